# Optimizing a Trainium2 kernel written in Bass

```python
import math
import jax, jax.numpy as jnp
from jax import lax
import numpy as np

D_MODEL = 1024
BATCH = 8
SEQ = 4096
DEPTH = 1

RW_HEADS = 8
RW_HEAD_DIM = 64
RW_DIM = RW_HEADS * RW_HEAD_DIM
RW_DECAY_LORA = 64
RW_A_LORA = 64
RW_GATE_LORA = 128
RW_SIZES = (RW_DIM, RW_DIM, RW_DIM, RW_DECAY_LORA, RW_A_LORA, RW_GATE_LORA)
RW_COLS = 3 * RW_DIM + RW_DECAY_LORA + RW_A_LORA + RW_GATE_LORA
RW_GN_EPS = 64e-5

DSA_HEADS = 8
DSA_LATENT = 128
DSA_Q_DIM = DSA_HEADS * DSA_LATENT
IDX_HEADS = 4
IDX_DIM = 64
TOPK_MAX = 256
Q_BLOCK = 128

REL_BUCKETS = 32
REL_MAX_DIST = 128

IN_SIZES = (RW_COLS, DSA_Q_DIM, DSA_LATENT, IDX_HEADS * IDX_DIM, IDX_DIM, IDX_HEADS, D_MODEL, D_MODEL)
IN_COLS = RW_COLS + DSA_Q_DIM + DSA_LATENT + IDX_HEADS * IDX_DIM + IDX_DIM + IDX_HEADS + 2 * D_MODEL

PEER_HEADS = 8
PEER_N_KEYS = 128
PEER_N_EXPERTS = PEER_N_KEYS * PEER_N_KEYS
PEER_KEY_DIM = 128
PEER_HALF = PEER_KEY_DIM // 2
PEER_TOPK = 16
PEER_CHUNK = 128

LN_EPS = 1e-5
DEEPNORM_ALPHA = (2.0 * DEPTH) ** 0.25
DEEPNORM_BETA = (8.0 * DEPTH) ** -0.25

kernel_name = "rwkv7_dsa_peer_hybrid_block"


def _split_points(sizes):
    return np.cumsum(sizes)[:-1].tolist()


def _layer_norm(x, g, b):
    xf = x.astype(jnp.float32)
    mu = jnp.mean(xf, -1, keepdims=True)
    var = jnp.mean(jnp.square(xf - mu), -1, keepdims=True)
    return ((xf - mu) * lax.rsqrt(var + LN_EPS)).astype(x.dtype) * g + b


def _rms_norm(x, g):
    xf = x.astype(jnp.float32)
    ms = jnp.mean(jnp.square(xf), -1, keepdims=True)
    return (xf * lax.rsqrt(ms + LN_EPS)).astype(x.dtype) * g


def _heads(t, n_heads):
    return t.reshape(*t.shape[:-1], n_heads, t.shape[-1] // n_heads)


def _token_shift(z):
    return jnp.pad(z, ((0, 0), (1, 0), (0, 0)))[:, :-1]


def _t5_bucket(n):
    n = jnp.maximum(n, 0)
    max_exact = REL_BUCKETS // 2
    nf = jnp.maximum(n, 1).astype(jnp.float32)
    large = max_exact + (jnp.log(nf / max_exact) / math.log(REL_MAX_DIST / max_exact)
                         * (REL_BUCKETS - max_exact)).astype(jnp.int32)
    large = jnp.minimum(large, REL_BUCKETS - 1)
    return jnp.where(n < max_exact, n, large)


def _rwkv7_time_mix(z_rw, mu, w0, w2, a0, a2, g2, k_k, k_a, r_k, gn_g, gn_b):
    B, S, _ = z_rw.shape
    zs = z_rw + (_token_shift(z_rw) - z_rw) * mu
    r, k, v, wl, al, gl = jnp.split(zs, _split_points(RW_SIZES), axis=-1)
    log_w = -jax.nn.softplus(-(w0 + jnp.tanh(wl) @ w2)) - 0.5
    decay = jnp.exp(-jnp.exp(log_w))
    a = jax.nn.sigmoid(a0 + al @ a2)
    g = jax.nn.sigmoid(gl) @ g2
    kk = _heads(k * k_k, RW_HEADS)
    kk = kk * lax.rsqrt(jnp.maximum(jnp.sum(kk * kk, -1, keepdims=True), 1e-24))
    k = k * (1 + (a - 1) * k_a)
    rh, kh, vh, dh, ah = [_heads(t, RW_HEADS) for t in (r, k, v, decay, a)]
    a_vec = -kk
    b_vec = kk * ah

    def step(state, inp):
        r_t, w_t, k_t, v_t, a_t, b_t = inp
        sa = jnp.einsum('bhvk,bhk->bhv', state, a_t)
        state = (state * w_t[:, :, None, :] + sa[..., None] * b_t[:, :, None, :]
                 + v_t[..., None] * k_t[:, :, None, :])
        return state, jnp.einsum('bhvk,bhk->bhv', state, r_t)

    seq = tuple(jnp.moveaxis(t.astype(jnp.float32), 1, 0) for t in (rh, dh, kh, vh, a_vec, b_vec))
    state0 = jnp.zeros((B, RW_HEADS, RW_HEAD_DIM, RW_HEAD_DIM), jnp.float32)
    _, y = lax.scan(step, state0, seq)
    y = jnp.moveaxis(y, 0, 1)
    mean = jnp.mean(y, -1, keepdims=True)
    var = jnp.mean(jnp.square(y - mean), -1, keepdims=True)
    y = ((y - mean) * lax.rsqrt(var + RW_GN_EPS)).reshape(B, S, RW_DIM).astype(z_rw.dtype) * gn_g + gn_b
    bonus = (jnp.sum(rh * kh * r_k, -1, keepdims=True) * vh).reshape(B, S, RW_DIM)
    return (y + bonus) * g


def _dsa_attention(z_q, z_kv, z_qi, z_ki, z_wi, kv_g, idx_k_g, idx_k_b, rel_bias):
    B, S, _ = z_q.shape
    c_kv = _rms_norm(z_kv, kv_g)
    k_idx = _layer_norm(z_ki, idx_k_g, idx_k_b)
    q = _heads(z_q, DSA_HEADS)
    q_idx = _heads(z_qi, IDX_HEADS)
    w_idx = z_wi * (IDX_HEADS ** -0.5)
    topk = min(TOPK_MAX, S // 4)
    nb = S // Q_BLOCK

    def blockify(t):
        return jnp.swapaxes(t.reshape(B, nb, Q_BLOCK, *t.shape[2:]), 0, 1)

    q_pos = jnp.arange(S, dtype=jnp.int32).reshape(nb, Q_BLOCK)
    key_pos = jnp.arange(S, dtype=jnp.int32)

    def one_block(args):
        qb, qib, wb, tpos = args
        dots = jnp.einsum('bqhd,bsd->bqhs', qib, k_idx) * (IDX_DIM ** -0.5)
        score = jnp.einsum('bqh,bqhs->bqs', wb, jax.nn.relu(dots))
        causal = key_pos[None, :] <= tpos[:, None]
        score = jnp.where(causal[None], score, -jnp.inf)
        _, sel = lax.top_k(score, topk)
        valid = sel <= tpos[None, :, None]
        kv_sel = jax.vmap(lambda cb, ib: cb[ib])(c_kv, sel)
        logits = jnp.einsum('bqhd,bqkd->bqhk', qb, kv_sel) * (DSA_LATENT ** -0.5)
        bias = rel_bias[_t5_bucket(tpos[None, :, None] - sel)]
        logits = logits + jnp.transpose(bias, (0, 1, 3, 2))
        logits = jnp.where(valid[:, :, None, :], logits, -jnp.inf)
        p = jax.nn.softmax(logits.astype(jnp.float32), axis=-1).astype(kv_sel.dtype)
        return jnp.einsum('bqhk,bqkd->bqhd', p, kv_sel)

    out = lax.map(one_block, (blockify(q), blockify(q_idx), blockify(w_idx), q_pos))
    return jnp.swapaxes(out, 0, 1).reshape(B, S, DSA_Q_DIM)


def _peer(h, w_pq, sub_keys, u_tab, v_tab):
    B, S, D = h.shape
    T = B * S
    ht = h.reshape(T, D)
    q = (ht @ w_pq).reshape(T, PEER_HEADS, 2, PEER_HALF)
    s = jnp.einsum('thcd,hcnd->thcn', q, sub_keys)
    s1, i1 = lax.top_k(s[:, :, 0], PEER_TOPK)
    s2, i2 = lax.top_k(s[:, :, 1], PEER_TOPK)
    cand = (s1[..., :, None] + s2[..., None, :]).reshape(T, PEER_HEADS, PEER_TOPK * PEER_TOPK)
    cand_idx = (i1[..., :, None] * PEER_N_KEYS + i2[..., None, :]).reshape(T, PEER_HEADS, PEER_TOPK * PEER_TOPK)
    top_s, pos = lax.top_k(cand, PEER_TOPK)
    experts = jnp.take_along_axis(cand_idx, pos, axis=-1)
    gates = jax.nn.softmax(top_s.astype(jnp.float32), axis=-1).astype(h.dtype)
    n_chunks = T // PEER_CHUNK

    def chunk(args):
        hc, ec, gc = args
        act = jax.nn.gelu(jnp.einsum('cd,chkd->chk', hc, u_tab[ec]), approximate=False) * gc
        return jnp.einsum('chk,chkd->cd', act, v_tab[ec])

    out = lax.map(chunk, (ht.reshape(n_chunks, PEER_CHUNK, D),
                          experts.reshape(n_chunks, PEER_CHUNK, PEER_HEADS, PEER_TOPK),
                          gates.reshape(n_chunks, PEER_CHUNK, PEER_HEADS, PEER_TOPK)))
    return out.reshape(B, S, D)


def setup_inputs(seed: int = 0) -> dict:
    key = jax.random.key(seed)
    ks = iter(jax.random.split(key, 40))
    L, D = DEPTH, D_MODEL

    def nrm(shape, std):
        return jax.random.normal(next(ks), shape, jnp.float32) * std

    def unif(shape, lo, hi):
        return jax.random.uniform(next(ks), shape, jnp.float32, lo, hi)

    col_scale = jnp.concatenate([jnp.ones((2 * RW_DIM,), jnp.float32),
                                 jnp.full((RW_DIM,), DEEPNORM_BETA, jnp.float32),
                                 jnp.ones((IN_COLS - 3 * RW_DIM,), jnp.float32)])
    return {
        "x": nrm((BATCH, SEQ, D), 1.0),
        "c": nrm((BATCH, D), 1.0),
        "w_ada": nrm((L, D, 6 * D), D ** -0.5),
        "b_ada": nrm((L, 6 * D), 0.02),
        "w_in": nrm((L, D, IN_COLS), D ** -0.5) * col_scale,
        "rw_mu": unif((L, RW_COLS), 0.0, 1.0),
        "rw_w0": unif((L, RW_DIM), -4.0, 0.0),
        "rw_w2": nrm((L, RW_DECAY_LORA, RW_DIM), 0.5 * RW_DECAY_LORA ** -0.5),
        "rw_a0": nrm((L, RW_DIM), 0.1),
        "rw_a2": nrm((L, RW_A_LORA, RW_DIM), 0.5 * RW_A_LORA ** -0.5),
        "rw_g2": nrm((L, RW_GATE_LORA, RW_DIM), RW_GATE_LORA ** -0.5),
        "rw_k_k": 0.85 + nrm((L, RW_DIM), 0.05),
        "rw_k_a": 1.0 + nrm((L, RW_DIM), 0.05),
        "rw_r_k": nrm((L, RW_HEADS, RW_HEAD_DIM), 0.1),
        "rw_gn_g": 1.0 + nrm((L, RW_DIM), 0.02),
        "rw_gn_b": nrm((L, RW_DIM), 0.02),
        "dsa_kv_g": 1.0 + nrm((L, DSA_LATENT), 0.02),
        "idx_k_g": 1.0 + nrm((L, IDX_DIM), 0.02),
        "idx_k_b": nrm((L, IDX_DIM), 0.02),
        "rel_bias": nrm((REL_BUCKETS, DSA_HEADS), 0.5),
        "w_br_a": nrm((L, RW_DIM, D), DEEPNORM_BETA * RW_DIM ** -0.5),
        "w_br_b": nrm((L, DSA_Q_DIM, D), DEEPNORM_BETA * DSA_Q_DIM ** -0.5),
        "w_out": nrm((L, D, D), DEEPNORM_BETA * D ** -0.5),
        "ln1_g": 1.0 + nrm((L, D), 0.02),
        "ln1_b": nrm((L, D), 0.02),
        "peer_wq": nrm((L, D, PEER_HEADS * PEER_KEY_DIM), D ** -0.5),
        "peer_keys": nrm((L, PEER_HEADS, 2, PEER_N_KEYS, PEER_HALF), PEER_HALF ** -0.5),
        "peer_u": nrm((L, PEER_N_EXPERTS, D), D ** -0.5),
        "peer_v": nrm((L, PEER_N_EXPERTS, D), DEEPNORM_BETA * PEER_HEADS ** -0.5),
        "ln2_g": 1.0 + nrm((L, D), 0.02),
        "ln2_b": nrm((L, D), 0.02),
    }


def reference(x, c, w_ada, b_ada, w_in, rw_mu, rw_w0, rw_w2, rw_a0, rw_a2, rw_g2, rw_k_k, rw_k_a,
              rw_r_k, rw_gn_g, rw_gn_b, dsa_kv_g, idx_k_g, idx_k_b, rel_bias, w_br_a, w_br_b, w_out,
              ln1_g, ln1_b, peer_wq, peer_keys, peer_u, peer_v, ln2_g, ln2_b):
    for l in range(DEPTH):
        mod = jax.nn.silu(c) @ w_ada[l] + b_ada[l]
        sh1, sc1, gt1, sh2, sc2, gt2 = jnp.split(mod, 6, axis=-1)

        h = x * (1 + sc1[:, None]) + sh1[:, None]
        z = h @ w_in[l]
        z_rw, z_q, z_kv, z_qi, z_ki, z_wi, z_ga, z_gb = jnp.split(z, _split_points(IN_SIZES), axis=-1)
        y_a = _rwkv7_time_mix(z_rw, rw_mu[l], rw_w0[l], rw_w2[l], rw_a0[l], rw_a2[l], rw_g2[l],
                              rw_k_k[l], rw_k_a[l], rw_r_k[l], rw_gn_g[l], rw_gn_b[l]) @ w_br_a[l]
        y_b = _dsa_attention(z_q, z_kv, z_qi, z_ki, z_wi, dsa_kv_g[l], idx_k_g[l], idx_k_b[l],
                             rel_bias) @ w_br_b[l]
        merged = jax.nn.sigmoid(z_ga) * y_a + jax.nn.sigmoid(z_gb) * y_b
        mix = merged @ w_out[l]
        x = _layer_norm(DEEPNORM_ALPHA * x + gt1[:, None] * mix, ln1_g[l], ln1_b[l])

        h2 = x * (1 + sc2[:, None]) + sh2[:, None]
        y2 = _peer(h2, peer_wq[l], peer_keys[l], peer_u[l], peer_v[l])
        x = _layer_norm(DEEPNORM_ALPHA * x + gt2[:, None] * y2, ln2_g[l], ln2_b[l])
    return x
```

```python
import numpy as np
import ml_dtypes
import concourse.bass as bass
import concourse.mybir as mybir
from concourse.bass_utils import run_bass_kernel_spmd

F32 = mybir.dt.float32
BF16 = mybir.dt.bfloat16
I32 = mybir.dt.int32
U32 = mybir.dt.uint32
AF = mybir.ActivationFunctionType
ALU = mybir.AluOpType
AX = mybir.AxisListType

S = 4096
D = 1024
NT = S // 128
IN_COLS = 5316
DBG = {}
STOP_AFTER = None


class Buf:
    __slots__ = ("name", "lw", "rd")

    def __init__(self, name):
        self.name = name
        self.lw = None
        self.rd = []


class _Rec:
    def __init__(self):
        self.call = None

    def __getattr__(self, name):
        def f(*args, **kwargs):
            self.call = (name, args, kwargs)
            return self
        return f


class Sched:
    ENGS = ("pe", "act", "dve", "pool", "sp")

    def __init__(self, nc, n_dma_sems=40):
        self.nc = nc
        self.ops = {e: [] for e in self.ENGS}
        self.sem = {e: nc.alloc_semaphore("c_" + e) for e in self.ENGS}
        self.cnt = {e: 0 for e in self.ENGS}
        self.seen = {e: {} for e in self.ENGS}
        self.dsem = [nc.alloc_semaphore("d%d" % i) for i in range(n_dma_sems)]
        self.dval = [0] * n_dma_sems
        self.drr = 0
        self.drr_sw = 0
        self.NSW = 8
        self.NHW = n_dma_sems - 8
        self.all_events = []

    def _waits(self, eng, reads, writes):
        deps = []
        for b in reads:
            if b.lw is not None:
                deps.append(b.lw)
        for b in writes:
            if b.lw is not None:
                deps.append(b.lw)
            deps.extend(b.rd)
        out = {}
        for (sem, val, src) in deps:
            if src == "pe" and eng == "pe":
                continue
            k = sem.num
            if self.seen[eng].get(k, 0) >= val:
                continue
            if out.get(k, (None, 0))[1] < val:
                out[k] = (sem, val)
        for k, (sem, val) in out.items():
            self.seen[eng][k] = val
        return list(out.values())

    def op(self, eng, fn, reads=(), writes=()):
        waits = self._waits(eng, reads, writes)
        self.cnt[eng] += 1
        sem = self.sem[eng]
        val = self.cnt[eng]
        ev = (sem, val, eng)

        rec = _Rec()
        fn(rec)
        name, args, kwargs = rec.call

        def emit(e, waits=waits, sem=sem, name=name, args=args, kwargs=kwargs):
            for (s, v) in waits:
                e.wait_ge(s, v)
            getattr(e, name)(*args, **kwargs).then_inc(sem, 1)
        self.ops[eng].append(emit)
        for b in reads:
            b.rd.append(ev)
        for b in writes:
            b.lw = ev
            b.rd = []
        return ev

    def dma(self, eng, out, in_, reads=(), writes=(), fn=None, **kw):
        if fn is not None:
            rec = _Rec()
            fn(rec)
            mname, margs, mkw = rec.call
        else:
            mname, margs, mkw = "dma_start", (), dict(out=out, in_=in_, **kw)
        if eng == "pool":
            i = self.NHW + (self.drr_sw % self.NSW)
            self.drr_sw += 1
        else:
            i = self.drr
            self.drr = (self.drr + 1) % self.NHW
        sem = self.dsem[i]
        waits = self._waits(eng, reads, writes)
        prev = self.dval[i]
        if prev > 0 and self.seen[eng].get(sem.num, 0) < prev:
            waits.append((sem, prev))
            self.seen[eng][sem.num] = prev
        self.dval[i] += 16
        val = self.dval[i]
        ev = (sem, val, "dma")

        def emit(e, waits=waits, sem=sem, mname=mname, margs=margs, mkw=mkw):
            for (s, v) in waits:
                e.wait_ge(s, v)
            getattr(e, mname)(*margs, **mkw).then_inc(sem, 16)
        self.ops[eng].append(emit)
        for b in reads:
            b.rd.append(ev)
        for b in writes:
            b.lw = ev
            b.rd = []
        self.all_events.append(ev)
        return ev

    def raw(self, eng, fn):
        self.ops[eng].append(fn)

    def barrier(self):
        targets = []
        for en in self.ENGS:
            if self.cnt[en] > 0:
                targets.append((self.sem[en], self.cnt[en], en))
        for i, s in enumerate(self.dsem):
            if self.dval[i] > 0:
                targets.append((s, self.dval[i], "dma"))
        for eng in self.ENGS:
            waits = []
            for (s, v, src) in targets:
                if src == eng:
                    continue
                if self.seen[eng].get(s.num, 0) >= v:
                    continue
                self.seen[eng][s.num] = v
                waits.append((s, v))

            def emit(e, waits=waits):
                for (s, v) in waits:
                    e.wait_ge(s, v)
            self.ops[eng].append(emit)

    def finish(self, final_events):
        nc = self.nc
        with nc.Block() as block:
            def run(name):
                def f(e):
                    for emit in self.ops[name]:
                        emit(e)
                    if name == "sp":
                        for (s, v, _) in final_events:
                            e.wait_ge(s, v)
                        for i, s in enumerate(self.dsem):
                            if self.dval[i] > 0:
                                e.wait_ge(s, self.dval[i])
                        for en in ("pe", "act", "dve", "pool"):
                            if self.cnt[en] > 0:
                                e.wait_ge(self.sem[en], self.cnt[en])
                return f
            block.tensor(run("pe"))
            block.scalar(run("act"))
            block.vector(run("dve"))
            block.gpsimd(run("pool"))
            block.sync(run("sp"))


class Arena:
    def __init__(self, nc, base=0, top=192 * 1024):
        self.nc = nc
        self.off = base
        self.top = top
        self.n = 0
        self.sc = None

    def mark(self):
        return self.off

    def release(self, m):
        self.off = m
        if self.sc is not None:
            self.sc.barrier()

    def alloc(self, shape, dtype, name="t"):
        esz = {F32: 4, BF16: 2, I32: 4, U32: 4}[dtype]
        per = esz
        for s in shape[1:]:
            per *= s
        per = (per + 63) // 64 * 64
        self.n += 1
        t = self.nc.alloc_sbuf_tensor_at("%s_%d" % (name, self.n), list(shape), dtype, offset=self.off)
        self.off += per
        assert self.off <= self.top, ("SBUF overflow", name, self.off)
        return t


def build(dbg=None, stop_after=None, phases=None, feed=(), nblk=8):
    dbg = dbg or {}
    phases = phases or {'0', 'A', 'B', 'C', 'D', 'E', 'F'}
    nc = bass.Bass("TRN2", target_bir_lowering=False)
    sc = Sched(nc)
    ar = Arena(nc, base=(nc.sbuf_base + 63) // 64 * 64, top=nc.sbuf_top // 64 * 64)
    ar.sc = sc

    def din(name, shape, dt=F32):
        return nc.dram_tensor(name, list(shape), dt, kind="ExternalInput").ap()

    def dscratch(name, shape, dt=F32):
        kind = "ExternalOutput" if name in dbg else ("ExternalInput" if name in feed else "Internal")
        return nc.dram_tensor(name, list(shape), dt, kind=kind).ap()

    x_d = din("x", [S, D])
    c_d = din("c_col", [128, 8])
    wada_d = din("w_ada", [D, 6 * D])
    bada_col_d = din("b_ada_col", [128, 48])
    bada_bc_d = din("b_ada_bc", [128, 6 * D])
    win_d = din("w_in", [D, IN_COLS])
    ident_d = din("ident", [128, 128])
    out_d = nc.dram_tensor("out", [S, D], F32, kind="ExternalOutput").ap()

    mu_d = din("mu_col", [128, 14])
    rwvec_d = din("rwvec", [128, 20])
    w2a2_d = din("w2a2", [128, 512])
    g2_d = din("g2", [128, 512])
    gnbc_d = din("gn_bc", [128, 2, 256])
    cst_d = din("cst", [128, 1024])
    wbra_d = din("w_br_a", [512, D])
    wbrb_d = din("w_br_b", [D, D])
    wout_d = din("w_out", [D, D])
    lnbc_d = din("ln_bc", [128, 4, D])
    wq_d = din("peer_wq", [D, D])
    pkeys_d = din("peer_keysT", [128, 8, 128])
    pu_d = din("peer_u", [16384, D])
    pv_d = din("peer_v", [16384, D])
    cvec_d = din("cvec", [128, 256])
    biasT_d = din("biasT", [128, 3, 1024])
    negm_d = din("negm", [128, 128])
    zrw_d = dscratch("zrw", [1792, S], F32)
    zq_d = dscratch("zq", [1024, S], BF16)
    zqi_d = dscratch("zqi", [256, S], BF16)
    zg_d = dscratch("zg", [2048, S], BF16)
    ztok_d = dscratch("ztok", [S, 196], F32)
    rwo_d = dscratch("rwo", [512, S], BF16)
    dsao_d = dscratch("dsao", [1024, S], BF16)
    x1_d = dscratch("x1dbg", [S, D], F32)
    y2_d = dscratch("y2dbg", [S, D], F32)
    x1s_d = dscratch("x1s", [S, D], F32)
    h2T_d = dscratch("h2T", [D, S], BF16)
    selT_d = dscratch("selT", [3, 128, S], F32)
    uv_d = dscratch("uv16", [128, 128, 2048], BF16)
    iota_d = din("iota128", [128, 128])
    iota128 = ar.alloc([128, 128], F32, "iota128")
    B_iota = Buf("iota")
    iota16 = iota128[:, 0:16]

    ident = ar.alloc([128, 128], F32, "ident")
    identb = ar.alloc([128, 128], BF16, "identb")
    modcol = ar.alloc([128, 48], F32, "modcol")
    onep1 = ar.alloc([128, 8], F32, "onep1")
    onep2 = ar.alloc([128, 8], F32, "onep2")
    gt_bc = ar.alloc([128, 4, D], F32, "gt_bc")
    B_ident = Buf("ident")
    B_mod = Buf("mod")
    B_gt = Buf("gt")

    ps = [nc.alloc_psum_tensor("ps%d" % i, [128, 512], F32) for i in range(8)]
    B_ps = [Buf("ps%d" % i) for i in range(8)]

    sc.dma("sp", ident[:, :], ident_d[:, :], writes=[B_ident])
    sc.dma("sp", iota128[:, :], iota_d[:, :], writes=[B_iota])

    mark_stg = ar.mark()
    STG = 1024
    stg = [ar.alloc([128, STG], F32, "stg%d" % i) for i in range(3)]
    B_stg = [Buf("stg%d" % i) for i in range(3)]
    stg_i = [0]

    def load_bf16(dst, src, n, bdst, eng="pool"):
        P = dst.shape[0]
        for o in range(0, n, STG):
            w = min(STG, n - o)
            k = stg_i[0] % 3
            stg_i[0] += 1
            sc.dma("sp", stg[k][0:P, 0:w], src[:, o:o + w], writes=[B_stg[k]])
            sc.op(eng, lambda e, k=k, o=o, w=w, P=P, dst=dst: e.tensor_copy(out=dst[:, o:o + w], in_=stg[k][0:P, 0:w]),
                  reads=[B_stg[k]], writes=[bdst])
    sc.op("dve", lambda e: e.tensor_copy(out=identb[:, :], in_=ident[:, :]), reads=[B_ident], writes=[B_ident])

    if '0' in phases:
        m0 = ar.mark()
        c_sb = ar.alloc([128, 8], F32, "c_sb")
        sil = ar.alloc([128, 8], F32, "sil")
        silbc = ar.alloc([128, 8, 128], F32, "silbc")
        bcol = ar.alloc([128, 48], F32, "bcol")
        bbc = ar.alloc([128, 4, D], F32, "bbc")
        wa = [ar.alloc([128, 8, 1024], F32, "wa%d" % i) for i in range(4)]
        B_c = Buf("c")
        B_wa = [Buf("wa0"), Buf("wa1"), Buf("wa2"), Buf("wa3")]
        B_b = Buf("bcol")
        sc.dma("sp", c_sb[:, :], c_d[:, :], writes=[B_c])
        sc.dma("sp", bcol[:, :], bada_col_d[:, :], writes=[B_b])
        for gi_, g_ in enumerate((2, 3, 4, 5)):
            sc.dma("sp", bbc[:, gi_, :], bada_bc_d[:, g_ * D:(g_ + 1) * D], writes=[B_b])
        sc.op("act", lambda e: e.activation(out=sil[:, :], in_=c_sb[:, :], func=AF.Silu), reads=[B_c], writes=[B_c])
        for kc in range(8):
            sc.op("dve", lambda e, kc=kc: e.tensor_copy(out=silbc[:, kc, :], in_=sil[:, kc:kc + 1].to_broadcast([128, 128])),
                  reads=[B_c], writes=[B_c])
        wada_v = wada_d.rearrange("(kc p) n -> p kc n", p=128)
        for g in range(6):
            w = wa[g % 4]
            bw = B_wa[g % 4]
            for kc in range(8):
                sc.dma("sp", w[:, kc, :], wada_v[:, kc, g * 1024:(g + 1) * 1024], writes=[bw])
            if g in (2, 3, 4, 5):
                gi = g - 2
                for half in range(2):
                    p = ps[half]
                    for kc in range(8):
                        sc.op("pe", lambda e, p=p, w=w, kc=kc, half=half: e.matmul(
                            p[:, :], lhsT=silbc[:, kc, :], rhs=w[:, kc, half * 512:(half + 1) * 512],
                            start=(kc == 0), stop=(kc == 7)), reads=[bw, B_c], writes=[B_ps[half]])
                    sc.op("dve", lambda e, p=p, gi=gi, half=half: e.tensor_tensor(
                        out=gt_bc[:, gi, half * 512:(half + 1) * 512], in0=p[:, :],
                        in1=bbc[:, gi, half * 512:(half + 1) * 512], op=ALU.add),
                        reads=[B_ps[half], B_b], writes=[B_gt])
            if g in (0, 1, 3, 4):
                p = ps[2]
                for fc in range(8):
                    for kc in range(8):
                        sc.op("pe", lambda e, p=p, w=w, kc=kc, fc=fc: e.matmul(
                            p[:, fc:fc + 1], lhsT=w[:, kc, fc * 128:(fc + 1) * 128], rhs=sil[:, kc:kc + 1],
                            start=(kc == 0), stop=(kc == 7)), reads=[bw, B_c], writes=[B_ps[2]])
                sc.op("dve", lambda e, p=p, g=g: e.tensor_tensor(
                    out=modcol[:, g * 8:(g + 1) * 8], in0=p[:, 0:8], in1=bcol[:, g * 8:(g + 1) * 8], op=ALU.add),
                    reads=[B_ps[2], B_b], writes=[B_mod])
        sc.op("dve", lambda e: e.tensor_scalar(out=onep1[:, :], in0=modcol[:, 8:16], scalar1=1.0, scalar2=None, op0=ALU.add),
              reads=[B_mod], writes=[B_mod])
        sc.op("dve", lambda e: e.tensor_scalar(out=onep2[:, :], in0=modcol[:, 32:40], scalar1=1.0, scalar2=None, op0=ALU.add),
              reads=[B_mod], writes=[B_mod])
        sc.op("dve", lambda e: e.tensor_scalar(out=gt_bc[:, 2, :], in0=gt_bc[:, 2, :], scalar1=1.0, scalar2=None, op0=ALU.add),
              reads=[B_gt], writes=[B_gt])
        ar.release(m0)
        if "modcol" in dbg:
            dd = nc.dram_tensor("modcol_o", [128, 48], F32, kind="ExternalOutput").ap()
            sc.dma("sp", dd[:, :], modcol[:, :], reads=[B_mod])
            dd2 = nc.dram_tensor("gt_o", [128, 4 * D], F32, kind="ExternalOutput").ap()
            sc.dma("sp", dd2[:, :], gt_bc[:, :, :].rearrange("p a b -> p (a b)"), reads=[B_gt])

    if 'A' in phases:
        mA = ar.mark()
        fm_chunks = []
        for i in range(14):
            fm_chunks.append((i * 128, "rw", i))
        for i in range(8):
            fm_chunks.append((1792 + i * 128, "q", i))
        for i in range(2):
            fm_chunks.append((2944 + i * 128, "qi", i))
        for i in range(16):
            fm_chunks.append((3268 + i * 128, "g", i))
        winb = ar.alloc([128, 8, IN_COLS], BF16, "winb")
        B_win = Buf("win")
        win_v = win_d.rearrange("(kc p) n -> p kc n", p=128)
        for kc in range(8):
            load_bf16(winb[:, kc, :], win_v[:, kc, :], IN_COLS, B_win)
        xt = [ar.alloc([128, 4, D], F32, "xt%d" % i) for i in range(2)]
        B_xt = [Buf("xt0"), Buf("xt1")]
        hT = [ar.alloc([128, 8, 512], BF16, "hT%d" % i) for i in range(2)]
        B_hT = [Buf("hT0"), Buf("hT1")]
        NEV = 8
        ev32 = [ar.alloc([128, 512], F32, "ev32_%d" % i) for i in range(NEV)]
        ev16 = [ar.alloc([128, 512], BF16, "ev16_%d" % i) for i in range(NEV)]
        B_ev32 = [Buf("ev32_%d" % i) for i in range(NEV)]
        B_ev16 = [Buf("ev16_%d" % i) for i in range(NEV)]
        ztk = [ar.alloc([128, 196], F32, "ztk%d" % i) for i in range(2)]
        B_ztk = [Buf("ztk0"), Buf("ztk1")]
        x_v = x_d.rearrange("(n p) m -> p n m", p=128)
        pi = 0
        evi = 0
        tok_cols = [(2816, 128, 0), (3200, 68, 128)]
        for tb in range(8):
            xb = xt[tb % 2]
            bx = B_xt[tb % 2]
            hb = hT[tb % 2]
            bh = B_hT[tb % 2]
            for j in range(4):
                sc.dma("sp", xb[:, j, :], x_v[:, tb * 4 + j, :], writes=[bx])
            for kc in range(8):
                p = ps[pi % 8]
                bp = B_ps[pi % 8]
                pi += 1
                for j in range(4):
                    sc.op("pe", lambda e, p=p, xb=xb, j=j, kc=kc: e.transpose(
                        p[:, j * 128:(j + 1) * 128], xb[:, j, kc * 128:(kc + 1) * 128], ident[:, :]),
                        reads=[bx, B_ident], writes=[bp])
                sc.op("act", lambda e, p=p, hb=hb, kc=kc: e.activation(
                    out=hb[:, kc, :], in_=p[:, :], func=AF.Identity,
                    scale=onep1[:, kc:kc + 1], bias=modcol[:, kc:kc + 1]),
                    reads=[bp, B_mod], writes=[bh])
            for ci, (col0, kind, idx) in enumerate(fm_chunks):
                p = ps[pi % 8]
                bp = B_ps[pi % 8]
                pi += 1
                for kc in range(8):
                    sc.op("pe", lambda e, p=p, kc=kc, col0=col0, hb=hb: e.matmul(
                        p[:, :], lhsT=winb[:, kc, col0:col0 + 128], rhs=hb[:, kc, :],
                        start=(kc == 0), stop=(kc == 7)), reads=[B_win, bh], writes=[bp])
                k = evi % NEV
                evi += 1
                eng = "dve" if (ci % 2 == 0) else "act"
                tsl = slice(tb * 512, (tb + 1) * 512)
                if kind == "rw":
                    dst = ev32[k]
                    bd = B_ev32[k]
                    if eng == "dve":
                        sc.op("dve", lambda e, p=p, dst=dst: e.tensor_copy(out=dst[:, :], in_=p[:, :]), reads=[bp], writes=[bd])
                    else:
                        sc.op("act", lambda e, p=p, dst=dst: e.activation(out=dst[:, :], in_=p[:, :], func=AF.Copy), reads=[bp], writes=[bd])
                    sc.dma("sp", zrw_d[idx * 128:(idx + 1) * 128, tsl], dst[:, :], reads=[bd])
                elif kind in ("q", "qi"):
                    dst = ev16[k]
                    bd = B_ev16[k]
                    if eng == "dve":
                        sc.op("dve", lambda e, p=p, dst=dst: e.tensor_copy(out=dst[:, :], in_=p[:, :]), reads=[bp], writes=[bd])
                    else:
                        sc.op("act", lambda e, p=p, dst=dst: e.activation(out=dst[:, :], in_=p[:, :], func=AF.Copy), reads=[bp], writes=[bd])
                    dd = zq_d if kind == "q" else zqi_d
                    sc.dma("sp", dd[idx * 128:(idx + 1) * 128, tsl], dst[:, :], reads=[bd])
                else:
                    dst = ev16[k]
                    bd = B_ev16[k]
                    sc.op("act", lambda e, p=p, dst=dst: e.activation(out=dst[:, :], in_=p[:, :], func=AF.Sigmoid), reads=[bp], writes=[bd])
                    sc.dma("sp", zg_d[idx * 128:(idx + 1) * 128, tsl], dst[:, :], reads=[bd])
            for j in range(4):
                p = ps[pi % 8]
                bp = B_ps[pi % 8]
                pi += 1
                for (c0, ncol, o0) in tok_cols:
                    for kc in range(8):
                        sc.op("pe", lambda e, p=p, kc=kc, j=j, c0=c0, ncol=ncol, o0=o0, hb=hb: e.matmul(
                            p[:, o0:o0 + ncol], lhsT=hb[:, kc, j * 128:(j + 1) * 128], rhs=winb[:, kc, c0:c0 + ncol],
                            start=(kc == 0), stop=(kc == 7)), reads=[B_win, bh], writes=[bp])
                zt = ztk[j % 2]
                bz = B_ztk[j % 2]
                sc.op("dve", lambda e, p=p, zt=zt: e.tensor_copy(out=zt[:, :], in_=p[:, 0:196]), reads=[bp], writes=[bz])
                r0 = (tb * 4 + j) * 128
                sc.dma("sp", ztok_d[r0:r0 + 128, :], zt[:, :], reads=[bz])
        ar.release(mA)

    e0_done_flag = [False]
    if 'B' in phases:
        phase_B(locals())
    if 'C' in phases:
        phase_C(locals())
    if 'D' in phases:
        phase_D(locals())
    if 'E' in phases:
        phase_E(locals())

    sc.finish([])
    return nc


def _prep_inputs(inputs):
    f = lambda a: np.ascontiguousarray(np.asarray(a, dtype=np.float32))
    x = f(inputs["x"])
    c = f(inputs["c"])
    b_ada = f(inputs["b_ada"])[0]
    shared = {
        "w_ada": f(inputs["w_ada"])[0],
        "b_ada_col": np.ascontiguousarray(b_ada.reshape(48, 128).T),
        "b_ada_bc": np.ascontiguousarray(np.broadcast_to(b_ada[None, :], (128, 6 * D))),
        "w_in": f(inputs["w_in"])[0],
        "ident": np.eye(128, dtype=np.float32),
        "iota128": np.ascontiguousarray(np.broadcast_to(np.arange(128, dtype=np.float32)[None], (128, 128))),
    }
    col = lambda v, n: np.ascontiguousarray(f(v).reshape(n, 128).T)
    shared["mu_col"] = col(inputs["rw_mu"][0], 14)
    shared["rwvec"] = np.ascontiguousarray(np.concatenate([col(inputs["rw_w0"][0], 4), col(inputs["rw_a0"][0], 4), col(inputs["rw_k_k"][0], 4),
                                                            col(inputs["rw_k_a"][0], 4), col(f(inputs["rw_r_k"])[0].reshape(-1), 4)], axis=1))
    shared["w2a2"] = np.ascontiguousarray(np.concatenate([f(inputs["rw_w2"])[0], f(inputs["rw_a2"])[0]], axis=0))
    shared["g2"] = f(inputs["rw_g2"])[0]
    gn2 = np.stack([f(inputs["rw_gn_g"])[0], f(inputs["rw_gn_b"])[0]]).reshape(2, 4, 2, 64)
    gnl = np.zeros((128, 2, 4, 64), np.float32)
    gnl[0:64] = gn2[:, :, 0, :][None]
    gnl[64:128] = gn2[:, :, 1, :][None]
    shared["gn_bc"] = np.ascontiguousarray(gnl.reshape(128, 2, 256))
    cst = np.zeros((128, 1024), np.float32)
    iu = np.triu(np.ones((64, 64), np.float32), 1)
    il = np.triu(np.ones((64, 64), np.float32), 0)
    cst[:, 0:128] = np.block([[iu, il], [iu, il]])
    cst[0:64, 128:192] = iu.T
    cst[64:128, 128:192] = iu.T
    cst[0:64, 192:256] = 1.0
    cst[64:128, 256:320] = 1.0
    sm = np.ones(512, np.float32); sm[::64] = 0.0
    cst[:, 320:832] = sm[None, :]
    cst[0:64, 832:896] = np.eye(64, dtype=np.float32)
    cst[64:128, 832:896] = np.eye(64, dtype=np.float32)
    cst[:, 896] = 1.0
    shared["cst"] = cst
    shared["w_br_a"] = f(inputs["w_br_a"])[0]
    shared["w_br_b"] = f(inputs["w_br_b"])[0]
    shared["w_out"] = f(inputs["w_out"])[0]
    lnr = np.stack([f(inputs["ln1_g"])[0], f(inputs["ln1_b"])[0], f(inputs["ln2_g"])[0], f(inputs["ln2_b"])[0]])
    shared["ln_bc"] = np.ascontiguousarray(np.broadcast_to(lnr[None], (128, 4, D)))
    shared["peer_wq"] = f(inputs["peer_wq"])[0]
    pk = f(inputs["peer_keys"])[0]
    shared["peer_keysT"] = np.ascontiguousarray(pk.transpose(1, 3, 0, 2).reshape(128, 8, 128))
    shared["peer_u"] = f(inputs["peer_u"])[0]
    cv = np.concatenate([f(inputs["dsa_kv_g"])[0], f(inputs["idx_k_g"])[0], f(inputs["idx_k_b"])[0]])
    shared["cvec"] = np.ascontiguousarray(np.broadcast_to(cv[None], (128, 256)))
    rb = f(inputs["rel_bias"])
    nn_ = np.arange(0, 256)
    nf = np.maximum(nn_, 1).astype(np.float32)
    large = 16 + (np.log(nf / np.float32(16)) / np.float32(np.log(8.0)) * np.float32(16)).astype(np.int32)
    bucket = np.where(nn_ < 16, nn_, np.minimum(large, 31))
    sI = np.arange(128)[:, None]
    qI = np.arange(128)[None, :]
    bd = bucket[np.clip(qI - sI, 0, 255)]
    bp = bucket[np.clip(qI + 128 - sI, 0, 255)]
    bT = np.zeros((128, 3, 8, 128), np.float32)
    bT[:, 0] = rb[bd].transpose(0, 2, 1)
    bT[:, 1] = rb[bp].transpose(0, 2, 1)
    bT[:, 2] = rb[31][None, :, None]
    shared["biasT"] = np.ascontiguousarray(bT.reshape(128, 3, 1024))
    shared["negm"] = np.where(np.arange(128)[None, :] <= np.arange(128)[:, None], 0.0, -1e30).astype(np.float32)
    shared["peer_v"] = f(inputs["peer_v"])[0]
    maps = []
    for b in range(8):
        m = dict(shared)
        m["x"] = x[b]
        m["c_col"] = np.ascontiguousarray(c[b].reshape(8, 128).T)
        maps.append(m)
    return maps


def kernel(**inputs):
    nc = build()
    maps = _prep_inputs(inputs)
    res = run_bass_kernel_spmd(nc, maps, core_ids=list(range(8)))
    out = np.stack([np.asarray(r["out"], dtype=np.float32) for r in res.results], axis=0)
    return out


def _bc_mid(ap, n):
    sh = list(ap.shape)
    return ap.unsqueeze(1).to_broadcast([sh[0], n] + sh[1:])


def phase_B(L):
    nc, sc, ar, ps, B_ps = L["nc"], L["sc"], L["ar"], L["ps"], L["B_ps"]
    identb, B_ident, load_bf16 = L["identb"], L["B_ident"], L["load_bf16"]
    zrw_d, rwo_d = L["zrw_d"], L["rwo_d"]
    V = lambda fn, r=(), w=(): sc.op("dve", fn, r, w)
    A = lambda fn, r=(), w=(): sc.op("act", fn, r, w)
    P = lambda fn, r=(), w=(): sc.op("pe", fn, r, w)
    mB = ar.mark()
    cst = ar.alloc([128, 1024], F32, "cst")
    mu = ar.alloc([128, 14], F32, "mu")
    rwvec = ar.alloc([128, 20], F32, "rwvec")
    omka = ar.alloc([128, 4], F32, "omka")
    w2a2b = ar.alloc([128, 512], BF16, "w2a2b")
    g2b = ar.alloc([128, 512], BF16, "g2b")
    gnbc = ar.alloc([128, 2, 256], F32, "gnbc")
    bones = ar.alloc([128, 128], BF16, "bones")
    onesb = ar.alloc([128, 1], BF16, "onesb")
    B_c = Buf("cstB")
    sc.dma("sp", cst[:, :], L["cst_d"][:, :], writes=[B_c])
    sc.dma("sp", mu[:, :], L["mu_d"][:, :], writes=[B_c])
    sc.dma("sp", rwvec[:, :], L["rwvec_d"][:, :], writes=[B_c])
    sc.dma("sp", gnbc[:, :, :], L["gnbc_d"][:, :, :], writes=[B_c])
    load_bf16(w2a2b[:, :], L["w2a2_d"][:, :], 512, B_c)
    load_bf16(g2b[:, :], L["g2_d"][:, :], 512, B_c)
    V(lambda e: e.tensor_scalar(out=omka[:, :], in0=rwvec[:, 12:16], scalar1=-1.0, scalar2=1.0, op0=ALU.mult, op1=ALU.add), [B_c], [B_c])
    V(lambda e: e.tensor_copy(out=bones[:, :], in_=cst[:, 192:320]), [B_c], [B_c])
    V(lambda e: e.tensor_copy(out=onesb[:, :], in_=cst[:, 896:897]), [B_c], [B_c])
    maskA = cst[:, 0:128]
    maskT = cst[:, 128:192]
    scanmask = cst[:, 320:832]
    eye64 = cst[:, 832:896]
    w0c, a0c, kkc, kac, rkc = (rwvec[:, 0:4], rwvec[:, 4:8], rwvec[:, 8:12], rwvec[:, 12:16], rwvec[:, 16:20])

    zb = ar.alloc([128, 14, 513], F32, "zb")
    zs = ar.alloc([128, 14, 512], F32, "zs")
    tmp = [ar.alloc([128, 512], F32, "tmpB%d" % i) for i in range(3)]
    B_tmp = [Buf("tmpB%d" % i) for i in range(3)]
    th = ar.alloc([128, 512], BF16, "th")
    al16 = ar.alloc([128, 512], BF16, "al16")
    sgl = ar.alloc([128, 512], BF16, "sgl")
    sq16 = ar.alloc([128, 512], BF16, "sq16")
    asg = ar.alloc([128, 512], F32, "asg")
    kk = ar.alloc([128, 512], F32, "kk")
    kp = ar.alloc([128, 512], F32, "kp")
    bv = ar.alloc([128, 512], F32, "bv")
    lw = ar.alloc([128, 512], F32, "lw")
    cs = ar.alloc([128, 512], F32, "cs")
    E = [ar.alloc([128, 512], F32, "E%d" % i) for i in range(4)]
    E5 = ar.alloc([128, 4, 8], F32, "E5")
    AR = ar.alloc([128, 4, 8, 2, 64], BF16, "AR")
    BK = ar.alloc([128, 4, 8, 2, 64], BF16, "BK")
    KH = ar.alloc([128, 4, 512], BF16, "KH")
    BH = ar.alloc([128, 4, 512], BF16, "BH")
    Vb = ar.alloc([128, 4, 512], BF16, "Vb")
    rkr = ar.alloc([128, 4, 512], BF16, "rkr")
    rwoT = ar.alloc([128, 4, 512], BF16, "rwoT")
    B_zb, B_zs, B_pre, B_blk, B_rwoT = Buf("zb"), Buf("zs"), Buf("pre"), Buf("blk"), Buf("rwoT")
    Vt2p = [ar.alloc([128, 512], BF16, "Vt2_%d" % i) for i in range(2)]
    KHt2p = [ar.alloc([128, 512], BF16, "KHt2_%d" % i) for i in range(2)]
    BHt2p = [ar.alloc([128, 512], BF16, "BHt2_%d" % i) for i in range(2)]
    sABp = [ar.alloc([128, 4, 128], BF16, "sAB%d" % i) for i in range(2)]
    sAKp = [ar.alloc([128, 4, 128], BF16, "sAK%d" % i) for i in range(2)]
    Xfp = [ar.alloc([128, 4, 64], BF16, "Xf%d" % i) for i in range(2)]
    B_Vtp = [Buf("Vt0"), Buf("Vt1")]
    B_sAp = [Buf("sA0"), Buf("sA1")]
    B_Xfp = [Buf("Xf0"), Buf("Xf1")]
    Mx = [ar.alloc([128, 4, 64], BF16, "Mx%d" % i) for i in range(2)]
    MT = [ar.alloc([128, 4, 64], BF16, "MT%d" % i) for i in range(2)]
    X = [ar.alloc([128, 4, 64], BF16, "X%d" % i) for i in range(2)]
    RHSs = ar.alloc([128, 4, 64], BF16, "RHSs")
    SAs = ar.alloc([128, 4, 64], BF16, "SAs")
    ST = ar.alloc([128, 4, 64], BF16, "ST")
    STf = ar.alloc([128, 4, 64], F32, "STf")
    sqy = ar.alloc([128, 256], F32, "sqy")
    yn = ar.alloc([128, 256], F32, "yn")
    bon = ar.alloc([128, 256], F32, "bon")
    O16 = ar.alloc([128, 256], BF16, "O16")
    st8 = ar.alloc([128, 4, 8], F32, "st8")
    B_Vt, B_sA, B_M, B_MT, B_X, B_R, B_SA, B_ST, B_ep, B_st8, B_O = (Buf("Vt"), Buf("sA"), [Buf("M0"), Buf("M1")], [Buf("MT0"), Buf("MT1")],
                                                                    [Buf("X0"), Buf("X1")], Buf("R"), Buf("SA"), Buf("ST"), Buf("ep"), Buf("st8"), Buf("O"))
    e0 = make_e0(L, 2, 7) if ('E' in L["phases"] or 'E0' in L["phases"]) else iter(())
    V(lambda e: e.memset(STf[:, :, :], 0.0), [], [B_ST])
    V(lambda e: e.memset(ST[:, :, :], 0.0), [], [B_ST])
    V(lambda e: e.memset(zb[:, :, 0:1], 0.0), [], [B_zb])

    def v3(ap2):
        return ap2.rearrange("p (c t) -> p c t", t=64)

    for tb in range(L['nblk']):
        for i in range(14):
            if tb == 0:
                sc.dma("sp", zb[:, i, 1:513], zrw_d[i * 128:(i + 1) * 128, 0:512], writes=[B_zb])
            else:
                sc.dma("sp", zb[:, i, 0:513], zrw_d[i * 128:(i + 1) * 128, tb * 512 - 1:tb * 512 + 512], writes=[B_zb])
        for i in range(14):
            t0 = tmp[i % 2]
            bt0 = B_tmp[i % 2]
            V(lambda e, i=i, t0=t0: e.tensor_tensor(out=t0[:, :], in0=zb[:, i, 0:512], in1=zb[:, i, 1:513], op=ALU.subtract), [B_zb], [bt0])
            V(lambda e, i=i, t0=t0: e.scalar_tensor_tensor(out=zs[:, i, :], in0=t0[:, :], scalar=mu[:, i:i + 1], in1=zb[:, i, 1:513],
                                                           op0=ALU.mult, op1=ALU.add), [bt0, B_zb, B_c], [B_zs])
        A(lambda e: e.activation(out=th[:, :], in_=zs[:, 12, :], func=AF.Tanh), [B_zs], [B_pre])
        V(lambda e: e.tensor_copy(out=al16[:, :], in_=zs[:, 12, :]), [B_zs], [B_pre])
        A(lambda e: e.activation(out=sgl[:, :], in_=zs[:, 13, :], func=AF.Sigmoid), [B_zs], [B_blk])
        for j in range(4):
            pW, bW = ps[0], B_ps[0]
            pA, bA = ps[1], B_ps[1]
            pQ, bQ = ps[2], B_ps[2]
            P(lambda e, j=j: e.matmul(pW[:, :], lhsT=w2a2b[0:64, j * 128:(j + 1) * 128], rhs=th[0:64, :], start=True, stop=True), [B_c, B_pre], [bW])
            P(lambda e, j=j: e.matmul(pA[:, :], lhsT=w2a2b[64:128, j * 128:(j + 1) * 128], rhs=al16[64:128, :], start=True, stop=True), [B_c, B_pre], [bA])
            A(lambda e, j=j: e.activation(out=lw[:, :], in_=pW[:, :], func=AF.Sigmoid, bias=w0c[:, j:j + 1]), [bW, B_c], [B_pre])
            A(lambda e, j=j: e.activation(out=asg[:, :], in_=pA[:, :], func=AF.Sigmoid, bias=a0c[:, j:j + 1]), [bA, B_c], [B_pre])
            A(lambda e, j=j: e.activation(out=sq16[:, :], in_=zs[:, 4 + j, :], func=AF.Square, scale=kkc[:, j:j + 1]), [B_zs, B_c], [B_pre])
            P(lambda e: e.matmul(pQ[:, :], lhsT=bones[:, :], rhs=sq16[:, :], start=True, stop=True), [B_c, B_pre], [bQ])
            V(lambda e: e.tensor_scalar(out=tmp[2][:, :], in0=pQ[:, :], scalar1=1e-24, scalar2=None, op0=ALU.max), [bQ], [B_tmp[2]])
            A(lambda e: e.activation(out=tmp[2][:, :], in_=tmp[2][:, :], func=AF.Sqrt), [B_tmp[2]], [B_tmp[2]])
            V(lambda e: e.reciprocal(out=tmp[2][:, :], in_=tmp[2][:, :]), [B_tmp[2]], [B_tmp[2]])
            V(lambda e, j=j: e.scalar_tensor_tensor(out=kk[:, :], in0=zs[:, 4 + j, :], scalar=kkc[:, j:j + 1], in1=tmp[2][:, :],
                                                    op0=ALU.mult, op1=ALU.mult), [B_zs, B_tmp[2], B_c], [B_pre])
            V(lambda e, j=j: e.tensor_scalar(out=tmp[0][:, :], in0=asg[:, :], scalar1=kac[:, j:j + 1], scalar2=omka[:, j:j + 1],
                                             op0=ALU.mult, op1=ALU.add), [B_pre, B_c], [B_tmp[0]])
            V(lambda e, j=j: e.tensor_tensor(out=kp[:, :], in0=zs[:, 4 + j, :], in1=tmp[0][:, :], op=ALU.mult), [B_zs, B_tmp[0]], [B_pre])
            V(lambda e: e.tensor_tensor(out=bv[:, :], in0=kk[:, :], in1=asg[:, :], op=ALU.mult), [B_pre], [B_pre])
            V(lambda e: e.tensor_scalar(out=lw[:, :], in0=lw[:, :], scalar1=-0.6065306597126334, scalar2=None, op0=ALU.mult), [B_pre], [B_pre])
            V(lambda e: e.tensor_tensor_scan(out=cs[:, :], data0=scanmask, data1=lw[:, :], initial=0.0, op0=ALU.mult, op1=ALU.add), [B_pre, B_c], [B_pre])
            V(lambda e: e.tensor_tensor(out=tmp[0][:, :], in0=cs[:, :], in1=lw[:, :], op=ALU.subtract), [B_pre], [B_tmp[0]])
            V(lambda e: e.tensor_tensor(out=v3(tmp[1][:, :]), in0=v3(cs[:, :])[:, :, 63:64].to_broadcast([128, 8, 64]), in1=v3(cs[:, :]),
                                        op=ALU.subtract), [B_pre], [B_tmp[1]])
            A(lambda e: e.activation(out=E[0][:, :], in_=cs[:, :], func=AF.Exp), [B_pre], [B_pre])
            A(lambda e: e.activation(out=E[1][:, :], in_=cs[:, :], func=AF.Exp, scale=-1.0), [B_pre], [B_pre])
            A(lambda e: e.activation(out=E[2][:, :], in_=tmp[0][:, :], func=AF.Exp), [B_tmp[0]], [B_pre])
            A(lambda e: e.activation(out=E[3][:, :], in_=tmp[1][:, :], func=AF.Exp), [B_tmp[1]], [B_pre])
            V(lambda e, j=j: e.tensor_copy(out=E5[:, j, :], in_=v3(E[0][:, :])[:, :, 63]), [B_pre], [B_blk])
            V(lambda e, j=j: e.tensor_tensor(out=AR[:, j, :, 1, :], in0=v3(zs[:, j, :]), in1=v3(E[0][:, :]), op=ALU.mult), [B_zs, B_pre], [B_blk])
            V(lambda e, j=j: e.tensor_tensor(out=BK[:, j, :, 1, :], in0=v3(kp[:, :]), in1=v3(E[1][:, :]), op=ALU.mult), [B_pre], [B_blk])
            V(lambda e, j=j: e.tensor_tensor(out=BK[:, j, :, 0, :], in0=v3(bv[:, :]), in1=v3(E[1][:, :]), op=ALU.mult), [B_pre], [B_blk])
            V(lambda e, j=j: e.scalar_tensor_tensor(out=AR[:, j, :, 0, :], in0=v3(kk[:, :]), scalar=-1.0, in1=v3(E[2][:, :]),
                                                    op0=ALU.mult, op1=ALU.mult), [B_pre], [B_blk])
            V(lambda e, j=j: e.tensor_tensor(out=KH[:, j, :], in0=kp[:, :], in1=E[3][:, :], op=ALU.mult), [B_pre], [B_blk])
            V(lambda e, j=j: e.tensor_tensor(out=BH[:, j, :], in0=bv[:, :], in1=E[3][:, :], op=ALU.mult), [B_pre], [B_blk])
            A(lambda e, j=j: e.activation(out=Vb[:, j, :], in_=zs[:, 8 + j, :], func=AF.Copy), [B_zs], [B_blk])
            V(lambda e, j=j: e.scalar_tensor_tensor(out=rkr[:, j, :], in0=zs[:, j, :], scalar=rkc[:, j:j + 1], in1=kp[:, :],
                                                    op0=ALU.mult, op1=ALU.mult), [B_zs, B_pre, B_c], [B_blk])

        def v4(ap2):
            return ap2.rearrange("p (j t) -> p j t", t=64)
        hl = [(h // 2, slice((h % 2) * 64, (h % 2) * 64 + 64), slice((h // 2) * 64, (h // 2) * 64 + 64), slice(h * 64, (h + 1) * 64)) for h in range(8)]

        def pre(c):
            q = c % 2
            csl = slice(c * 64, (c + 1) * 64)
            Vt2, KHt2, BHt2, sAB, sAK = Vt2p[q], KHt2p[q], BHt2p[q], sABp[q], sAKp[q]
            B_Vt, B_sA = B_Vtp[q], B_sAp[q]
            pT = ps[3][:, :].bitcast(BF16)
            pT2 = ps[4][:, :].bitcast(BF16)
            for half in range(2):
                hp = slice(half * 64, half * 64 + 64)
                for j in range(4):
                    P(lambda e: e.transpose(pT[hp, j * 128:(j + 1) * 128], Vb[:, j, csl], identb[:, :]), [B_blk, B_ident], [B_ps[3]])
                    P(lambda e: e.transpose(pT[hp, 512 + j * 128:512 + (j + 1) * 128], KH[:, j, csl], identb[:, :]), [B_blk, B_ident], [B_ps[3]])
                    P(lambda e: e.transpose(pT2[hp, j * 128:(j + 1) * 128], BH[:, j, csl], identb[:, :]), [B_blk, B_ident], [B_ps[4]])
            yield
            V(lambda e: e.tensor_copy(out=Vt2[:, :], in_=pT[:, 0:512]), [B_ps[3]], [B_Vt])
            A(lambda e: e.activation(out=KHt2[:, :], in_=pT[:, 512:1024], func=AF.Copy), [B_ps[3]], [B_Vt])
            A(lambda e: e.activation(out=BHt2[:, :], in_=pT2[:, 0:512], func=AF.Copy), [B_ps[4]], [B_Vt])
            for (j, pp, js, hs) in hl:
                P(lambda e: e.matmul(ps[0][pp, j * 128:(j + 1) * 128], lhsT=BK[pp, j, c, 0, :], rhs=AR[pp, j, c, :, :], start=True, stop=True), [B_blk], [B_ps[0]])
                P(lambda e: e.matmul(ps[1][pp, j * 128:(j + 1) * 128], lhsT=BK[pp, j, c, 1, :], rhs=AR[pp, j, c, :, :], start=True, stop=True), [B_blk], [B_ps[1]])
                P(lambda e: e.matmul(ps[2][pp, j * 64:(j + 1) * 64], lhsT=AR[pp, j, c, 0, :], rhs=BK[pp, j, c, 0, :], start=True, stop=True), [B_blk], [B_ps[2]])
            yield
            V(lambda e: e.tensor_tensor(out=sAB[:, :, :], in0=ps[0][:, :].rearrange("p (j t) -> p j t", t=128), in1=_bc_mid(maskA, 4), op=ALU.mult), [B_ps[0], B_c], [B_sA])
            V(lambda e: e.tensor_tensor(out=sAK[:, :, :], in0=ps[1][:, :].rearrange("p (j t) -> p j t", t=128), in1=_bc_mid(maskA, 4), op=ALU.mult), [B_ps[1], B_c], [B_sA])
            V(lambda e: e.tensor_tensor(out=MT[0][:, :, :], in0=v4(ps[2][:, 0:256]), in1=_bc_mid(maskT, 4), op=ALU.mult), [B_ps[2], B_c], [B_MT[0]])
            V(lambda e: e.tensor_copy(out=Mx[0][:, :, :], in_=sAB[:, :, 0:64]), [B_sA], [B_M[0]])
            V(lambda e: e.tensor_tensor(out=X[0][:, :, :], in0=sAB[:, :, 0:64], in1=_bc_mid(eye64, 4), op=ALU.add), [B_sA, B_c], [B_X[0]])
            yield
            cur = 0
            pa, pb, pc = ps[2], ps[3], ps[4]
            for rd in range(5):
                nxt = 1 - cur
                for (j, pp, js, hs) in hl:
                    P(lambda e: e.matmul(pa[pp, js], lhsT=MT[cur][pp, j, :], rhs=Mx[cur][pp, j, :], start=True, stop=True), [B_MT[cur], B_M[cur]], [B_ps[2]])
                    P(lambda e: e.matmul(pb[pp, js], lhsT=Mx[cur][pp, j, :], rhs=MT[cur][pp, j, :], start=True, stop=True), [B_MT[cur], B_M[cur]], [B_ps[3]])
                yield
                V(lambda e: e.tensor_copy(out=Mx[nxt][:, :, :], in_=v4(pa[:, 0:256])), [B_ps[2]], [B_M[nxt]])
                A(lambda e: e.activation(out=MT[nxt][:, :, :], in_=v4(pb[:, 0:256]), func=AF.Copy), [B_ps[3]], [B_MT[nxt]])
                for (j, pp, js, hs) in hl:
                    P(lambda e: e.matmul(pc[pp, js], lhsT=MT[nxt][pp, j, :], rhs=X[cur][pp, j, :], start=True, stop=True), [B_MT[nxt], B_X[cur]], [B_ps[4]])
                yield
                if rd < 4:
                    V(lambda e: e.tensor_tensor(out=X[nxt][:, :, :], in0=X[cur][:, :, :], in1=v4(pc[:, 0:256]), op=ALU.add), [B_X[cur], B_ps[4]], [B_X[nxt]])
                else:
                    V(lambda e: e.tensor_tensor(out=Xfp[q][:, :, :], in0=X[cur][:, :, :], in1=v4(pc[:, 0:256]), op=ALU.add), [B_X[cur], B_ps[4]], [B_Xfp[q]])
                cur = nxt
                yield

        def post(c):
            q = c % 2
            csl = slice(c * 64, (c + 1) * 64)
            Vt2, KHt2, BHt2, sAB, sAK, Xf = Vt2p[q], KHt2p[q], BHt2p[q], sABp[q], sAKp[q], Xfp[q]
            B_Vt, B_sA, bXf = B_Vtp[q], B_sAp[q], B_Xfp[q]
            pR, bR = ps[5], B_ps[5]
            pY, bY = ps[6], B_ps[6]
            pU, bU = ps[7], B_ps[7]
            for (j, pp, js, hs) in hl:
                P(lambda e: e.matmul(pR[pp, js], lhsT=AR[pp, j, c, 0, :], rhs=ST[pp, j, :], start=True, stop=False), [B_blk, B_ST], [bR])
                P(lambda e: e.matmul(pR[pp, js], lhsT=sAK[pp, j, 0:64], rhs=Vt2[pp, hs], start=False, stop=True), [B_sA, B_Vt], [bR])
            yield
            V(lambda e: e.tensor_copy(out=RHSs[:, :, :], in_=v4(pR[:, 0:256])), [bR], [B_R])
            for (j, pp, js, hs) in hl:
                P(lambda e: e.matmul(pR[pp, js], lhsT=Xf[pp, j, :], rhs=RHSs[pp, j, :], start=True, stop=True), [bXf, B_R], [bR])
            yield
            V(lambda e: e.tensor_copy(out=SAs[:, :, :], in_=v4(pR[:, 0:256])), [bR], [B_SA])
            for (j, pp, js, hs) in hl:
                P(lambda e: e.matmul(pY[pp, js], lhsT=AR[pp, j, c, 1, :], rhs=ST[pp, j, :], start=True, stop=False), [B_blk, B_ST], [bY])
                P(lambda e: e.matmul(pY[pp, js], lhsT=sAK[pp, j, 64:128], rhs=Vt2[pp, hs], start=False, stop=False), [B_sA, B_Vt], [bY])
                P(lambda e: e.matmul(pY[pp, js], lhsT=sAB[pp, j, 64:128], rhs=SAs[pp, j, :], start=False, stop=True), [B_sA, B_SA], [bY])
            for (j, pp, js, hs) in hl:
                P(lambda e: e.matmul(pU[pp, js], lhsT=KHt2[pp, hs], rhs=Vt2[pp, hs], start=True, stop=False), [B_Vt], [bU])
                P(lambda e: e.matmul(pU[pp, js], lhsT=BHt2[pp, hs], rhs=SAs[pp, j, :], start=False, stop=True), [B_Vt, B_SA], [bU])
            yield
            V(lambda e: e.tensor_tensor(out=STf[:, :, :], in0=STf[:, :, :], in1=E5[:, :, c:c + 1].to_broadcast([128, 4, 64]), op=ALU.mult), [B_blk, B_ST, bY, bR], [B_ST])
            V(lambda e: e.tensor_tensor(out=STf[:, :, :], in0=STf[:, :, :], in1=v4(pU[:, 0:256]), op=ALU.add), [bU, B_ST], [B_ST])
            V(lambda e: e.tensor_copy(out=ST[:, :, :], in_=STf[:, :, :]), [B_ST], [B_ST])
            pG, bG = ps[5], B_ps[5]
            for (j, pp, js, hs) in hl:
                P(lambda e: e.matmul(pG[pp, 256 + j:256 + j + 1], lhsT=rkr[pp, j, csl], rhs=onesb[pp, 0:1], start=True, stop=True), [B_blk, B_c, B_SA], [bG])
                P(lambda e: e.matmul(pG[pp, js], lhsT=sgl[:, csl], rhs=g2b[:, hs], start=True, stop=True), [B_blk, B_c, B_SA, B_R], [bG])
            y3 = v4(pY[:, 0:256])
            V(lambda e: e.tensor_reduce(out=st8[:, :, 0], in_=y3, axis=AX.X, op=ALU.add), [bY], [B_st8])
            A(lambda e: e.activation(out=sqy[:, :], in_=pY[:, 0:256], func=AF.Square), [bY], [B_ep])
            yield
            V(lambda e: e.tensor_reduce(out=st8[:, :, 1], in_=v4(sqy[:, :]), axis=AX.X, op=ALU.add), [B_ep], [B_st8])
            V(lambda e: e.tensor_scalar(out=st8[:, :, 2], in0=st8[:, :, 0], scalar1=1.0 / 64, scalar2=None, op0=ALU.mult), [B_st8], [B_st8])
            V(lambda e: e.tensor_tensor(out=st8[:, :, 3], in0=st8[:, :, 2], in1=st8[:, :, 2], op=ALU.mult), [B_st8], [B_st8])
            V(lambda e: e.scalar_tensor_tensor(out=st8[:, :, 4], in0=st8[:, :, 1], scalar=1.0 / 64, in1=st8[:, :, 3], op0=ALU.mult, op1=ALU.subtract), [B_st8], [B_st8])
            V(lambda e: e.tensor_scalar(out=st8[:, :, 4], in0=st8[:, :, 4], scalar1=64e-5, scalar2=None, op0=ALU.add), [B_st8], [B_st8])
            A(lambda e: e.activation(out=st8[:, :, 5], in_=st8[:, :, 4], func=AF.Sqrt), [B_st8], [B_st8])
            yield
            V(lambda e: e.reciprocal(out=st8[:, :, 5], in_=st8[:, :, 5]), [B_st8], [B_st8])
            yn3 = v4(yn[:, :])
            V(lambda e: e.tensor_tensor(out=yn3, in0=y3, in1=st8[:, :, 2:3].to_broadcast([128, 4, 64]), op=ALU.subtract), [bY, B_st8], [B_ep])
            V(lambda e: e.tensor_tensor(out=yn3, in0=yn3, in1=st8[:, :, 5:6].to_broadcast([128, 4, 64]), op=ALU.mult), [B_ep, B_st8], [B_ep])
            V(lambda e: e.tensor_tensor(out=yn[:, :], in0=yn[:, :], in1=gnbc[:, 0, :], op=ALU.mult), [B_ep, B_c], [B_ep])
            V(lambda e: e.tensor_tensor(out=yn[:, :], in0=yn[:, :], in1=gnbc[:, 1, :], op=ALU.add), [B_ep, B_c], [B_ep])
            V(lambda e: e.tensor_copy(out=st8[:, :, 6], in_=pG[:, 256:260]), [bG], [B_st8])
            for half in range(2):
                hp = slice(half * 64, half * 64 + 64)
                V(lambda e: e.tensor_tensor(out=v4(bon[hp, :]), in0=Vt2[hp, :].rearrange("p (j q t) -> p j q t", q=2, t=64)[:, :, half, :],
                                            in1=st8[hp, :, 6:7].to_broadcast([64, 4, 64]), op=ALU.mult), [B_Vt, B_st8], [B_ep])
            V(lambda e: e.tensor_tensor(out=yn[:, :], in0=yn[:, :], in1=bon[:, :], op=ALU.add), [B_ep], [B_ep])
            V(lambda e: e.tensor_tensor(out=O16[:, :], in0=yn[:, :], in1=pG[:, 0:256], op=ALU.mult), [B_ep, bG], [B_O])
            pO = pU[:, 384:512].bitcast(BF16)
            for (j, pp, js, hs) in hl:
                P(lambda e: e.transpose(pO[pp, js], O16[pp, js], identb[pp, pp]), [B_O, B_ident, B_ST], [bU])
            yield
            V(lambda e: e.tensor_copy(out=rwoT[:, :, csl], in_=v4(pO[:, 0:256])), [bU], [B_rwoT])

        def run_both(ga, gb):
            alive_a, alive_b = ga is not None, gb is not None
            while alive_a or alive_b:
                if alive_a:
                    try:
                        next(ga)
                    except StopIteration:
                        alive_a = False
                if alive_b:
                    try:
                        next(gb)
                    except StopIteration:
                        alive_b = False

        run_both(pre(0), None)
        for c in range(8):
            for _ in range(4):
                next(e0, None)
            run_both(post(c), pre(c + 1) if c + 1 < 8 else None)
        for j in range(4):
            sc.dma("sp", rwo_d[j * 128:(j + 1) * 128, tb * 512:(tb + 1) * 512], rwoT[:, j, :], reads=[B_rwoT])
    for _ in e0:
        pass
    if 'E' in L["phases"]:
        L["e0_done_flag"][0] = True
    ar.release(mB)


def phase_D(L):
    nc, sc, ar, ps, B_ps = L["nc"], L["sc"], L["ar"], L["ps"], L["B_ps"]
    ident, identb, B_ident, load_bf16 = L["ident"], L["identb"], L["B_ident"], L["load_bf16"]
    gt_bc, B_gt = L["gt_bc"], L["B_gt"]
    dbg = L["dbg"]
    V = lambda fn, r=(), w=(): sc.op("dve", fn, r, w)
    A = lambda fn, r=(), w=(): sc.op("act", fn, r, w)
    P = lambda fn, r=(), w=(): sc.op("pe", fn, r, w)
    ALPHA = 2.0 ** 0.25
    mD = ar.mark()
    wbra = ar.alloc([128, 4, D], BF16, "wbra")
    wbrb = ar.alloc([128, 8, D], BF16, "wbrb")
    wout = ar.alloc([128, 8, D], BF16, "wout")
    wqb = ar.alloc([128, 8, D], BF16, "wqb")
    keysT = ar.alloc([128, 8, 128], BF16, "keysT")
    lnbc = ar.alloc([128, 2, D], F32, "lnbc")
    B_w = Buf("wD")
    for kc in range(4):
        load_bf16(wbra[:, kc, :], L["wbra_d"][kc * 128:(kc + 1) * 128, :], D, B_w)
    for kc in range(8):
        load_bf16(wbrb[:, kc, :], L["wbrb_d"][kc * 128:(kc + 1) * 128, :], D, B_w)
        load_bf16(wout[:, kc, :], L["wout_d"][kc * 128:(kc + 1) * 128, :], D, B_w)
        load_bf16(wqb[:, kc, :], L["wq_d"][kc * 128:(kc + 1) * 128, :], D, B_w)
    load_bf16(keysT[:, :, :].rearrange("p a b -> p (a b)"), L["pkeys_d"][:, :, :].rearrange("p a b -> p (a b)"), 1024, B_w)
    sc.dma("sp", lnbc[:, :, :], L["lnbc_d"][:, 0:2, :], writes=[B_w])
    rwoB = ar.alloc([128, 4, 512], BF16, "rwoB")
    dsaB = ar.alloc([128, 8, 512], BF16, "dsaB")
    zgr = [ar.alloc([128, 2, 512], BF16, "zgr%d" % i) for i in range(2)]
    B_zgr = [Buf("zgr0"), Buf("zgr1")]
    mg = ar.alloc([128, 8, 512], BF16, "mg")
    t1 = ar.alloc([128, 512], F32, "t1")
    t2 = ar.alloc([128, 512], F32, "t2")
    B_in, B_mg, B_t = Buf("inD"), Buf("mg"), Buf("tD")
    xt = ar.alloc([128, D], F32, "xtD")
    u = ar.alloc([128, D], F32, "uD")
    x1 = ar.alloc([128, D], F32, "x1")
    h2 = ar.alloc([128, D], F32, "h2")
    st = ar.alloc([128, 8], F32, "stD")
    h2T = ar.alloc([128, 8, 128], BF16, "h2T")
    qT = ar.alloc([128, 8, 128], BF16, "qT")
    ssb = ar.alloc([128, 16, 128], F32, "ssb")
    stmp = ar.alloc([128, 256], F32, "stmp")
    tv = ar.alloc([128, 16, 16], F32, "tv")
    ti = ar.alloc([128, 16, 16], U32, "ti")
    tif = ar.alloc([128, 16, 16], F32, "tif")
    cand = ar.alloc([128, 8, 256], F32, "cand")
    mv = ar.alloc([128, 8, 16], F32, "mv")
    posu = ar.alloc([128, 8, 16], U32, "posu")
    au = ar.alloc([128, 8, 16], U32, "au")
    bu = ar.alloc([128, 8, 16], U32, "bu")
    abf = ar.alloc([128, 2, 128], F32, "abf")
    oh16 = ar.alloc([128, 128, 16], F32, "oh16")
    junk = oh16[:, 0:64, :].rearrange("p a b -> p (a b)")
    sel = ar.alloc([128, 3, 128], F32, "sel")
    selT = ar.alloc([128, 3, 128], F32, "selT")
    gate = ar.alloc([128, 8, 16], F32, "gate")
    gs = ar.alloc([128, 8], F32, "gs")
    iota16 = L["iota16"]
    B_oh, B_sel, B_selT = Buf("oh"), Buf("sel"), Buf("selT")
    B_x, B_u, B_x1, B_h2, B_st, B_h2T, B_qT, B_s, B_tk, B_c, B_e, B_hu, B_acc, B_j = [Buf(n) for n in
        ("x", "u", "x1", "h2", "st", "h2T", "qT", "s", "tk", "cand", "eid", "hu", "acc", "junk")]
    x_v = L["x_d"].rearrange("(n p) m -> p n m", p=128)
    out_v = L["out_d"].rearrange("(n p) m -> p n m", p=128)

    def layer_norm(src, bsrc, dst, bdst, gi):
        V(lambda e: e.tensor_reduce(out=st[:, 0:1], in_=src[:, :], axis=AX.X, op=ALU.add), [bsrc], [B_st])
        A(lambda e: e.activation(out=junk, in_=src[:, :], func=AF.Square, accum_out=st[:, 1:2]), [bsrc], [B_st, B_oh])
        V(lambda e: e.tensor_scalar(out=st[:, 2:3], in0=st[:, 0:1], scalar1=1.0 / D, scalar2=None, op0=ALU.mult), [B_st], [B_st])
        V(lambda e: e.tensor_tensor(out=st[:, 3:4], in0=st[:, 2:3], in1=st[:, 2:3], op=ALU.mult), [B_st], [B_st])
        V(lambda e: e.scalar_tensor_tensor(out=st[:, 4:5], in0=st[:, 1:2], scalar=1.0 / D, in1=st[:, 3:4], op0=ALU.mult, op1=ALU.subtract), [B_st], [B_st])
        V(lambda e: e.tensor_scalar(out=st[:, 4:5], in0=st[:, 4:5], scalar1=1e-5, scalar2=None, op0=ALU.add), [B_st], [B_st])
        A(lambda e: e.activation(out=st[:, 5:6], in_=st[:, 4:5], func=AF.Sqrt), [B_st], [B_st])
        V(lambda e: e.reciprocal(out=st[:, 5:6], in_=st[:, 5:6]), [B_st], [B_st])
        V(lambda e: e.tensor_scalar(out=dst[:, :], in0=src[:, :], scalar1=st[:, 2:3], scalar2=st[:, 5:6], op0=ALU.subtract, op1=ALU.mult), [bsrc, B_st], [bdst])
        V(lambda e: e.tensor_tensor(out=dst[:, :], in0=dst[:, :], in1=lnbc[:, gi, :], op=ALU.mult), [bdst, B_w], [bdst])
        V(lambda e: e.tensor_tensor(out=dst[:, :], in0=dst[:, :], in1=lnbc[:, gi + 1, :], op=ALU.add), [bdst, B_w], [bdst])

    x1p = [x1, ar.alloc([128, D], F32, "x1b")]
    B_x1p = [B_x1, Buf("x1b")]
    h2Tp_ = [h2T, ar.alloc([128, 8, 128], BF16, "h2Tb")]
    B_h2Tp_ = [B_h2T, Buf("h2Tb")]
    ssbp = [ssb, ar.alloc([128, 16, 128], F32, "ssbb")]
    B_sp = [B_s, Buf("ssbb")]
    prevn = [None]
    B_ohh = [Buf("ohh%d" % i) for i in range(8)]
    stmp16 = ar.alloc([128, 16, 128], F32, "stmp16")
    stmp8 = stmp16[:, :, :].rearrange("p (h a) b -> p h (a b)", a=2)
    B_tkg = [Buf("tkg%d" % i) for i in range(16)]
    B_tkg2 = [Buf("tkgb%d" % i) for i in range(16)]
    B_stg16 = [Buf("stg16_%d" % i) for i in range(16)]
    B_tig = [Buf("tig%d" % i) for i in range(16)]
    B_tig2 = [Buf("tigb%d" % i) for i in range(16)]
    B_mvh = [Buf("mvh%d" % i) for i in range(8)]
    B_mvh2 = [Buf("mvhb%d" % i) for i in range(8)]
    B_st8h = [B_stg16[2 * i] for i in range(8)]
    B_posh = [Buf("posh%d" % i) for i in range(8)]
    B_posh2 = [Buf("poshb%d" % i) for i in range(8)]

    def stageA(n, jt):
        q = n % 2
        x1_, bx1_, h2T_, bh2T_, ssb_, bs_ = x1p[q], B_x1p[q], h2Tp_[q], B_h2Tp_[q], ssbp[q], B_sp[q]
        sc.dma("sp", xt[:, :], x_v[:, n, :], writes=[B_x])
        for half in range(2):
            pM, bM = ps[4 + half], B_ps[4 + half]
            hsl = slice(half * 512, (half + 1) * 512)
            for dc in range(8):
                P(lambda e: e.matmul(pM[:, :], lhsT=mg[:, dc, jt * 128:(jt + 1) * 128], rhs=wout[:, dc, hsl], start=(dc == 0), stop=(dc == 7)), [B_mg, B_w], [bM])
            V(lambda e: e.tensor_tensor(out=u[:, hsl], in0=pM[:, :], in1=gt_bc[:, 0, hsl], op=ALU.mult), [bM, B_gt], [B_u])
            V(lambda e: e.scalar_tensor_tensor(out=u[:, hsl], in0=xt[:, hsl], scalar=ALPHA, in1=u[:, hsl], op0=ALU.mult, op1=ALU.add), [B_x, B_u], [B_u])
        layer_norm(u, B_u, x1_, bx1_, 0)
        if "x1dbg" in dbg:
            sc.dma("sp", L["x1_d"][n * 128:(n + 1) * 128, :], x1_[:, :], reads=[bx1_])
        sc.dma("sp", L["x1s_d"][n * 128:(n + 1) * 128, :], x1_[:, :], reads=[bx1_])
        V(lambda e: e.tensor_tensor(out=h2[:, :], in0=x1_[:, :], in1=gt_bc[:, 2, :], op=ALU.mult), [bx1_, B_gt], [B_h2])
        V(lambda e: e.tensor_tensor(out=h2[:, :], in0=h2[:, :], in1=gt_bc[:, 1, :], op=ALU.add), [B_h2, B_gt], [B_h2])
        for kc in range(8):
            pp_, bp_ = ps[kc // 4], B_ps[kc // 4]
            P(lambda e: e.transpose(pp_[:, (kc % 4) * 128:(kc % 4 + 1) * 128], h2[:, kc * 128:(kc + 1) * 128], ident[:, :]), [B_h2, B_ident], [bp_])
        for k2 in range(2):
            A(lambda e: e.activation(out=h2T_[:, k2 * 4:(k2 + 1) * 4, :], in_=ps[k2][:, :].rearrange("p (a b) -> p a b", b=128), func=AF.Copy), [B_ps[k2]], [bh2T_])
        for kc in range(8):
            sc.dma("sp", L["h2T_d"][kc * 128:(kc + 1) * 128, n * 128:(n + 1) * 128], h2T_[:, kc, :], reads=[bh2T_])
        for hh in range(8):
            pq, bq = ps[2 + hh // 4], B_ps[2 + hh // 4]
            for kc in range(8):
                P(lambda e: e.matmul(pq[:, (hh % 4) * 128:(hh % 4 + 1) * 128], lhsT=wqb[:, kc, hh * 128:(hh + 1) * 128], rhs=h2T_[:, kc, :],
                                     start=(kc == 0), stop=(kc == 7)), [B_w, bh2T_], [bq])
        for k2 in range(2):
            A(lambda e: e.activation(out=qT[:, k2 * 4:(k2 + 1) * 4, :], in_=ps[2 + k2][:, :].rearrange("p (a b) -> p a b", b=128), func=AF.Copy), [B_ps[2 + k2]], [B_qT])
        for g in range(16):
            hh, cc = g % 8, g // 8
            pS, bS = ps[4 + g // 4], B_ps[4 + g // 4]
            P(lambda e: e.matmul(pS[:, (g % 4) * 128:(g % 4 + 1) * 128], lhsT=qT[cc * 64:(cc + 1) * 64, hh, :], rhs=keysT[cc * 64:(cc + 1) * 64, hh, :],
                                 start=True, stop=True), [B_qT, B_w], [bS])
        for k4 in range(4):
            A(lambda e: e.activation(out=ssb_[:, k4 * 4:(k4 + 1) * 4, :], in_=ps[4 + k4][:, :].rearrange("p (a b) -> p a b", b=128), func=AF.Copy), [B_ps[4 + k4]], [bs_])

    def stageB(n):
        q = n % 2
        ssb_, bs_ = ssbp[q], B_sp[q]
        for g in range(16):
            V(lambda e: e.max(out=tv[:, g, 0:8], in_=ssb_[:, g, :]), [bs_], [B_tkg[g]])
        for g in range(16):
            V(lambda e: e.match_replace(out=stmp16[:, g, :], in_to_replace=tv[:, g, 0:8], in_values=ssb_[:, g, :], imm_value=-1e30), [bs_, B_tkg[g]], [B_stg16[g]])
        for g in range(16):
            V(lambda e: e.max(out=tv[:, g, 8:16], in_=stmp16[:, g, :]), [B_stg16[g]], [B_tkg2[g]])
        for g in range(16):
            V(lambda e: e.max_index(out=ti[:, g, 0:8], in_max=tv[:, g, 0:8], in_values=ssb_[:, g, :]), [bs_, B_tkg[g]], [B_tig[g]])
        for g in range(16):
            V(lambda e: e.max_index(out=ti[:, g, 8:16], in_max=tv[:, g, 8:16], in_values=ssb_[:, g, :]), [bs_, B_tkg2[g]], [B_tig2[g]])
        V(lambda e: e.tensor_copy(out=tif[:, :, :], in_=ti[:, :, :]), B_tig + B_tig2, [B_tk])
        V(lambda e: e.tensor_copy(out=tv[:, 0:1, 0:1], in_=tv[:, 0:1, 0:1]), B_tkg + B_tkg2, [B_tk])
        tvv = tv[:, :, :].rearrange("p (c h) k -> p h c k", c=2)
        tfv = tif[:, :, :].rearrange("p (c h) k -> p h c k", c=2)
        c4 = cand[:, :, :].rearrange("p h (a b) -> p h a b", b=16)
        for hh in range(8):
            V(lambda e: e.tensor_tensor(out=c4[:, hh, :, :], in0=tvv[:, hh, 0, :].unsqueeze(2).to_broadcast([128, 16, 16]),
                                        in1=tvv[:, hh, 1, :].unsqueeze(1).to_broadcast([128, 16, 16]), op=ALU.add), [B_tk], [B_c])
        for hh in range(8):
            V(lambda e: e.max(out=mv[:, hh, 0:8], in_=cand[:, hh, :]), [B_c], [B_mvh[hh]])
        for hh in range(8):
            V(lambda e: e.match_replace(out=stmp8[:, hh, :], in_to_replace=mv[:, hh, 0:8], in_values=cand[:, hh, :], imm_value=-1e30), [B_c, B_mvh[hh]], [B_stg16[2 * hh], B_stg16[2 * hh + 1]])
        for hh in range(8):
            V(lambda e: e.max(out=mv[:, hh, 8:16], in_=stmp8[:, hh, :]), [B_stg16[2 * hh], B_stg16[2 * hh + 1]], [B_mvh2[hh]])
        for hh in range(8):
            V(lambda e: e.max_index(out=posu[:, hh, 0:8], in_max=mv[:, hh, 0:8], in_values=cand[:, hh, :]), [B_c, B_mvh[hh]], [B_posh[hh]])
        for hh in range(8):
            V(lambda e: e.max_index(out=posu[:, hh, 8:16], in_max=mv[:, hh, 8:16], in_values=cand[:, hh, :]), [B_c, B_mvh2[hh]], [B_posh2[hh]])
        V(lambda e: e.tensor_copy(out=mv[:, 0:1, 0:1], in_=mv[:, 0:1, 0:1]), B_mvh + B_mvh2 + B_posh + B_posh2, [B_e])
        V(lambda e: e.tensor_scalar(out=au[:, :, :], in0=posu[:, :, :], scalar1=4, scalar2=None, op0=ALU.logical_shift_right), [B_e], [B_e])
        V(lambda e: e.tensor_scalar(out=bu[:, :, :], in0=posu[:, :, :], scalar1=15, scalar2=None, op0=ALU.bitwise_and), [B_e], [B_e])
        V(lambda e: e.tensor_copy(out=abf[:, 0, :], in_=au[:, :, :].rearrange("p a b -> p (a b)")), [B_e], [B_e])
        V(lambda e: e.tensor_copy(out=abf[:, 1, :], in_=bu[:, :, :].rearrange("p a b -> p (a b)")), [B_e], [B_e])
        for cc in range(2):
            V(lambda e: e.tensor_tensor(out=oh16[:, :, :], in0=abf[:, cc, :].unsqueeze(2).to_broadcast([128, 128, 16]),
                                        in1=iota16.unsqueeze(1).to_broadcast([128, 128, 16]), op=ALU.is_equal), [B_e, L["B_iota"]], [B_oh] + B_ohh)
            for hh in range(8):
                V(lambda e: e.tensor_tensor(out=oh16[:, hh * 16:(hh + 1) * 16, :], in0=oh16[:, hh * 16:(hh + 1) * 16, :],
                                            in1=tfv[:, hh, cc, :].unsqueeze(1).to_broadcast([128, 16, 16]), op=ALU.mult), [B_oh, B_tk], [B_ohh[hh]])
            V(lambda e: e.tensor_reduce(out=sel[:, cc, :], in_=oh16[:, :, :], axis=AX.X, op=ALU.add), B_ohh, [B_sel, B_oh])
        V(lambda e: e.tensor_tensor(out=gate[:, :, :], in0=mv[:, :, :], in1=mv[:, :, 0:1].to_broadcast([128, 8, 16]), op=ALU.subtract), [B_e], [B_hu])
        A(lambda e: e.activation(out=gate[:, :, :], in_=gate[:, :, :], func=AF.Exp), [B_hu], [B_hu])
        V(lambda e: e.tensor_reduce(out=gs[:, :], in_=gate[:, :, :], axis=AX.X, op=ALU.add), [B_hu], [B_hu])
        V(lambda e: e.reciprocal(out=gs[:, :], in_=gs[:, :]), [B_hu], [B_hu])
        V(lambda e: e.tensor_tensor(out=sel[:, 2, :].rearrange("p (a b) -> p a b", b=16), in0=gate[:, :, :], in1=gs[:, :].unsqueeze(2).to_broadcast([128, 8, 16]), op=ALU.mult),
          [B_hu], [B_sel])
        pI, bI = ps[6], B_ps[6]
        for q3 in range(3):
            P(lambda e: e.transpose(pI[:, q3 * 128:(q3 + 1) * 128], sel[:, q3, :], ident[:, :]), [B_sel, B_ident], [bI])
        A(lambda e: e.activation(out=selT[:, :, :], in_=pI[:, 0:384].rearrange("p (a b) -> p a b", b=128), func=AF.Copy), [bI], [B_selT])
        for q3 in range(3):
            sc.dma("sp", L["selT_d"][q3, :, n * 128:(n + 1) * 128], selT[:, q3, :], reads=[B_selT])

    for tb in range(L["nblk"]):
        tsl = slice(tb * 512, (tb + 1) * 512)
        for kc in range(4):
            sc.dma("sp", rwoB[:, kc, :], L["rwo_d"][kc * 128:(kc + 1) * 128, tsl], writes=[B_in])
        for kc in range(8):
            sc.dma("sp", dsaB[:, kc, :], L["dsao_d"][kc * 128:(kc + 1) * 128, tsl], writes=[B_in])
        for dc in range(8):
            pA, bA = ps[dc % 2], B_ps[dc % 2]
            pB, bB = ps[2 + dc % 2], B_ps[2 + dc % 2]
            zg_, bz_ = zgr[dc % 2], B_zgr[dc % 2]
            sc.dma("sp", zg_[:, 0, :], L["zg_d"][dc * 128:(dc + 1) * 128, tsl], writes=[bz_])
            sc.dma("sp", zg_[:, 1, :], L["zg_d"][(8 + dc) * 128:(9 + dc) * 128, tsl], writes=[bz_])
            for kc in range(4):
                P(lambda e: e.matmul(pA[:, :], lhsT=wbra[:, kc, dc * 128:(dc + 1) * 128], rhs=rwoB[:, kc, :], start=(kc == 0), stop=(kc == 3)), [B_w, B_in], [bA])
            for kc in range(8):
                P(lambda e: e.matmul(pB[:, :], lhsT=wbrb[:, kc, dc * 128:(dc + 1) * 128], rhs=dsaB[:, kc, :], start=(kc == 0), stop=(kc == 7)), [B_w, B_in], [bB])
            V(lambda e: e.tensor_tensor(out=t1[:, :], in0=pA[:, :], in1=zg_[:, 0, :], op=ALU.mult), [bA, bz_], [B_t])
            V(lambda e: e.tensor_tensor(out=t2[:, :], in0=pB[:, :], in1=zg_[:, 1, :], op=ALU.mult), [bB, bz_], [B_t])
            V(lambda e: e.tensor_tensor(out=mg[:, dc, :], in0=t1[:, :], in1=t2[:, :], op=ALU.add), [B_t], [B_mg])
        for jt in range(4):
            n = tb * 4 + jt
            stageA(n, jt)
            if prevn[0] is not None:
                stageB(prevn[0])
            prevn[0] = n
    stageB(prevn[0])
    ar.release(mD)


def phase_C(L):
    nc, sc, ar, ps, B_ps = L["nc"], L["sc"], L["ar"], L["ps"], L["B_ps"]
    identb, B_ident = L["identb"], L["B_ident"]
    V = lambda fn, r=(), w=(): sc.op("dve", fn, r, w)
    A = lambda fn, r=(), w=(): sc.op("act", fn, r, w)
    P = lambda fn, r=(), w=(): sc.op("pe", fn, r, w)
    G = lambda fn, r=(), w=(): sc.op("pool", fn, r, w)
    NQB = L["nblk"] * 4
    mC = ar.mark()
    cvec = ar.alloc([128, 256], F32, "cvec")
    biasT = ar.alloc([128, 3, 1024], F32, "biasT")
    negm = ar.alloc([128, 128], F32, "negm")
    ckv_tok = ar.alloc([128, 32, 129], BF16, "ckv_tok")
    ckvT = ar.alloc([128, S], BF16, "ckvT")
    kiT2 = ar.alloc([128, S], BF16, "kiT2")
    wall = ar.alloc([128, 32, 4], F32, "wall")
    B_cc, B_kv, B_ki, B_wl = Buf("cc"), Buf("kv"), Buf("ki"), Buf("wl")
    sc.dma("sp", cvec[:, :], L["cvec_d"][:, :], writes=[B_cc])
    sc.dma("sp", biasT[:, :, :], L["biasT_d"][:, :, :], writes=[B_cc])
    sc.dma("sp", negm[:, :], L["negm_d"][:, :], writes=[B_cc])
    V(lambda e: e.memset(ckv_tok[:, :, 128:129], 1.0), [], [B_kv])
    for bi_ in range(2):
        V(lambda e: e.tensor_tensor(out=biasT[:, bi_, :], in0=biasT[:, bi_, :], in1=biasT[:, 2, :], op=ALU.subtract), [B_cc], [B_cc])
    zt = [ar.alloc([128, 196], F32, "ztC%d" % i) for i in range(2)]
    B_zt = [Buf("ztC0"), Buf("ztC1")]
    sq = ar.alloc([128, 128], F32, "sqC")
    c16 = ar.alloc([128, 128], BF16, "c16")
    k32 = ar.alloc([128, 64], F32, "k32")
    k16 = ar.alloc([128, 128], BF16, "k16")
    stc = ar.alloc([128, 8], F32, "stc")
    B_sq, B_c16, B_k, B_stc = Buf("sqC"), Buf("c16"), Buf("k"), Buf("stc")
    for n in range(NQB):
        z, bz = zt[n % 2], B_zt[n % 2]
        sc.dma("sp", z[:, :], L["ztok_d"][n * 128:(n + 1) * 128, :], writes=[bz])
        A(lambda e: e.activation(out=sq[:, :], in_=z[:, 0:128], func=AF.Square, accum_out=stc[:, 0:1]), [bz], [B_sq, B_stc])
        V(lambda e: e.tensor_scalar(out=stc[:, 1:2], in0=stc[:, 0:1], scalar1=1.0 / 128, scalar2=1e-5, op0=ALU.mult, op1=ALU.add), [B_stc], [B_stc])
        A(lambda e: e.activation(out=stc[:, 1:2], in_=stc[:, 1:2], func=AF.Sqrt), [B_stc], [B_stc])
        V(lambda e: e.reciprocal(out=stc[:, 1:2], in_=stc[:, 1:2]), [B_stc], [B_stc])
        V(lambda e: e.scalar_tensor_tensor(out=ckv_tok[:, n, 0:128], in0=z[:, 0:128], scalar=stc[:, 1:2], in1=cvec[:, 0:128], op0=ALU.mult, op1=ALU.mult),
          [bz, B_stc, B_cc], [B_kv])
        V(lambda e: e.tensor_reduce(out=stc[:, 2:3], in_=z[:, 128:192], axis=AX.X, op=ALU.add), [bz], [B_stc])
        A(lambda e: e.activation(out=sq[:, 0:64], in_=z[:, 128:192], func=AF.Square, accum_out=stc[:, 3:4]), [bz], [B_sq, B_stc])
        V(lambda e: e.tensor_scalar(out=stc[:, 4:5], in0=stc[:, 2:3], scalar1=1.0 / 64, scalar2=None, op0=ALU.mult), [B_stc], [B_stc])
        V(lambda e: e.tensor_tensor(out=stc[:, 5:6], in0=stc[:, 4:5], in1=stc[:, 4:5], op=ALU.mult), [B_stc], [B_stc])
        V(lambda e: e.scalar_tensor_tensor(out=stc[:, 6:7], in0=stc[:, 3:4], scalar=1.0 / 64, in1=stc[:, 5:6], op0=ALU.mult, op1=ALU.subtract), [B_stc], [B_stc])
        V(lambda e: e.tensor_scalar(out=stc[:, 6:7], in0=stc[:, 6:7], scalar1=1e-5, scalar2=None, op0=ALU.add), [B_stc], [B_stc])
        A(lambda e: e.activation(out=stc[:, 6:7], in_=stc[:, 6:7], func=AF.Sqrt), [B_stc], [B_stc])
        V(lambda e: e.reciprocal(out=stc[:, 6:7], in_=stc[:, 6:7]), [B_stc], [B_stc])
        V(lambda e: e.tensor_scalar(out=k32[:, :], in0=z[:, 128:192], scalar1=stc[:, 4:5], scalar2=stc[:, 6:7], op0=ALU.subtract, op1=ALU.mult), [bz, B_stc], [B_k])
        V(lambda e: e.tensor_tensor(out=k32[:, :], in0=k32[:, :], in1=cvec[:, 128:192], op=ALU.mult), [B_k, B_cc], [B_k])
        V(lambda e: e.tensor_tensor(out=k16[:, 0:64], in0=k32[:, :], in1=cvec[:, 192:256], op=ALU.add), [B_k, B_cc], [B_k])
        V(lambda e: e.tensor_copy(out=k16[:, 64:128], in_=k16[:, 0:64]), [B_k], [B_k])
        V(lambda e: e.tensor_scalar(out=wall[:, n, :], in0=z[:, 192:196], scalar1=0.0625, scalar2=None, op0=ALU.mult), [bz], [B_wl])
        pT = ps[n % 2][:, :].bitcast(BF16)
        bT = B_ps[n % 2]
        P(lambda e: e.transpose(pT[:, 0:128], ckv_tok[:, n, 0:128], identb[:, :]), [B_kv, B_ident], [bT])
        P(lambda e: e.transpose(pT[:, 128:256], k16[:, :], identb[:, :]), [B_k, B_ident], [bT])
        A(lambda e: e.activation(out=ckvT[:, n * 128:(n + 1) * 128], in_=pT[:, 0:128], func=AF.Copy, scale=128.0 ** -0.5), [bT], [B_kv])
        V(lambda e: e.tensor_copy(out=kiT2[:, n * 128:(n + 1) * 128], in_=pT[:, 128:256]), [bT], [B_ki])
    qiB = [ar.alloc([128, 2, 128], BF16, "qiB%d" % i) for i in range(2)]
    zqB = [ar.alloc([128, 8, 128], BF16, "zqB%d" % i) for i in range(2)]
    B_qi = [Buf("qi0"), Buf("qi1")]
    B_zq = [Buf("zq0"), Buf("zq1")]
    score2 = [ar.alloc([128, S], F32, "score%d" % i) for i in range(2)]
    maskb2 = [ar.alloc([128, S], BF16, "maskb%d" % i) for i in range(2)]
    maskT2 = [ar.alloc([128, 32, 128], BF16, "maskT%d" % i) for i in range(2)]
    bis2 = [ar.alloc([128, 8], F32, "bis%d" % i) for i in range(2)]
    junk = ar.alloc([128, S], BF16, "junkC")
    junkA = ar.alloc([128, S // 2], BF16, "junkA")
    rl = [ar.alloc([128, 512], F32, "rl%d" % i) for i in range(2)]
    B_rl = [Buf("rl0"), Buf("rl1")]
    lg = ar.alloc([128, 1024], F32, "lg")
    PT2 = [ar.alloc([128, 8, 128], BF16, "PT%d" % i) for i in range(2)]
    B_PT2 = [Buf("PT0"), Buf("PT1")]
    Oacc = ar.alloc([128, 8, 129], F32, "Oacc")
    rec = ar.alloc([128, 8], F32, "rec")
    Oo = ar.alloc([128, 8, 128], BF16, "Oo")
    dsT = ar.alloc([128, 8, 128], BF16, "dsT")
    B_sc2 = [Buf("score0"), Buf("score1")]
    B_mb2 = [Buf("maskb0"), Buf("maskb1")]
    B_mT2 = [Buf("maskT0"), Buf("maskT1")]
    B_bisA = [Buf("bisA0"), Buf("bisA1")]
    B_bisB = [Buf("bisB0"), Buf("bisB1")]
    B_bisC = [Buf("bisC0"), Buf("bisC1")]
    B_j, B_jA, B_lg, B_O, B_Oo, B_dsT = [Buf(n_) for n_ in ("junkC", "junkA", "lg", "Oacc", "Oo", "dsT")]

    def stage1(j):
        q = j % 2
        Lk = (j + 1) * 128
        qi, bqi, zq, bzq = qiB[q], B_qi[q], zqB[q], B_zq[q]
        score, B_sc = score2[q], B_sc2[q]
        qsl = slice(j * 128, (j + 1) * 128)
        for cch in range(2):
            sc.dma("sp", qi[:, cch, :], L["zqi_d"][cch * 128:(cch + 1) * 128, qsl], writes=[bqi])
        for h in range(8):
            sc.dma("sp", zq[:, h, :], L["zq_d"][h * 128:(h + 1) * 128, qsl], writes=[bzq])
        nkc = (Lk + 511) // 512
        ri = 0
        for kc in range(nkc):
            wd = min(512, Lk - kc * 512)
            ksl = slice(kc * 512, kc * 512 + wd)
            for hi in range(4):
                po = (hi % 2) * 64
                pD, bD = ps[hi], B_ps[hi]
                P(lambda e: e.matmul(pD[:, 0:wd], lhsT=qi[po:po + 64, hi // 2, :], rhs=kiT2[po:po + 64, ksl], start=True, stop=True), [bqi, B_ki], [bD])
                r_, br_ = rl[ri % 2], B_rl[ri % 2]
                ri += 1
                A(lambda e: e.activation(out=r_[:, 0:wd], in_=pD[:, 0:wd], func=AF.Relu), [bD], [br_])
                if hi == 0:
                    V(lambda e: e.tensor_scalar(out=score[:, ksl], in0=r_[:, 0:wd], scalar1=wall[:, j, 0:1], scalar2=None, op0=ALU.mult), [br_, B_wl], [B_sc])
                else:
                    V(lambda e: e.scalar_tensor_tensor(out=score[:, ksl], in0=r_[:, 0:wd], scalar=wall[:, j, hi:hi + 1], in1=score[:, ksl], op0=ALU.mult, op1=ALU.add),
                      [br_, B_wl, B_sc], [B_sc])
        V(lambda e: e.tensor_tensor(out=score[:, qsl], in0=score[:, qsl], in1=negm[:, :], op=ALU.add), [B_sc, B_cc], [B_sc])

    def bisect_iters(j):
        q = j % 2
        Lk = (j + 1) * 128
        score, B_sc, bis = score2[q], B_sc2[q], bis2[q]
        bA, bB, bC = B_bisA[q], B_bisB[q], B_bisC[q]
        if Lk > 256:
            na = (Lk // 2) // 128 * 128
            V(lambda e: e.memset(bis[:, 6:7], 0.0), [bA], [bA])
            step = 32.0
            for it in range(27):
                V(lambda e: e.tensor_scalar(out=bis[:, 7:8], in0=bis[:, 6:7], scalar1=-1.0, scalar2=None, op0=ALU.mult), [bA], [bB])
                A(lambda e: e.activation(out=junkA[:, 0:na], in_=score[:, 0:na], func=AF.Sign, bias=bis[:, 7:8], accum_out=bis[:, 2:3]), [B_sc, bB], [B_jA, bC])
                V(lambda e: e.tensor_scalar(out=junk[:, na:Lk], in0=score[:, na:Lk], scalar1=bis[:, 6:7], scalar2=None, op0=ALU.is_ge, op1=ALU.add, accum_out=bis[:, 3:4]),
                  [B_sc, bA], [B_j, bA])
                V(lambda e: e.scalar_tensor_tensor(out=bis[:, 4:5], in0=bis[:, 2:3], scalar=0.5, in1=bis[:, 3:4], op0=ALU.mult, op1=ALU.add), [bA, bC], [bA])
                V(lambda e: e.tensor_scalar(out=bis[:, 5:6], in0=bis[:, 4:5], scalar1=255.5 - na / 2.0, scalar2=2.0 * step, op0=ALU.is_ge, op1=ALU.mult), [bA], [bA])
                V(lambda e: e.scalar_tensor_tensor(out=bis[:, 6:7], in0=bis[:, 5:6], scalar=-step, in1=bis[:, 6:7], op0=ALU.add, op1=ALU.add), [bA, bB], [bA])
                step *= 0.5
                yield
            V(lambda e: e.tensor_scalar(out=bis[:, 6:7], in0=bis[:, 6:7], scalar1=-4.0 * step, scalar2=None, op0=ALU.add), [bA], [bA])
        else:
            V(lambda e: e.memset(bis[:, 6:7], -1e29), [bA], [bA])

    def stage3(j):
        q = j % 2
        Lk = (j + 1) * 128
        score, B_sc, bis, maskb, B_mb, maskT, B_mT = score2[q], B_sc2[q], bis2[q], maskb2[q], B_mb2[q], maskT2[q], B_mT2[q]
        V(lambda e: e.tensor_scalar(out=maskb[:, 0:Lk], in0=score[:, 0:Lk], scalar1=bis[:, 6:7], scalar2=None, op0=ALU.is_ge), [B_sc, B_bisA[q]], [B_mb])
        for k8 in range((j + 8) // 8):
            nn = min(8, j + 1 - k8 * 8)
            pM = ps[4][:, :].bitcast(BF16)
            for kk_ in range(nn):
                kt = k8 * 8 + kk_
                P(lambda e: e.transpose(pM[:, kk_ * 128:(kk_ + 1) * 128], maskb[:, kt * 128:(kt + 1) * 128], identb[:, :]), [B_mb, B_ident], [B_ps[4]])
            A(lambda e: e.activation(out=maskT[:, k8 * 8:k8 * 8 + nn, :], in_=pM[:, 0:nn * 128].rearrange("p (a b) -> p a b", b=128), func=AF.Copy), [B_ps[4]], [B_mT])

    def stage4(j, side):
        q = j % 2
        zq, bzq, maskT, B_mT = zqB[q], B_zq[q], maskT2[q], B_mT2[q]
        qsl = slice(j * 128, (j + 1) * 128)
        for kt in range(j + 1):
            near = kt >= j - 1
            bsel = 0 if kt == j else 1
            PTk, bPT = PT2[kt % 2], B_PT2[kt % 2]
            for half in range(2):
                pL, bL = ps[(kt % 2) * 2 + half], B_ps[(kt % 2) * 2 + half]
                P(lambda e: e.matmul(pL[:, :], lhsT=ckvT[:, kt * 128:(kt + 1) * 128], rhs=zq[:, half * 4:(half + 1) * 4, :], start=True, stop=True), [B_kv, bzq], [bL])
                if near:
                    V(lambda e: e.tensor_tensor(out=lg[:, half * 512:(half + 1) * 512], in0=pL[:, :], in1=biasT[:, bsel, half * 512:(half + 1) * 512], op=ALU.add),
                      [bL, B_cc], [B_lg])
                    A(lambda e: e.activation(out=PTk[:, half * 4:(half + 1) * 4, :], in_=lg[:, half * 512:(half + 1) * 512].rearrange("p (h q) -> p h q", q=128), func=AF.Exp),
                      [B_lg], [bPT])
                else:
                    A(lambda e: e.activation(out=PTk[:, half * 4:(half + 1) * 4, :], in_=pL[:, :].rearrange("p (h q) -> p h q", q=128), func=AF.Exp), [bL], [bPT])
            V(lambda e: e.tensor_tensor(out=PTk[:, :, :], in0=PTk[:, :, :], in1=maskT[:, kt, :].unsqueeze(1).to_broadcast([128, 8, 128]), op=ALU.mult), [bPT, B_mT], [bPT])
            for h in range(8):
                pO, bO = ps[5 + h // 3], B_ps[5 + h // 3]
                P(lambda e: e.matmul(pO[:, (h % 3) * 129:(h % 3 + 1) * 129], lhsT=PTk[:, h, :], rhs=ckv_tok[:, kt, :], start=(kt == 0 and h % 3 == 0), stop=(kt == j),
                                     skip_group_check=True), [bPT, B_kv], [bO])
            if side is not None:
                next(side, None)
        for b3 in range(3):
            nh = 3 if b3 < 2 else 2
            V(lambda e: e.tensor_copy(out=Oacc[:, b3 * 3:b3 * 3 + nh, :], in_=ps[5 + b3][:, 0:nh * 129].rearrange("p (h d) -> p h d", d=129)), [B_ps[5 + b3]], [B_O])
        V(lambda e: e.reciprocal(out=rec[:, :], in_=Oacc[:, :, 128]), [B_O], [B_Oo])
        V(lambda e: e.tensor_tensor(out=Oo[:, :, :], in0=Oacc[:, :, 0:128], in1=rec[:, :].unsqueeze(2).to_broadcast([128, 8, 128]), op=ALU.mult), [B_O, B_Oo], [B_Oo])
        pX = ps[4][:, :].bitcast(BF16)
        for h in range(8):
            P(lambda e: e.transpose(pX[:, h * 128:(h + 1) * 128], Oo[:, h, :], identb[:, :]), [B_Oo, B_ident], [B_ps[4]])
        V(lambda e: e.tensor_copy(out=dsT[:, :, :], in_=pX[:, :].rearrange("p (a b) -> p a b", b=128)), [B_ps[4]], [B_dsT])
        for h in range(8):
            sc.dma("sp", L["dsao_d"][h * 128:(h + 1) * 128, qsl], dsT[:, h, :], reads=[B_dsT])

    stage1(0)
    for _ in bisect_iters(0):
        pass
    stage3(0)
    for j in range(NQB):
        side = None
        if j + 1 < NQB:
            stage1(j + 1)
            side = bisect_iters(j + 1)
        stage4(j, side)
        if side is not None:
            for _ in side:
                pass
            stage3(j + 1)
    ar.release(mC)


def phase_E(L):
    nc, sc, ar, ps, B_ps = L["nc"], L["sc"], L["ar"], L["ps"], L["B_ps"]
    identb, B_ident = L["identb"], L["B_ident"]
    gt_bc, B_gt = L["gt_bc"], L["B_gt"]
    iota128, B_iota = L["iota128"], L["B_iota"]
    dbg = L["dbg"]
    V = lambda fn, r=(), w=(): sc.op("dve", fn, r, w)
    A = lambda fn, r=(), w=(): sc.op("act", fn, r, w)
    P = lambda fn, r=(), w=(): sc.op("pe", fn, r, w)
    G = lambda fn, r=(), w=(): sc.op("pool", fn, r, w)
    ALPHA = 2.0 ** 0.25
    if not L["e0_done_flag"][0]:
        m0 = ar.mark()
        for _ in make_e0(L, 3, 0):
            pass
        ar.release(m0)
    ar.release(L["mark_stg"])
    mE = ar.mark()
    TP = 256
    lnbc = ar.alloc([128, 2, D], F32, "lnbcE")
    B_ln = Buf("lnE")
    sc.dma("sp", lnbc[:, :, :], L["lnbc_d"][:, 2:4, :], writes=[B_ln])
    Gs2 = [ar.alloc([128, 128, TP], BF16, "Gs%d" % i) for i in range(2)]
    h2Tp2 = [ar.alloc([128, 8, TP], BF16, "h2Tp%d" % i) for i in range(2)]
    IT1 = ar.alloc([128, 3, TP], F32, "IT1")
    IT2 = [IT1, IT1]
    ITb2 = [ar.alloc([128, 3, TP], BF16, "ITb%d" % i) for i in range(2)]
    iotab = ar.alloc([128, 128], BF16, "iotab")
    NBT = 8
    eqb = [ar.alloc([128, NBT, 128], BF16, "eqb%d" % i) for i in range(1)]
    Lb = [ar.alloc([128, NBT, 128], BF16, "Lb%d" % i) for i in range(2)]
    Rb = [ar.alloc([128, NBT, 128], BF16, "Rb%d" % i) for i in range(2)]
    B_Gs2 = [Buf("Gs0"), Buf("Gs1")]
    B_h2Tp2 = [Buf("h2Tp0"), Buf("h2Tp1")]
    B_IT2 = [Buf("IT0"), Buf("IT1")]
    B_ITf1 = Buf("ITf")
    B_ITf2 = [B_ITf1, B_ITf1]
    B_eq = [Buf("eqb0")]
    B_eqh = [Buf("eqh0"), Buf("eqh1")]
    V(lambda e: e.tensor_copy(out=iotab[:, :], in_=iota128[:, :]), [B_iota], [B_iota])
    B_Lb = [Buf("Lb0"), Buf("Lb1")]
    B_Rb = [Buf("Rb0"), Buf("Rb1")]
    NS = 4
    uTc = [ar.alloc([128, 1024], BF16, "uTc%d" % i) for i in range(NS)]
    vcb = [ar.alloc([128, 1024], BF16, "vcb%d" % i) for i in range(NS)]
    B_uTc = [Buf("uTc%d" % i) for i in range(NS)]
    B_vcb = [Buf("vcb%d" % i) for i in range(NS)]
    gl = [ar.alloc([128, TP], BF16, "gl%d" % i) for i in range(3)]
    AT = [ar.alloc([128, TP], BF16, "AT%d" % i) for i in range(3)]
    B_gl = [Buf("gl0"), Buf("gl1"), Buf("gl2")]
    B_AT = [Buf("AT0"), Buf("AT1"), Buf("AT2")]
    x1t = ar.alloc([128, D], F32, "x1t")
    oo = ar.alloc([128, D], F32, "ooE")
    jk = oo
    st = ar.alloc([128, 8], F32, "stE")
    B_x1t, B_oo, B_st = Buf("x1t"), Buf("ooE"), Buf("stE")
    B_jk = B_oo
    out_v = L["out_d"].rearrange("(n p) m -> p n m", p=128)
    iota3 = iotab[:, :].unsqueeze(1).to_broadcast([128, NBT, 128])
    npass = L["nblk"] * 2
    NBATCH = TP // NBT

    def emit_loads(p_):
        q = p_ % 2
        tsl = slice(p_ * TP, (p_ + 1) * TP)
        for kc in range(8):
            sc.dma("sp", h2Tp2[q][:, kc, :], L["h2T_d"][kc * 128:(kc + 1) * 128, tsl], writes=[B_h2Tp2[q]])
        for q3 in range(3):
            sc.dma("sp", IT2[q][:, q3, :], L["selT_d"][q3, :, tsl], writes=[B_ITf2[q]])
        A(lambda e: e.activation(out=ITb2[q][:, :, :], in_=IT2[q][:, :, :], func=AF.Copy), [B_ITf2[q]], [B_IT2[q]])

    gcount = [0]

    def gbatch_gen(p_, b):
        q = p_ % 2
        Gs, B_Gs, ITb, B_IT = Gs2[q], B_Gs2[q], ITb2[q], B_IT2[q]
        gi = gcount[0]
        gcount[0] += 1
        Lk, bLk = Lb[gi % 2], B_Lb[gi % 2]
        Rk, bRk = Rb[gi % 2], B_Rb[gi % 2]
        eq_ = eqb[0]
        H = NBT // 2
        for hf in range(2):
            hs_ = slice(hf * H, (hf + 1) * H)
            bs_ = slice(b * NBT + hf * H, b * NBT + (hf + 1) * H)
            io_ = iotab[:, :].unsqueeze(1).to_broadcast([128, H, 128])
            V(lambda e: e.tensor_tensor(out=eq_[:, hs_, :], in0=io_, in1=ITb[:, 0, bs_].unsqueeze(2).to_broadcast([128, H, 128]), op=ALU.is_equal), [B_IT, B_iota], [B_eqh[hf]])
            yield
            V(lambda e: e.tensor_tensor(out=Lk[:, hs_, :], in0=eq_[:, hs_, :], in1=ITb[:, 2, bs_].unsqueeze(2).to_broadcast([128, H, 128]), op=ALU.mult), [B_eqh[hf], B_IT], [bLk])
            yield
            V(lambda e: e.tensor_tensor(out=Rk[:, hs_, :], in0=io_, in1=ITb[:, 1, bs_].unsqueeze(2).to_broadcast([128, H, 128]), op=ALU.is_equal), [B_IT, B_iota], [bRk])
            yield
        yield
        yield
        for t4 in range(NBT // 4):
            pg, bpg = ps[7], B_ps[7]
            for tt in range(4):
                t = t4 * 4 + tt
                P(lambda e: e.matmul(pg[:, tt * 128:(tt + 1) * 128], lhsT=Lk[:, t, :], rhs=Rk[:, t, :], start=True, stop=True), [bLk, bRk], [bpg])
            t0 = b * NBT + t4 * 4
            src = pg[:, :].rearrange("p (t i) -> p i t", i=128)
            A(lambda e: e.activation(out=Gs[:, :, t0:t0 + 4], in_=src, func=AF.Copy), [bpg], [B_Gs])
            yield

    def gall_gen(p_):
        for b in range(NBATCH):
            for _ in gbatch_gen(p_, b):
                yield

    emit_loads(0)
    for _ in gall_gen(0):
        pass
    for p_ in range(npass):
        q = p_ % 2
        Gs, B_Gs, h2Tp, B_h2Tp = Gs2[q], B_Gs2[q], h2Tp2[q], B_h2Tp2[q]
        if p_ + 1 < npass:
            emit_loads(p_ + 1)

        def load_u(c):
            sc.dma("sp", uTc[c % NS][:, :], L["uv_d"][c, :, 0:1024], writes=[B_uTc[c % NS]])

        def emit_hu(c):
            k = c % NS
            if c == 0:
                load_u(0)
                load_u(1)
            if c + 2 < 128:
                load_u(c + 2)
            sc.dma("sp", vcb[k][:, :], L["uv_d"][c, :, 1024:2048], writes=[B_vcb[k]])
            pH, bH = ps[4 + c % 3], B_ps[4 + c % 3]
            for kc in range(8):
                P(lambda e: e.matmul(pH[:, 0:TP], lhsT=uTc[k][:, kc * 128:(kc + 1) * 128], rhs=h2Tp[:, kc, :], start=(kc == 0), stop=(kc == 7)), [B_uTc[k], B_h2Tp], [bH])

        def emit_y2(c):
            k = c % NS
            pH, bH = ps[4 + c % 3], B_ps[4 + c % 3]
            g_, bg_ = gl[c % 3], B_gl[c % 3]
            a_, ba_ = AT[c % 3], B_AT[c % 3]
            A(lambda e: e.activation(out=g_[:, :], in_=pH[:, 0:TP], func=AF.Gelu), [bH], [bg_])
            V(lambda e: e.tensor_tensor(out=a_[:, :], in0=g_[:, :], in1=Gs[:, c, :], op=ALU.mult), [bg_, B_Gs], [ba_])
            for tt in range(2):
                for half in range(2):
                    py, bpy = ps[tt * 2 + half], B_ps[tt * 2 + half]
                    P(lambda e: e.matmul(py[:, :], lhsT=a_[:, tt * 128:(tt + 1) * 128], rhs=vcb[k][:, half * 512:(half + 1) * 512], start=(c == 0), stop=(c == 127)),
                      [ba_, B_vcb[k]], [bpy])
        emit_hu(0)
        emit_hu(1)
        gg = gall_gen(p_ + 1) if p_ + 1 < npass else None
        steps_per_chunk = (NBATCH * 10 + 127) // 128
        for c in range(128):
            if c + 2 < 128:
                emit_hu(c + 2)
            emit_y2(c)
            if gg is not None:
                for _ in range(steps_per_chunk):
                    next(gg, None)
        if gg is not None:
            for _ in gg:
                pass
        for tt in range(2):
            n = p_ * 2 + tt
            sc.dma("sp", x1t[:, :], L["x1s_d"][n * 128:(n + 1) * 128, :], writes=[B_x1t])
            for half in range(2):
                hsl = slice(half * 512, (half + 1) * 512)
                py, bpy = ps[tt * 2 + half], B_ps[tt * 2 + half]
                if "y2dbg" in dbg:
                    V(lambda e: e.tensor_copy(out=oo[:, hsl], in_=py[:, :]), [bpy], [B_oo])
                    sc.dma("sp", L["y2_d"][n * 128:(n + 1) * 128, hsl], oo[:, hsl], reads=[B_oo])
                V(lambda e: e.tensor_tensor(out=oo[:, hsl], in0=py[:, :], in1=gt_bc[:, 3, hsl], op=ALU.mult), [bpy, B_gt], [B_oo])
            V(lambda e: e.scalar_tensor_tensor(out=x1t[:, :], in0=x1t[:, :], scalar=ALPHA, in1=oo[:, :], op0=ALU.mult, op1=ALU.add), [B_x1t, B_oo], [B_x1t])
            V(lambda e: e.tensor_reduce(out=st[:, 0:1], in_=x1t[:, :], axis=AX.X, op=ALU.add), [B_x1t], [B_st])
            A(lambda e: e.activation(out=oo[:, :], in_=x1t[:, :], func=AF.Square, accum_out=st[:, 1:2]), [B_x1t], [B_st, B_oo])
            V(lambda e: e.tensor_scalar(out=st[:, 2:3], in0=st[:, 0:1], scalar1=1.0 / D, scalar2=None, op0=ALU.mult), [B_st], [B_st])
            V(lambda e: e.tensor_tensor(out=st[:, 3:4], in0=st[:, 2:3], in1=st[:, 2:3], op=ALU.mult), [B_st], [B_st])
            V(lambda e: e.scalar_tensor_tensor(out=st[:, 4:5], in0=st[:, 1:2], scalar=1.0 / D, in1=st[:, 3:4], op0=ALU.mult, op1=ALU.subtract), [B_st], [B_st])
            V(lambda e: e.tensor_scalar(out=st[:, 4:5], in0=st[:, 4:5], scalar1=1e-5, scalar2=None, op0=ALU.add), [B_st], [B_st])
            A(lambda e: e.activation(out=st[:, 5:6], in_=st[:, 4:5], func=AF.Sqrt), [B_st], [B_st])
            V(lambda e: e.reciprocal(out=st[:, 5:6], in_=st[:, 5:6]), [B_st], [B_st])
            V(lambda e: e.tensor_scalar(out=oo[:, :], in0=x1t[:, :], scalar1=st[:, 2:3], scalar2=st[:, 5:6], op0=ALU.subtract, op1=ALU.mult), [B_x1t, B_st], [B_oo])
            V(lambda e: e.tensor_tensor(out=oo[:, :], in0=oo[:, :], in1=lnbc[:, 0, :], op=ALU.mult), [B_oo, B_ln], [B_oo])
            V(lambda e: e.tensor_tensor(out=oo[:, :], in0=oo[:, :], in1=lnbc[:, 1, :], op=ALU.add), [B_oo, B_ln], [B_oo])
            sc.dma("sp", out_v[:, n, :], oo[:, :], reads=[B_oo])
    ar.release(mE)


def make_e0(L, NB, bank):
    sc, ar, ps, B_ps = L["sc"], L["ar"], L["ps"], L["B_ps"]
    identb, B_ident = L["identb"], L["B_ident"]
    A = lambda fn, r=(), w=(): sc.op("act", fn, r, w)
    P = lambda fn, r=(), w=(): sc.op("pe", fn, r, w)
    G = lambda fn, r=(), w=(): sc.op("pool", fn, r, w)
    NF = 3
    stf = [ar.alloc([128, D], F32, "e0f%d" % i) for i in range(NF)]
    o16 = [ar.alloc([128, D], BF16, "e0h%d" % i) for i in range(NF)]
    uT = [ar.alloc([128, D], BF16, "e0t%d" % i) for i in range(2)]
    B_f = [Buf("e0f%d" % i) for i in range(NF)]
    B_o = [Buf("e0h%d" % i) for i in range(NF)]
    B_t = [Buf("e0t0"), Buf("e0t1")]
    pu_v = L["pu_d"].rearrange("(i1 i2) d -> i2 i1 d", i2=128)
    pv_v = L["pv_d"].rearrange("(i1 i2) d -> i2 i1 d", i2=128)
    NI = 256

    def load(i):
        c, isv = i // 2, i % 2
        sc.dma("sp", stf[i % NF][:, :], (pv_v if isv else pu_v)[c, :, :], writes=[B_f[i % NF]])

    def cast(i):
        G(lambda e: e.tensor_copy(out=o16[i % NF][:, :], in_=stf[i % NF][:, :]), [B_f[i % NF]], [B_o[i % NF]])

    def finish(i):
        c, isv = i // 2, i % 2
        if isv:
            sc.dma("sp", L["uv_d"][c, :, 1024:2048], o16[i % NF][:, :], reads=[B_o[i % NF]])
        else:
            pT = ps[bank][:, :].bitcast(BF16)
            for kc in range(8):
                P(lambda e: e.transpose(pT[:, kc * 128:(kc + 1) * 128], o16[i % NF][:, kc * 128:(kc + 1) * 128], identb[:, :]), [B_o[i % NF], B_ident], [B_ps[bank]])
            A(lambda e: e.activation(out=uT[c % 2][:, :], in_=pT[:, :], func=AF.Copy), [B_ps[bank]], [B_t[c % 2]])
            sc.dma("sp", L["uv_d"][c, :, 0:1024], uT[c % 2][:, :], reads=[B_t[c % 2]])
    load(0)
    load(1)
    cast(0)
    for k in range(NI):
        if k + 2 < NI:
            load(k + 2)
        if k + 1 < NI:
            cast(k + 1)
        finish(k)
        yield
```

```python
import numpy as np
import ml_dtypes
import concourse.bass as bass
import concourse.mybir as mybir
from concourse.bass_utils import run_bass_kernel_spmd

F32 = mybir.dt.float32
BF16 = mybir.dt.bfloat16
I32 = mybir.dt.int32
U32 = mybir.dt.uint32
AF = mybir.ActivationFunctionType
ALU = mybir.AluOpType
AX = mybir.AxisListType

S = 4096
D = 1024
NT = S // 128
IN_COLS = 5316
DBG = {}
STOP_AFTER = None


class Buf:
    __slots__ = ("name", "lw", "rd")

    def __init__(self, name):
        self.name = name
        self.lw = None
        self.rd = []


class _Rec:
    def __init__(self):
        self.call = None

    def __getattr__(self, name):
        def f(*args, **kwargs):
            self.call = (name, args, kwargs)
            return self
        return f


class Sched:
    ENGS = ("pe", "act", "dve", "pool", "sp")

    def __init__(self, nc, n_dma_sems=40):
        self.nc = nc
        self.ops = {e: [] for e in self.ENGS}
        self.sem = {e: nc.alloc_semaphore("c_" + e) for e in self.ENGS}
        self.cnt = {e: 0 for e in self.ENGS}
        self.seen = {e: {} for e in self.ENGS}
        self.dsem = [nc.alloc_semaphore("d%d" % i) for i in range(n_dma_sems)]
        self.dval = [0] * n_dma_sems
        self.drr = 0
        self.drr_sw = 0
        self.NSW = 8
        self.NHW = n_dma_sems - 8
        self.all_events = []

    def _waits(self, eng, reads, writes):
        deps = []
        for b in reads:
            if b.lw is not None:
                deps.append(b.lw)
        for b in writes:
            if b.lw is not None:
                deps.append(b.lw)
            deps.extend(b.rd)
        out = {}
        for (sem, val, src) in deps:
            if src == "pe" and eng == "pe":
                continue
            k = sem.num
            if self.seen[eng].get(k, 0) >= val:
                continue
            if out.get(k, (None, 0))[1] < val:
                out[k] = (sem, val)
        for k, (sem, val) in out.items():
            self.seen[eng][k] = val
        return list(out.values())

    def op(self, eng, fn, reads=(), writes=()):
        waits = self._waits(eng, reads, writes)
        self.cnt[eng] += 1
        sem = self.sem[eng]
        val = self.cnt[eng]
        ev = (sem, val, eng)

        rec = _Rec()
        fn(rec)
        name, args, kwargs = rec.call

        def emit(e, waits=waits, sem=sem, name=name, args=args, kwargs=kwargs):
            for (s, v) in waits:
                e.wait_ge(s, v)
            getattr(e, name)(*args, **kwargs).then_inc(sem, 1)
        self.ops[eng].append(emit)
        for b in reads:
            b.rd.append(ev)
        for b in writes:
            b.lw = ev
            b.rd = []
        return ev

    def dma(self, eng, out, in_, reads=(), writes=(), fn=None, **kw):
        if fn is not None:
            rec = _Rec()
            fn(rec)
            mname, margs, mkw = rec.call
        else:
            mname, margs, mkw = "dma_start", (), dict(out=out, in_=in_, **kw)
        if eng == "pool":
            i = self.NHW + (self.drr_sw % self.NSW)
            self.drr_sw += 1
        else:
            i = self.drr
            self.drr = (self.drr + 1) % self.NHW
        sem = self.dsem[i]
        waits = self._waits(eng, reads, writes)
        prev = self.dval[i]
        if prev > 0 and self.seen[eng].get(sem.num, 0) < prev:
            waits.append((sem, prev))
            self.seen[eng][sem.num] = prev
        self.dval[i] += 16
        val = self.dval[i]
        ev = (sem, val, "dma")

        def emit(e, waits=waits, sem=sem, mname=mname, margs=margs, mkw=mkw):
            for (s, v) in waits:
                e.wait_ge(s, v)
            getattr(e, mname)(*margs, **mkw).then_inc(sem, 16)
        self.ops[eng].append(emit)
        for b in reads:
            b.rd.append(ev)
        for b in writes:
            b.lw = ev
            b.rd = []
        self.all_events.append(ev)
        return ev

    def raw(self, eng, fn):
        self.ops[eng].append(fn)

    def barrier(self):
        targets = []
        for en in self.ENGS:
            if self.cnt[en] > 0:
                targets.append((self.sem[en], self.cnt[en], en))
        for i, s in enumerate(self.dsem):
            if self.dval[i] > 0:
                targets.append((s, self.dval[i], "dma"))
        for eng in self.ENGS:
            waits = []
            for (s, v, src) in targets:
                if src == eng:
                    continue
                if self.seen[eng].get(s.num, 0) >= v:
                    continue
                self.seen[eng][s.num] = v
                waits.append((s, v))

            def emit(e, waits=waits):
                for (s, v) in waits:
                    e.wait_ge(s, v)
            self.ops[eng].append(emit)

    def finish(self, final_events):
        nc = self.nc
        with nc.Block() as block:
            def run(name):
                def f(e):
                    for emit in self.ops[name]:
                        emit(e)
                    if name == "sp":
                        for (s, v, _) in final_events:
                            e.wait_ge(s, v)
                        for i, s in enumerate(self.dsem):
                            if self.dval[i] > 0:
                                e.wait_ge(s, self.dval[i])
                        for en in ("pe", "act", "dve", "pool"):
                            if self.cnt[en] > 0:
                                e.wait_ge(self.sem[en], self.cnt[en])
                return f
            block.tensor(run("pe"))
            block.scalar(run("act"))
            block.vector(run("dve"))
            block.gpsimd(run("pool"))
            block.sync(run("sp"))


class Arena:
    def __init__(self, nc, base=0, top=192 * 1024):
        self.nc = nc
        self.off = base
        self.top = top
        self.n = 0
        self.sc = None

    def mark(self):
        return self.off

    def release(self, m):
        self.off = m
        if self.sc is not None:
            self.sc.barrier()

    def alloc(self, shape, dtype, name="t"):
        esz = {F32: 4, BF16: 2, I32: 4, U32: 4}[dtype]
        per = esz
        for s in shape[1:]:
            per *= s
        per = (per + 63) // 64 * 64
        self.n += 1
        t = self.nc.alloc_sbuf_tensor_at("%s_%d" % (name, self.n), list(shape), dtype, offset=self.off)
        self.off += per
        assert self.off <= self.top, ("SBUF overflow", name, self.off)
        return t


def build(dbg=None, stop_after=None, phases=None, feed=(), nblk=8):
    dbg = dbg or {}
    phases = phases or {'0', 'A', 'B', 'C', 'D', 'E', 'F'}
    nc = bass.Bass("TRN2", target_bir_lowering=False)
    sc = Sched(nc)
    ar = Arena(nc, base=(nc.sbuf_base + 63) // 64 * 64, top=nc.sbuf_top // 64 * 64)
    ar.sc = sc

    def din(name, shape, dt=F32):
        return nc.dram_tensor(name, list(shape), dt, kind="ExternalInput").ap()

    def dscratch(name, shape, dt=F32):
        kind = "ExternalOutput" if name in dbg else ("ExternalInput" if name in feed else "Internal")
        return nc.dram_tensor(name, list(shape), dt, kind=kind).ap()

    x_d = din("x", [S, D])
    c_d = din("c_col", [128, 8])
    wada_d = din("w_ada", [D, 6 * D])
    bada_col_d = din("b_ada_col", [128, 48])
    bada_bc_d = din("b_ada_bc", [128, 6 * D])
    win_d = din("w_in", [D, IN_COLS])
    ident_d = din("ident", [128, 128])
    out_d = nc.dram_tensor("out", [S, D], F32, kind="ExternalOutput").ap()

    mu_d = din("mu_col", [128, 14])
    rwvec_d = din("rwvec", [128, 20])
    w2a2_d = din("w2a2", [128, 512])
    g2_d = din("g2", [128, 512])
    gnbc_d = din("gn_bc", [128, 2, 256])
    cst_d = din("cst", [128, 1024])
    wbra_d = din("w_br_a", [512, D])
    wbrb_d = din("w_br_b", [D, D])
    wout_d = din("w_out", [D, D])
    lnbc_d = din("ln_bc", [128, 4, D])
    wq_d = din("peer_wq", [D, D])
    pkeys_d = din("peer_keysT", [128, 8, 128])
    pu_d = din("peer_u", [16384, D])
    pv_d = din("peer_v", [16384, D])
    cvec_d = din("cvec", [128, 256])
    biasT_d = din("biasT", [128, 3, 1024])
    negm_d = din("negm", [128, 128])
    zrw_d = dscratch("zrw", [1792, S], F32)
    zq_d = dscratch("zq", [1024, S], BF16)
    zqi_d = dscratch("zqi", [256, S], BF16)
    zg_d = dscratch("zg", [2048, S], BF16)
    ztok_d = dscratch("ztok", [S, 196], F32)
    rwo_d = dscratch("rwo", [512, S], BF16)
    dsao_d = dscratch("dsao", [1024, S], BF16)
    x1_d = dscratch("x1dbg", [S, D], F32)
    y2_d = dscratch("y2dbg", [S, D], F32)
    x1s_d = dscratch("x1s", [S, D], F32)
    h2T_d = dscratch("h2T", [D, S], BF16)
    selT_d = dscratch("selT", [3, 128, S], F32)
    uv_d = dscratch("uv16", [128, 128, 2048], BF16)
    iota_d = din("iota128", [128, 128])
    iota128 = ar.alloc([128, 128], F32, "iota128")
    B_iota = Buf("iota")
    iota16 = iota128[:, 0:16]

    ident = ar.alloc([128, 128], F32, "ident")
    identb = ar.alloc([128, 128], BF16, "identb")
    modcol = ar.alloc([128, 48], F32, "modcol")
    onep1 = ar.alloc([128, 8], F32, "onep1")
    onep2 = ar.alloc([128, 8], F32, "onep2")
    gt_bc = ar.alloc([128, 4, D], F32, "gt_bc")
    B_ident = Buf("ident")
    B_mod = Buf("mod")
    B_gt = Buf("gt")

    ps = [nc.alloc_psum_tensor("ps%d" % i, [128, 512], F32) for i in range(8)]
    B_ps = [Buf("ps%d" % i) for i in range(8)]

    sc.dma("sp", ident[:, :], ident_d[:, :], writes=[B_ident])
    sc.dma("sp", iota128[:, :], iota_d[:, :], writes=[B_iota])

    mark_stg = ar.mark()
    STG = 1024
    stg = [ar.alloc([128, STG], F32, "stg%d" % i) for i in range(3)]
    B_stg = [Buf("stg%d" % i) for i in range(3)]
    stg_i = [0]

    def load_bf16(dst, src, n, bdst, eng="pool"):
        P = dst.shape[0]
        for o in range(0, n, STG):
            w = min(STG, n - o)
            k = stg_i[0] % 3
            stg_i[0] += 1
            sc.dma("sp", stg[k][0:P, 0:w], src[:, o:o + w], writes=[B_stg[k]])
            sc.op(eng, lambda e, k=k, o=o, w=w, P=P, dst=dst: e.tensor_copy(out=dst[:, o:o + w], in_=stg[k][0:P, 0:w]),
                  reads=[B_stg[k]], writes=[bdst])
    sc.op("dve", lambda e: e.tensor_copy(out=identb[:, :], in_=ident[:, :]), reads=[B_ident], writes=[B_ident])

    if '0' in phases:
        m0 = ar.mark()
        c_sb = ar.alloc([128, 8], F32, "c_sb")
        sil = ar.alloc([128, 8], F32, "sil")
        silbc = ar.alloc([128, 8, 128], F32, "silbc")
        bcol = ar.alloc([128, 48], F32, "bcol")
        bbc = ar.alloc([128, 4, D], F32, "bbc")
        wa = [ar.alloc([128, 8, 1024], F32, "wa%d" % i) for i in range(4)]
        B_c = Buf("c")
        B_wa = [Buf("wa0"), Buf("wa1"), Buf("wa2"), Buf("wa3")]
        B_b = Buf("bcol")
        sc.dma("sp", c_sb[:, :], c_d[:, :], writes=[B_c])
        sc.dma("sp", bcol[:, :], bada_col_d[:, :], writes=[B_b])
        for gi_, g_ in enumerate((2, 3, 4, 5)):
            sc.dma("sp", bbc[:, gi_, :], bada_bc_d[:, g_ * D:(g_ + 1) * D], writes=[B_b])
        sc.op("act", lambda e: e.activation(out=sil[:, :], in_=c_sb[:, :], func=AF.Silu), reads=[B_c], writes=[B_c])
        for kc in range(8):
            sc.op("dve", lambda e, kc=kc: e.tensor_copy(out=silbc[:, kc, :], in_=sil[:, kc:kc + 1].to_broadcast([128, 128])),
                  reads=[B_c], writes=[B_c])
        wada_v = wada_d.rearrange("(kc p) n -> p kc n", p=128)
        for g in range(6):
            w = wa[g % 4]
            bw = B_wa[g % 4]
            for kc in range(8):
                sc.dma("sp", w[:, kc, :], wada_v[:, kc, g * 1024:(g + 1) * 1024], writes=[bw])
            if g in (2, 3, 4, 5):
                gi = g - 2
                for half in range(2):
                    p = ps[half]
                    for kc in range(8):
                        sc.op("pe", lambda e, p=p, w=w, kc=kc, half=half: e.matmul(
                            p[:, :], lhsT=silbc[:, kc, :], rhs=w[:, kc, half * 512:(half + 1) * 512],
                            start=(kc == 0), stop=(kc == 7)), reads=[bw, B_c], writes=[B_ps[half]])
                    sc.op("dve", lambda e, p=p, gi=gi, half=half: e.tensor_tensor(
                        out=gt_bc[:, gi, half * 512:(half + 1) * 512], in0=p[:, :],
                        in1=bbc[:, gi, half * 512:(half + 1) * 512], op=ALU.add),
                        reads=[B_ps[half], B_b], writes=[B_gt])
            if g in (0, 1, 3, 4):
                p = ps[2]
                for fc in range(8):
                    for kc in range(8):
                        sc.op("pe", lambda e, p=p, w=w, kc=kc, fc=fc: e.matmul(
                            p[:, fc:fc + 1], lhsT=w[:, kc, fc * 128:(fc + 1) * 128], rhs=sil[:, kc:kc + 1],
                            start=(kc == 0), stop=(kc == 7)), reads=[bw, B_c], writes=[B_ps[2]])
                sc.op("dve", lambda e, p=p, g=g: e.tensor_tensor(
                    out=modcol[:, g * 8:(g + 1) * 8], in0=p[:, 0:8], in1=bcol[:, g * 8:(g + 1) * 8], op=ALU.add),
                    reads=[B_ps[2], B_b], writes=[B_mod])
        sc.op("dve", lambda e: e.tensor_scalar(out=onep1[:, :], in0=modcol[:, 8:16], scalar1=1.0, scalar2=None, op0=ALU.add),
              reads=[B_mod], writes=[B_mod])
        sc.op("dve", lambda e: e.tensor_scalar(out=onep2[:, :], in0=modcol[:, 32:40], scalar1=1.0, scalar2=None, op0=ALU.add),
              reads=[B_mod], writes=[B_mod])
        sc.op("dve", lambda e: e.tensor_scalar(out=gt_bc[:, 2, :], in0=gt_bc[:, 2, :], scalar1=1.0, scalar2=None, op0=ALU.add),
              reads=[B_gt], writes=[B_gt])
        ar.release(m0)
        if "modcol" in dbg:
            dd = nc.dram_tensor("modcol_o", [128, 48], F32, kind="ExternalOutput").ap()
            sc.dma("sp", dd[:, :], modcol[:, :], reads=[B_mod])
            dd2 = nc.dram_tensor("gt_o", [128, 4 * D], F32, kind="ExternalOutput").ap()
            sc.dma("sp", dd2[:, :], gt_bc[:, :, :].rearrange("p a b -> p (a b)"), reads=[B_gt])

    if 'A' in phases:
        mA = ar.mark()
        fm_chunks = []
        for i in range(14):
            fm_chunks.append((i * 128, "rw", i))
        for i in range(8):
            fm_chunks.append((1792 + i * 128, "q", i))
        for i in range(2):
            fm_chunks.append((2944 + i * 128, "qi", i))
        for i in range(16):
            fm_chunks.append((3268 + i * 128, "g", i))
        winb = ar.alloc([128, 8, IN_COLS], BF16, "winb")
        B_win = Buf("win")
        win_v = win_d.rearrange("(kc p) n -> p kc n", p=128)
        for kc in range(8):
            load_bf16(winb[:, kc, :], win_v[:, kc, :], IN_COLS, B_win)
        xt = [ar.alloc([128, 4, D], F32, "xt%d" % i) for i in range(2)]
        B_xt = [Buf("xt0"), Buf("xt1")]
        hT = [ar.alloc([128, 8, 512], BF16, "hT%d" % i) for i in range(2)]
        B_hT = [Buf("hT0"), Buf("hT1")]
        NEV = 8
        ev32 = [ar.alloc([128, 512], F32, "ev32_%d" % i) for i in range(NEV)]
        ev16 = [ar.alloc([128, 512], BF16, "ev16_%d" % i) for i in range(NEV)]
        B_ev32 = [Buf("ev32_%d" % i) for i in range(NEV)]
        B_ev16 = [Buf("ev16_%d" % i) for i in range(NEV)]
        ztk = [ar.alloc([128, 196], F32, "ztk%d" % i) for i in range(2)]
        B_ztk = [Buf("ztk0"), Buf("ztk1")]
        x_v = x_d.rearrange("(n p) m -> p n m", p=128)
        pi = 0
        evi = 0
        tok_cols = [(2816, 128, 0), (3200, 68, 128)]
        for tb in range(8):
            xb = xt[tb % 2]
            bx = B_xt[tb % 2]
            hb = hT[tb % 2]
            bh = B_hT[tb % 2]
            for j in range(4):
                sc.dma("sp", xb[:, j, :], x_v[:, tb * 4 + j, :], writes=[bx])
            for kc in range(8):
                p = ps[pi % 8]
                bp = B_ps[pi % 8]
                pi += 1
                for j in range(4):
                    sc.op("pe", lambda e, p=p, xb=xb, j=j, kc=kc: e.transpose(
                        p[:, j * 128:(j + 1) * 128], xb[:, j, kc * 128:(kc + 1) * 128], ident[:, :]),
                        reads=[bx, B_ident], writes=[bp])
                sc.op("act", lambda e, p=p, hb=hb, kc=kc: e.activation(
                    out=hb[:, kc, :], in_=p[:, :], func=AF.Identity,
                    scale=onep1[:, kc:kc + 1], bias=modcol[:, kc:kc + 1]),
                    reads=[bp, B_mod], writes=[bh])
            for ci, (col0, kind, idx) in enumerate(fm_chunks):
                p = ps[pi % 8]
                bp = B_ps[pi % 8]
                pi += 1
                for kc in range(8):
                    sc.op("pe", lambda e, p=p, kc=kc, col0=col0, hb=hb: e.matmul(
                        p[:, :], lhsT=winb[:, kc, col0:col0 + 128], rhs=hb[:, kc, :],
                        start=(kc == 0), stop=(kc == 7)), reads=[B_win, bh], writes=[bp])
                k = evi % NEV
                evi += 1
                eng = "dve" if (ci % 2 == 0) else "act"
                tsl = slice(tb * 512, (tb + 1) * 512)
                if kind == "rw":
                    dst = ev32[k]
                    bd = B_ev32[k]
                    if eng == "dve":
                        sc.op("dve", lambda e, p=p, dst=dst: e.tensor_copy(out=dst[:, :], in_=p[:, :]), reads=[bp], writes=[bd])
                    else:
                        sc.op("act", lambda e, p=p, dst=dst: e.activation(out=dst[:, :], in_=p[:, :], func=AF.Copy), reads=[bp], writes=[bd])
                    sc.dma("sp", zrw_d[idx * 128:(idx + 1) * 128, tsl], dst[:, :], reads=[bd])
                elif kind in ("q", "qi"):
                    dst = ev16[k]
                    bd = B_ev16[k]
                    if eng == "dve":
                        sc.op("dve", lambda e, p=p, dst=dst: e.tensor_copy(out=dst[:, :], in_=p[:, :]), reads=[bp], writes=[bd])
                    else:
                        sc.op("act", lambda e, p=p, dst=dst: e.activation(out=dst[:, :], in_=p[:, :], func=AF.Copy), reads=[bp], writes=[bd])
                    dd = zq_d if kind == "q" else zqi_d
                    sc.dma("sp", dd[idx * 128:(idx + 1) * 128, tsl], dst[:, :], reads=[bd])
                else:
                    dst = ev16[k]
                    bd = B_ev16[k]
                    sc.op("act", lambda e, p=p, dst=dst: e.activation(out=dst[:, :], in_=p[:, :], func=AF.Sigmoid), reads=[bp], writes=[bd])
                    sc.dma("sp", zg_d[idx * 128:(idx + 1) * 128, tsl], dst[:, :], reads=[bd])
            for j in range(4):
                p = ps[pi % 8]
                bp = B_ps[pi % 8]
                pi += 1
                for (c0, ncol, o0) in tok_cols:
                    for kc in range(8):
                        sc.op("pe", lambda e, p=p, kc=kc, j=j, c0=c0, ncol=ncol, o0=o0, hb=hb: e.matmul(
                            p[:, o0:o0 + ncol], lhsT=hb[:, kc, j * 128:(j + 1) * 128], rhs=winb[:, kc, c0:c0 + ncol],
                            start=(kc == 0), stop=(kc == 7)), reads=[B_win, bh], writes=[bp])
                zt = ztk[j % 2]
                bz = B_ztk[j % 2]
                sc.op("dve", lambda e, p=p, zt=zt: e.tensor_copy(out=zt[:, :], in_=p[:, 0:196]), reads=[bp], writes=[bz])
                r0 = (tb * 4 + j) * 128
                sc.dma("sp", ztok_d[r0:r0 + 128, :], zt[:, :], reads=[bz])
        ar.release(mA)

    e0_done_flag = [False]
    if 'B' in phases:
        phase_B(locals())
    if 'C' in phases:
        phase_C(locals())
    if 'D' in phases:
        phase_D(locals())
    if 'E' in phases:
        phase_E(locals())

    sc.finish([])
    return nc


def _prep_inputs(inputs):
    f = lambda a: np.ascontiguousarray(np.asarray(a, dtype=np.float32))
    x = f(inputs["x"])
    c = f(inputs["c"])
    b_ada = f(inputs["b_ada"])[0]
    shared = {
        "w_ada": f(inputs["w_ada"])[0],
        "b_ada_col": np.ascontiguousarray(b_ada.reshape(48, 128).T),
        "b_ada_bc": np.ascontiguousarray(np.broadcast_to(b_ada[None, :], (128, 6 * D))),
        "w_in": f(inputs["w_in"])[0],
        "ident": np.eye(128, dtype=np.float32),
        "iota128": np.ascontiguousarray(np.broadcast_to(np.arange(128, dtype=np.float32)[None], (128, 128))),
    }
    col = lambda v, n: np.ascontiguousarray(f(v).reshape(n, 128).T)
    shared["mu_col"] = col(inputs["rw_mu"][0], 14)
    shared["rwvec"] = np.ascontiguousarray(np.concatenate([col(inputs["rw_w0"][0], 4), col(inputs["rw_a0"][0], 4), col(inputs["rw_k_k"][0], 4),
                                                            col(inputs["rw_k_a"][0], 4), col(f(inputs["rw_r_k"])[0].reshape(-1), 4)], axis=1))
    shared["w2a2"] = np.ascontiguousarray(np.concatenate([f(inputs["rw_w2"])[0], f(inputs["rw_a2"])[0]], axis=0))
    shared["g2"] = f(inputs["rw_g2"])[0]
    gn2 = np.stack([f(inputs["rw_gn_g"])[0], f(inputs["rw_gn_b"])[0]]).reshape(2, 4, 2, 64)
    gnl = np.zeros((128, 2, 4, 64), np.float32)
    gnl[0:64] = gn2[:, :, 0, :][None]
    gnl[64:128] = gn2[:, :, 1, :][None]
    shared["gn_bc"] = np.ascontiguousarray(gnl.reshape(128, 2, 256))
    cst = np.zeros((128, 1024), np.float32)
    iu = np.triu(np.ones((64, 64), np.float32), 1)
    il = np.triu(np.ones((64, 64), np.float32), 0)
    cst[:, 0:128] = np.block([[iu, il], [iu, il]])
    cst[0:64, 128:192] = iu.T
    cst[64:128, 128:192] = iu.T
    cst[0:64, 192:256] = 1.0
    cst[64:128, 256:320] = 1.0
    sm = np.ones(512, np.float32); sm[::64] = 0.0
    cst[:, 320:832] = sm[None, :]
    cst[0:64, 832:896] = np.eye(64, dtype=np.float32)
    cst[64:128, 832:896] = np.eye(64, dtype=np.float32)
    cst[:, 896] = 1.0
    shared["cst"] = cst
    shared["w_br_a"] = f(inputs["w_br_a"])[0]
    shared["w_br_b"] = f(inputs["w_br_b"])[0]
    shared["w_out"] = f(inputs["w_out"])[0]
    lnr = np.stack([f(inputs["ln1_g"])[0], f(inputs["ln1_b"])[0], f(inputs["ln2_g"])[0], f(inputs["ln2_b"])[0]])
    shared["ln_bc"] = np.ascontiguousarray(np.broadcast_to(lnr[None], (128, 4, D)))
    shared["peer_wq"] = f(inputs["peer_wq"])[0]
    pk = f(inputs["peer_keys"])[0]
    shared["peer_keysT"] = np.ascontiguousarray(pk.transpose(1, 3, 0, 2).reshape(128, 8, 128))
    shared["peer_u"] = f(inputs["peer_u"])[0]
    cv = np.concatenate([f(inputs["dsa_kv_g"])[0], f(inputs["idx_k_g"])[0], f(inputs["idx_k_b"])[0]])
    shared["cvec"] = np.ascontiguousarray(np.broadcast_to(cv[None], (128, 256)))
    rb = f(inputs["rel_bias"])
    nn_ = np.arange(0, 256)
    nf = np.maximum(nn_, 1).astype(np.float32)
    large = 16 + (np.log(nf / np.float32(16)) / np.float32(np.log(8.0)) * np.float32(16)).astype(np.int32)
    bucket = np.where(nn_ < 16, nn_, np.minimum(large, 31))
    sI = np.arange(128)[:, None]
    qI = np.arange(128)[None, :]
    bd = bucket[np.clip(qI - sI, 0, 255)]
    bp = bucket[np.clip(qI + 128 - sI, 0, 255)]
    bT = np.zeros((128, 3, 8, 128), np.float32)
    bT[:, 0] = rb[bd].transpose(0, 2, 1)
    bT[:, 1] = rb[bp].transpose(0, 2, 1)
    bT[:, 2] = rb[31][None, :, None]
    shared["biasT"] = np.ascontiguousarray(bT.reshape(128, 3, 1024))
    shared["negm"] = np.where(np.arange(128)[None, :] <= np.arange(128)[:, None], 0.0, -1e30).astype(np.float32)
    shared["peer_v"] = f(inputs["peer_v"])[0]
    maps = []
    for b in range(8):
        m = dict(shared)
        m["x"] = x[b]
        m["c_col"] = np.ascontiguousarray(c[b].reshape(8, 128).T)
        maps.append(m)
    return maps


def kernel(**inputs):
    nc = build()
    maps = _prep_inputs(inputs)
    res = run_bass_kernel_spmd(nc, maps, core_ids=list(range(8)))
    out = np.stack([np.asarray(r["out"], dtype=np.float32) for r in res.results], axis=0)
    return out


def _bc_mid(ap, n):
    sh = list(ap.shape)
    return ap.unsqueeze(1).to_broadcast([sh[0], n] + sh[1:])


def phase_B(L):
    nc, sc, ar, ps, B_ps = L["nc"], L["sc"], L["ar"], L["ps"], L["B_ps"]
    identb, B_ident, load_bf16 = L["identb"], L["B_ident"], L["load_bf16"]
    zrw_d, rwo_d = L["zrw_d"], L["rwo_d"]
    V = lambda fn, r=(), w=(): sc.op("dve", fn, r, w)
    A = lambda fn, r=(), w=(): sc.op("act", fn, r, w)
    P = lambda fn, r=(), w=(): sc.op("pe", fn, r, w)
    mB = ar.mark()
    cst = ar.alloc([128, 1024], F32, "cst")
    mu = ar.alloc([128, 14], F32, "mu")
    rwvec = ar.alloc([128, 20], F32, "rwvec")
    omka = ar.alloc([128, 4], F32, "omka")
    w2a2b = ar.alloc([128, 512], BF16, "w2a2b")
    g2b = ar.alloc([128, 512], BF16, "g2b")
    gnbc = ar.alloc([128, 2, 256], F32, "gnbc")
    bones = ar.alloc([128, 128], BF16, "bones")
    onesb = ar.alloc([128, 1], BF16, "onesb")
    B_c = Buf("cstB")
    sc.dma("sp", cst[:, :], L["cst_d"][:, :], writes=[B_c])
    sc.dma("sp", mu[:, :], L["mu_d"][:, :], writes=[B_c])
    sc.dma("sp", rwvec[:, :], L["rwvec_d"][:, :], writes=[B_c])
    sc.dma("sp", gnbc[:, :, :], L["gnbc_d"][:, :, :], writes=[B_c])
    load_bf16(w2a2b[:, :], L["w2a2_d"][:, :], 512, B_c)
    load_bf16(g2b[:, :], L["g2_d"][:, :], 512, B_c)
    V(lambda e: e.tensor_scalar(out=omka[:, :], in0=rwvec[:, 12:16], scalar1=-1.0, scalar2=1.0, op0=ALU.mult, op1=ALU.add), [B_c], [B_c])
    V(lambda e: e.tensor_copy(out=bones[:, :], in_=cst[:, 192:320]), [B_c], [B_c])
    V(lambda e: e.tensor_copy(out=onesb[:, :], in_=cst[:, 896:897]), [B_c], [B_c])
    maskA = cst[:, 0:128]
    maskT = cst[:, 128:192]
    scanmask = cst[:, 320:832]
    eye64 = cst[:, 832:896]
    w0c, a0c, kkc, kac, rkc = (rwvec[:, 0:4], rwvec[:, 4:8], rwvec[:, 8:12], rwvec[:, 12:16], rwvec[:, 16:20])

    zb = ar.alloc([128, 14, 513], F32, "zb")
    zs = ar.alloc([128, 14, 512], F32, "zs")
    tmp = [ar.alloc([128, 512], F32, "tmpB%d" % i) for i in range(3)]
    B_tmp = [Buf("tmpB%d" % i) for i in range(3)]
    th = ar.alloc([128, 512], BF16, "th")
    al16 = ar.alloc([128, 512], BF16, "al16")
    sgl = ar.alloc([128, 512], BF16, "sgl")
    sq16 = ar.alloc([128, 512], BF16, "sq16")
    asg = ar.alloc([128, 512], F32, "asg")
    kk = ar.alloc([128, 512], F32, "kk")
    kp = ar.alloc([128, 512], F32, "kp")
    bv = ar.alloc([128, 512], F32, "bv")
    lw = ar.alloc([128, 512], F32, "lw")
    cs = ar.alloc([128, 512], F32, "cs")
    E = [ar.alloc([128, 512], F32, "E%d" % i) for i in range(4)]
    E5 = ar.alloc([128, 4, 8], F32, "E5")
    AR = ar.alloc([128, 4, 8, 2, 64], BF16, "AR")
    BK = ar.alloc([128, 4, 8, 2, 64], BF16, "BK")
    KH = ar.alloc([128, 4, 512], BF16, "KH")
    BH = ar.alloc([128, 4, 512], BF16, "BH")
    Vb = ar.alloc([128, 4, 512], BF16, "Vb")
    rkr = ar.alloc([128, 4, 512], BF16, "rkr")
    rwoT = ar.alloc([128, 4, 512], BF16, "rwoT")
    B_zb, B_zs, B_pre, B_blk, B_rwoT = Buf("zb"), Buf("zs"), Buf("pre"), Buf("blk"), Buf("rwoT")
    Vt2p = [ar.alloc([128, 512], BF16, "Vt2_%d" % i) for i in range(2)]
    KHt2p = [ar.alloc([128, 512], BF16, "KHt2_%d" % i) for i in range(2)]
    BHt2p = [ar.alloc([128, 512], BF16, "BHt2_%d" % i) for i in range(2)]
    sABp = [ar.alloc([128, 4, 128], BF16, "sAB%d" % i) for i in range(2)]
    sAKp = [ar.alloc([128, 4, 128], BF16, "sAK%d" % i) for i in range(2)]
    Xfp = [ar.alloc([128, 4, 64], BF16, "Xf%d" % i) for i in range(2)]
    B_Vtp = [Buf("Vt0"), Buf("Vt1")]
    B_sAp = [Buf("sA0"), Buf("sA1")]
    B_Xfp = [Buf("Xf0"), Buf("Xf1")]
    Mx = [ar.alloc([128, 4, 64], BF16, "Mx%d" % i) for i in range(2)]
    MT = [ar.alloc([128, 4, 64], BF16, "MT%d" % i) for i in range(2)]
    X = [ar.alloc([128, 4, 64], BF16, "X%d" % i) for i in range(2)]
    RHSs = ar.alloc([128, 4, 64], BF16, "RHSs")
    SAs = ar.alloc([128, 4, 64], BF16, "SAs")
    ST = ar.alloc([128, 4, 64], BF16, "ST")
    STf = ar.alloc([128, 4, 64], F32, "STf")
    sqy = ar.alloc([128, 256], F32, "sqy")
    yn = ar.alloc([128, 256], F32, "yn")
    bon = ar.alloc([128, 256], F32, "bon")
    O16 = ar.alloc([128, 256], BF16, "O16")
    st8 = ar.alloc([128, 4, 8], F32, "st8")
    B_Vt, B_sA, B_M, B_MT, B_X, B_R, B_SA, B_ST, B_ep, B_st8, B_O = (Buf("Vt"), Buf("sA"), [Buf("M0"), Buf("M1")], [Buf("MT0"), Buf("MT1")],
                                                                    [Buf("X0"), Buf("X1")], Buf("R"), Buf("SA"), Buf("ST"), Buf("ep"), Buf("st8"), Buf("O"))
    e0 = make_e0(L, 2, 7) if ('E' in L["phases"] or 'E0' in L["phases"]) else iter(())
    V(lambda e: e.memset(STf[:, :, :], 0.0), [], [B_ST])
    V(lambda e: e.memset(ST[:, :, :], 0.0), [], [B_ST])
    V(lambda e: e.memset(zb[:, :, 0:1], 0.0), [], [B_zb])

    def v3(ap2):
        return ap2.rearrange("p (c t) -> p c t", t=64)

    for tb in range(L['nblk']):
        for i in range(14):
            if tb == 0:
                sc.dma("sp", zb[:, i, 1:513], zrw_d[i * 128:(i + 1) * 128, 0:512], writes=[B_zb])
            else:
                sc.dma("sp", zb[:, i, 0:513], zrw_d[i * 128:(i + 1) * 128, tb * 512 - 1:tb * 512 + 512], writes=[B_zb])
        for i in range(14):
            t0 = tmp[i % 2]
            bt0 = B_tmp[i % 2]
            V(lambda e, i=i, t0=t0: e.tensor_tensor(out=t0[:, :], in0=zb[:, i, 0:512], in1=zb[:, i, 1:513], op=ALU.subtract), [B_zb], [bt0])
            V(lambda e, i=i, t0=t0: e.scalar_tensor_tensor(out=zs[:, i, :], in0=t0[:, :], scalar=mu[:, i:i + 1], in1=zb[:, i, 1:513],
                                                           op0=ALU.mult, op1=ALU.add), [bt0, B_zb, B_c], [B_zs])
        A(lambda e: e.activation(out=th[:, :], in_=zs[:, 12, :], func=AF.Tanh), [B_zs], [B_pre])
        V(lambda e: e.tensor_copy(out=al16[:, :], in_=zs[:, 12, :]), [B_zs], [B_pre])
        A(lambda e: e.activation(out=sgl[:, :], in_=zs[:, 13, :], func=AF.Sigmoid), [B_zs], [B_blk])
        for j in range(4):
            pW, bW = ps[0], B_ps[0]
            pA, bA = ps[1], B_ps[1]
            pQ, bQ = ps[2], B_ps[2]
            P(lambda e, j=j: e.matmul(pW[:, :], lhsT=w2a2b[0:64, j * 128:(j + 1) * 128], rhs=th[0:64, :], start=True, stop=True), [B_c, B_pre], [bW])
            P(lambda e, j=j: e.matmul(pA[:, :], lhsT=w2a2b[64:128, j * 128:(j + 1) * 128], rhs=al16[64:128, :], start=True, stop=True), [B_c, B_pre], [bA])
            A(lambda e, j=j: e.activation(out=lw[:, :], in_=pW[:, :], func=AF.Sigmoid, bias=w0c[:, j:j + 1]), [bW, B_c], [B_pre])
            A(lambda e, j=j: e.activation(out=asg[:, :], in_=pA[:, :], func=AF.Sigmoid, bias=a0c[:, j:j + 1]), [bA, B_c], [B_pre])
            A(lambda e, j=j: e.activation(out=sq16[:, :], in_=zs[:, 4 + j, :], func=AF.Square, scale=kkc[:, j:j + 1]), [B_zs, B_c], [B_pre])
            P(lambda e: e.matmul(pQ[:, :], lhsT=bones[:, :], rhs=sq16[:, :], start=True, stop=True), [B_c, B_pre], [bQ])
            V(lambda e: e.tensor_scalar(out=tmp[2][:, :], in0=pQ[:, :], scalar1=1e-24, scalar2=None, op0=ALU.max), [bQ], [B_tmp[2]])
            A(lambda e: e.activation(out=tmp[2][:, :], in_=tmp[2][:, :], func=AF.Sqrt), [B_tmp[2]], [B_tmp[2]])
            V(lambda e: e.reciprocal(out=tmp[2][:, :], in_=tmp[2][:, :]), [B_tmp[2]], [B_tmp[2]])
            V(lambda e, j=j: e.scalar_tensor_tensor(out=kk[:, :], in0=zs[:, 4 + j, :], scalar=kkc[:, j:j + 1], in1=tmp[2][:, :],
                                                    op0=ALU.mult, op1=ALU.mult), [B_zs, B_tmp[2], B_c], [B_pre])
            V(lambda e, j=j: e.tensor_scalar(out=tmp[0][:, :], in0=asg[:, :], scalar1=kac[:, j:j + 1], scalar2=omka[:, j:j + 1],
                                             op0=ALU.mult, op1=ALU.add), [B_pre, B_c], [B_tmp[0]])
            V(lambda e, j=j: e.tensor_tensor(out=kp[:, :], in0=zs[:, 4 + j, :], in1=tmp[0][:, :], op=ALU.mult), [B_zs, B_tmp[0]], [B_pre])
            V(lambda e: e.tensor_tensor(out=bv[:, :], in0=kk[:, :], in1=asg[:, :], op=ALU.mult), [B_pre], [B_pre])
            V(lambda e: e.tensor_scalar(out=lw[:, :], in0=lw[:, :], scalar1=-0.6065306597126334, scalar2=None, op0=ALU.mult), [B_pre], [B_pre])
            V(lambda e: e.tensor_tensor_scan(out=cs[:, :], data0=scanmask, data1=lw[:, :], initial=0.0, op0=ALU.mult, op1=ALU.add), [B_pre, B_c], [B_pre])
            V(lambda e: e.tensor_tensor(out=tmp[0][:, :], in0=cs[:, :], in1=lw[:, :], op=ALU.subtract), [B_pre], [B_tmp[0]])
            V(lambda e: e.tensor_tensor(out=v3(tmp[1][:, :]), in0=v3(cs[:, :])[:, :, 63:64].to_broadcast([128, 8, 64]), in1=v3(cs[:, :]),
                                        op=ALU.subtract), [B_pre], [B_tmp[1]])
            A(lambda e: e.activation(out=E[0][:, :], in_=cs[:, :], func=AF.Exp), [B_pre], [B_pre])
            A(lambda e: e.activation(out=E[1][:, :], in_=cs[:, :], func=AF.Exp, scale=-1.0), [B_pre], [B_pre])
            A(lambda e: e.activation(out=E[2][:, :], in_=tmp[0][:, :], func=AF.Exp), [B_tmp[0]], [B_pre])
            A(lambda e: e.activation(out=E[3][:, :], in_=tmp[1][:, :], func=AF.Exp), [B_tmp[1]], [B_pre])
            V(lambda e, j=j: e.tensor_copy(out=E5[:, j, :], in_=v3(E[0][:, :])[:, :, 63]), [B_pre], [B_blk])
            V(lambda e, j=j: e.tensor_tensor(out=AR[:, j, :, 1, :], in0=v3(zs[:, j, :]), in1=v3(E[0][:, :]), op=ALU.mult), [B_zs, B_pre], [B_blk])
            V(lambda e, j=j: e.tensor_tensor(out=BK[:, j, :, 1, :], in0=v3(kp[:, :]), in1=v3(E[1][:, :]), op=ALU.mult), [B_pre], [B_blk])
            V(lambda e, j=j: e.tensor_tensor(out=BK[:, j, :, 0, :], in0=v3(bv[:, :]), in1=v3(E[1][:, :]), op=ALU.mult), [B_pre], [B_blk])
            V(lambda e, j=j: e.scalar_tensor_tensor(out=AR[:, j, :, 0, :], in0=v3(kk[:, :]), scalar=-1.0, in1=v3(E[2][:, :]),
                                                    op0=ALU.mult, op1=ALU.mult), [B_pre], [B_blk])
            V(lambda e, j=j: e.tensor_tensor(out=KH[:, j, :], in0=kp[:, :], in1=E[3][:, :], op=ALU.mult), [B_pre], [B_blk])
            V(lambda e, j=j: e.tensor_tensor(out=BH[:, j, :], in0=bv[:, :], in1=E[3][:, :], op=ALU.mult), [B_pre], [B_blk])
            A(lambda e, j=j: e.activation(out=Vb[:, j, :], in_=zs[:, 8 + j, :], func=AF.Copy), [B_zs], [B_blk])
            V(lambda e, j=j: e.scalar_tensor_tensor(out=rkr[:, j, :], in0=zs[:, j, :], scalar=rkc[:, j:j + 1], in1=kp[:, :],
                                                    op0=ALU.mult, op1=ALU.mult), [B_zs, B_pre, B_c], [B_blk])

        def v4(ap2):
            return ap2.rearrange("p (j t) -> p j t", t=64)
        hl = [(h // 2, slice((h % 2) * 64, (h % 2) * 64 + 64), slice((h // 2) * 64, (h // 2) * 64 + 64), slice(h * 64, (h + 1) * 64)) for h in range(8)]

        def pre(c):
            q = c % 2
            csl = slice(c * 64, (c + 1) * 64)
            Vt2, KHt2, BHt2, sAB, sAK = Vt2p[q], KHt2p[q], BHt2p[q], sABp[q], sAKp[q]
            B_Vt, B_sA = B_Vtp[q], B_sAp[q]
            pT = ps[3][:, :].bitcast(BF16)
            pT2 = ps[4][:, :].bitcast(BF16)
            for half in range(2):
                hp = slice(half * 64, half * 64 + 64)
                for j in range(4):
                    P(lambda e: e.transpose(pT[hp, j * 128:(j + 1) * 128], Vb[:, j, csl], identb[:, :]), [B_blk, B_ident], [B_ps[3]])
                    P(lambda e: e.transpose(pT[hp, 512 + j * 128:512 + (j + 1) * 128], KH[:, j, csl], identb[:, :]), [B_blk, B_ident], [B_ps[3]])
                    P(lambda e: e.transpose(pT2[hp, j * 128:(j + 1) * 128], BH[:, j, csl], identb[:, :]), [B_blk, B_ident], [B_ps[4]])
            yield
            V(lambda e: e.tensor_copy(out=Vt2[:, :], in_=pT[:, 0:512]), [B_ps[3]], [B_Vt])
            A(lambda e: e.activation(out=KHt2[:, :], in_=pT[:, 512:1024], func=AF.Copy), [B_ps[3]], [B_Vt])
            A(lambda e: e.activation(out=BHt2[:, :], in_=pT2[:, 0:512], func=AF.Copy), [B_ps[4]], [B_Vt])
            for (j, pp, js, hs) in hl:
                P(lambda e: e.matmul(ps[0][pp, j * 128:(j + 1) * 128], lhsT=BK[pp, j, c, 0, :], rhs=AR[pp, j, c, :, :], start=True, stop=True), [B_blk], [B_ps[0]])
                P(lambda e: e.matmul(ps[1][pp, j * 128:(j + 1) * 128], lhsT=BK[pp, j, c, 1, :], rhs=AR[pp, j, c, :, :], start=True, stop=True), [B_blk], [B_ps[1]])
                P(lambda e: e.matmul(ps[2][pp, j * 64:(j + 1) * 64], lhsT=AR[pp, j, c, 0, :], rhs=BK[pp, j, c, 0, :], start=True, stop=True), [B_blk], [B_ps[2]])
            yield
            V(lambda e: e.tensor_tensor(out=sAB[:, :, :], in0=ps[0][:, :].rearrange("p (j t) -> p j t", t=128), in1=_bc_mid(maskA, 4), op=ALU.mult), [B_ps[0], B_c], [B_sA])
            V(lambda e: e.tensor_tensor(out=sAK[:, :, :], in0=ps[1][:, :].rearrange("p (j t) -> p j t", t=128), in1=_bc_mid(maskA, 4), op=ALU.mult), [B_ps[1], B_c], [B_sA])
            V(lambda e: e.tensor_tensor(out=MT[0][:, :, :], in0=v4(ps[2][:, 0:256]), in1=_bc_mid(maskT, 4), op=ALU.mult), [B_ps[2], B_c], [B_MT[0]])
            V(lambda e: e.tensor_copy(out=Mx[0][:, :, :], in_=sAB[:, :, 0:64]), [B_sA], [B_M[0]])
            V(lambda e: e.tensor_tensor(out=X[0][:, :, :], in0=sAB[:, :, 0:64], in1=_bc_mid(eye64, 4), op=ALU.add), [B_sA, B_c], [B_X[0]])
            yield
            cur = 0
            pa, pb, pc = ps[2], ps[3], ps[4]
            for rd in range(5):
                nxt = 1 - cur
                for (j, pp, js, hs) in hl:
                    P(lambda e: e.matmul(pa[pp, js], lhsT=MT[cur][pp, j, :], rhs=Mx[cur][pp, j, :], start=True, stop=True), [B_MT[cur], B_M[cur]], [B_ps[2]])
                    P(lambda e: e.matmul(pb[pp, js], lhsT=Mx[cur][pp, j, :], rhs=MT[cur][pp, j, :], start=True, stop=True), [B_MT[cur], B_M[cur]], [B_ps[3]])
                yield
                V(lambda e: e.tensor_copy(out=Mx[nxt][:, :, :], in_=v4(pa[:, 0:256])), [B_ps[2]], [B_M[nxt]])
                A(lambda e: e.activation(out=MT[nxt][:, :, :], in_=v4(pb[:, 0:256]), func=AF.Copy), [B_ps[3]], [B_MT[nxt]])
                for (j, pp, js, hs) in hl:
                    P(lambda e: e.matmul(pc[pp, js], lhsT=MT[nxt][pp, j, :], rhs=X[cur][pp, j, :], start=True, stop=True), [B_MT[nxt], B_X[cur]], [B_ps[4]])
                yield
                if rd < 4:
                    V(lambda e: e.tensor_tensor(out=X[nxt][:, :, :], in0=X[cur][:, :, :], in1=v4(pc[:, 0:256]), op=ALU.add), [B_X[cur], B_ps[4]], [B_X[nxt]])
                else:
                    V(lambda e: e.tensor_tensor(out=Xfp[q][:, :, :], in0=X[cur][:, :, :], in1=v4(pc[:, 0:256]), op=ALU.add), [B_X[cur], B_ps[4]], [B_Xfp[q]])
                cur = nxt
                yield

        def post(c):
            q = c % 2
            csl = slice(c * 64, (c + 1) * 64)
            Vt2, KHt2, BHt2, sAB, sAK, Xf = Vt2p[q], KHt2p[q], BHt2p[q], sABp[q], sAKp[q], Xfp[q]
            B_Vt, B_sA, bXf = B_Vtp[q], B_sAp[q], B_Xfp[q]
            pR, bR = ps[5], B_ps[5]
            pY, bY = ps[6], B_ps[6]
            pU, bU = ps[7], B_ps[7]
            for (j, pp, js, hs) in hl:
                P(lambda e: e.matmul(pR[pp, js], lhsT=AR[pp, j, c, 0, :], rhs=ST[pp, j, :], start=True, stop=False), [B_blk, B_ST], [bR])
                P(lambda e: e.matmul(pR[pp, js], lhsT=sAK[pp, j, 0:64], rhs=Vt2[pp, hs], start=False, stop=True), [B_sA, B_Vt], [bR])
            yield
            V(lambda e: e.tensor_copy(out=RHSs[:, :, :], in_=v4(pR[:, 0:256])), [bR], [B_R])
            for (j, pp, js, hs) in hl:
                P(lambda e: e.matmul(pR[pp, js], lhsT=Xf[pp, j, :], rhs=RHSs[pp, j, :], start=True, stop=True), [bXf, B_R], [bR])
            yield
            V(lambda e: e.tensor_copy(out=SAs[:, :, :], in_=v4(pR[:, 0:256])), [bR], [B_SA])
            for (j, pp, js, hs) in hl:
                P(lambda e: e.matmul(pY[pp, js], lhsT=AR[pp, j, c, 1, :], rhs=ST[pp, j, :], start=True, stop=False), [B_blk, B_ST], [bY])
                P(lambda e: e.matmul(pY[pp, js], lhsT=sAK[pp, j, 64:128], rhs=Vt2[pp, hs], start=False, stop=False), [B_sA, B_Vt], [bY])
                P(lambda e: e.matmul(pY[pp, js], lhsT=sAB[pp, j, 64:128], rhs=SAs[pp, j, :], start=False, stop=True), [B_sA, B_SA], [bY])
            for (j, pp, js, hs) in hl:
                P(lambda e: e.matmul(pU[pp, js], lhsT=KHt2[pp, hs], rhs=Vt2[pp, hs], start=True, stop=False), [B_Vt], [bU])
                P(lambda e: e.matmul(pU[pp, js], lhsT=BHt2[pp, hs], rhs=SAs[pp, j, :], start=False, stop=True), [B_Vt, B_SA], [bU])
            yield
            V(lambda e: e.tensor_tensor(out=STf[:, :, :], in0=STf[:, :, :], in1=E5[:, :, c:c + 1].to_broadcast([128, 4, 64]), op=ALU.mult), [B_blk, B_ST, bY, bR], [B_ST])
            V(lambda e: e.tensor_tensor(out=STf[:, :, :], in0=STf[:, :, :], in1=v4(pU[:, 0:256]), op=ALU.add), [bU, B_ST], [B_ST])
            V(lambda e: e.tensor_copy(out=ST[:, :, :], in_=STf[:, :, :]), [B_ST], [B_ST])
            pG, bG = ps[5], B_ps[5]
            for (j, pp, js, hs) in hl:
                P(lambda e: e.matmul(pG[pp, 256 + j:256 + j + 1], lhsT=rkr[pp, j, csl], rhs=onesb[pp, 0:1], start=True, stop=True), [B_blk, B_c, B_SA], [bG])
                P(lambda e: e.matmul(pG[pp, js], lhsT=sgl[:, csl], rhs=g2b[:, hs], start=True, stop=True), [B_blk, B_c, B_SA, B_R], [bG])
            y3 = v4(pY[:, 0:256])
            V(lambda e: e.tensor_reduce(out=st8[:, :, 0], in_=y3, axis=AX.X, op=ALU.add), [bY], [B_st8])
            A(lambda e: e.activation(out=sqy[:, :], in_=pY[:, 0:256], func=AF.Square), [bY], [B_ep])
            yield
            V(lambda e: e.tensor_reduce(out=st8[:, :, 1], in_=v4(sqy[:, :]), axis=AX.X, op=ALU.add), [B_ep], [B_st8])
            V(lambda e: e.tensor_scalar(out=st8[:, :, 2], in0=st8[:, :, 0], scalar1=1.0 / 64, scalar2=None, op0=ALU.mult), [B_st8], [B_st8])
            V(lambda e: e.tensor_tensor(out=st8[:, :, 3], in0=st8[:, :, 2], in1=st8[:, :, 2], op=ALU.mult), [B_st8], [B_st8])
            V(lambda e: e.scalar_tensor_tensor(out=st8[:, :, 4], in0=st8[:, :, 1], scalar=1.0 / 64, in1=st8[:, :, 3], op0=ALU.mult, op1=ALU.subtract), [B_st8], [B_st8])
            V(lambda e: e.tensor_scalar(out=st8[:, :, 4], in0=st8[:, :, 4], scalar1=64e-5, scalar2=None, op0=ALU.add), [B_st8], [B_st8])
            A(lambda e: e.activation(out=st8[:, :, 5], in_=st8[:, :, 4], func=AF.Sqrt), [B_st8], [B_st8])
            yield
            V(lambda e: e.reciprocal(out=st8[:, :, 5], in_=st8[:, :, 5]), [B_st8], [B_st8])
            yn3 = v4(yn[:, :])
            V(lambda e: e.tensor_tensor(out=yn3, in0=y3, in1=st8[:, :, 2:3].to_broadcast([128, 4, 64]), op=ALU.subtract), [bY, B_st8], [B_ep])
            V(lambda e: e.tensor_tensor(out=yn3, in0=yn3, in1=st8[:, :, 5:6].to_broadcast([128, 4, 64]), op=ALU.mult), [B_ep, B_st8], [B_ep])
            V(lambda e: e.tensor_tensor(out=yn[:, :], in0=yn[:, :], in1=gnbc[:, 0, :], op=ALU.mult), [B_ep, B_c], [B_ep])
            V(lambda e: e.tensor_tensor(out=yn[:, :], in0=yn[:, :], in1=gnbc[:, 1, :], op=ALU.add), [B_ep, B_c], [B_ep])
            V(lambda e: e.tensor_copy(out=st8[:, :, 6], in_=pG[:, 256:260]), [bG], [B_st8])
            for half in range(2):
                hp = slice(half * 64, half * 64 + 64)
                V(lambda e: e.tensor_tensor(out=v4(bon[hp, :]), in0=Vt2[hp, :].rearrange("p (j q t) -> p j q t", q=2, t=64)[:, :, half, :],
                                            in1=st8[hp, :, 6:7].to_broadcast([64, 4, 64]), op=ALU.mult), [B_Vt, B_st8], [B_ep])
            V(lambda e: e.tensor_tensor(out=yn[:, :], in0=yn[:, :], in1=bon[:, :], op=ALU.add), [B_ep], [B_ep])
            V(lambda e: e.tensor_tensor(out=O16[:, :], in0=yn[:, :], in1=pG[:, 0:256], op=ALU.mult), [B_ep, bG], [B_O])
            pO = pU[:, 384:512].bitcast(BF16)
            for (j, pp, js, hs) in hl:
                P(lambda e: e.transpose(pO[pp, js], O16[pp, js], identb[pp, pp]), [B_O, B_ident, B_ST], [bU])
            yield
            V(lambda e: e.tensor_copy(out=rwoT[:, :, csl], in_=v4(pO[:, 0:256])), [bU], [B_rwoT])

        def run_both(ga, gb):
            alive_a, alive_b = ga is not None, gb is not None
            while alive_a or alive_b:
                if alive_a:
                    try:
                        next(ga)
                    except StopIteration:
                        alive_a = False
                if alive_b:
                    try:
                        next(gb)
                    except StopIteration:
                        alive_b = False

        run_both(pre(0), None)
        for c in range(8):
            for _ in range(4):
                next(e0, None)
            run_both(post(c), pre(c + 1) if c + 1 < 8 else None)
        for j in range(4):
            sc.dma("sp", rwo_d[j * 128:(j + 1) * 128, tb * 512:(tb + 1) * 512], rwoT[:, j, :], reads=[B_rwoT])
    for _ in e0:
        pass
    if 'E' in L["phases"]:
        L["e0_done_flag"][0] = True
    ar.release(mB)


def phase_D(L):
    nc, sc, ar, ps, B_ps = L["nc"], L["sc"], L["ar"], L["ps"], L["B_ps"]
    ident, identb, B_ident, load_bf16 = L["ident"], L["identb"], L["B_ident"], L["load_bf16"]
    gt_bc, B_gt = L["gt_bc"], L["B_gt"]
    dbg = L["dbg"]
    V = lambda fn, r=(), w=(): sc.op("dve", fn, r, w)
    A = lambda fn, r=(), w=(): sc.op("act", fn, r, w)
    P = lambda fn, r=(), w=(): sc.op("pe", fn, r, w)
    ALPHA = 2.0 ** 0.25
    mD = ar.mark()
    wbra = ar.alloc([128, 4, D], BF16, "wbra")
    wbrb = ar.alloc([128, 8, D], BF16, "wbrb")
    wout = ar.alloc([128, 8, D], BF16, "wout")
    wqb = ar.alloc([128, 8, D], BF16, "wqb")
    keysT = ar.alloc([128, 8, 128], BF16, "keysT")
    lnbc = ar.alloc([128, 2, D], F32, "lnbc")
    B_w = Buf("wD")
    for kc in range(4):
        load_bf16(wbra[:, kc, :], L["wbra_d"][kc * 128:(kc + 1) * 128, :], D, B_w)
    for kc in range(8):
        load_bf16(wbrb[:, kc, :], L["wbrb_d"][kc * 128:(kc + 1) * 128, :], D, B_w)
        load_bf16(wout[:, kc, :], L["wout_d"][kc * 128:(kc + 1) * 128, :], D, B_w)
        load_bf16(wqb[:, kc, :], L["wq_d"][kc * 128:(kc + 1) * 128, :], D, B_w)
    load_bf16(keysT[:, :, :].rearrange("p a b -> p (a b)"), L["pkeys_d"][:, :, :].rearrange("p a b -> p (a b)"), 1024, B_w)
    sc.dma("sp", lnbc[:, :, :], L["lnbc_d"][:, 0:2, :], writes=[B_w])
    rwoB = ar.alloc([128, 4, 512], BF16, "rwoB")
    dsaB = ar.alloc([128, 8, 512], BF16, "dsaB")
    zgr = [ar.alloc([128, 2, 512], BF16, "zgr%d" % i) for i in range(2)]
    B_zgr = [Buf("zgr0"), Buf("zgr1")]
    mg = ar.alloc([128, 8, 512], BF16, "mg")
    t1 = ar.alloc([128, 512], F32, "t1")
    t2 = ar.alloc([128, 512], F32, "t2")
    B_in, B_mg, B_t = Buf("inD"), Buf("mg"), Buf("tD")
    xt = ar.alloc([128, D], F32, "xtD")
    u = ar.alloc([128, D], F32, "uD")
    x1 = ar.alloc([128, D], F32, "x1")
    h2 = ar.alloc([128, D], F32, "h2")
    st = ar.alloc([128, 8], F32, "stD")
    h2T = ar.alloc([128, 8, 128], BF16, "h2T")
    qT = ar.alloc([128, 8, 128], BF16, "qT")
    ssb = ar.alloc([128, 16, 128], F32, "ssb")
    stmp = ar.alloc([128, 256], F32, "stmp")
    tv = ar.alloc([128, 16, 16], F32, "tv")
    ti = ar.alloc([128, 16, 16], U32, "ti")
    tif = ar.alloc([128, 16, 16], F32, "tif")
    cand = ar.alloc([128, 8, 256], F32, "cand")
    mv = ar.alloc([128, 8, 16], F32, "mv")
    posu = ar.alloc([128, 8, 16], U32, "posu")
    au = ar.alloc([128, 8, 16], U32, "au")
    bu = ar.alloc([128, 8, 16], U32, "bu")
    abf = ar.alloc([128, 2, 128], F32, "abf")
    oh16 = ar.alloc([128, 128, 16], F32, "oh16")
    junk = oh16[:, 0:64, :].rearrange("p a b -> p (a b)")
    sel = ar.alloc([128, 3, 128], F32, "sel")
    selT = ar.alloc([128, 3, 128], F32, "selT")
    gate = ar.alloc([128, 8, 16], F32, "gate")
    gs = ar.alloc([128, 8], F32, "gs")
    iota16 = L["iota16"]
    B_oh, B_sel, B_selT = Buf("oh"), Buf("sel"), Buf("selT")
    B_x, B_u, B_x1, B_h2, B_st, B_h2T, B_qT, B_s, B_tk, B_c, B_e, B_hu, B_acc, B_j = [Buf(n) for n in
        ("x", "u", "x1", "h2", "st", "h2T", "qT", "s", "tk", "cand", "eid", "hu", "acc", "junk")]
    x_v = L["x_d"].rearrange("(n p) m -> p n m", p=128)
    out_v = L["out_d"].rearrange("(n p) m -> p n m", p=128)

    def layer_norm(src, bsrc, dst, bdst, gi):
        V(lambda e: e.tensor_reduce(out=st[:, 0:1], in_=src[:, :], axis=AX.X, op=ALU.add), [bsrc], [B_st])
        A(lambda e: e.activation(out=junk, in_=src[:, :], func=AF.Square, accum_out=st[:, 1:2]), [bsrc], [B_st, B_oh])
        V(lambda e: e.tensor_scalar(out=st[:, 2:3], in0=st[:, 0:1], scalar1=1.0 / D, scalar2=None, op0=ALU.mult), [B_st], [B_st])
        V(lambda e: e.tensor_tensor(out=st[:, 3:4], in0=st[:, 2:3], in1=st[:, 2:3], op=ALU.mult), [B_st], [B_st])
        V(lambda e: e.scalar_tensor_tensor(out=st[:, 4:5], in0=st[:, 1:2], scalar=1.0 / D, in1=st[:, 3:4], op0=ALU.mult, op1=ALU.subtract), [B_st], [B_st])
        V(lambda e: e.tensor_scalar(out=st[:, 4:5], in0=st[:, 4:5], scalar1=1e-5, scalar2=None, op0=ALU.add), [B_st], [B_st])
        A(lambda e: e.activation(out=st[:, 5:6], in_=st[:, 4:5], func=AF.Sqrt), [B_st], [B_st])
        V(lambda e: e.reciprocal(out=st[:, 5:6], in_=st[:, 5:6]), [B_st], [B_st])
        V(lambda e: e.tensor_scalar(out=dst[:, :], in0=src[:, :], scalar1=st[:, 2:3], scalar2=st[:, 5:6], op0=ALU.subtract, op1=ALU.mult), [bsrc, B_st], [bdst])
        V(lambda e: e.tensor_tensor(out=dst[:, :], in0=dst[:, :], in1=lnbc[:, gi, :], op=ALU.mult), [bdst, B_w], [bdst])
        V(lambda e: e.tensor_tensor(out=dst[:, :], in0=dst[:, :], in1=lnbc[:, gi + 1, :], op=ALU.add), [bdst, B_w], [bdst])

    x1p = [x1, ar.alloc([128, D], F32, "x1b")]
    B_x1p = [B_x1, Buf("x1b")]
    h2Tp_ = [h2T, ar.alloc([128, 8, 128], BF16, "h2Tb")]
    B_h2Tp_ = [B_h2T, Buf("h2Tb")]
    ssbp = [ssb, ar.alloc([128, 16, 128], F32, "ssbb")]
    B_sp = [B_s, Buf("ssbb")]
    prevn = [None]
    B_ohh = [Buf("ohh%d" % i) for i in range(8)]
    stmp16 = ar.alloc([128, 16, 128], F32, "stmp16")
    stmp8 = stmp16[:, :, :].rearrange("p (h a) b -> p h (a b)", a=2)
    B_tkg = [Buf("tkg%d" % i) for i in range(16)]
    B_tkg2 = [Buf("tkgb%d" % i) for i in range(16)]
    B_stg16 = [Buf("stg16_%d" % i) for i in range(16)]
    B_tig = [Buf("tig%d" % i) for i in range(16)]
    B_tig2 = [Buf("tigb%d" % i) for i in range(16)]
    B_mvh = [Buf("mvh%d" % i) for i in range(8)]
    B_mvh2 = [Buf("mvhb%d" % i) for i in range(8)]
    B_st8h = [B_stg16[2 * i] for i in range(8)]
    B_posh = [Buf("posh%d" % i) for i in range(8)]
    B_posh2 = [Buf("poshb%d" % i) for i in range(8)]

    def stageA(n, jt):
        q = n % 2
        x1_, bx1_, h2T_, bh2T_, ssb_, bs_ = x1p[q], B_x1p[q], h2Tp_[q], B_h2Tp_[q], ssbp[q], B_sp[q]
        sc.dma("sp", xt[:, :], x_v[:, n, :], writes=[B_x])
        for half in range(2):
            pM, bM = ps[4 + half], B_ps[4 + half]
            hsl = slice(half * 512, (half + 1) * 512)
            for dc in range(8):
                P(lambda e: e.matmul(pM[:, :], lhsT=mg[:, dc, jt * 128:(jt + 1) * 128], rhs=wout[:, dc, hsl], start=(dc == 0), stop=(dc == 7)), [B_mg, B_w], [bM])
            V(lambda e: e.tensor_tensor(out=u[:, hsl], in0=pM[:, :], in1=gt_bc[:, 0, hsl], op=ALU.mult), [bM, B_gt], [B_u])
            V(lambda e: e.scalar_tensor_tensor(out=u[:, hsl], in0=xt[:, hsl], scalar=ALPHA, in1=u[:, hsl], op0=ALU.mult, op1=ALU.add), [B_x, B_u], [B_u])
        layer_norm(u, B_u, x1_, bx1_, 0)
        if "x1dbg" in dbg:
            sc.dma("sp", L["x1_d"][n * 128:(n + 1) * 128, :], x1_[:, :], reads=[bx1_])
        sc.dma("sp", L["x1s_d"][n * 128:(n + 1) * 128, :], x1_[:, :], reads=[bx1_])
        V(lambda e: e.tensor_tensor(out=h2[:, :], in0=x1_[:, :], in1=gt_bc[:, 2, :], op=ALU.mult), [bx1_, B_gt], [B_h2])
        V(lambda e: e.tensor_tensor(out=h2[:, :], in0=h2[:, :], in1=gt_bc[:, 1, :], op=ALU.add), [B_h2, B_gt], [B_h2])
        for kc in range(8):
            pp_, bp_ = ps[kc // 4], B_ps[kc // 4]
            P(lambda e: e.transpose(pp_[:, (kc % 4) * 128:(kc % 4 + 1) * 128], h2[:, kc * 128:(kc + 1) * 128], ident[:, :]), [B_h2, B_ident], [bp_])
        for k2 in range(2):
            A(lambda e: e.activation(out=h2T_[:, k2 * 4:(k2 + 1) * 4, :], in_=ps[k2][:, :].rearrange("p (a b) -> p a b", b=128), func=AF.Copy), [B_ps[k2]], [bh2T_])
        for kc in range(8):
            sc.dma("sp", L["h2T_d"][kc * 128:(kc + 1) * 128, n * 128:(n + 1) * 128], h2T_[:, kc, :], reads=[bh2T_])
        for hh in range(8):
            pq, bq = ps[2 + hh // 4], B_ps[2 + hh // 4]
            for kc in range(8):
                P(lambda e: e.matmul(pq[:, (hh % 4) * 128:(hh % 4 + 1) * 128], lhsT=wqb[:, kc, hh * 128:(hh + 1) * 128], rhs=h2T_[:, kc, :],
                                     start=(kc == 0), stop=(kc == 7)), [B_w, bh2T_], [bq])
        for k2 in range(2):
            A(lambda e: e.activation(out=qT[:, k2 * 4:(k2 + 1) * 4, :], in_=ps[2 + k2][:, :].rearrange("p (a b) -> p a b", b=128), func=AF.Copy), [B_ps[2 + k2]], [B_qT])
        for g in range(16):
            hh, cc = g % 8, g // 8
            pS, bS = ps[4 + g // 4], B_ps[4 + g // 4]
            P(lambda e: e.matmul(pS[:, (g % 4) * 128:(g % 4 + 1) * 128], lhsT=qT[cc * 64:(cc + 1) * 64, hh, :], rhs=keysT[cc * 64:(cc + 1) * 64, hh, :],
                                 start=True, stop=True), [B_qT, B_w], [bS])
        for k4 in range(4):
            A(lambda e: e.activation(out=ssb_[:, k4 * 4:(k4 + 1) * 4, :], in_=ps[4 + k4][:, :].rearrange("p (a b) -> p a b", b=128), func=AF.Copy), [B_ps[4 + k4]], [bs_])

    def stageB(n):
        q = n % 2
        ssb_, bs_ = ssbp[q], B_sp[q]
        for g in range(16):
            V(lambda e: e.max(out=tv[:, g, 0:8], in_=ssb_[:, g, :]), [bs_], [B_tkg[g]])
        for g in range(16):
            V(lambda e: e.match_replace(out=stmp16[:, g, :], in_to_replace=tv[:, g, 0:8], in_values=ssb_[:, g, :], imm_value=-1e30), [bs_, B_tkg[g]], [B_stg16[g]])
        for g in range(16):
            V(lambda e: e.max(out=tv[:, g, 8:16], in_=stmp16[:, g, :]), [B_stg16[g]], [B_tkg2[g]])
        for g in range(16):
            V(lambda e: e.max_index(out=ti[:, g, 0:8], in_max=tv[:, g, 0:8], in_values=ssb_[:, g, :]), [bs_, B_tkg[g]], [B_tig[g]])
        for g in range(16):
            V(lambda e: e.max_index(out=ti[:, g, 8:16], in_max=tv[:, g, 8:16], in_values=ssb_[:, g, :]), [bs_, B_tkg2[g]], [B_tig2[g]])
        V(lambda e: e.tensor_copy(out=tif[:, :, :], in_=ti[:, :, :]), B_tig + B_tig2, [B_tk])
        V(lambda e: e.tensor_copy(out=tv[:, 0:1, 0:1], in_=tv[:, 0:1, 0:1]), B_tkg + B_tkg2, [B_tk])
        tvv = tv[:, :, :].rearrange("p (c h) k -> p h c k", c=2)
        tfv = tif[:, :, :].rearrange("p (c h) k -> p h c k", c=2)
        c4 = cand[:, :, :].rearrange("p h (a b) -> p h a b", b=16)
        for hh in range(8):
            V(lambda e: e.tensor_tensor(out=c4[:, hh, :, :], in0=tvv[:, hh, 0, :].unsqueeze(2).to_broadcast([128, 16, 16]),
                                        in1=tvv[:, hh, 1, :].unsqueeze(1).to_broadcast([128, 16, 16]), op=ALU.add), [B_tk], [B_c])
        for hh in range(8):
            V(lambda e: e.max(out=mv[:, hh, 0:8], in_=cand[:, hh, :]), [B_c], [B_mvh[hh]])
        for hh in range(8):
            V(lambda e: e.match_replace(out=stmp8[:, hh, :], in_to_replace=mv[:, hh, 0:8], in_values=cand[:, hh, :], imm_value=-1e30), [B_c, B_mvh[hh]], [B_stg16[2 * hh], B_stg16[2 * hh + 1]])
        for hh in range(8):
            V(lambda e: e.max(out=mv[:, hh, 8:16], in_=stmp8[:, hh, :]), [B_stg16[2 * hh], B_stg16[2 * hh + 1]], [B_mvh2[hh]])
        for hh in range(8):
            V(lambda e: e.max_index(out=posu[:, hh, 0:8], in_max=mv[:, hh, 0:8], in_values=cand[:, hh, :]), [B_c, B_mvh[hh]], [B_posh[hh]])
        for hh in range(8):
            V(lambda e: e.max_index(out=posu[:, hh, 8:16], in_max=mv[:, hh, 8:16], in_values=cand[:, hh, :]), [B_c, B_mvh2[hh]], [B_posh2[hh]])
        V(lambda e: e.tensor_copy(out=mv[:, 0:1, 0:1], in_=mv[:, 0:1, 0:1]), B_mvh + B_mvh2 + B_posh + B_posh2, [B_e])
        V(lambda e: e.tensor_scalar(out=au[:, :, :], in0=posu[:, :, :], scalar1=4, scalar2=None, op0=ALU.logical_shift_right), [B_e], [B_e])
        V(lambda e: e.tensor_scalar(out=bu[:, :, :], in0=posu[:, :, :], scalar1=15, scalar2=None, op0=ALU.bitwise_and), [B_e], [B_e])
        V(lambda e: e.tensor_copy(out=abf[:, 0, :], in_=au[:, :, :].rearrange("p a b -> p (a b)")), [B_e], [B_e])
        V(lambda e: e.tensor_copy(out=abf[:, 1, :], in_=bu[:, :, :].rearrange("p a b -> p (a b)")), [B_e], [B_e])
        for cc in range(2):
            V(lambda e: e.tensor_tensor(out=oh16[:, :, :], in0=abf[:, cc, :].unsqueeze(2).to_broadcast([128, 128, 16]),
                                        in1=iota16.unsqueeze(1).to_broadcast([128, 128, 16]), op=ALU.is_equal), [B_e, L["B_iota"]], [B_oh] + B_ohh)
            for hh in range(8):
                V(lambda e: e.tensor_tensor(out=oh16[:, hh * 16:(hh + 1) * 16, :], in0=oh16[:, hh * 16:(hh + 1) * 16, :],
                                            in1=tfv[:, hh, cc, :].unsqueeze(1).to_broadcast([128, 16, 16]), op=ALU.mult), [B_oh, B_tk], [B_ohh[hh]])
            V(lambda e: e.tensor_reduce(out=sel[:, cc, :], in_=oh16[:, :, :], axis=AX.X, op=ALU.add), B_ohh, [B_sel, B_oh])
        V(lambda e: e.tensor_tensor(out=gate[:, :, :], in0=mv[:, :, :], in1=mv[:, :, 0:1].to_broadcast([128, 8, 16]), op=ALU.subtract), [B_e], [B_hu])
        A(lambda e: e.activation(out=gate[:, :, :], in_=gate[:, :, :], func=AF.Exp), [B_hu], [B_hu])
        V(lambda e: e.tensor_reduce(out=gs[:, :], in_=gate[:, :, :], axis=AX.X, op=ALU.add), [B_hu], [B_hu])
        V(lambda e: e.reciprocal(out=gs[:, :], in_=gs[:, :]), [B_hu], [B_hu])
        V(lambda e: e.tensor_tensor(out=sel[:, 2, :].rearrange("p (a b) -> p a b", b=16), in0=gate[:, :, :], in1=gs[:, :].unsqueeze(2).to_broadcast([128, 8, 16]), op=ALU.mult),
          [B_hu], [B_sel])
        pI, bI = ps[6], B_ps[6]
        for q3 in range(3):
            P(lambda e: e.transpose(pI[:, q3 * 128:(q3 + 1) * 128], sel[:, q3, :], ident[:, :]), [B_sel, B_ident], [bI])
        A(lambda e: e.activation(out=selT[:, :, :], in_=pI[:, 0:384].rearrange("p (a b) -> p a b", b=128), func=AF.Copy), [bI], [B_selT])
        for q3 in range(3):
            sc.dma("sp", L["selT_d"][q3, :, n * 128:(n + 1) * 128], selT[:, q3, :], reads=[B_selT])

    for tb in range(L["nblk"]):
        tsl = slice(tb * 512, (tb + 1) * 512)
        for kc in range(4):
            sc.dma("sp", rwoB[:, kc, :], L["rwo_d"][kc * 128:(kc + 1) * 128, tsl], writes=[B_in])
        for kc in range(8):
            sc.dma("sp", dsaB[:, kc, :], L["dsao_d"][kc * 128:(kc + 1) * 128, tsl], writes=[B_in])
        for dc in range(8):
            pA, bA = ps[dc % 2], B_ps[dc % 2]
            pB, bB = ps[2 + dc % 2], B_ps[2 + dc % 2]
            zg_, bz_ = zgr[dc % 2], B_zgr[dc % 2]
            sc.dma("sp", zg_[:, 0, :], L["zg_d"][dc * 128:(dc + 1) * 128, tsl], writes=[bz_])
            sc.dma("sp", zg_[:, 1, :], L["zg_d"][(8 + dc) * 128:(9 + dc) * 128, tsl], writes=[bz_])
            for kc in range(4):
                P(lambda e: e.matmul(pA[:, :], lhsT=wbra[:, kc, dc * 128:(dc + 1) * 128], rhs=rwoB[:, kc, :], start=(kc == 0), stop=(kc == 3)), [B_w, B_in], [bA])
            for kc in range(8):
                P(lambda e: e.matmul(pB[:, :], lhsT=wbrb[:, kc, dc * 128:(dc + 1) * 128], rhs=dsaB[:, kc, :], start=(kc == 0), stop=(kc == 7)), [B_w, B_in], [bB])
            V(lambda e: e.tensor_tensor(out=t1[:, :], in0=pA[:, :], in1=zg_[:, 0, :], op=ALU.mult), [bA, bz_], [B_t])
            V(lambda e: e.tensor_tensor(out=t2[:, :], in0=pB[:, :], in1=zg_[:, 1, :], op=ALU.mult), [bB, bz_], [B_t])
            V(lambda e: e.tensor_tensor(out=mg[:, dc, :], in0=t1[:, :], in1=t2[:, :], op=ALU.add), [B_t], [B_mg])
        for jt in range(4):
            n = tb * 4 + jt
            stageA(n, jt)
            if prevn[0] is not None:
                stageB(prevn[0])
            prevn[0] = n
    stageB(prevn[0])
    ar.release(mD)


def phase_C(L):
    nc, sc, ar, ps, B_ps = L["nc"], L["sc"], L["ar"], L["ps"], L["B_ps"]
    identb, B_ident = L["identb"], L["B_ident"]
    V = lambda fn, r=(), w=(): sc.op("dve", fn, r, w)
    A = lambda fn, r=(), w=(): sc.op("act", fn, r, w)
    P = lambda fn, r=(), w=(): sc.op("pe", fn, r, w)
    G = lambda fn, r=(), w=(): sc.op("pool", fn, r, w)
    NQB = L["nblk"] * 4
    mC = ar.mark()
    cvec = ar.alloc([128, 256], F32, "cvec")
    biasT = ar.alloc([128, 3, 1024], F32, "biasT")
    negm = ar.alloc([128, 128], F32, "negm")
    ckv_tok = ar.alloc([128, 32, 129], BF16, "ckv_tok")
    ckvT = ar.alloc([128, S], BF16, "ckvT")
    kiT2 = ar.alloc([128, S], BF16, "kiT2")
    wall = ar.alloc([128, 32, 4], F32, "wall")
    B_cc, B_kv, B_ki, B_wl = Buf("cc"), Buf("kv"), Buf("ki"), Buf("wl")
    sc.dma("sp", cvec[:, :], L["cvec_d"][:, :], writes=[B_cc])
    sc.dma("sp", biasT[:, :, :], L["biasT_d"][:, :, :], writes=[B_cc])
    sc.dma("sp", negm[:, :], L["negm_d"][:, :], writes=[B_cc])
    V(lambda e: e.memset(ckv_tok[:, :, 128:129], 1.0), [], [B_kv])
    for bi_ in range(2):
        V(lambda e: e.tensor_tensor(out=biasT[:, bi_, :], in0=biasT[:, bi_, :], in1=biasT[:, 2, :], op=ALU.subtract), [B_cc], [B_cc])
    zt = [ar.alloc([128, 196], F32, "ztC%d" % i) for i in range(2)]
    B_zt = [Buf("ztC0"), Buf("ztC1")]
    sq = ar.alloc([128, 128], F32, "sqC")
    c16 = ar.alloc([128, 128], BF16, "c16")
    k32 = ar.alloc([128, 64], F32, "k32")
    k16 = ar.alloc([128, 128], BF16, "k16")
    stc = ar.alloc([128, 8], F32, "stc")
    B_sq, B_c16, B_k, B_stc = Buf("sqC"), Buf("c16"), Buf("k"), Buf("stc")
    for n in range(NQB):
        z, bz = zt[n % 2], B_zt[n % 2]
        sc.dma("sp", z[:, :], L["ztok_d"][n * 128:(n + 1) * 128, :], writes=[bz])
        A(lambda e: e.activation(out=sq[:, :], in_=z[:, 0:128], func=AF.Square, accum_out=stc[:, 0:1]), [bz], [B_sq, B_stc])
        V(lambda e: e.tensor_scalar(out=stc[:, 1:2], in0=stc[:, 0:1], scalar1=1.0 / 128, scalar2=1e-5, op0=ALU.mult, op1=ALU.add), [B_stc], [B_stc])
        A(lambda e: e.activation(out=stc[:, 1:2], in_=stc[:, 1:2], func=AF.Sqrt), [B_stc], [B_stc])
        V(lambda e: e.reciprocal(out=stc[:, 1:2], in_=stc[:, 1:2]), [B_stc], [B_stc])
        V(lambda e: e.scalar_tensor_tensor(out=ckv_tok[:, n, 0:128], in0=z[:, 0:128], scalar=stc[:, 1:2], in1=cvec[:, 0:128], op0=ALU.mult, op1=ALU.mult),
          [bz, B_stc, B_cc], [B_kv])
        V(lambda e: e.tensor_reduce(out=stc[:, 2:3], in_=z[:, 128:192], axis=AX.X, op=ALU.add), [bz], [B_stc])
        A(lambda e: e.activation(out=sq[:, 0:64], in_=z[:, 128:192], func=AF.Square, accum_out=stc[:, 3:4]), [bz], [B_sq, B_stc])
        V(lambda e: e.tensor_scalar(out=stc[:, 4:5], in0=stc[:, 2:3], scalar1=1.0 / 64, scalar2=None, op0=ALU.mult), [B_stc], [B_stc])
        V(lambda e: e.tensor_tensor(out=stc[:, 5:6], in0=stc[:, 4:5], in1=stc[:, 4:5], op=ALU.mult), [B_stc], [B_stc])
        V(lambda e: e.scalar_tensor_tensor(out=stc[:, 6:7], in0=stc[:, 3:4], scalar=1.0 / 64, in1=stc[:, 5:6], op0=ALU.mult, op1=ALU.subtract), [B_stc], [B_stc])
        V(lambda e: e.tensor_scalar(out=stc[:, 6:7], in0=stc[:, 6:7], scalar1=1e-5, scalar2=None, op0=ALU.add), [B_stc], [B_stc])
        A(lambda e: e.activation(out=stc[:, 6:7], in_=stc[:, 6:7], func=AF.Sqrt), [B_stc], [B_stc])
        V(lambda e: e.reciprocal(out=stc[:, 6:7], in_=stc[:, 6:7]), [B_stc], [B_stc])
        V(lambda e: e.tensor_scalar(out=k32[:, :], in0=z[:, 128:192], scalar1=stc[:, 4:5], scalar2=stc[:, 6:7], op0=ALU.subtract, op1=ALU.mult), [bz, B_stc], [B_k])
        V(lambda e: e.tensor_tensor(out=k32[:, :], in0=k32[:, :], in1=cvec[:, 128:192], op=ALU.mult), [B_k, B_cc], [B_k])
        V(lambda e: e.tensor_tensor(out=k16[:, 0:64], in0=k32[:, :], in1=cvec[:, 192:256], op=ALU.add), [B_k, B_cc], [B_k])
        V(lambda e: e.tensor_copy(out=k16[:, 64:128], in_=k16[:, 0:64]), [B_k], [B_k])
        V(lambda e: e.tensor_scalar(out=wall[:, n, :], in0=z[:, 192:196], scalar1=0.0625, scalar2=None, op0=ALU.mult), [bz], [B_wl])
        pT = ps[n % 2][:, :].bitcast(BF16)
        bT = B_ps[n % 2]
        P(lambda e: e.transpose(pT[:, 0:128], ckv_tok[:, n, 0:128], identb[:, :]), [B_kv, B_ident], [bT])
        P(lambda e: e.transpose(pT[:, 128:256], k16[:, :], identb[:, :]), [B_k, B_ident], [bT])
        A(lambda e: e.activation(out=ckvT[:, n * 128:(n + 1) * 128], in_=pT[:, 0:128], func=AF.Copy, scale=128.0 ** -0.5), [bT], [B_kv])
        V(lambda e: e.tensor_copy(out=kiT2[:, n * 128:(n + 1) * 128], in_=pT[:, 128:256]), [bT], [B_ki])
    qiB = [ar.alloc([128, 2, 128], BF16, "qiB%d" % i) for i in range(2)]
    zqB = [ar.alloc([128, 8, 128], BF16, "zqB%d" % i) for i in range(2)]
    B_qi = [Buf("qi0"), Buf("qi1")]
    B_zq = [Buf("zq0"), Buf("zq1")]
    score2 = [ar.alloc([128, S], F32, "score%d" % i) for i in range(2)]
    maskb2 = [ar.alloc([128, S], BF16, "maskb%d" % i) for i in range(2)]
    maskT2 = [ar.alloc([128, 32, 128], BF16, "maskT%d" % i) for i in range(2)]
    bis2 = [ar.alloc([128, 8], F32, "bis%d" % i) for i in range(2)]
    junk = ar.alloc([128, S], BF16, "junkC")
    junkA = ar.alloc([128, S // 2], BF16, "junkA")
    rl = [ar.alloc([128, 512], F32, "rl%d" % i) for i in range(2)]
    B_rl = [Buf("rl0"), Buf("rl1")]
    lg = ar.alloc([128, 1024], F32, "lg")
    PT2 = [ar.alloc([128, 8, 128], BF16, "PT%d" % i) for i in range(2)]
    B_PT2 = [Buf("PT0"), Buf("PT1")]
    Oacc = ar.alloc([128, 8, 129], F32, "Oacc")
    rec = ar.alloc([128, 8], F32, "rec")
    Oo = ar.alloc([128, 8, 128], BF16, "Oo")
    dsT = ar.alloc([128, 8, 128], BF16, "dsT")
    B_sc2 = [Buf("score0"), Buf("score1")]
    B_mb2 = [Buf("maskb0"), Buf("maskb1")]
    B_mT2 = [Buf("maskT0"), Buf("maskT1")]
    B_bisA = [Buf("bisA0"), Buf("bisA1")]
    B_bisB = [Buf("bisB0"), Buf("bisB1")]
    B_bisC = [Buf("bisC0"), Buf("bisC1")]
    B_j, B_jA, B_lg, B_O, B_Oo, B_dsT = [Buf(n_) for n_ in ("junkC", "junkA", "lg", "Oacc", "Oo", "dsT")]

    def stage1(j):
        q = j % 2
        Lk = (j + 1) * 128
        qi, bqi, zq, bzq = qiB[q], B_qi[q], zqB[q], B_zq[q]
        score, B_sc = score2[q], B_sc2[q]
        qsl = slice(j * 128, (j + 1) * 128)
        for cch in range(2):
            sc.dma("sp", qi[:, cch, :], L["zqi_d"][cch * 128:(cch + 1) * 128, qsl], writes=[bqi])
        for h in range(8):
            sc.dma("sp", zq[:, h, :], L["zq_d"][h * 128:(h + 1) * 128, qsl], writes=[bzq])
        nkc = (Lk + 511) // 512
        ri = 0
        for kc in range(nkc):
            wd = min(512, Lk - kc * 512)
            ksl = slice(kc * 512, kc * 512 + wd)
            for hi in range(4):
                po = (hi % 2) * 64
                pD, bD = ps[hi], B_ps[hi]
                P(lambda e: e.matmul(pD[:, 0:wd], lhsT=qi[po:po + 64, hi // 2, :], rhs=kiT2[po:po + 64, ksl], start=True, stop=True), [bqi, B_ki], [bD])
                r_, br_ = rl[ri % 2], B_rl[ri % 2]
                ri += 1
                A(lambda e: e.activation(out=r_[:, 0:wd], in_=pD[:, 0:wd], func=AF.Relu), [bD], [br_])
                if hi == 0:
                    V(lambda e: e.tensor_scalar(out=score[:, ksl], in0=r_[:, 0:wd], scalar1=wall[:, j, 0:1], scalar2=None, op0=ALU.mult), [br_, B_wl], [B_sc])
                else:
                    V(lambda e: e.scalar_tensor_tensor(out=score[:, ksl], in0=r_[:, 0:wd], scalar=wall[:, j, hi:hi + 1], in1=score[:, ksl], op0=ALU.mult, op1=ALU.add),
                      [br_, B_wl, B_sc], [B_sc])
        V(lambda e: e.tensor_tensor(out=score[:, qsl], in0=score[:, qsl], in1=negm[:, :], op=ALU.add), [B_sc, B_cc], [B_sc])

    def bisect_iters(j):
        q = j % 2
        Lk = (j + 1) * 128
        score, B_sc, bis = score2[q], B_sc2[q], bis2[q]
        bA, bB, bC = B_bisA[q], B_bisB[q], B_bisC[q]
        if Lk > 256:
            na = (Lk // 2) // 128 * 128
            V(lambda e: e.memset(bis[:, 6:7], 0.0), [bA], [bA])
            step = 32.0
            for it in range(21):
                V(lambda e: e.tensor_scalar(out=bis[:, 7:8], in0=bis[:, 6:7], scalar1=-1.0, scalar2=None, op0=ALU.mult), [bA], [bB])
                A(lambda e: e.activation(out=junkA[:, 0:na], in_=score[:, 0:na], func=AF.Sign, bias=bis[:, 7:8], accum_out=bis[:, 2:3]), [B_sc, bB], [B_jA, bC])
                V(lambda e: e.tensor_scalar(out=junk[:, na:Lk], in0=score[:, na:Lk], scalar1=bis[:, 6:7], scalar2=None, op0=ALU.is_ge, op1=ALU.add, accum_out=bis[:, 3:4]),
                  [B_sc, bA], [B_j, bA])
                V(lambda e: e.scalar_tensor_tensor(out=bis[:, 4:5], in0=bis[:, 2:3], scalar=0.5, in1=bis[:, 3:4], op0=ALU.mult, op1=ALU.add), [bA, bC], [bA])
                V(lambda e: e.tensor_scalar(out=bis[:, 5:6], in0=bis[:, 4:5], scalar1=255.5 - na / 2.0, scalar2=2.0 * step, op0=ALU.is_ge, op1=ALU.mult), [bA], [bA])
                V(lambda e: e.scalar_tensor_tensor(out=bis[:, 6:7], in0=bis[:, 5:6], scalar=-step, in1=bis[:, 6:7], op0=ALU.add, op1=ALU.add), [bA, bB], [bA])
                step *= 0.5
                yield
            V(lambda e: e.tensor_scalar(out=bis[:, 6:7], in0=bis[:, 6:7], scalar1=-4.0 * step, scalar2=None, op0=ALU.add), [bA], [bA])
        else:
            V(lambda e: e.memset(bis[:, 6:7], -1e29), [bA], [bA])

    def stage3(j):
        q = j % 2
        Lk = (j + 1) * 128
        score, B_sc, bis, maskb, B_mb, maskT, B_mT = score2[q], B_sc2[q], bis2[q], maskb2[q], B_mb2[q], maskT2[q], B_mT2[q]
        V(lambda e: e.tensor_scalar(out=maskb[:, 0:Lk], in0=score[:, 0:Lk], scalar1=bis[:, 6:7], scalar2=None, op0=ALU.is_ge), [B_sc, B_bisA[q]], [B_mb])
        for k8 in range((j + 8) // 8):
            nn = min(8, j + 1 - k8 * 8)
            pM = ps[4][:, :].bitcast(BF16)
            for kk_ in range(nn):
                kt = k8 * 8 + kk_
                P(lambda e: e.transpose(pM[:, kk_ * 128:(kk_ + 1) * 128], maskb[:, kt * 128:(kt + 1) * 128], identb[:, :]), [B_mb, B_ident], [B_ps[4]])
            A(lambda e: e.activation(out=maskT[:, k8 * 8:k8 * 8 + nn, :], in_=pM[:, 0:nn * 128].rearrange("p (a b) -> p a b", b=128), func=AF.Copy), [B_ps[4]], [B_mT])

    def stage4(j, side):
        q = j % 2
        zq, bzq, maskT, B_mT = zqB[q], B_zq[q], maskT2[q], B_mT2[q]
        qsl = slice(j * 128, (j + 1) * 128)
        for kt in range(j + 1):
            near = kt >= j - 1
            bsel = 0 if kt == j else 1
            PTk, bPT = PT2[kt % 2], B_PT2[kt % 2]
            for half in range(2):
                pL, bL = ps[(kt % 2) * 2 + half], B_ps[(kt % 2) * 2 + half]
                P(lambda e: e.matmul(pL[:, :], lhsT=ckvT[:, kt * 128:(kt + 1) * 128], rhs=zq[:, half * 4:(half + 1) * 4, :], start=True, stop=True), [B_kv, bzq], [bL])
                if near:
                    V(lambda e: e.tensor_tensor(out=lg[:, half * 512:(half + 1) * 512], in0=pL[:, :], in1=biasT[:, bsel, half * 512:(half + 1) * 512], op=ALU.add),
                      [bL, B_cc], [B_lg])
                    A(lambda e: e.activation(out=PTk[:, half * 4:(half + 1) * 4, :], in_=lg[:, half * 512:(half + 1) * 512].rearrange("p (h q) -> p h q", q=128), func=AF.Exp),
                      [B_lg], [bPT])
                else:
                    A(lambda e: e.activation(out=PTk[:, half * 4:(half + 1) * 4, :], in_=pL[:, :].rearrange("p (h q) -> p h q", q=128), func=AF.Exp), [bL], [bPT])
            V(lambda e: e.tensor_tensor(out=PTk[:, :, :], in0=PTk[:, :, :], in1=maskT[:, kt, :].unsqueeze(1).to_broadcast([128, 8, 128]), op=ALU.mult), [bPT, B_mT], [bPT])
            for h in range(8):
                pO, bO = ps[5 + h // 3], B_ps[5 + h // 3]
                P(lambda e: e.matmul(pO[:, (h % 3) * 129:(h % 3 + 1) * 129], lhsT=PTk[:, h, :], rhs=ckv_tok[:, kt, :], start=(kt == 0 and h % 3 == 0), stop=(kt == j),
                                     skip_group_check=True), [bPT, B_kv], [bO])
            if side is not None:
                next(side, None)
        for b3 in range(3):
            nh = 3 if b3 < 2 else 2
            V(lambda e: e.tensor_copy(out=Oacc[:, b3 * 3:b3 * 3 + nh, :], in_=ps[5 + b3][:, 0:nh * 129].rearrange("p (h d) -> p h d", d=129)), [B_ps[5 + b3]], [B_O])
        V(lambda e: e.reciprocal(out=rec[:, :], in_=Oacc[:, :, 128]), [B_O], [B_Oo])
        V(lambda e: e.tensor_tensor(out=Oo[:, :, :], in0=Oacc[:, :, 0:128], in1=rec[:, :].unsqueeze(2).to_broadcast([128, 8, 128]), op=ALU.mult), [B_O, B_Oo], [B_Oo])
        pX = ps[4][:, :].bitcast(BF16)
        for h in range(8):
            P(lambda e: e.transpose(pX[:, h * 128:(h + 1) * 128], Oo[:, h, :], identb[:, :]), [B_Oo, B_ident], [B_ps[4]])
        V(lambda e: e.tensor_copy(out=dsT[:, :, :], in_=pX[:, :].rearrange("p (a b) -> p a b", b=128)), [B_ps[4]], [B_dsT])
        for h in range(8):
            sc.dma("sp", L["dsao_d"][h * 128:(h + 1) * 128, qsl], dsT[:, h, :], reads=[B_dsT])

    stage1(0)
    for _ in bisect_iters(0):
        pass
    stage3(0)
    for j in range(NQB):
        side = None
        if j + 1 < NQB:
            stage1(j + 1)
            side = bisect_iters(j + 1)
        stage4(j, side)
        if side is not None:
            for _ in side:
                pass
            stage3(j + 1)
    ar.release(mC)


def phase_E(L):
    nc, sc, ar, ps, B_ps = L["nc"], L["sc"], L["ar"], L["ps"], L["B_ps"]
    identb, B_ident = L["identb"], L["B_ident"]
    gt_bc, B_gt = L["gt_bc"], L["B_gt"]
    iota128, B_iota = L["iota128"], L["B_iota"]
    dbg = L["dbg"]
    V = lambda fn, r=(), w=(): sc.op("dve", fn, r, w)
    A = lambda fn, r=(), w=(): sc.op("act", fn, r, w)
    P = lambda fn, r=(), w=(): sc.op("pe", fn, r, w)
    G = lambda fn, r=(), w=(): sc.op("pool", fn, r, w)
    ALPHA = 2.0 ** 0.25
    if not L["e0_done_flag"][0]:
        m0 = ar.mark()
        for _ in make_e0(L, 3, 0):
            pass
        ar.release(m0)
    ar.release(L["mark_stg"])
    mE = ar.mark()
    TP = 256
    lnbc = ar.alloc([128, 2, D], F32, "lnbcE")
    B_ln = Buf("lnE")
    sc.dma("sp", lnbc[:, :, :], L["lnbc_d"][:, 2:4, :], writes=[B_ln])
    Gs2 = [ar.alloc([128, 128, TP], BF16, "Gs%d" % i) for i in range(2)]
    h2Tp2 = [ar.alloc([128, 8, TP], BF16, "h2Tp%d" % i) for i in range(2)]
    IT1 = ar.alloc([128, 3, TP], F32, "IT1")
    IT2 = [IT1, IT1]
    ITb2 = [ar.alloc([128, 3, TP], BF16, "ITb%d" % i) for i in range(2)]
    iotab = ar.alloc([128, 128], BF16, "iotab")
    NBT = 8
    eqb = [ar.alloc([128, NBT, 128], BF16, "eqb%d" % i) for i in range(1)]
    Lb = [ar.alloc([128, NBT, 128], BF16, "Lb%d" % i) for i in range(2)]
    Rb = [ar.alloc([128, NBT, 128], BF16, "Rb%d" % i) for i in range(2)]
    B_Gs2 = [Buf("Gs0"), Buf("Gs1")]
    B_h2Tp2 = [Buf("h2Tp0"), Buf("h2Tp1")]
    B_IT2 = [Buf("IT0"), Buf("IT1")]
    B_ITf1 = Buf("ITf")
    B_ITf2 = [B_ITf1, B_ITf1]
    B_eq = [Buf("eqb0")]
    B_eqh = [Buf("eqh0"), Buf("eqh1")]
    V(lambda e: e.tensor_copy(out=iotab[:, :], in_=iota128[:, :]), [B_iota], [B_iota])
    B_Lb = [Buf("Lb0"), Buf("Lb1")]
    B_Rb = [Buf("Rb0"), Buf("Rb1")]
    NS = 4
    uTc = [ar.alloc([128, 1024], BF16, "uTc%d" % i) for i in range(NS)]
    vcb = [ar.alloc([128, 1024], BF16, "vcb%d" % i) for i in range(NS)]
    B_uTc = [Buf("uTc%d" % i) for i in range(NS)]
    B_vcb = [Buf("vcb%d" % i) for i in range(NS)]
    gl = [ar.alloc([128, TP], BF16, "gl%d" % i) for i in range(3)]
    AT = [ar.alloc([128, TP], BF16, "AT%d" % i) for i in range(3)]
    B_gl = [Buf("gl0"), Buf("gl1"), Buf("gl2")]
    B_AT = [Buf("AT0"), Buf("AT1"), Buf("AT2")]
    x1t = ar.alloc([128, D], F32, "x1t")
    oo = ar.alloc([128, D], F32, "ooE")
    jk = oo
    st = ar.alloc([128, 8], F32, "stE")
    B_x1t, B_oo, B_st = Buf("x1t"), Buf("ooE"), Buf("stE")
    B_jk = B_oo
    out_v = L["out_d"].rearrange("(n p) m -> p n m", p=128)
    iota3 = iotab[:, :].unsqueeze(1).to_broadcast([128, NBT, 128])
    npass = L["nblk"] * 2
    NBATCH = TP // NBT

    def emit_loads(p_):
        q = p_ % 2
        tsl = slice(p_ * TP, (p_ + 1) * TP)
        for kc in range(8):
            sc.dma("sp", h2Tp2[q][:, kc, :], L["h2T_d"][kc * 128:(kc + 1) * 128, tsl], writes=[B_h2Tp2[q]])
        for q3 in range(3):
            sc.dma("sp", IT2[q][:, q3, :], L["selT_d"][q3, :, tsl], writes=[B_ITf2[q]])
        A(lambda e: e.activation(out=ITb2[q][:, :, :], in_=IT2[q][:, :, :], func=AF.Copy), [B_ITf2[q]], [B_IT2[q]])

    gcount = [0]

    def gbatch_gen(p_, b):
        q = p_ % 2
        Gs, B_Gs, ITb, B_IT = Gs2[q], B_Gs2[q], ITb2[q], B_IT2[q]
        gi = gcount[0]
        gcount[0] += 1
        Lk, bLk = Lb[gi % 2], B_Lb[gi % 2]
        Rk, bRk = Rb[gi % 2], B_Rb[gi % 2]
        eq_ = eqb[0]
        H = NBT // 2
        for hf in range(2):
            hs_ = slice(hf * H, (hf + 1) * H)
            bs_ = slice(b * NBT + hf * H, b * NBT + (hf + 1) * H)
            io_ = iotab[:, :].unsqueeze(1).to_broadcast([128, H, 128])
            V(lambda e: e.tensor_tensor(out=eq_[:, hs_, :], in0=io_, in1=ITb[:, 0, bs_].unsqueeze(2).to_broadcast([128, H, 128]), op=ALU.is_equal), [B_IT, B_iota], [B_eqh[hf]])
            yield
            V(lambda e: e.tensor_tensor(out=Lk[:, hs_, :], in0=eq_[:, hs_, :], in1=ITb[:, 2, bs_].unsqueeze(2).to_broadcast([128, H, 128]), op=ALU.mult), [B_eqh[hf], B_IT], [bLk])
            yield
            V(lambda e: e.tensor_tensor(out=Rk[:, hs_, :], in0=io_, in1=ITb[:, 1, bs_].unsqueeze(2).to_broadcast([128, H, 128]), op=ALU.is_equal), [B_IT, B_iota], [bRk])
            yield
        yield
        yield
        for t4 in range(NBT // 4):
            pg, bpg = ps[7], B_ps[7]
            for tt in range(4):
                t = t4 * 4 + tt
                P(lambda e: e.matmul(pg[:, tt * 128:(tt + 1) * 128], lhsT=Lk[:, t, :], rhs=Rk[:, t, :], start=True, stop=True), [bLk, bRk], [bpg])
            t0 = b * NBT + t4 * 4
            src = pg[:, :].rearrange("p (t i) -> p i t", i=128)
            A(lambda e: e.activation(out=Gs[:, :, t0:t0 + 4], in_=src, func=AF.Copy), [bpg], [B_Gs])
            yield

    def gall_gen(p_):
        for b in range(NBATCH):
            for _ in gbatch_gen(p_, b):
                yield

    emit_loads(0)
    for _ in gall_gen(0):
        pass
    for p_ in range(npass):
        q = p_ % 2
        Gs, B_Gs, h2Tp, B_h2Tp = Gs2[q], B_Gs2[q], h2Tp2[q], B_h2Tp2[q]
        if p_ + 1 < npass:
            emit_loads(p_ + 1)

        def load_u(c):
            sc.dma("sp", uTc[c % NS][:, :], L["uv_d"][c, :, 0:1024], writes=[B_uTc[c % NS]])

        def emit_hu(c):
            k = c % NS
            if c == 0:
                load_u(0)
                load_u(1)
            if c + 2 < 128:
                load_u(c + 2)
            sc.dma("sp", vcb[k][:, :], L["uv_d"][c, :, 1024:2048], writes=[B_vcb[k]])
            pH, bH = ps[4 + c % 3], B_ps[4 + c % 3]
            for kc in range(8):
                P(lambda e: e.matmul(pH[:, 0:TP], lhsT=uTc[k][:, kc * 128:(kc + 1) * 128], rhs=h2Tp[:, kc, :], start=(kc == 0), stop=(kc == 7)), [B_uTc[k], B_h2Tp], [bH])

        def emit_y2(c):
            k = c % NS
            pH, bH = ps[4 + c % 3], B_ps[4 + c % 3]
            g_, bg_ = gl[c % 3], B_gl[c % 3]
            a_, ba_ = AT[c % 3], B_AT[c % 3]
            A(lambda e: e.activation(out=g_[:, :], in_=pH[:, 0:TP], func=AF.Gelu), [bH], [bg_])
            V(lambda e: e.tensor_tensor(out=a_[:, :], in0=g_[:, :], in1=Gs[:, c, :], op=ALU.mult), [bg_, B_Gs], [ba_])
            for tt in range(2):
                for half in range(2):
                    py, bpy = ps[tt * 2 + half], B_ps[tt * 2 + half]
                    P(lambda e: e.matmul(py[:, :], lhsT=a_[:, tt * 128:(tt + 1) * 128], rhs=vcb[k][:, half * 512:(half + 1) * 512], start=(c == 0), stop=(c == 127)),
                      [ba_, B_vcb[k]], [bpy])
        emit_hu(0)
        emit_hu(1)
        gg = gall_gen(p_ + 1) if p_ + 1 < npass else None
        steps_per_chunk = (NBATCH * 10 + 127) // 128
        for c in range(128):
            if c + 2 < 128:
                emit_hu(c + 2)
            emit_y2(c)
            if gg is not None:
                for _ in range(steps_per_chunk):
                    next(gg, None)
        if gg is not None:
            for _ in gg:
                pass
        for tt in range(2):
            n = p_ * 2 + tt
            sc.dma("sp", x1t[:, :], L["x1s_d"][n * 128:(n + 1) * 128, :], writes=[B_x1t])
            for half in range(2):
                hsl = slice(half * 512, (half + 1) * 512)
                py, bpy = ps[tt * 2 + half], B_ps[tt * 2 + half]
                if "y2dbg" in dbg:
                    V(lambda e: e.tensor_copy(out=oo[:, hsl], in_=py[:, :]), [bpy], [B_oo])
                    sc.dma("sp", L["y2_d"][n * 128:(n + 1) * 128, hsl], oo[:, hsl], reads=[B_oo])
                V(lambda e: e.tensor_tensor(out=oo[:, hsl], in0=py[:, :], in1=gt_bc[:, 3, hsl], op=ALU.mult), [bpy, B_gt], [B_oo])
            V(lambda e: e.scalar_tensor_tensor(out=x1t[:, :], in0=x1t[:, :], scalar=ALPHA, in1=oo[:, :], op0=ALU.mult, op1=ALU.add), [B_x1t, B_oo], [B_x1t])
            V(lambda e: e.tensor_reduce(out=st[:, 0:1], in_=x1t[:, :], axis=AX.X, op=ALU.add), [B_x1t], [B_st])
            A(lambda e: e.activation(out=oo[:, :], in_=x1t[:, :], func=AF.Square, accum_out=st[:, 1:2]), [B_x1t], [B_st, B_oo])
            V(lambda e: e.tensor_scalar(out=st[:, 2:3], in0=st[:, 0:1], scalar1=1.0 / D, scalar2=None, op0=ALU.mult), [B_st], [B_st])
            V(lambda e: e.tensor_tensor(out=st[:, 3:4], in0=st[:, 2:3], in1=st[:, 2:3], op=ALU.mult), [B_st], [B_st])
            V(lambda e: e.scalar_tensor_tensor(out=st[:, 4:5], in0=st[:, 1:2], scalar=1.0 / D, in1=st[:, 3:4], op0=ALU.mult, op1=ALU.subtract), [B_st], [B_st])
            V(lambda e: e.tensor_scalar(out=st[:, 4:5], in0=st[:, 4:5], scalar1=1e-5, scalar2=None, op0=ALU.add), [B_st], [B_st])
            A(lambda e: e.activation(out=st[:, 5:6], in_=st[:, 4:5], func=AF.Sqrt), [B_st], [B_st])
            V(lambda e: e.reciprocal(out=st[:, 5:6], in_=st[:, 5:6]), [B_st], [B_st])
            V(lambda e: e.tensor_scalar(out=oo[:, :], in0=x1t[:, :], scalar1=st[:, 2:3], scalar2=st[:, 5:6], op0=ALU.subtract, op1=ALU.mult), [B_x1t, B_st], [B_oo])
            V(lambda e: e.tensor_tensor(out=oo[:, :], in0=oo[:, :], in1=lnbc[:, 0, :], op=ALU.mult), [B_oo, B_ln], [B_oo])
            V(lambda e: e.tensor_tensor(out=oo[:, :], in0=oo[:, :], in1=lnbc[:, 1, :], op=ALU.add), [B_oo, B_ln], [B_oo])
            sc.dma("sp", out_v[:, n, :], oo[:, :], reads=[B_oo])
    ar.release(mE)


def make_e0(L, NB, bank):
    sc, ar, ps, B_ps = L["sc"], L["ar"], L["ps"], L["B_ps"]
    identb, B_ident = L["identb"], L["B_ident"]
    A = lambda fn, r=(), w=(): sc.op("act", fn, r, w)
    P = lambda fn, r=(), w=(): sc.op("pe", fn, r, w)
    G = lambda fn, r=(), w=(): sc.op("pool", fn, r, w)
    NF = 3
    stf = [ar.alloc([128, D], F32, "e0f%d" % i) for i in range(NF)]
    o16 = [ar.alloc([128, D], BF16, "e0h%d" % i) for i in range(NF)]
    uT = [ar.alloc([128, D], BF16, "e0t%d" % i) for i in range(2)]
    B_f = [Buf("e0f%d" % i) for i in range(NF)]
    B_o = [Buf("e0h%d" % i) for i in range(NF)]
    B_t = [Buf("e0t0"), Buf("e0t1")]
    pu_v = L["pu_d"].rearrange("(i1 i2) d -> i2 i1 d", i2=128)
    pv_v = L["pv_d"].rearrange("(i1 i2) d -> i2 i1 d", i2=128)
    NI = 256

    def load(i):
        c, isv = i // 2, i % 2
        sc.dma("sp", stf[i % NF][:, :], (pv_v if isv else pu_v)[c, :, :], writes=[B_f[i % NF]])

    def cast(i):
        G(lambda e: e.tensor_copy(out=o16[i % NF][:, :], in_=stf[i % NF][:, :]), [B_f[i % NF]], [B_o[i % NF]])

    def finish(i):
        c, isv = i // 2, i % 2
        if isv:
            sc.dma("sp", L["uv_d"][c, :, 1024:2048], o16[i % NF][:, :], reads=[B_o[i % NF]])
        else:
            pT = ps[bank][:, :].bitcast(BF16)
            for kc in range(8):
                P(lambda e: e.transpose(pT[:, kc * 128:(kc + 1) * 128], o16[i % NF][:, kc * 128:(kc + 1) * 128], identb[:, :]), [B_o[i % NF], B_ident], [B_ps[bank]])
            A(lambda e: e.activation(out=uT[c % 2][:, :], in_=pT[:, :], func=AF.Copy), [B_ps[bank]], [B_t[c % 2]])
            sc.dma("sp", L["uv_d"][c, :, 0:1024], uT[c % 2][:, :], reads=[B_t[c % 2]])
    load(0)
    load(1)
    cast(0)
    for k in range(NI):
        if k + 2 < NI:
            load(k + 2)
        if k + 1 < NI:
            cast(k + 1)
        finish(k)
        yield
```

```python
import numpy as np
import ml_dtypes
import concourse.bass as bass
import concourse.mybir as mybir
from concourse.bass_utils import run_bass_kernel_spmd

F32 = mybir.dt.float32
BF16 = mybir.dt.bfloat16
I32 = mybir.dt.int32
U32 = mybir.dt.uint32
AF = mybir.ActivationFunctionType
ALU = mybir.AluOpType
AX = mybir.AxisListType

S = 4096
D = 1024
NT = S // 128
IN_COLS = 5316
DBG = {}
STOP_AFTER = None


class Buf:
    __slots__ = ("name", "lw", "rd")

    def __init__(self, name):
        self.name = name
        self.lw = None
        self.rd = []


class _Rec:
    def __init__(self):
        self.call = None

    def __getattr__(self, name):
        def f(*args, **kwargs):
            self.call = (name, args, kwargs)
            return self
        return f


class Sched:
    ENGS = ("pe", "act", "dve", "pool", "sp")

    def __init__(self, nc, n_dma_sems=40):
        self.nc = nc
        self.ops = {e: [] for e in self.ENGS}
        self.sem = {e: nc.alloc_semaphore("c_" + e) for e in self.ENGS}
        self.cnt = {e: 0 for e in self.ENGS}
        self.seen = {e: {} for e in self.ENGS}
        self.dsem = [nc.alloc_semaphore("d%d" % i) for i in range(n_dma_sems)]
        self.dval = [0] * n_dma_sems
        self.drr = 0
        self.drr_sw = 0
        self.NSW = 8
        self.NHW = n_dma_sems - 8
        self.all_events = []

    def _waits(self, eng, reads, writes):
        deps = []
        for b in reads:
            if b.lw is not None:
                deps.append(b.lw)
        for b in writes:
            if b.lw is not None:
                deps.append(b.lw)
            deps.extend(b.rd)
        out = {}
        for (sem, val, src) in deps:
            if src == "pe" and eng == "pe":
                continue
            k = sem.num
            if self.seen[eng].get(k, 0) >= val:
                continue
            if out.get(k, (None, 0))[1] < val:
                out[k] = (sem, val)
        for k, (sem, val) in out.items():
            self.seen[eng][k] = val
        return list(out.values())

    def op(self, eng, fn, reads=(), writes=()):
        waits = self._waits(eng, reads, writes)
        self.cnt[eng] += 1
        sem = self.sem[eng]
        val = self.cnt[eng]
        ev = (sem, val, eng)

        rec = _Rec()
        fn(rec)
        name, args, kwargs = rec.call

        def emit(e, waits=waits, sem=sem, name=name, args=args, kwargs=kwargs):
            for (s, v) in waits:
                e.wait_ge(s, v)
            getattr(e, name)(*args, **kwargs).then_inc(sem, 1)
        self.ops[eng].append(emit)
        for b in reads:
            b.rd.append(ev)
        for b in writes:
            b.lw = ev
            b.rd = []
        return ev

    def dma(self, eng, out, in_, reads=(), writes=(), fn=None, **kw):
        if fn is not None:
            rec = _Rec()
            fn(rec)
            mname, margs, mkw = rec.call
        else:
            mname, margs, mkw = "dma_start", (), dict(out=out, in_=in_, **kw)
        if eng == "pool":
            i = self.NHW + (self.drr_sw % self.NSW)
            self.drr_sw += 1
        else:
            i = self.drr
            self.drr = (self.drr + 1) % self.NHW
        sem = self.dsem[i]
        waits = self._waits(eng, reads, writes)
        prev = self.dval[i]
        if prev > 0 and self.seen[eng].get(sem.num, 0) < prev:
            waits.append((sem, prev))
            self.seen[eng][sem.num] = prev
        self.dval[i] += 16
        val = self.dval[i]
        ev = (sem, val, "dma")

        def emit(e, waits=waits, sem=sem, mname=mname, margs=margs, mkw=mkw):
            for (s, v) in waits:
                e.wait_ge(s, v)
            getattr(e, mname)(*margs, **mkw).then_inc(sem, 16)
        self.ops[eng].append(emit)
        for b in reads:
            b.rd.append(ev)
        for b in writes:
            b.lw = ev
            b.rd = []
        self.all_events.append(ev)
        return ev

    def raw(self, eng, fn):
        self.ops[eng].append(fn)

    def barrier(self):
        targets = []
        for en in self.ENGS:
            if self.cnt[en] > 0:
                targets.append((self.sem[en], self.cnt[en], en))
        for i, s in enumerate(self.dsem):
            if self.dval[i] > 0:
                targets.append((s, self.dval[i], "dma"))
        for eng in self.ENGS:
            waits = []
            for (s, v, src) in targets:
                if src == eng:
                    continue
                if self.seen[eng].get(s.num, 0) >= v:
                    continue
                self.seen[eng][s.num] = v
                waits.append((s, v))

            def emit(e, waits=waits):
                for (s, v) in waits:
                    e.wait_ge(s, v)
            self.ops[eng].append(emit)

    def finish(self, final_events):
        nc = self.nc
        with nc.Block() as block:
            def run(name):
                def f(e):
                    for emit in self.ops[name]:
                        emit(e)
                    if name == "sp":
                        for (s, v, _) in final_events:
                            e.wait_ge(s, v)
                        for i, s in enumerate(self.dsem):
                            if self.dval[i] > 0:
                                e.wait_ge(s, self.dval[i])
                        for en in ("pe", "act", "dve", "pool"):
                            if self.cnt[en] > 0:
                                e.wait_ge(self.sem[en], self.cnt[en])
                return f
            block.tensor(run("pe"))
            block.scalar(run("act"))
            block.vector(run("dve"))
            block.gpsimd(run("pool"))
            block.sync(run("sp"))


class Arena:
    def __init__(self, nc, base=0, top=192 * 1024):
        self.nc = nc
        self.off = base
        self.top = top
        self.n = 0
        self.sc = None

    def mark(self):
        return self.off

    def release(self, m):
        self.off = m
        if self.sc is not None:
            self.sc.barrier()

    def alloc(self, shape, dtype, name="t"):
        esz = {F32: 4, BF16: 2, I32: 4, U32: 4}[dtype]
        per = esz
        for s in shape[1:]:
            per *= s
        per = (per + 63) // 64 * 64
        self.n += 1
        t = self.nc.alloc_sbuf_tensor_at("%s_%d" % (name, self.n), list(shape), dtype, offset=self.off)
        self.off += per
        assert self.off <= self.top, ("SBUF overflow", name, self.off)
        return t


def build(dbg=None, stop_after=None, phases=None, feed=(), nblk=8):
    dbg = dbg or {}
    phases = phases or {'0', 'A', 'B', 'C', 'D', 'E', 'F'}
    nc = bass.Bass("TRN2", target_bir_lowering=False)
    sc = Sched(nc)
    ar = Arena(nc, base=(nc.sbuf_base + 63) // 64 * 64, top=nc.sbuf_top // 64 * 64)
    ar.sc = sc

    def din(name, shape, dt=F32):
        return nc.dram_tensor(name, list(shape), dt, kind="ExternalInput").ap()

    def dscratch(name, shape, dt=F32):
        kind = "ExternalOutput" if name in dbg else ("ExternalInput" if name in feed else "Internal")
        return nc.dram_tensor(name, list(shape), dt, kind=kind).ap()

    x_d = din("x", [S, D])
    c_d = din("c_col", [128, 8])
    wada_d = din("w_ada", [D, 6 * D])
    bada_col_d = din("b_ada_col", [128, 48])
    bada_bc_d = din("b_ada_bc", [128, 6 * D])
    win_d = din("w_in", [D, IN_COLS])
    ident_d = din("ident", [128, 128])
    out_d = nc.dram_tensor("out", [S, D], F32, kind="ExternalOutput").ap()

    mu_d = din("mu_col", [128, 14])
    rwvec_d = din("rwvec", [128, 20])
    w2a2_d = din("w2a2", [128, 512])
    g2_d = din("g2", [128, 512])
    gnbc_d = din("gn_bc", [128, 2, 256])
    cst_d = din("cst", [128, 1024])
    wbra_d = din("w_br_a", [512, D])
    wbrb_d = din("w_br_b", [D, D])
    wout_d = din("w_out", [D, D])
    lnbc_d = din("ln_bc", [128, 4, D])
    wq_d = din("peer_wq", [D, D])
    pkeys_d = din("peer_keysT", [128, 8, 128])
    pu_d = din("peer_u", [16384, D])
    pv_d = din("peer_v", [16384, D])
    cvec_d = din("cvec", [128, 256])
    biasT_d = din("biasT", [128, 3, 1024])
    negm_d = din("negm", [128, 128])
    zrw_d = dscratch("zrw", [1792, S], F32)
    zq_d = dscratch("zq", [1024, S], BF16)
    zqi_d = dscratch("zqi", [256, S], BF16)
    zg_d = dscratch("zg", [2048, S], BF16)
    ztok_d = dscratch("ztok", [S, 196], F32)
    rwo_d = dscratch("rwo", [512, S], BF16)
    dsao_d = dscratch("dsao", [1024, S], BF16)
    x1_d = dscratch("x1dbg", [S, D], F32)
    y2_d = dscratch("y2dbg", [S, D], F32)
    x1s_d = dscratch("x1s", [S, D], F32)
    h2T_d = dscratch("h2T", [D, S], BF16)
    selT_d = dscratch("selT", [3, 128, S], F32)
    uv_d = dscratch("uv16", [128, 128, 2048], BF16)
    iota_d = din("iota128", [128, 128])
    iota128 = ar.alloc([128, 128], F32, "iota128")
    B_iota = Buf("iota")
    iota16 = iota128[:, 0:16]

    ident = ar.alloc([128, 128], F32, "ident")
    identb = ar.alloc([128, 128], BF16, "identb")
    modcol = ar.alloc([128, 48], F32, "modcol")
    onep1 = ar.alloc([128, 8], F32, "onep1")
    onep2 = ar.alloc([128, 8], F32, "onep2")
    gt_bc = ar.alloc([128, 4, D], F32, "gt_bc")
    B_ident = Buf("ident")
    B_mod = Buf("mod")
    B_gt = Buf("gt")

    ps = [nc.alloc_psum_tensor("ps%d" % i, [128, 512], F32) for i in range(8)]
    B_ps = [Buf("ps%d" % i) for i in range(8)]

    sc.dma("sp", ident[:, :], ident_d[:, :], writes=[B_ident])
    sc.dma("sp", iota128[:, :], iota_d[:, :], writes=[B_iota])

    mark_stg = ar.mark()
    STG = 1024
    stg = [ar.alloc([128, STG], F32, "stg%d" % i) for i in range(3)]
    B_stg = [Buf("stg%d" % i) for i in range(3)]
    stg_i = [0]

    def load_bf16(dst, src, n, bdst, eng="pool"):
        P = dst.shape[0]
        for o in range(0, n, STG):
            w = min(STG, n - o)
            k = stg_i[0] % 3
            stg_i[0] += 1
            sc.dma("sp", stg[k][0:P, 0:w], src[:, o:o + w], writes=[B_stg[k]])
            sc.op(eng, lambda e, k=k, o=o, w=w, P=P, dst=dst: e.tensor_copy(out=dst[:, o:o + w], in_=stg[k][0:P, 0:w]),
                  reads=[B_stg[k]], writes=[bdst])
    sc.op("dve", lambda e: e.tensor_copy(out=identb[:, :], in_=ident[:, :]), reads=[B_ident], writes=[B_ident])

    if '0' in phases:
        m0 = ar.mark()
        c_sb = ar.alloc([128, 8], F32, "c_sb")
        sil = ar.alloc([128, 8], F32, "sil")
        silbc = ar.alloc([128, 8, 128], F32, "silbc")
        bcol = ar.alloc([128, 48], F32, "bcol")
        bbc = ar.alloc([128, 4, D], F32, "bbc")
        wa = [ar.alloc([128, 8, 1024], F32, "wa%d" % i) for i in range(4)]
        B_c = Buf("c")
        B_wa = [Buf("wa0"), Buf("wa1"), Buf("wa2"), Buf("wa3")]
        B_b = Buf("bcol")
        sc.dma("sp", c_sb[:, :], c_d[:, :], writes=[B_c])
        sc.dma("sp", bcol[:, :], bada_col_d[:, :], writes=[B_b])
        for gi_, g_ in enumerate((2, 3, 4, 5)):
            sc.dma("sp", bbc[:, gi_, :], bada_bc_d[:, g_ * D:(g_ + 1) * D], writes=[B_b])
        sc.op("act", lambda e: e.activation(out=sil[:, :], in_=c_sb[:, :], func=AF.Silu), reads=[B_c], writes=[B_c])
        for kc in range(8):
            sc.op("dve", lambda e, kc=kc: e.tensor_copy(out=silbc[:, kc, :], in_=sil[:, kc:kc + 1].to_broadcast([128, 128])),
                  reads=[B_c], writes=[B_c])
        wada_v = wada_d.rearrange("(kc p) n -> p kc n", p=128)
        for g in range(6):
            w = wa[g % 4]
            bw = B_wa[g % 4]
            for kc in range(8):
                sc.dma("sp", w[:, kc, :], wada_v[:, kc, g * 1024:(g + 1) * 1024], writes=[bw])
            if g in (2, 3, 4, 5):
                gi = g - 2
                for half in range(2):
                    p = ps[half]
                    for kc in range(8):
                        sc.op("pe", lambda e, p=p, w=w, kc=kc, half=half: e.matmul(
                            p[:, :], lhsT=silbc[:, kc, :], rhs=w[:, kc, half * 512:(half + 1) * 512],
                            start=(kc == 0), stop=(kc == 7)), reads=[bw, B_c], writes=[B_ps[half]])
                    sc.op("dve", lambda e, p=p, gi=gi, half=half: e.tensor_tensor(
                        out=gt_bc[:, gi, half * 512:(half + 1) * 512], in0=p[:, :],
                        in1=bbc[:, gi, half * 512:(half + 1) * 512], op=ALU.add),
                        reads=[B_ps[half], B_b], writes=[B_gt])
            if g in (0, 1, 3, 4):
                p = ps[2]
                for fc in range(8):
                    for kc in range(8):
                        sc.op("pe", lambda e, p=p, w=w, kc=kc, fc=fc: e.matmul(
                            p[:, fc:fc + 1], lhsT=w[:, kc, fc * 128:(fc + 1) * 128], rhs=sil[:, kc:kc + 1],
                            start=(kc == 0), stop=(kc == 7)), reads=[bw, B_c], writes=[B_ps[2]])
                sc.op("dve", lambda e, p=p, g=g: e.tensor_tensor(
                    out=modcol[:, g * 8:(g + 1) * 8], in0=p[:, 0:8], in1=bcol[:, g * 8:(g + 1) * 8], op=ALU.add),
                    reads=[B_ps[2], B_b], writes=[B_mod])
        sc.op("dve", lambda e: e.tensor_scalar(out=onep1[:, :], in0=modcol[:, 8:16], scalar1=1.0, scalar2=None, op0=ALU.add),
              reads=[B_mod], writes=[B_mod])
        sc.op("dve", lambda e: e.tensor_scalar(out=onep2[:, :], in0=modcol[:, 32:40], scalar1=1.0, scalar2=None, op0=ALU.add),
              reads=[B_mod], writes=[B_mod])
        sc.op("dve", lambda e: e.tensor_scalar(out=gt_bc[:, 2, :], in0=gt_bc[:, 2, :], scalar1=1.0, scalar2=None, op0=ALU.add),
              reads=[B_gt], writes=[B_gt])
        ar.release(m0)
        if "modcol" in dbg:
            dd = nc.dram_tensor("modcol_o", [128, 48], F32, kind="ExternalOutput").ap()
            sc.dma("sp", dd[:, :], modcol[:, :], reads=[B_mod])
            dd2 = nc.dram_tensor("gt_o", [128, 4 * D], F32, kind="ExternalOutput").ap()
            sc.dma("sp", dd2[:, :], gt_bc[:, :, :].rearrange("p a b -> p (a b)"), reads=[B_gt])

    if 'A' in phases:
        mA = ar.mark()
        fm_chunks = []
        for i in range(14):
            fm_chunks.append((i * 128, "rw", i))
        for i in range(8):
            fm_chunks.append((1792 + i * 128, "q", i))
        for i in range(2):
            fm_chunks.append((2944 + i * 128, "qi", i))
        for i in range(16):
            fm_chunks.append((3268 + i * 128, "g", i))
        winb = ar.alloc([128, 8, IN_COLS], BF16, "winb")
        B_win = Buf("win")
        win_v = win_d.rearrange("(kc p) n -> p kc n", p=128)
        for kc in range(8):
            load_bf16(winb[:, kc, :], win_v[:, kc, :], IN_COLS, B_win)
        xt = [ar.alloc([128, 4, D], F32, "xt%d" % i) for i in range(2)]
        B_xt = [Buf("xt0"), Buf("xt1")]
        hT = [ar.alloc([128, 8, 512], BF16, "hT%d" % i) for i in range(2)]
        B_hT = [Buf("hT0"), Buf("hT1")]
        NEV = 8
        ev32 = [ar.alloc([128, 512], F32, "ev32_%d" % i) for i in range(NEV)]
        ev16 = [ar.alloc([128, 512], BF16, "ev16_%d" % i) for i in range(NEV)]
        B_ev32 = [Buf("ev32_%d" % i) for i in range(NEV)]
        B_ev16 = [Buf("ev16_%d" % i) for i in range(NEV)]
        ztk = [ar.alloc([128, 196], F32, "ztk%d" % i) for i in range(2)]
        B_ztk = [Buf("ztk0"), Buf("ztk1")]
        x_v = x_d.rearrange("(n p) m -> p n m", p=128)
        pi = 0
        evi = 0
        tok_cols = [(2816, 128, 0), (3200, 68, 128)]
        for tb in range(8):
            xb = xt[tb % 2]
            bx = B_xt[tb % 2]
            hb = hT[tb % 2]
            bh = B_hT[tb % 2]
            for j in range(4):
                sc.dma("sp", xb[:, j, :], x_v[:, tb * 4 + j, :], writes=[bx])
            for kc in range(8):
                p = ps[pi % 8]
                bp = B_ps[pi % 8]
                pi += 1
                for j in range(4):
                    sc.op("pe", lambda e, p=p, xb=xb, j=j, kc=kc: e.transpose(
                        p[:, j * 128:(j + 1) * 128], xb[:, j, kc * 128:(kc + 1) * 128], ident[:, :]),
                        reads=[bx, B_ident], writes=[bp])
                sc.op("act", lambda e, p=p, hb=hb, kc=kc: e.activation(
                    out=hb[:, kc, :], in_=p[:, :], func=AF.Identity,
                    scale=onep1[:, kc:kc + 1], bias=modcol[:, kc:kc + 1]),
                    reads=[bp, B_mod], writes=[bh])
            for ci, (col0, kind, idx) in enumerate(fm_chunks):
                p = ps[pi % 8]
                bp = B_ps[pi % 8]
                pi += 1
                for kc in range(8):
                    sc.op("pe", lambda e, p=p, kc=kc, col0=col0, hb=hb: e.matmul(
                        p[:, :], lhsT=winb[:, kc, col0:col0 + 128], rhs=hb[:, kc, :],
                        start=(kc == 0), stop=(kc == 7)), reads=[B_win, bh], writes=[bp])
                k = evi % NEV
                evi += 1
                eng = "dve" if (ci % 2 == 0) else "act"
                tsl = slice(tb * 512, (tb + 1) * 512)
                if kind == "rw":
                    dst = ev32[k]
                    bd = B_ev32[k]
                    if eng == "dve":
                        sc.op("dve", lambda e, p=p, dst=dst: e.tensor_copy(out=dst[:, :], in_=p[:, :]), reads=[bp], writes=[bd])
                    else:
                        sc.op("act", lambda e, p=p, dst=dst: e.activation(out=dst[:, :], in_=p[:, :], func=AF.Copy), reads=[bp], writes=[bd])
                    sc.dma("sp", zrw_d[idx * 128:(idx + 1) * 128, tsl], dst[:, :], reads=[bd])
                elif kind in ("q", "qi"):
                    dst = ev16[k]
                    bd = B_ev16[k]
                    if eng == "dve":
                        sc.op("dve", lambda e, p=p, dst=dst: e.tensor_copy(out=dst[:, :], in_=p[:, :]), reads=[bp], writes=[bd])
                    else:
                        sc.op("act", lambda e, p=p, dst=dst: e.activation(out=dst[:, :], in_=p[:, :], func=AF.Copy), reads=[bp], writes=[bd])
                    dd = zq_d if kind == "q" else zqi_d
                    sc.dma("sp", dd[idx * 128:(idx + 1) * 128, tsl], dst[:, :], reads=[bd])
                else:
                    dst = ev16[k]
                    bd = B_ev16[k]
                    sc.op("act", lambda e, p=p, dst=dst: e.activation(out=dst[:, :], in_=p[:, :], func=AF.Sigmoid), reads=[bp], writes=[bd])
                    sc.dma("sp", zg_d[idx * 128:(idx + 1) * 128, tsl], dst[:, :], reads=[bd])
            for j in range(4):
                p = ps[pi % 8]
                bp = B_ps[pi % 8]
                pi += 1
                for (c0, ncol, o0) in tok_cols:
                    for kc in range(8):
                        sc.op("pe", lambda e, p=p, kc=kc, j=j, c0=c0, ncol=ncol, o0=o0, hb=hb: e.matmul(
                            p[:, o0:o0 + ncol], lhsT=hb[:, kc, j * 128:(j + 1) * 128], rhs=winb[:, kc, c0:c0 + ncol],
                            start=(kc == 0), stop=(kc == 7)), reads=[B_win, bh], writes=[bp])
                zt = ztk[j % 2]
                bz = B_ztk[j % 2]
                sc.op("dve", lambda e, p=p, zt=zt: e.tensor_copy(out=zt[:, :], in_=p[:, 0:196]), reads=[bp], writes=[bz])
                r0 = (tb * 4 + j) * 128
                sc.dma("sp", ztok_d[r0:r0 + 128, :], zt[:, :], reads=[bz])
        ar.release(mA)

    e0_done_flag = [False]
    if 'B' in phases:
        phase_B(locals())
    if 'C' in phases:
        phase_C(locals())
    if 'D' in phases:
        phase_D(locals())
    if 'E' in phases:
        phase_E(locals())

    sc.finish([])
    return nc


def _prep_inputs(inputs):
    f = lambda a: np.ascontiguousarray(np.asarray(a, dtype=np.float32))
    x = f(inputs["x"])
    c = f(inputs["c"])
    b_ada = f(inputs["b_ada"])[0]
    shared = {
        "w_ada": f(inputs["w_ada"])[0],
        "b_ada_col": np.ascontiguousarray(b_ada.reshape(48, 128).T),
        "b_ada_bc": np.ascontiguousarray(np.broadcast_to(b_ada[None, :], (128, 6 * D))),
        "w_in": f(inputs["w_in"])[0],
        "ident": np.eye(128, dtype=np.float32),
        "iota128": np.ascontiguousarray(np.broadcast_to(np.arange(128, dtype=np.float32)[None], (128, 128))),
    }
    col = lambda v, n: np.ascontiguousarray(f(v).reshape(n, 128).T)
    shared["mu_col"] = col(inputs["rw_mu"][0], 14)
    shared["rwvec"] = np.ascontiguousarray(np.concatenate([col(inputs["rw_w0"][0], 4), col(inputs["rw_a0"][0], 4), col(inputs["rw_k_k"][0], 4),
                                                            col(inputs["rw_k_a"][0], 4), col(f(inputs["rw_r_k"])[0].reshape(-1), 4)], axis=1))
    shared["w2a2"] = np.ascontiguousarray(np.concatenate([f(inputs["rw_w2"])[0], f(inputs["rw_a2"])[0]], axis=0))
    shared["g2"] = f(inputs["rw_g2"])[0]
    gn2 = np.stack([f(inputs["rw_gn_g"])[0], f(inputs["rw_gn_b"])[0]]).reshape(2, 4, 2, 64)
    gnl = np.zeros((128, 2, 4, 64), np.float32)
    gnl[0:64] = gn2[:, :, 0, :][None]
    gnl[64:128] = gn2[:, :, 1, :][None]
    shared["gn_bc"] = np.ascontiguousarray(gnl.reshape(128, 2, 256))
    cst = np.zeros((128, 1024), np.float32)
    iu = np.triu(np.ones((64, 64), np.float32), 1)
    il = np.triu(np.ones((64, 64), np.float32), 0)
    cst[:, 0:128] = np.block([[iu, il], [iu, il]])
    cst[0:64, 128:192] = iu.T
    cst[64:128, 128:192] = iu.T
    cst[0:64, 192:256] = 1.0
    cst[64:128, 256:320] = 1.0
    sm = np.ones(512, np.float32); sm[::64] = 0.0
    cst[:, 320:832] = sm[None, :]
    cst[0:64, 832:896] = np.eye(64, dtype=np.float32)
    cst[64:128, 832:896] = np.eye(64, dtype=np.float32)
    cst[:, 896] = 1.0
    shared["cst"] = cst
    shared["w_br_a"] = f(inputs["w_br_a"])[0]
    shared["w_br_b"] = f(inputs["w_br_b"])[0]
    shared["w_out"] = f(inputs["w_out"])[0]
    lnr = np.stack([f(inputs["ln1_g"])[0], f(inputs["ln1_b"])[0], f(inputs["ln2_g"])[0], f(inputs["ln2_b"])[0]])
    shared["ln_bc"] = np.ascontiguousarray(np.broadcast_to(lnr[None], (128, 4, D)))
    shared["peer_wq"] = f(inputs["peer_wq"])[0]
    pk = f(inputs["peer_keys"])[0]
    shared["peer_keysT"] = np.ascontiguousarray(pk.transpose(1, 3, 0, 2).reshape(128, 8, 128))
    shared["peer_u"] = f(inputs["peer_u"])[0]
    cv = np.concatenate([f(inputs["dsa_kv_g"])[0], f(inputs["idx_k_g"])[0], f(inputs["idx_k_b"])[0]])
    shared["cvec"] = np.ascontiguousarray(np.broadcast_to(cv[None], (128, 256)))
    rb = f(inputs["rel_bias"])
    nn_ = np.arange(0, 256)
    nf = np.maximum(nn_, 1).astype(np.float32)
    large = 16 + (np.log(nf / np.float32(16)) / np.float32(np.log(8.0)) * np.float32(16)).astype(np.int32)
    bucket = np.where(nn_ < 16, nn_, np.minimum(large, 31))
    sI = np.arange(128)[:, None]
    qI = np.arange(128)[None, :]
    bd = bucket[np.clip(qI - sI, 0, 255)]
    bp = bucket[np.clip(qI + 128 - sI, 0, 255)]
    bT = np.zeros((128, 3, 8, 128), np.float32)
    bT[:, 0] = rb[bd].transpose(0, 2, 1)
    bT[:, 1] = rb[bp].transpose(0, 2, 1)
    bT[:, 2] = rb[31][None, :, None]
    shared["biasT"] = np.ascontiguousarray(bT.reshape(128, 3, 1024))
    shared["negm"] = np.where(np.arange(128)[None, :] <= np.arange(128)[:, None], 0.0, -1e30).astype(np.float32)
    shared["peer_v"] = f(inputs["peer_v"])[0]
    maps = []
    for b in range(8):
        m = dict(shared)
        m["x"] = x[b]
        m["c_col"] = np.ascontiguousarray(c[b].reshape(8, 128).T)
        maps.append(m)
    return maps


def kernel(**inputs):
    nc = build()
    maps = _prep_inputs(inputs)
    res = run_bass_kernel_spmd(nc, maps, core_ids=list(range(8)))
    out = np.stack([np.asarray(r["out"], dtype=np.float32) for r in res.results], axis=0)
    return out


def _bc_mid(ap, n):
    sh = list(ap.shape)
    return ap.unsqueeze(1).to_broadcast([sh[0], n] + sh[1:])


def phase_B(L):
    nc, sc, ar, ps, B_ps = L["nc"], L["sc"], L["ar"], L["ps"], L["B_ps"]
    identb, B_ident, load_bf16 = L["identb"], L["B_ident"], L["load_bf16"]
    zrw_d, rwo_d = L["zrw_d"], L["rwo_d"]
    V = lambda fn, r=(), w=(): sc.op("dve", fn, r, w)
    A = lambda fn, r=(), w=(): sc.op("act", fn, r, w)
    P = lambda fn, r=(), w=(): sc.op("pe", fn, r, w)
    mB = ar.mark()
    cst = ar.alloc([128, 1024], F32, "cst")
    mu = ar.alloc([128, 14], F32, "mu")
    rwvec = ar.alloc([128, 20], F32, "rwvec")
    omka = ar.alloc([128, 4], F32, "omka")
    w2a2b = ar.alloc([128, 512], BF16, "w2a2b")
    g2b = ar.alloc([128, 512], BF16, "g2b")
    gnbc = ar.alloc([128, 2, 256], F32, "gnbc")
    bones = ar.alloc([128, 128], BF16, "bones")
    onesb = ar.alloc([128, 1], BF16, "onesb")
    B_c = Buf("cstB")
    sc.dma("sp", cst[:, :], L["cst_d"][:, :], writes=[B_c])
    sc.dma("sp", mu[:, :], L["mu_d"][:, :], writes=[B_c])
    sc.dma("sp", rwvec[:, :], L["rwvec_d"][:, :], writes=[B_c])
    sc.dma("sp", gnbc[:, :, :], L["gnbc_d"][:, :, :], writes=[B_c])
    load_bf16(w2a2b[:, :], L["w2a2_d"][:, :], 512, B_c)
    load_bf16(g2b[:, :], L["g2_d"][:, :], 512, B_c)
    V(lambda e: e.tensor_scalar(out=omka[:, :], in0=rwvec[:, 12:16], scalar1=-1.0, scalar2=1.0, op0=ALU.mult, op1=ALU.add), [B_c], [B_c])
    V(lambda e: e.tensor_copy(out=bones[:, :], in_=cst[:, 192:320]), [B_c], [B_c])
    V(lambda e: e.tensor_copy(out=onesb[:, :], in_=cst[:, 896:897]), [B_c], [B_c])
    maskA = cst[:, 0:128]
    maskT = cst[:, 128:192]
    scanmask = cst[:, 320:832]
    eye64 = cst[:, 832:896]
    w0c, a0c, kkc, kac, rkc = (rwvec[:, 0:4], rwvec[:, 4:8], rwvec[:, 8:12], rwvec[:, 12:16], rwvec[:, 16:20])

    zb = ar.alloc([128, 14, 513], F32, "zb")
    zs = ar.alloc([128, 14, 512], F32, "zs")
    tmp = [ar.alloc([128, 512], F32, "tmpB%d" % i) for i in range(3)]
    B_tmp = [Buf("tmpB%d" % i) for i in range(3)]
    th = ar.alloc([128, 512], BF16, "th")
    al16 = ar.alloc([128, 512], BF16, "al16")
    sgl = ar.alloc([128, 512], BF16, "sgl")
    sq16 = ar.alloc([128, 512], BF16, "sq16")
    asg = ar.alloc([128, 512], F32, "asg")
    kk = ar.alloc([128, 512], F32, "kk")
    kp = ar.alloc([128, 512], F32, "kp")
    bv = ar.alloc([128, 512], F32, "bv")
    lw = ar.alloc([128, 512], F32, "lw")
    cs = ar.alloc([128, 512], F32, "cs")
    E = [ar.alloc([128, 512], F32, "E%d" % i) for i in range(4)]
    E5 = ar.alloc([128, 4, 8], F32, "E5")
    AR = ar.alloc([128, 4, 8, 2, 64], BF16, "AR")
    BK = ar.alloc([128, 4, 8, 2, 64], BF16, "BK")
    KH = ar.alloc([128, 4, 512], BF16, "KH")
    BH = ar.alloc([128, 4, 512], BF16, "BH")
    Vb = ar.alloc([128, 4, 512], BF16, "Vb")
    rkr = ar.alloc([128, 4, 512], BF16, "rkr")
    rwoT = ar.alloc([128, 4, 512], BF16, "rwoT")
    B_zb, B_zs, B_pre, B_blk, B_rwoT = Buf("zb"), Buf("zs"), Buf("pre"), Buf("blk"), Buf("rwoT")
    Vt2p = [ar.alloc([128, 512], BF16, "Vt2_%d" % i) for i in range(2)]
    KHt2p = [ar.alloc([128, 512], BF16, "KHt2_%d" % i) for i in range(2)]
    BHt2p = [ar.alloc([128, 512], BF16, "BHt2_%d" % i) for i in range(2)]
    sABp = [ar.alloc([128, 4, 128], BF16, "sAB%d" % i) for i in range(2)]
    sAKp = [ar.alloc([128, 4, 128], BF16, "sAK%d" % i) for i in range(2)]
    Xfp = [ar.alloc([128, 4, 64], BF16, "Xf%d" % i) for i in range(2)]
    B_Vtp = [Buf("Vt0"), Buf("Vt1")]
    B_sAp = [Buf("sA0"), Buf("sA1")]
    B_Xfp = [Buf("Xf0"), Buf("Xf1")]
    Mx = [ar.alloc([128, 4, 64], BF16, "Mx%d" % i) for i in range(2)]
    MT = [ar.alloc([128, 4, 64], BF16, "MT%d" % i) for i in range(2)]
    X = [ar.alloc([128, 4, 64], BF16, "X%d" % i) for i in range(2)]
    RHSs = ar.alloc([128, 4, 64], BF16, "RHSs")
    SAs = ar.alloc([128, 4, 64], BF16, "SAs")
    ST = ar.alloc([128, 4, 64], BF16, "ST")
    STf = ar.alloc([128, 4, 64], F32, "STf")
    sqy = ar.alloc([128, 256], F32, "sqy")
    yn = ar.alloc([128, 256], F32, "yn")
    bon = ar.alloc([128, 256], F32, "bon")
    O16 = ar.alloc([128, 256], BF16, "O16")
    st8 = ar.alloc([128, 4, 8], F32, "st8")
    B_Vt, B_sA, B_M, B_MT, B_X, B_R, B_SA, B_ST, B_ep, B_st8, B_O = (Buf("Vt"), Buf("sA"), [Buf("M0"), Buf("M1")], [Buf("MT0"), Buf("MT1")],
                                                                    [Buf("X0"), Buf("X1")], Buf("R"), Buf("SA"), Buf("ST"), Buf("ep"), Buf("st8"), Buf("O"))
    e0 = make_e0(L, 2, 7) if ('E' in L["phases"] or 'E0' in L["phases"]) else iter(())
    V(lambda e: e.memset(STf[:, :, :], 0.0), [], [B_ST])
    V(lambda e: e.memset(ST[:, :, :], 0.0), [], [B_ST])
    V(lambda e: e.memset(zb[:, :, 0:1], 0.0), [], [B_zb])

    def v3(ap2):
        return ap2.rearrange("p (c t) -> p c t", t=64)

    for tb in range(L['nblk']):
        for i in range(14):
            if tb == 0:
                sc.dma("sp", zb[:, i, 1:513], zrw_d[i * 128:(i + 1) * 128, 0:512], writes=[B_zb])
            else:
                sc.dma("sp", zb[:, i, 0:513], zrw_d[i * 128:(i + 1) * 128, tb * 512 - 1:tb * 512 + 512], writes=[B_zb])
        for i in range(14):
            t0 = tmp[i % 2]
            bt0 = B_tmp[i % 2]
            V(lambda e, i=i, t0=t0: e.tensor_tensor(out=t0[:, :], in0=zb[:, i, 0:512], in1=zb[:, i, 1:513], op=ALU.subtract), [B_zb], [bt0])
            V(lambda e, i=i, t0=t0: e.scalar_tensor_tensor(out=zs[:, i, :], in0=t0[:, :], scalar=mu[:, i:i + 1], in1=zb[:, i, 1:513],
                                                           op0=ALU.mult, op1=ALU.add), [bt0, B_zb, B_c], [B_zs])
        A(lambda e: e.activation(out=th[:, :], in_=zs[:, 12, :], func=AF.Tanh), [B_zs], [B_pre])
        V(lambda e: e.tensor_copy(out=al16[:, :], in_=zs[:, 12, :]), [B_zs], [B_pre])
        A(lambda e: e.activation(out=sgl[:, :], in_=zs[:, 13, :], func=AF.Sigmoid), [B_zs], [B_blk])
        for j in range(4):
            pW, bW = ps[0], B_ps[0]
            pA, bA = ps[1], B_ps[1]
            pQ, bQ = ps[2], B_ps[2]
            P(lambda e, j=j: e.matmul(pW[:, :], lhsT=w2a2b[0:64, j * 128:(j + 1) * 128], rhs=th[0:64, :], start=True, stop=True), [B_c, B_pre], [bW])
            P(lambda e, j=j: e.matmul(pA[:, :], lhsT=w2a2b[64:128, j * 128:(j + 1) * 128], rhs=al16[64:128, :], start=True, stop=True), [B_c, B_pre], [bA])
            A(lambda e, j=j: e.activation(out=lw[:, :], in_=pW[:, :], func=AF.Sigmoid, bias=w0c[:, j:j + 1]), [bW, B_c], [B_pre])
            A(lambda e, j=j: e.activation(out=asg[:, :], in_=pA[:, :], func=AF.Sigmoid, bias=a0c[:, j:j + 1]), [bA, B_c], [B_pre])
            A(lambda e, j=j: e.activation(out=sq16[:, :], in_=zs[:, 4 + j, :], func=AF.Square, scale=kkc[:, j:j + 1]), [B_zs, B_c], [B_pre])
            P(lambda e: e.matmul(pQ[:, :], lhsT=bones[:, :], rhs=sq16[:, :], start=True, stop=True), [B_c, B_pre], [bQ])
            V(lambda e: e.tensor_scalar(out=tmp[2][:, :], in0=pQ[:, :], scalar1=1e-24, scalar2=None, op0=ALU.max), [bQ], [B_tmp[2]])
            A(lambda e: e.activation(out=tmp[2][:, :], in_=tmp[2][:, :], func=AF.Sqrt), [B_tmp[2]], [B_tmp[2]])
            V(lambda e: e.reciprocal(out=tmp[2][:, :], in_=tmp[2][:, :]), [B_tmp[2]], [B_tmp[2]])
            V(lambda e, j=j: e.scalar_tensor_tensor(out=kk[:, :], in0=zs[:, 4 + j, :], scalar=kkc[:, j:j + 1], in1=tmp[2][:, :],
                                                    op0=ALU.mult, op1=ALU.mult), [B_zs, B_tmp[2], B_c], [B_pre])
            V(lambda e, j=j: e.tensor_scalar(out=tmp[0][:, :], in0=asg[:, :], scalar1=kac[:, j:j + 1], scalar2=omka[:, j:j + 1],
                                             op0=ALU.mult, op1=ALU.add), [B_pre, B_c], [B_tmp[0]])
            V(lambda e, j=j: e.tensor_tensor(out=kp[:, :], in0=zs[:, 4 + j, :], in1=tmp[0][:, :], op=ALU.mult), [B_zs, B_tmp[0]], [B_pre])
            V(lambda e: e.tensor_tensor(out=bv[:, :], in0=kk[:, :], in1=asg[:, :], op=ALU.mult), [B_pre], [B_pre])
            V(lambda e: e.tensor_scalar(out=lw[:, :], in0=lw[:, :], scalar1=-0.6065306597126334, scalar2=None, op0=ALU.mult), [B_pre], [B_pre])
            V(lambda e: e.tensor_tensor_scan(out=cs[:, :], data0=scanmask, data1=lw[:, :], initial=0.0, op0=ALU.mult, op1=ALU.add), [B_pre, B_c], [B_pre])
            V(lambda e: e.tensor_tensor(out=tmp[0][:, :], in0=cs[:, :], in1=lw[:, :], op=ALU.subtract), [B_pre], [B_tmp[0]])
            V(lambda e: e.tensor_tensor(out=v3(tmp[1][:, :]), in0=v3(cs[:, :])[:, :, 63:64].to_broadcast([128, 8, 64]), in1=v3(cs[:, :]),
                                        op=ALU.subtract), [B_pre], [B_tmp[1]])
            A(lambda e: e.activation(out=E[0][:, :], in_=cs[:, :], func=AF.Exp), [B_pre], [B_pre])
            A(lambda e: e.activation(out=E[1][:, :], in_=cs[:, :], func=AF.Exp, scale=-1.0), [B_pre], [B_pre])
            A(lambda e: e.activation(out=E[2][:, :], in_=tmp[0][:, :], func=AF.Exp), [B_tmp[0]], [B_pre])
            A(lambda e: e.activation(out=E[3][:, :], in_=tmp[1][:, :], func=AF.Exp), [B_tmp[1]], [B_pre])
            V(lambda e, j=j: e.tensor_copy(out=E5[:, j, :], in_=v3(E[0][:, :])[:, :, 63]), [B_pre], [B_blk])
            V(lambda e, j=j: e.tensor_tensor(out=AR[:, j, :, 1, :], in0=v3(zs[:, j, :]), in1=v3(E[0][:, :]), op=ALU.mult), [B_zs, B_pre], [B_blk])
            V(lambda e, j=j: e.tensor_tensor(out=BK[:, j, :, 1, :], in0=v3(kp[:, :]), in1=v3(E[1][:, :]), op=ALU.mult), [B_pre], [B_blk])
            V(lambda e, j=j: e.tensor_tensor(out=BK[:, j, :, 0, :], in0=v3(bv[:, :]), in1=v3(E[1][:, :]), op=ALU.mult), [B_pre], [B_blk])
            V(lambda e, j=j: e.scalar_tensor_tensor(out=AR[:, j, :, 0, :], in0=v3(kk[:, :]), scalar=-1.0, in1=v3(E[2][:, :]),
                                                    op0=ALU.mult, op1=ALU.mult), [B_pre], [B_blk])
            V(lambda e, j=j: e.tensor_tensor(out=KH[:, j, :], in0=kp[:, :], in1=E[3][:, :], op=ALU.mult), [B_pre], [B_blk])
            V(lambda e, j=j: e.tensor_tensor(out=BH[:, j, :], in0=bv[:, :], in1=E[3][:, :], op=ALU.mult), [B_pre], [B_blk])
            A(lambda e, j=j: e.activation(out=Vb[:, j, :], in_=zs[:, 8 + j, :], func=AF.Copy), [B_zs], [B_blk])
            V(lambda e, j=j: e.scalar_tensor_tensor(out=rkr[:, j, :], in0=zs[:, j, :], scalar=rkc[:, j:j + 1], in1=kp[:, :],
                                                    op0=ALU.mult, op1=ALU.mult), [B_zs, B_pre, B_c], [B_blk])

        def v4(ap2):
            return ap2.rearrange("p (j t) -> p j t", t=64)
        hl = [(h // 2, slice((h % 2) * 64, (h % 2) * 64 + 64), slice((h // 2) * 64, (h // 2) * 64 + 64), slice(h * 64, (h + 1) * 64)) for h in range(8)]

        def pre(c):
            q = c % 2
            csl = slice(c * 64, (c + 1) * 64)
            Vt2, KHt2, BHt2, sAB, sAK = Vt2p[q], KHt2p[q], BHt2p[q], sABp[q], sAKp[q]
            B_Vt, B_sA = B_Vtp[q], B_sAp[q]
            pT = ps[3][:, :].bitcast(BF16)
            pT2 = ps[4][:, :].bitcast(BF16)
            for half in range(2):
                hp = slice(half * 64, half * 64 + 64)
                for j in range(4):
                    P(lambda e: e.transpose(pT[hp, j * 128:(j + 1) * 128], Vb[:, j, csl], identb[:, :]), [B_blk, B_ident], [B_ps[3]])
                    P(lambda e: e.transpose(pT[hp, 512 + j * 128:512 + (j + 1) * 128], KH[:, j, csl], identb[:, :]), [B_blk, B_ident], [B_ps[3]])
                    P(lambda e: e.transpose(pT2[hp, j * 128:(j + 1) * 128], BH[:, j, csl], identb[:, :]), [B_blk, B_ident], [B_ps[4]])
            yield
            V(lambda e: e.tensor_copy(out=Vt2[:, :], in_=pT[:, 0:512]), [B_ps[3]], [B_Vt])
            A(lambda e: e.activation(out=KHt2[:, :], in_=pT[:, 512:1024], func=AF.Copy), [B_ps[3]], [B_Vt])
            A(lambda e: e.activation(out=BHt2[:, :], in_=pT2[:, 0:512], func=AF.Copy), [B_ps[4]], [B_Vt])
            for (j, pp, js, hs) in hl:
                P(lambda e: e.matmul(ps[0][pp, j * 128:(j + 1) * 128], lhsT=BK[pp, j, c, 0, :], rhs=AR[pp, j, c, :, :], start=True, stop=True), [B_blk], [B_ps[0]])
                P(lambda e: e.matmul(ps[1][pp, j * 128:(j + 1) * 128], lhsT=BK[pp, j, c, 1, :], rhs=AR[pp, j, c, :, :], start=True, stop=True), [B_blk], [B_ps[1]])
                P(lambda e: e.matmul(ps[2][pp, j * 64:(j + 1) * 64], lhsT=AR[pp, j, c, 0, :], rhs=BK[pp, j, c, 0, :], start=True, stop=True), [B_blk], [B_ps[2]])
            yield
            V(lambda e: e.tensor_tensor(out=sAB[:, :, :], in0=ps[0][:, :].rearrange("p (j t) -> p j t", t=128), in1=_bc_mid(maskA, 4), op=ALU.mult), [B_ps[0], B_c], [B_sA])
            V(lambda e: e.tensor_tensor(out=sAK[:, :, :], in0=ps[1][:, :].rearrange("p (j t) -> p j t", t=128), in1=_bc_mid(maskA, 4), op=ALU.mult), [B_ps[1], B_c], [B_sA])
            V(lambda e: e.tensor_tensor(out=MT[0][:, :, :], in0=v4(ps[2][:, 0:256]), in1=_bc_mid(maskT, 4), op=ALU.mult), [B_ps[2], B_c], [B_MT[0]])
            V(lambda e: e.tensor_copy(out=Mx[0][:, :, :], in_=sAB[:, :, 0:64]), [B_sA], [B_M[0]])
            V(lambda e: e.tensor_tensor(out=X[0][:, :, :], in0=sAB[:, :, 0:64], in1=_bc_mid(eye64, 4), op=ALU.add), [B_sA, B_c], [B_X[0]])
            yield
            cur = 0
            pa, pb, pc = ps[2], ps[3], ps[4]
            for rd in range(5):
                nxt = 1 - cur
                for (j, pp, js, hs) in hl:
                    P(lambda e: e.matmul(pa[pp, js], lhsT=MT[cur][pp, j, :], rhs=Mx[cur][pp, j, :], start=True, stop=True), [B_MT[cur], B_M[cur]], [B_ps[2]])
                    P(lambda e: e.matmul(pb[pp, js], lhsT=Mx[cur][pp, j, :], rhs=MT[cur][pp, j, :], start=True, stop=True), [B_MT[cur], B_M[cur]], [B_ps[3]])
                yield
                V(lambda e: e.tensor_copy(out=Mx[nxt][:, :, :], in_=v4(pa[:, 0:256])), [B_ps[2]], [B_M[nxt]])
                A(lambda e: e.activation(out=MT[nxt][:, :, :], in_=v4(pb[:, 0:256]), func=AF.Copy), [B_ps[3]], [B_MT[nxt]])
                for (j, pp, js, hs) in hl:
                    P(lambda e: e.matmul(pc[pp, js], lhsT=MT[nxt][pp, j, :], rhs=X[cur][pp, j, :], start=True, stop=True), [B_MT[nxt], B_X[cur]], [B_ps[4]])
                yield
                if rd < 4:
                    V(lambda e: e.tensor_tensor(out=X[nxt][:, :, :], in0=X[cur][:, :, :], in1=v4(pc[:, 0:256]), op=ALU.add), [B_X[cur], B_ps[4]], [B_X[nxt]])
                else:
                    V(lambda e: e.tensor_tensor(out=Xfp[q][:, :, :], in0=X[cur][:, :, :], in1=v4(pc[:, 0:256]), op=ALU.add), [B_X[cur], B_ps[4]], [B_Xfp[q]])
                cur = nxt
                yield

        def post(c):
            q = c % 2
            csl = slice(c * 64, (c + 1) * 64)
            Vt2, KHt2, BHt2, sAB, sAK, Xf = Vt2p[q], KHt2p[q], BHt2p[q], sABp[q], sAKp[q], Xfp[q]
            B_Vt, B_sA, bXf = B_Vtp[q], B_sAp[q], B_Xfp[q]
            pR, bR = ps[5], B_ps[5]
            pY, bY = ps[6], B_ps[6]
            pU, bU = ps[7], B_ps[7]
            for (j, pp, js, hs) in hl:
                P(lambda e: e.matmul(pR[pp, js], lhsT=AR[pp, j, c, 0, :], rhs=ST[pp, j, :], start=True, stop=False), [B_blk, B_ST], [bR])
                P(lambda e: e.matmul(pR[pp, js], lhsT=sAK[pp, j, 0:64], rhs=Vt2[pp, hs], start=False, stop=True), [B_sA, B_Vt], [bR])
            yield
            V(lambda e: e.tensor_copy(out=RHSs[:, :, :], in_=v4(pR[:, 0:256])), [bR], [B_R])
            for (j, pp, js, hs) in hl:
                P(lambda e: e.matmul(pR[pp, js], lhsT=Xf[pp, j, :], rhs=RHSs[pp, j, :], start=True, stop=True), [bXf, B_R], [bR])
            yield
            V(lambda e: e.tensor_copy(out=SAs[:, :, :], in_=v4(pR[:, 0:256])), [bR], [B_SA])
            for (j, pp, js, hs) in hl:
                P(lambda e: e.matmul(pY[pp, js], lhsT=AR[pp, j, c, 1, :], rhs=ST[pp, j, :], start=True, stop=False), [B_blk, B_ST], [bY])
                P(lambda e: e.matmul(pY[pp, js], lhsT=sAK[pp, j, 64:128], rhs=Vt2[pp, hs], start=False, stop=False), [B_sA, B_Vt], [bY])
                P(lambda e: e.matmul(pY[pp, js], lhsT=sAB[pp, j, 64:128], rhs=SAs[pp, j, :], start=False, stop=True), [B_sA, B_SA], [bY])
            for (j, pp, js, hs) in hl:
                P(lambda e: e.matmul(pU[pp, js], lhsT=KHt2[pp, hs], rhs=Vt2[pp, hs], start=True, stop=False), [B_Vt], [bU])
                P(lambda e: e.matmul(pU[pp, js], lhsT=BHt2[pp, hs], rhs=SAs[pp, j, :], start=False, stop=True), [B_Vt, B_SA], [bU])
            yield
            V(lambda e: e.tensor_tensor(out=STf[:, :, :], in0=STf[:, :, :], in1=E5[:, :, c:c + 1].to_broadcast([128, 4, 64]), op=ALU.mult), [B_blk, B_ST, bY, bR], [B_ST])
            V(lambda e: e.tensor_tensor(out=STf[:, :, :], in0=STf[:, :, :], in1=v4(pU[:, 0:256]), op=ALU.add), [bU, B_ST], [B_ST])
            V(lambda e: e.tensor_copy(out=ST[:, :, :], in_=STf[:, :, :]), [B_ST], [B_ST])
            pG, bG = ps[5], B_ps[5]
            for (j, pp, js, hs) in hl:
                P(lambda e: e.matmul(pG[pp, 256 + j:256 + j + 1], lhsT=rkr[pp, j, csl], rhs=onesb[pp, 0:1], start=True, stop=True), [B_blk, B_c, B_SA], [bG])
                P(lambda e: e.matmul(pG[pp, js], lhsT=sgl[:, csl], rhs=g2b[:, hs], start=True, stop=True), [B_blk, B_c, B_SA, B_R], [bG])
            y3 = v4(pY[:, 0:256])
            V(lambda e: e.tensor_reduce(out=st8[:, :, 0], in_=y3, axis=AX.X, op=ALU.add), [bY], [B_st8])
            A(lambda e: e.activation(out=sqy[:, :], in_=pY[:, 0:256], func=AF.Square), [bY], [B_ep])
            yield
            V(lambda e: e.tensor_reduce(out=st8[:, :, 1], in_=v4(sqy[:, :]), axis=AX.X, op=ALU.add), [B_ep], [B_st8])
            V(lambda e: e.tensor_scalar(out=st8[:, :, 2], in0=st8[:, :, 0], scalar1=1.0 / 64, scalar2=None, op0=ALU.mult), [B_st8], [B_st8])
            V(lambda e: e.tensor_tensor(out=st8[:, :, 3], in0=st8[:, :, 2], in1=st8[:, :, 2], op=ALU.mult), [B_st8], [B_st8])
            V(lambda e: e.scalar_tensor_tensor(out=st8[:, :, 4], in0=st8[:, :, 1], scalar=1.0 / 64, in1=st8[:, :, 3], op0=ALU.mult, op1=ALU.subtract), [B_st8], [B_st8])
            V(lambda e: e.tensor_scalar(out=st8[:, :, 4], in0=st8[:, :, 4], scalar1=64e-5, scalar2=None, op0=ALU.add), [B_st8], [B_st8])
            A(lambda e: e.activation(out=st8[:, :, 5], in_=st8[:, :, 4], func=AF.Sqrt), [B_st8], [B_st8])
            yield
            V(lambda e: e.reciprocal(out=st8[:, :, 5], in_=st8[:, :, 5]), [B_st8], [B_st8])
            yn3 = v4(yn[:, :])
            V(lambda e: e.tensor_tensor(out=yn3, in0=y3, in1=st8[:, :, 2:3].to_broadcast([128, 4, 64]), op=ALU.subtract), [bY, B_st8], [B_ep])
            V(lambda e: e.tensor_tensor(out=yn3, in0=yn3, in1=st8[:, :, 5:6].to_broadcast([128, 4, 64]), op=ALU.mult), [B_ep, B_st8], [B_ep])
            V(lambda e: e.tensor_tensor(out=yn[:, :], in0=yn[:, :], in1=gnbc[:, 0, :], op=ALU.mult), [B_ep, B_c], [B_ep])
            V(lambda e: e.tensor_tensor(out=yn[:, :], in0=yn[:, :], in1=gnbc[:, 1, :], op=ALU.add), [B_ep, B_c], [B_ep])
            V(lambda e: e.tensor_copy(out=st8[:, :, 6], in_=pG[:, 256:260]), [bG], [B_st8])
            for half in range(2):
                hp = slice(half * 64, half * 64 + 64)
                V(lambda e: e.tensor_tensor(out=v4(bon[hp, :]), in0=Vt2[hp, :].rearrange("p (j q t) -> p j q t", q=2, t=64)[:, :, half, :],
                                            in1=st8[hp, :, 6:7].to_broadcast([64, 4, 64]), op=ALU.mult), [B_Vt, B_st8], [B_ep])
            V(lambda e: e.tensor_tensor(out=yn[:, :], in0=yn[:, :], in1=bon[:, :], op=ALU.add), [B_ep], [B_ep])
            V(lambda e: e.tensor_tensor(out=O16[:, :], in0=yn[:, :], in1=pG[:, 0:256], op=ALU.mult), [B_ep, bG], [B_O])
            pO = pU[:, 384:512].bitcast(BF16)
            for (j, pp, js, hs) in hl:
                P(lambda e: e.transpose(pO[pp, js], O16[pp, js], identb[pp, pp]), [B_O, B_ident, B_ST], [bU])
            yield
            V(lambda e: e.tensor_copy(out=rwoT[:, :, csl], in_=v4(pO[:, 0:256])), [bU], [B_rwoT])

        def run_both(ga, gb):
            alive_a, alive_b = ga is not None, gb is not None
            while alive_a or alive_b:
                if alive_a:
                    try:
                        next(ga)
                    except StopIteration:
                        alive_a = False
                if alive_b:
                    try:
                        next(gb)
                    except StopIteration:
                        alive_b = False

        run_both(pre(0), None)
        for c in range(8):
            for _ in range(4):
                next(e0, None)
            run_both(post(c), pre(c + 1) if c + 1 < 8 else None)
        for j in range(4):
            sc.dma("sp", rwo_d[j * 128:(j + 1) * 128, tb * 512:(tb + 1) * 512], rwoT[:, j, :], reads=[B_rwoT])
    for _ in e0:
        pass
    if 'E' in L["phases"]:
        L["e0_done_flag"][0] = True
    ar.release(mB)


def phase_D(L):
    nc, sc, ar, ps, B_ps = L["nc"], L["sc"], L["ar"], L["ps"], L["B_ps"]
    ident, identb, B_ident, load_bf16 = L["ident"], L["identb"], L["B_ident"], L["load_bf16"]
    gt_bc, B_gt = L["gt_bc"], L["B_gt"]
    dbg = L["dbg"]
    V = lambda fn, r=(), w=(): sc.op("dve", fn, r, w)
    A = lambda fn, r=(), w=(): sc.op("act", fn, r, w)
    P = lambda fn, r=(), w=(): sc.op("pe", fn, r, w)
    G = lambda fn, r=(), w=(): sc.op("pool", fn, r, w)
    ALPHA = 2.0 ** 0.25
    mD = ar.mark()
    wbra = ar.alloc([128, 4, D], BF16, "wbra")
    wbrb = ar.alloc([128, 8, D], BF16, "wbrb")
    wout = ar.alloc([128, 8, D], BF16, "wout")
    wqb = ar.alloc([128, 8, D], BF16, "wqb")
    keysT = ar.alloc([128, 8, 128], BF16, "keysT")
    lnbc = ar.alloc([128, 2, D], F32, "lnbc")
    B_w = Buf("wD")
    for kc in range(4):
        load_bf16(wbra[:, kc, :], L["wbra_d"][kc * 128:(kc + 1) * 128, :], D, B_w)
    for kc in range(8):
        load_bf16(wbrb[:, kc, :], L["wbrb_d"][kc * 128:(kc + 1) * 128, :], D, B_w)
        load_bf16(wout[:, kc, :], L["wout_d"][kc * 128:(kc + 1) * 128, :], D, B_w)
        load_bf16(wqb[:, kc, :], L["wq_d"][kc * 128:(kc + 1) * 128, :], D, B_w)
    load_bf16(keysT[:, :, :].rearrange("p a b -> p (a b)"), L["pkeys_d"][:, :, :].rearrange("p a b -> p (a b)"), 1024, B_w)
    sc.dma("sp", lnbc[:, :, :], L["lnbc_d"][:, 0:2, :], writes=[B_w])
    rwoB = ar.alloc([128, 4, 512], BF16, "rwoB")
    dsaB = ar.alloc([128, 8, 512], BF16, "dsaB")
    zgr = [ar.alloc([128, 2, 512], BF16, "zgr%d" % i) for i in range(2)]
    B_zgr = [Buf("zgr0"), Buf("zgr1")]
    mg = ar.alloc([128, 8, 512], BF16, "mg")
    t1 = ar.alloc([128, 512], F32, "t1")
    t2 = ar.alloc([128, 512], F32, "t2")
    B_in, B_mg, B_t = Buf("inD"), Buf("mg"), Buf("tD")
    xt = ar.alloc([128, D], F32, "xtD")
    u = ar.alloc([128, D], F32, "uD")
    x1 = ar.alloc([128, D], F32, "x1")
    h2 = ar.alloc([128, D], F32, "h2")
    st = ar.alloc([128, 8], F32, "stD")
    h2T = ar.alloc([128, 8, 128], BF16, "h2T")
    qT = ar.alloc([128, 8, 128], BF16, "qT")
    ssb = ar.alloc([128, 16, 128], F32, "ssb")
    stmp = ar.alloc([128, 256], F32, "stmp")
    tv = ar.alloc([128, 16, 16], F32, "tv")
    ti = ar.alloc([128, 16, 16], U32, "ti")
    tif = ar.alloc([128, 16, 16], F32, "tif")
    cand = ar.alloc([128, 8, 256], F32, "cand")
    mv = ar.alloc([128, 8, 16], F32, "mv")
    posu = ar.alloc([128, 8, 16], U32, "posu")
    au = ar.alloc([128, 8, 16], U32, "au")
    bu = ar.alloc([128, 8, 16], U32, "bu")
    abf = ar.alloc([128, 2, 128], F32, "abf")
    oh16 = ar.alloc([128, 128, 16], F32, "oh16")
    junk = oh16[:, 0:64, :].rearrange("p a b -> p (a b)")
    sel = ar.alloc([128, 3, 128], F32, "sel")
    selT = ar.alloc([128, 3, 128], F32, "selT")
    gate = ar.alloc([128, 8, 16], F32, "gate")
    gs = ar.alloc([128, 8], F32, "gs")
    iota16 = L["iota16"]
    B_oh, B_sel, B_selT = Buf("oh"), Buf("sel"), Buf("selT")
    B_x, B_u, B_x1, B_h2, B_st, B_h2T, B_qT, B_s, B_tk, B_c, B_e, B_hu, B_acc, B_j = [Buf(n) for n in
        ("x", "u", "x1", "h2", "st", "h2T", "qT", "s", "tk", "cand", "eid", "hu", "acc", "junk")]
    x_v = L["x_d"].rearrange("(n p) m -> p n m", p=128)
    out_v = L["out_d"].rearrange("(n p) m -> p n m", p=128)

    def layer_norm(src, bsrc, dst, bdst, gi):
        A(lambda e: e.activation(out=junk, in_=src[:, :], func=AF.Copy, accum_out=st[:, 0:1]), [bsrc], [B_st, B_oh])
        A(lambda e: e.activation(out=junk, in_=src[:, :], func=AF.Square, accum_out=st[:, 1:2]), [bsrc], [B_st, B_oh])
        V(lambda e: e.tensor_scalar(out=st[:, 2:3], in0=st[:, 0:1], scalar1=1.0 / D, scalar2=None, op0=ALU.mult), [B_st], [B_st])
        V(lambda e: e.tensor_tensor(out=st[:, 3:4], in0=st[:, 2:3], in1=st[:, 2:3], op=ALU.mult), [B_st], [B_st])
        V(lambda e: e.scalar_tensor_tensor(out=st[:, 4:5], in0=st[:, 1:2], scalar=1.0 / D, in1=st[:, 3:4], op0=ALU.mult, op1=ALU.subtract), [B_st], [B_st])
        V(lambda e: e.tensor_scalar(out=st[:, 4:5], in0=st[:, 4:5], scalar1=1e-5, scalar2=None, op0=ALU.add), [B_st], [B_st])
        A(lambda e: e.activation(out=st[:, 5:6], in_=st[:, 4:5], func=AF.Sqrt), [B_st], [B_st])
        V(lambda e: e.reciprocal(out=st[:, 5:6], in_=st[:, 5:6]), [B_st], [B_st])
        V(lambda e: e.scalar_tensor_tensor(out=st[:, 6:7], in0=st[:, 2:3], scalar=-1.0, in1=st[:, 5:6], op0=ALU.mult, op1=ALU.mult), [B_st], [B_st])
        A(lambda e: e.activation(out=dst[:, :], in_=src[:, :], func=AF.Identity, scale=st[:, 5:6], bias=st[:, 6:7]), [bsrc, B_st], [bdst])
        G(lambda e: e.tensor_tensor(out=dst[:, :], in0=dst[:, :], in1=lnbc[:, gi, :], op=ALU.mult), [bdst, B_w], [bdst])
        G(lambda e: e.tensor_tensor(out=dst[:, :], in0=dst[:, :], in1=lnbc[:, gi + 1, :], op=ALU.add), [bdst, B_w], [bdst])

    x1p = [x1, ar.alloc([128, D], F32, "x1b")]
    B_x1p = [B_x1, Buf("x1b")]
    h2Tp_ = [h2T, ar.alloc([128, 8, 128], BF16, "h2Tb")]
    B_h2Tp_ = [B_h2T, Buf("h2Tb")]
    ssbp = [ssb, ar.alloc([128, 16, 128], F32, "ssbb")]
    B_sp = [B_s, Buf("ssbb")]
    prevn = [None]
    B_ohh = [Buf("ohh%d" % i) for i in range(8)]
    stmp16 = ar.alloc([128, 16, 128], F32, "stmp16")
    stmp8 = stmp16[:, :, :].rearrange("p (h a) b -> p h (a b)", a=2)
    B_tkg = [Buf("tkg%d" % i) for i in range(16)]
    B_tkg2 = [Buf("tkgb%d" % i) for i in range(16)]
    B_stg16 = [Buf("stg16_%d" % i) for i in range(16)]
    B_tig = [Buf("tig%d" % i) for i in range(16)]
    B_tig2 = [Buf("tigb%d" % i) for i in range(16)]
    B_mvh = [Buf("mvh%d" % i) for i in range(8)]
    B_mvh2 = [Buf("mvhb%d" % i) for i in range(8)]
    B_st8h = [B_stg16[2 * i] for i in range(8)]
    B_posh = [Buf("posh%d" % i) for i in range(8)]
    B_posh2 = [Buf("poshb%d" % i) for i in range(8)]

    def stageA(n, jt):
        q = n % 2
        x1_, bx1_, h2T_, bh2T_, ssb_, bs_ = x1p[q], B_x1p[q], h2Tp_[q], B_h2Tp_[q], ssbp[q], B_sp[q]
        sc.dma("sp", xt[:, :], x_v[:, n, :], writes=[B_x])
        for half in range(2):
            pM, bM = ps[4 + half], B_ps[4 + half]
            hsl = slice(half * 512, (half + 1) * 512)
            for dc in range(8):
                P(lambda e: e.matmul(pM[:, :], lhsT=mg[:, dc, jt * 128:(jt + 1) * 128], rhs=wout[:, dc, hsl], start=(dc == 0), stop=(dc == 7)), [B_mg, B_w], [bM])
            V(lambda e: e.tensor_tensor(out=u[:, hsl], in0=pM[:, :], in1=gt_bc[:, 0, hsl], op=ALU.mult), [bM, B_gt], [B_u])
            V(lambda e: e.scalar_tensor_tensor(out=u[:, hsl], in0=xt[:, hsl], scalar=ALPHA, in1=u[:, hsl], op0=ALU.mult, op1=ALU.add), [B_x, B_u], [B_u])
        layer_norm(u, B_u, x1_, bx1_, 0)
        if "x1dbg" in dbg:
            sc.dma("sp", L["x1_d"][n * 128:(n + 1) * 128, :], x1_[:, :], reads=[bx1_])
        sc.dma("sp", L["x1s_d"][n * 128:(n + 1) * 128, :], x1_[:, :], reads=[bx1_])
        G(lambda e: e.tensor_tensor(out=h2[:, :], in0=x1_[:, :], in1=gt_bc[:, 2, :], op=ALU.mult), [bx1_, B_gt], [B_h2])
        G(lambda e: e.tensor_tensor(out=h2[:, :], in0=h2[:, :], in1=gt_bc[:, 1, :], op=ALU.add), [B_h2, B_gt], [B_h2])
        for kc in range(8):
            pp_, bp_ = ps[kc // 4], B_ps[kc // 4]
            P(lambda e: e.transpose(pp_[:, (kc % 4) * 128:(kc % 4 + 1) * 128], h2[:, kc * 128:(kc + 1) * 128], ident[:, :]), [B_h2, B_ident], [bp_])
        for k2 in range(2):
            A(lambda e: e.activation(out=h2T_[:, k2 * 4:(k2 + 1) * 4, :], in_=ps[k2][:, :].rearrange("p (a b) -> p a b", b=128), func=AF.Copy), [B_ps[k2]], [bh2T_])
        for kc in range(8):
            sc.dma("sp", L["h2T_d"][kc * 128:(kc + 1) * 128, n * 128:(n + 1) * 128], h2T_[:, kc, :], reads=[bh2T_])
        for hh in range(8):
            pq, bq = ps[2 + hh // 4], B_ps[2 + hh // 4]
            for kc in range(8):
                P(lambda e: e.matmul(pq[:, (hh % 4) * 128:(hh % 4 + 1) * 128], lhsT=wqb[:, kc, hh * 128:(hh + 1) * 128], rhs=h2T_[:, kc, :],
                                     start=(kc == 0), stop=(kc == 7)), [B_w, bh2T_], [bq])
        for k2 in range(2):
            A(lambda e: e.activation(out=qT[:, k2 * 4:(k2 + 1) * 4, :], in_=ps[2 + k2][:, :].rearrange("p (a b) -> p a b", b=128), func=AF.Copy), [B_ps[2 + k2]], [B_qT])
        for g in range(16):
            hh, cc = g % 8, g // 8
            pS, bS = ps[4 + g // 4], B_ps[4 + g // 4]
            P(lambda e: e.matmul(pS[:, (g % 4) * 128:(g % 4 + 1) * 128], lhsT=qT[cc * 64:(cc + 1) * 64, hh, :], rhs=keysT[cc * 64:(cc + 1) * 64, hh, :],
                                 start=True, stop=True), [B_qT, B_w], [bS])
        for k4 in range(4):
            A(lambda e: e.activation(out=ssb_[:, k4 * 4:(k4 + 1) * 4, :], in_=ps[4 + k4][:, :].rearrange("p (a b) -> p a b", b=128), func=AF.Copy), [B_ps[4 + k4]], [bs_])

    def stageB(n):
        q = n % 2
        ssb_, bs_ = ssbp[q], B_sp[q]
        for g in range(16):
            V(lambda e: e.max(out=tv[:, g, 0:8], in_=ssb_[:, g, :]), [bs_], [B_tkg[g]])
        for g in range(16):
            V(lambda e: e.match_replace(out=stmp16[:, g, :], in_to_replace=tv[:, g, 0:8], in_values=ssb_[:, g, :], imm_value=-1e30), [bs_, B_tkg[g]], [B_stg16[g]])
        for g in range(16):
            V(lambda e: e.max(out=tv[:, g, 8:16], in_=stmp16[:, g, :]), [B_stg16[g]], [B_tkg2[g]])
        for g in range(16):
            V(lambda e: e.max_index(out=ti[:, g, 0:8], in_max=tv[:, g, 0:8], in_values=ssb_[:, g, :]), [bs_, B_tkg[g]], [B_tig[g]])
        for g in range(16):
            V(lambda e: e.max_index(out=ti[:, g, 8:16], in_max=tv[:, g, 8:16], in_values=ssb_[:, g, :]), [bs_, B_tkg2[g]], [B_tig2[g]])
        V(lambda e: e.tensor_copy(out=tif[:, :, :], in_=ti[:, :, :]), B_tig + B_tig2, [B_tk])
        V(lambda e: e.tensor_copy(out=tv[:, 0:1, 0:1], in_=tv[:, 0:1, 0:1]), B_tkg + B_tkg2, [B_tk])
        tvv = tv[:, :, :].rearrange("p (c h) k -> p h c k", c=2)
        tfv = tif[:, :, :].rearrange("p (c h) k -> p h c k", c=2)
        c4 = cand[:, :, :].rearrange("p h (a b) -> p h a b", b=16)
        for hh in range(8):
            V(lambda e: e.tensor_tensor(out=c4[:, hh, :, :], in0=tvv[:, hh, 0, :].unsqueeze(2).to_broadcast([128, 16, 16]),
                                        in1=tvv[:, hh, 1, :].unsqueeze(1).to_broadcast([128, 16, 16]), op=ALU.add), [B_tk], [B_c])
        for hh in range(8):
            V(lambda e: e.max(out=mv[:, hh, 0:8], in_=cand[:, hh, :]), [B_c], [B_mvh[hh]])
        for hh in range(8):
            V(lambda e: e.match_replace(out=stmp8[:, hh, :], in_to_replace=mv[:, hh, 0:8], in_values=cand[:, hh, :], imm_value=-1e30), [B_c, B_mvh[hh]], [B_stg16[2 * hh], B_stg16[2 * hh + 1]])
        for hh in range(8):
            V(lambda e: e.max(out=mv[:, hh, 8:16], in_=stmp8[:, hh, :]), [B_stg16[2 * hh], B_stg16[2 * hh + 1]], [B_mvh2[hh]])
        for hh in range(8):
            V(lambda e: e.max_index(out=posu[:, hh, 0:8], in_max=mv[:, hh, 0:8], in_values=cand[:, hh, :]), [B_c, B_mvh[hh]], [B_posh[hh]])
        for hh in range(8):
            V(lambda e: e.max_index(out=posu[:, hh, 8:16], in_max=mv[:, hh, 8:16], in_values=cand[:, hh, :]), [B_c, B_mvh2[hh]], [B_posh2[hh]])
        V(lambda e: e.tensor_copy(out=mv[:, 0:1, 0:1], in_=mv[:, 0:1, 0:1]), B_mvh + B_mvh2 + B_posh + B_posh2, [B_e])
        V(lambda e: e.tensor_scalar(out=au[:, :, :], in0=posu[:, :, :], scalar1=4, scalar2=None, op0=ALU.logical_shift_right), [B_e], [B_e])
        V(lambda e: e.tensor_scalar(out=bu[:, :, :], in0=posu[:, :, :], scalar1=15, scalar2=None, op0=ALU.bitwise_and), [B_e], [B_e])
        V(lambda e: e.tensor_copy(out=abf[:, 0, :], in_=au[:, :, :].rearrange("p a b -> p (a b)")), [B_e], [B_e])
        V(lambda e: e.tensor_copy(out=abf[:, 1, :], in_=bu[:, :, :].rearrange("p a b -> p (a b)")), [B_e], [B_e])
        for cc in range(2):
            V(lambda e: e.tensor_tensor(out=oh16[:, :, :], in0=abf[:, cc, :].unsqueeze(2).to_broadcast([128, 128, 16]),
                                        in1=iota16.unsqueeze(1).to_broadcast([128, 128, 16]), op=ALU.is_equal), [B_e, L["B_iota"]], [B_oh] + B_ohh)
            for hh in range(8):
                V(lambda e: e.tensor_tensor(out=oh16[:, hh * 16:(hh + 1) * 16, :], in0=oh16[:, hh * 16:(hh + 1) * 16, :],
                                            in1=tfv[:, hh, cc, :].unsqueeze(1).to_broadcast([128, 16, 16]), op=ALU.mult), [B_oh, B_tk], [B_ohh[hh]])
            V(lambda e: e.tensor_reduce(out=sel[:, cc, :], in_=oh16[:, :, :], axis=AX.X, op=ALU.add), B_ohh, [B_sel, B_oh])
        V(lambda e: e.tensor_tensor(out=gate[:, :, :], in0=mv[:, :, :], in1=mv[:, :, 0:1].to_broadcast([128, 8, 16]), op=ALU.subtract), [B_e], [B_hu])
        A(lambda e: e.activation(out=gate[:, :, :], in_=gate[:, :, :], func=AF.Exp), [B_hu], [B_hu])
        V(lambda e: e.tensor_reduce(out=gs[:, :], in_=gate[:, :, :], axis=AX.X, op=ALU.add), [B_hu], [B_hu])
        V(lambda e: e.reciprocal(out=gs[:, :], in_=gs[:, :]), [B_hu], [B_hu])
        V(lambda e: e.tensor_tensor(out=sel[:, 2, :].rearrange("p (a b) -> p a b", b=16), in0=gate[:, :, :], in1=gs[:, :].unsqueeze(2).to_broadcast([128, 8, 16]), op=ALU.mult),
          [B_hu], [B_sel])
        pI, bI = ps[6], B_ps[6]
        for q3 in range(3):
            P(lambda e: e.transpose(pI[:, q3 * 128:(q3 + 1) * 128], sel[:, q3, :], ident[:, :]), [B_sel, B_ident], [bI])
        A(lambda e: e.activation(out=selT[:, :, :], in_=pI[:, 0:384].rearrange("p (a b) -> p a b", b=128), func=AF.Copy), [bI], [B_selT])
        for q3 in range(3):
            sc.dma("sp", L["selT_d"][q3, :, n * 128:(n + 1) * 128], selT[:, q3, :], reads=[B_selT])

    for tb in range(L["nblk"]):
        tsl = slice(tb * 512, (tb + 1) * 512)
        for kc in range(4):
            sc.dma("sp", rwoB[:, kc, :], L["rwo_d"][kc * 128:(kc + 1) * 128, tsl], writes=[B_in])
        for kc in range(8):
            sc.dma("sp", dsaB[:, kc, :], L["dsao_d"][kc * 128:(kc + 1) * 128, tsl], writes=[B_in])
        for dc in range(8):
            pA, bA = ps[dc % 2], B_ps[dc % 2]
            pB, bB = ps[2 + dc % 2], B_ps[2 + dc % 2]
            zg_, bz_ = zgr[dc % 2], B_zgr[dc % 2]
            sc.dma("sp", zg_[:, 0, :], L["zg_d"][dc * 128:(dc + 1) * 128, tsl], writes=[bz_])
            sc.dma("sp", zg_[:, 1, :], L["zg_d"][(8 + dc) * 128:(9 + dc) * 128, tsl], writes=[bz_])
            for kc in range(4):
                P(lambda e: e.matmul(pA[:, :], lhsT=wbra[:, kc, dc * 128:(dc + 1) * 128], rhs=rwoB[:, kc, :], start=(kc == 0), stop=(kc == 3)), [B_w, B_in], [bA])
            for kc in range(8):
                P(lambda e: e.matmul(pB[:, :], lhsT=wbrb[:, kc, dc * 128:(dc + 1) * 128], rhs=dsaB[:, kc, :], start=(kc == 0), stop=(kc == 7)), [B_w, B_in], [bB])
            V(lambda e: e.tensor_tensor(out=t1[:, :], in0=pA[:, :], in1=zg_[:, 0, :], op=ALU.mult), [bA, bz_], [B_t])
            V(lambda e: e.tensor_tensor(out=t2[:, :], in0=pB[:, :], in1=zg_[:, 1, :], op=ALU.mult), [bB, bz_], [B_t])
            V(lambda e: e.tensor_tensor(out=mg[:, dc, :], in0=t1[:, :], in1=t2[:, :], op=ALU.add), [B_t], [B_mg])
        for jt in range(4):
            n = tb * 4 + jt
            stageA(n, jt)
            if prevn[0] is not None:
                stageB(prevn[0])
            prevn[0] = n
    stageB(prevn[0])
    ar.release(mD)


def phase_C(L):
    nc, sc, ar, ps, B_ps = L["nc"], L["sc"], L["ar"], L["ps"], L["B_ps"]
    identb, B_ident = L["identb"], L["B_ident"]
    V = lambda fn, r=(), w=(): sc.op("dve", fn, r, w)
    A = lambda fn, r=(), w=(): sc.op("act", fn, r, w)
    P = lambda fn, r=(), w=(): sc.op("pe", fn, r, w)
    G = lambda fn, r=(), w=(): sc.op("pool", fn, r, w)
    NQB = L["nblk"] * 4
    mC = ar.mark()
    cvec = ar.alloc([128, 256], F32, "cvec")
    biasT = ar.alloc([128, 3, 1024], F32, "biasT")
    negm = ar.alloc([128, 128], F32, "negm")
    ckv_tok = ar.alloc([128, 32, 129], BF16, "ckv_tok")
    ckvT = ar.alloc([128, S], BF16, "ckvT")
    kiT2 = ar.alloc([128, S], BF16, "kiT2")
    wall = ar.alloc([128, 32, 4], F32, "wall")
    B_cc, B_kv, B_ki, B_wl = Buf("cc"), Buf("kv"), Buf("ki"), Buf("wl")
    sc.dma("sp", cvec[:, :], L["cvec_d"][:, :], writes=[B_cc])
    sc.dma("sp", biasT[:, :, :], L["biasT_d"][:, :, :], writes=[B_cc])
    sc.dma("sp", negm[:, :], L["negm_d"][:, :], writes=[B_cc])
    V(lambda e: e.memset(ckv_tok[:, :, 128:129], 1.0), [], [B_kv])
    for bi_ in range(2):
        V(lambda e: e.tensor_tensor(out=biasT[:, bi_, :], in0=biasT[:, bi_, :], in1=biasT[:, 2, :], op=ALU.subtract), [B_cc], [B_cc])
    zt = [ar.alloc([128, 196], F32, "ztC%d" % i) for i in range(2)]
    B_zt = [Buf("ztC0"), Buf("ztC1")]
    sq = ar.alloc([128, 128], F32, "sqC")
    c16 = ar.alloc([128, 128], BF16, "c16")
    k32 = ar.alloc([128, 64], F32, "k32")
    k16 = ar.alloc([128, 128], BF16, "k16")
    stc = ar.alloc([128, 8], F32, "stc")
    B_sq, B_c16, B_k, B_stc = Buf("sqC"), Buf("c16"), Buf("k"), Buf("stc")
    for n in range(NQB):
        z, bz = zt[n % 2], B_zt[n % 2]
        sc.dma("sp", z[:, :], L["ztok_d"][n * 128:(n + 1) * 128, :], writes=[bz])
        A(lambda e: e.activation(out=sq[:, :], in_=z[:, 0:128], func=AF.Square, accum_out=stc[:, 0:1]), [bz], [B_sq, B_stc])
        V(lambda e: e.tensor_scalar(out=stc[:, 1:2], in0=stc[:, 0:1], scalar1=1.0 / 128, scalar2=1e-5, op0=ALU.mult, op1=ALU.add), [B_stc], [B_stc])
        A(lambda e: e.activation(out=stc[:, 1:2], in_=stc[:, 1:2], func=AF.Sqrt), [B_stc], [B_stc])
        V(lambda e: e.reciprocal(out=stc[:, 1:2], in_=stc[:, 1:2]), [B_stc], [B_stc])
        V(lambda e: e.scalar_tensor_tensor(out=ckv_tok[:, n, 0:128], in0=z[:, 0:128], scalar=stc[:, 1:2], in1=cvec[:, 0:128], op0=ALU.mult, op1=ALU.mult),
          [bz, B_stc, B_cc], [B_kv])
        V(lambda e: e.tensor_reduce(out=stc[:, 2:3], in_=z[:, 128:192], axis=AX.X, op=ALU.add), [bz], [B_stc])
        A(lambda e: e.activation(out=sq[:, 0:64], in_=z[:, 128:192], func=AF.Square, accum_out=stc[:, 3:4]), [bz], [B_sq, B_stc])
        V(lambda e: e.tensor_scalar(out=stc[:, 4:5], in0=stc[:, 2:3], scalar1=1.0 / 64, scalar2=None, op0=ALU.mult), [B_stc], [B_stc])
        V(lambda e: e.tensor_tensor(out=stc[:, 5:6], in0=stc[:, 4:5], in1=stc[:, 4:5], op=ALU.mult), [B_stc], [B_stc])
        V(lambda e: e.scalar_tensor_tensor(out=stc[:, 6:7], in0=stc[:, 3:4], scalar=1.0 / 64, in1=stc[:, 5:6], op0=ALU.mult, op1=ALU.subtract), [B_stc], [B_stc])
        V(lambda e: e.tensor_scalar(out=stc[:, 6:7], in0=stc[:, 6:7], scalar1=1e-5, scalar2=None, op0=ALU.add), [B_stc], [B_stc])
        A(lambda e: e.activation(out=stc[:, 6:7], in_=stc[:, 6:7], func=AF.Sqrt), [B_stc], [B_stc])
        V(lambda e: e.reciprocal(out=stc[:, 6:7], in_=stc[:, 6:7]), [B_stc], [B_stc])
        V(lambda e: e.tensor_scalar(out=k32[:, :], in0=z[:, 128:192], scalar1=stc[:, 4:5], scalar2=stc[:, 6:7], op0=ALU.subtract, op1=ALU.mult), [bz, B_stc], [B_k])
        V(lambda e: e.tensor_tensor(out=k32[:, :], in0=k32[:, :], in1=cvec[:, 128:192], op=ALU.mult), [B_k, B_cc], [B_k])
        V(lambda e: e.tensor_tensor(out=k16[:, 0:64], in0=k32[:, :], in1=cvec[:, 192:256], op=ALU.add), [B_k, B_cc], [B_k])
        V(lambda e: e.tensor_copy(out=k16[:, 64:128], in_=k16[:, 0:64]), [B_k], [B_k])
        V(lambda e: e.tensor_scalar(out=wall[:, n, :], in0=z[:, 192:196], scalar1=0.0625, scalar2=None, op0=ALU.mult), [bz], [B_wl])
        pT = ps[n % 2][:, :].bitcast(BF16)
        bT = B_ps[n % 2]
        P(lambda e: e.transpose(pT[:, 0:128], ckv_tok[:, n, 0:128], identb[:, :]), [B_kv, B_ident], [bT])
        P(lambda e: e.transpose(pT[:, 128:256], k16[:, :], identb[:, :]), [B_k, B_ident], [bT])
        A(lambda e: e.activation(out=ckvT[:, n * 128:(n + 1) * 128], in_=pT[:, 0:128], func=AF.Copy, scale=128.0 ** -0.5), [bT], [B_kv])
        V(lambda e: e.tensor_copy(out=kiT2[:, n * 128:(n + 1) * 128], in_=pT[:, 128:256]), [bT], [B_ki])
    qiB = [ar.alloc([128, 2, 128], BF16, "qiB%d" % i) for i in range(2)]
    zqB = [ar.alloc([128, 8, 128], BF16, "zqB%d" % i) for i in range(2)]
    B_qi = [Buf("qi0"), Buf("qi1")]
    B_zq = [Buf("zq0"), Buf("zq1")]
    score2 = [ar.alloc([128, S], F32, "score%d" % i) for i in range(2)]
    maskb2 = [ar.alloc([128, S], BF16, "maskb%d" % i) for i in range(2)]
    maskT2 = [ar.alloc([128, 32, 128], BF16, "maskT%d" % i) for i in range(2)]
    bis2 = [ar.alloc([128, 8], F32, "bis%d" % i) for i in range(2)]
    junk = ar.alloc([128, S], BF16, "junkC")
    junkA = ar.alloc([128, S // 2], BF16, "junkA")
    rl = [ar.alloc([128, 512], F32, "rl%d" % i) for i in range(2)]
    B_rl = [Buf("rl0"), Buf("rl1")]
    lg = ar.alloc([128, 1024], F32, "lg")
    PT2 = [ar.alloc([128, 8, 128], BF16, "PT%d" % i) for i in range(2)]
    B_PT2 = [Buf("PT0"), Buf("PT1")]
    Oacc = ar.alloc([128, 8, 129], F32, "Oacc")
    rec = ar.alloc([128, 8], F32, "rec")
    Oo = ar.alloc([128, 8, 128], BF16, "Oo")
    dsT = ar.alloc([128, 8, 128], BF16, "dsT")
    B_sc2 = [Buf("score0"), Buf("score1")]
    B_mb2 = [Buf("maskb0"), Buf("maskb1")]
    B_mT2 = [Buf("maskT0"), Buf("maskT1")]
    B_bisA = [Buf("bisA0"), Buf("bisA1")]
    B_bisB = [Buf("bisB0"), Buf("bisB1")]
    B_bisC = [Buf("bisC0"), Buf("bisC1")]
    B_j, B_jA, B_lg, B_O, B_Oo, B_dsT = [Buf(n_) for n_ in ("junkC", "junkA", "lg", "Oacc", "Oo", "dsT")]

    def stage1(j):
        q = j % 2
        Lk = (j + 1) * 128
        qi, bqi, zq, bzq = qiB[q], B_qi[q], zqB[q], B_zq[q]
        score, B_sc = score2[q], B_sc2[q]
        qsl = slice(j * 128, (j + 1) * 128)
        for cch in range(2):
            sc.dma("sp", qi[:, cch, :], L["zqi_d"][cch * 128:(cch + 1) * 128, qsl], writes=[bqi])
        for h in range(8):
            sc.dma("sp", zq[:, h, :], L["zq_d"][h * 128:(h + 1) * 128, qsl], writes=[bzq])
        nkc = (Lk + 511) // 512
        ri = 0
        for kc in range(nkc):
            wd = min(512, Lk - kc * 512)
            ksl = slice(kc * 512, kc * 512 + wd)
            for hi in range(4):
                po = (hi % 2) * 64
                pD, bD = ps[hi], B_ps[hi]
                P(lambda e: e.matmul(pD[:, 0:wd], lhsT=qi[po:po + 64, hi // 2, :], rhs=kiT2[po:po + 64, ksl], start=True, stop=True), [bqi, B_ki], [bD])
                r_, br_ = rl[ri % 2], B_rl[ri % 2]
                ri += 1
                A(lambda e: e.activation(out=r_[:, 0:wd], in_=pD[:, 0:wd], func=AF.Relu), [bD], [br_])
                if hi == 0:
                    V(lambda e: e.tensor_scalar(out=score[:, ksl], in0=r_[:, 0:wd], scalar1=wall[:, j, 0:1], scalar2=None, op0=ALU.mult), [br_, B_wl], [B_sc])
                else:
                    V(lambda e: e.scalar_tensor_tensor(out=score[:, ksl], in0=r_[:, 0:wd], scalar=wall[:, j, hi:hi + 1], in1=score[:, ksl], op0=ALU.mult, op1=ALU.add),
                      [br_, B_wl, B_sc], [B_sc])
        V(lambda e: e.tensor_tensor(out=score[:, qsl], in0=score[:, qsl], in1=negm[:, :], op=ALU.add), [B_sc, B_cc], [B_sc])

    def bisect_iters(j):
        q = j % 2
        Lk = (j + 1) * 128
        score, B_sc, bis = score2[q], B_sc2[q], bis2[q]
        bA, bB, bC = B_bisA[q], B_bisB[q], B_bisC[q]
        if Lk > 256:
            na = (Lk // 2) // 128 * 128
            V(lambda e: e.memset(bis[:, 6:7], 0.0), [bA], [bA])
            step = 32.0
            for it in range(21):
                V(lambda e: e.tensor_scalar(out=bis[:, 7:8], in0=bis[:, 6:7], scalar1=-1.0, scalar2=None, op0=ALU.mult), [bA], [bB])
                A(lambda e: e.activation(out=junkA[:, 0:na], in_=score[:, 0:na], func=AF.Sign, bias=bis[:, 7:8], accum_out=bis[:, 2:3]), [B_sc, bB], [B_jA, bC])
                V(lambda e: e.tensor_scalar(out=junk[:, na:Lk], in0=score[:, na:Lk], scalar1=bis[:, 6:7], scalar2=None, op0=ALU.is_ge, op1=ALU.add, accum_out=bis[:, 3:4]),
                  [B_sc, bA], [B_j, bA])
                V(lambda e: e.scalar_tensor_tensor(out=bis[:, 4:5], in0=bis[:, 2:3], scalar=0.5, in1=bis[:, 3:4], op0=ALU.mult, op1=ALU.add), [bA, bC], [bA])
                V(lambda e: e.tensor_scalar(out=bis[:, 5:6], in0=bis[:, 4:5], scalar1=255.5 - na / 2.0, scalar2=2.0 * step, op0=ALU.is_ge, op1=ALU.mult), [bA], [bA])
                V(lambda e: e.scalar_tensor_tensor(out=bis[:, 6:7], in0=bis[:, 5:6], scalar=-step, in1=bis[:, 6:7], op0=ALU.add, op1=ALU.add), [bA, bB], [bA])
                step *= 0.5
                yield
            V(lambda e: e.tensor_scalar(out=bis[:, 6:7], in0=bis[:, 6:7], scalar1=-4.0 * step, scalar2=None, op0=ALU.add), [bA], [bA])
        else:
            V(lambda e: e.memset(bis[:, 6:7], -1e29), [bA], [bA])

    def stage3(j):
        q = j % 2
        Lk = (j + 1) * 128
        score, B_sc, bis, maskb, B_mb, maskT, B_mT = score2[q], B_sc2[q], bis2[q], maskb2[q], B_mb2[q], maskT2[q], B_mT2[q]
        V(lambda e: e.tensor_scalar(out=maskb[:, 0:Lk], in0=score[:, 0:Lk], scalar1=bis[:, 6:7], scalar2=None, op0=ALU.is_ge), [B_sc, B_bisA[q]], [B_mb])
        for k8 in range((j + 8) // 8):
            nn = min(8, j + 1 - k8 * 8)
            pM = ps[4][:, :].bitcast(BF16)
            for kk_ in range(nn):
                kt = k8 * 8 + kk_
                P(lambda e: e.transpose(pM[:, kk_ * 128:(kk_ + 1) * 128], maskb[:, kt * 128:(kt + 1) * 128], identb[:, :]), [B_mb, B_ident], [B_ps[4]])
            A(lambda e: e.activation(out=maskT[:, k8 * 8:k8 * 8 + nn, :], in_=pM[:, 0:nn * 128].rearrange("p (a b) -> p a b", b=128), func=AF.Copy), [B_ps[4]], [B_mT])

    def stage4(j, side):
        q = j % 2
        zq, bzq, maskT, B_mT = zqB[q], B_zq[q], maskT2[q], B_mT2[q]
        qsl = slice(j * 128, (j + 1) * 128)
        for kt in range(j + 1):
            near = kt >= j - 1
            bsel = 0 if kt == j else 1
            PTk, bPT = PT2[kt % 2], B_PT2[kt % 2]
            for half in range(2):
                pL, bL = ps[(kt % 2) * 2 + half], B_ps[(kt % 2) * 2 + half]
                P(lambda e: e.matmul(pL[:, :], lhsT=ckvT[:, kt * 128:(kt + 1) * 128], rhs=zq[:, half * 4:(half + 1) * 4, :], start=True, stop=True), [B_kv, bzq], [bL])
                if near:
                    V(lambda e: e.tensor_tensor(out=lg[:, half * 512:(half + 1) * 512], in0=pL[:, :], in1=biasT[:, bsel, half * 512:(half + 1) * 512], op=ALU.add),
                      [bL, B_cc], [B_lg])
                    A(lambda e: e.activation(out=PTk[:, half * 4:(half + 1) * 4, :], in_=lg[:, half * 512:(half + 1) * 512].rearrange("p (h q) -> p h q", q=128), func=AF.Exp),
                      [B_lg], [bPT])
                else:
                    A(lambda e: e.activation(out=PTk[:, half * 4:(half + 1) * 4, :], in_=pL[:, :].rearrange("p (h q) -> p h q", q=128), func=AF.Exp), [bL], [bPT])
            V(lambda e: e.tensor_tensor(out=PTk[:, :, :], in0=PTk[:, :, :], in1=maskT[:, kt, :].unsqueeze(1).to_broadcast([128, 8, 128]), op=ALU.mult), [bPT, B_mT], [bPT])
            for h in range(8):
                pO, bO = ps[5 + h // 3], B_ps[5 + h // 3]
                P(lambda e: e.matmul(pO[:, (h % 3) * 129:(h % 3 + 1) * 129], lhsT=PTk[:, h, :], rhs=ckv_tok[:, kt, :], start=(kt == 0 and h % 3 == 0), stop=(kt == j),
                                     skip_group_check=True), [bPT, B_kv], [bO])
            if side is not None:
                next(side, None)
        for b3 in range(3):
            nh = 3 if b3 < 2 else 2
            V(lambda e: e.tensor_copy(out=Oacc[:, b3 * 3:b3 * 3 + nh, :], in_=ps[5 + b3][:, 0:nh * 129].rearrange("p (h d) -> p h d", d=129)), [B_ps[5 + b3]], [B_O])
        V(lambda e: e.reciprocal(out=rec[:, :], in_=Oacc[:, :, 128]), [B_O], [B_Oo])
        V(lambda e: e.tensor_tensor(out=Oo[:, :, :], in0=Oacc[:, :, 0:128], in1=rec[:, :].unsqueeze(2).to_broadcast([128, 8, 128]), op=ALU.mult), [B_O, B_Oo], [B_Oo])
        pX = ps[4][:, :].bitcast(BF16)
        for h in range(8):
            P(lambda e: e.transpose(pX[:, h * 128:(h + 1) * 128], Oo[:, h, :], identb[:, :]), [B_Oo, B_ident], [B_ps[4]])
        V(lambda e: e.tensor_copy(out=dsT[:, :, :], in_=pX[:, :].rearrange("p (a b) -> p a b", b=128)), [B_ps[4]], [B_dsT])
        for h in range(8):
            sc.dma("sp", L["dsao_d"][h * 128:(h + 1) * 128, qsl], dsT[:, h, :], reads=[B_dsT])

    stage1(0)
    for _ in bisect_iters(0):
        pass
    stage3(0)
    for j in range(NQB):
        side = None
        if j + 1 < NQB:
            stage1(j + 1)
            side = bisect_iters(j + 1)
        stage4(j, side)
        if side is not None:
            for _ in side:
                pass
            stage3(j + 1)
    ar.release(mC)


def phase_E(L):
    nc, sc, ar, ps, B_ps = L["nc"], L["sc"], L["ar"], L["ps"], L["B_ps"]
    identb, B_ident = L["identb"], L["B_ident"]
    gt_bc, B_gt = L["gt_bc"], L["B_gt"]
    iota128, B_iota = L["iota128"], L["B_iota"]
    dbg = L["dbg"]
    V = lambda fn, r=(), w=(): sc.op("dve", fn, r, w)
    A = lambda fn, r=(), w=(): sc.op("act", fn, r, w)
    P = lambda fn, r=(), w=(): sc.op("pe", fn, r, w)
    G = lambda fn, r=(), w=(): sc.op("pool", fn, r, w)
    ALPHA = 2.0 ** 0.25
    if not L["e0_done_flag"][0]:
        m0 = ar.mark()
        for _ in make_e0(L, 3, 0):
            pass
        ar.release(m0)
    ar.release(L["mark_stg"])
    mE = ar.mark()
    TP = 256
    lnbc = ar.alloc([128, 2, D], F32, "lnbcE")
    B_ln = Buf("lnE")
    sc.dma("sp", lnbc[:, :, :], L["lnbc_d"][:, 2:4, :], writes=[B_ln])
    Gs2 = [ar.alloc([128, 128, TP], BF16, "Gs%d" % i) for i in range(2)]
    h2Tp2 = [ar.alloc([128, 8, TP], BF16, "h2Tp%d" % i) for i in range(2)]
    IT1 = ar.alloc([128, 3, TP], F32, "IT1")
    IT2 = [IT1, IT1]
    ITb2 = [ar.alloc([128, 3, TP], BF16, "ITb%d" % i) for i in range(2)]
    iotab = ar.alloc([128, 128], BF16, "iotab")
    NBT = 8
    eqb = [ar.alloc([128, NBT, 128], BF16, "eqb%d" % i) for i in range(1)]
    Lb = [ar.alloc([128, NBT, 128], BF16, "Lb%d" % i) for i in range(2)]
    Rb = [ar.alloc([128, NBT, 128], BF16, "Rb%d" % i) for i in range(2)]
    B_Gs2 = [Buf("Gs0"), Buf("Gs1")]
    B_h2Tp2 = [Buf("h2Tp0"), Buf("h2Tp1")]
    B_IT2 = [Buf("IT0"), Buf("IT1")]
    B_ITf1 = Buf("ITf")
    B_ITf2 = [B_ITf1, B_ITf1]
    B_eq = [Buf("eqb0")]
    B_eqh = [Buf("eqh0"), Buf("eqh1")]
    V(lambda e: e.tensor_copy(out=iotab[:, :], in_=iota128[:, :]), [B_iota], [B_iota])
    B_Lb = [Buf("Lb0"), Buf("Lb1")]
    B_Rb = [Buf("Rb0"), Buf("Rb1")]
    NS = 4
    uTc = [ar.alloc([128, 1024], BF16, "uTc%d" % i) for i in range(NS)]
    vcb = [ar.alloc([128, 1024], BF16, "vcb%d" % i) for i in range(NS)]
    B_uTc = [Buf("uTc%d" % i) for i in range(NS)]
    B_vcb = [Buf("vcb%d" % i) for i in range(NS)]
    gl = [ar.alloc([128, TP], BF16, "gl%d" % i) for i in range(3)]
    AT = [ar.alloc([128, TP], BF16, "AT%d" % i) for i in range(3)]
    B_gl = [Buf("gl0"), Buf("gl1"), Buf("gl2")]
    B_AT = [Buf("AT0"), Buf("AT1"), Buf("AT2")]
    x1t = ar.alloc([128, D], F32, "x1t")
    oo = ar.alloc([128, D], F32, "ooE")
    jk = oo
    st = ar.alloc([128, 8], F32, "stE")
    B_x1t, B_oo, B_st = Buf("x1t"), Buf("ooE"), Buf("stE")
    B_jk = B_oo
    out_v = L["out_d"].rearrange("(n p) m -> p n m", p=128)
    iota3 = iotab[:, :].unsqueeze(1).to_broadcast([128, NBT, 128])
    npass = L["nblk"] * 2
    NBATCH = TP // NBT

    def emit_loads(p_):
        q = p_ % 2
        tsl = slice(p_ * TP, (p_ + 1) * TP)
        for kc in range(8):
            sc.dma("sp", h2Tp2[q][:, kc, :], L["h2T_d"][kc * 128:(kc + 1) * 128, tsl], writes=[B_h2Tp2[q]])
        for q3 in range(3):
            sc.dma("sp", IT2[q][:, q3, :], L["selT_d"][q3, :, tsl], writes=[B_ITf2[q]])
        A(lambda e: e.activation(out=ITb2[q][:, :, :], in_=IT2[q][:, :, :], func=AF.Copy), [B_ITf2[q]], [B_IT2[q]])

    gcount = [0]

    def gbatch_gen(p_, b):
        q = p_ % 2
        Gs, B_Gs, ITb, B_IT = Gs2[q], B_Gs2[q], ITb2[q], B_IT2[q]
        gi = gcount[0]
        gcount[0] += 1
        Lk, bLk = Lb[gi % 2], B_Lb[gi % 2]
        Rk, bRk = Rb[gi % 2], B_Rb[gi % 2]
        eq_ = eqb[0]
        H = NBT // 2
        for hf in range(2):
            hs_ = slice(hf * H, (hf + 1) * H)
            bs_ = slice(b * NBT + hf * H, b * NBT + (hf + 1) * H)
            io_ = iotab[:, :].unsqueeze(1).to_broadcast([128, H, 128])
            V(lambda e: e.tensor_tensor(out=eq_[:, hs_, :], in0=io_, in1=ITb[:, 0, bs_].unsqueeze(2).to_broadcast([128, H, 128]), op=ALU.is_equal), [B_IT, B_iota], [B_eqh[hf]])
            yield
            V(lambda e: e.tensor_tensor(out=Lk[:, hs_, :], in0=eq_[:, hs_, :], in1=ITb[:, 2, bs_].unsqueeze(2).to_broadcast([128, H, 128]), op=ALU.mult), [B_eqh[hf], B_IT], [bLk])
            yield
            V(lambda e: e.tensor_tensor(out=Rk[:, hs_, :], in0=io_, in1=ITb[:, 1, bs_].unsqueeze(2).to_broadcast([128, H, 128]), op=ALU.is_equal), [B_IT, B_iota], [bRk])
            yield
        yield
        yield
        for t4 in range(NBT // 4):
            pg, bpg = ps[7], B_ps[7]
            for tt in range(4):
                t = t4 * 4 + tt
                P(lambda e: e.matmul(pg[:, tt * 128:(tt + 1) * 128], lhsT=Lk[:, t, :], rhs=Rk[:, t, :], start=True, stop=True), [bLk, bRk], [bpg])
            t0 = b * NBT + t4 * 4
            src = pg[:, :].rearrange("p (t i) -> p i t", i=128)
            A(lambda e: e.activation(out=Gs[:, :, t0:t0 + 4], in_=src, func=AF.Copy), [bpg], [B_Gs])
            yield

    def gall_gen(p_):
        for b in range(NBATCH):
            for _ in gbatch_gen(p_, b):
                yield

    emit_loads(0)
    for _ in gall_gen(0):
        pass
    for p_ in range(npass):
        q = p_ % 2
        Gs, B_Gs, h2Tp, B_h2Tp = Gs2[q], B_Gs2[q], h2Tp2[q], B_h2Tp2[q]
        if p_ + 1 < npass:
            emit_loads(p_ + 1)

        def load_u(c):
            sc.dma("sp", uTc[c % NS][:, :], L["uv_d"][c, :, 0:1024], writes=[B_uTc[c % NS]])

        def emit_hu(c):
            k = c % NS
            if c == 0:
                load_u(0)
                load_u(1)
            if c + 2 < 128:
                load_u(c + 2)
            sc.dma("sp", vcb[k][:, :], L["uv_d"][c, :, 1024:2048], writes=[B_vcb[k]])
            pH, bH = ps[4 + c % 3], B_ps[4 + c % 3]
            for kc in range(8):
                P(lambda e: e.matmul(pH[:, 0:TP], lhsT=uTc[k][:, kc * 128:(kc + 1) * 128], rhs=h2Tp[:, kc, :], start=(kc == 0), stop=(kc == 7)), [B_uTc[k], B_h2Tp], [bH])

        def emit_y2(c):
            k = c % NS
            pH, bH = ps[4 + c % 3], B_ps[4 + c % 3]
            g_, bg_ = gl[c % 3], B_gl[c % 3]
            a_, ba_ = AT[c % 3], B_AT[c % 3]
            A(lambda e: e.activation(out=g_[:, :], in_=pH[:, 0:TP], func=AF.Gelu), [bH], [bg_])
            V(lambda e: e.tensor_tensor(out=a_[:, :], in0=g_[:, :], in1=Gs[:, c, :], op=ALU.mult), [bg_, B_Gs], [ba_])
            for tt in range(2):
                for half in range(2):
                    py, bpy = ps[tt * 2 + half], B_ps[tt * 2 + half]
                    P(lambda e: e.matmul(py[:, :], lhsT=a_[:, tt * 128:(tt + 1) * 128], rhs=vcb[k][:, half * 512:(half + 1) * 512], start=(c == 0), stop=(c == 127)),
                      [ba_, B_vcb[k]], [bpy])
        emit_hu(0)
        emit_hu(1)
        gg = gall_gen(p_ + 1) if p_ + 1 < npass else None
        steps_per_chunk = (NBATCH * 10 + 127) // 128
        for c in range(128):
            if c + 2 < 128:
                emit_hu(c + 2)
            emit_y2(c)
            if gg is not None:
                for _ in range(steps_per_chunk):
                    next(gg, None)
        if gg is not None:
            for _ in gg:
                pass
        for tt in range(2):
            n = p_ * 2 + tt
            sc.dma("sp", x1t[:, :], L["x1s_d"][n * 128:(n + 1) * 128, :], writes=[B_x1t])
            for half in range(2):
                hsl = slice(half * 512, (half + 1) * 512)
                py, bpy = ps[tt * 2 + half], B_ps[tt * 2 + half]
                if "y2dbg" in dbg:
                    V(lambda e: e.tensor_copy(out=oo[:, hsl], in_=py[:, :]), [bpy], [B_oo])
                    sc.dma("sp", L["y2_d"][n * 128:(n + 1) * 128, hsl], oo[:, hsl], reads=[B_oo])
                V(lambda e: e.tensor_tensor(out=oo[:, hsl], in0=py[:, :], in1=gt_bc[:, 3, hsl], op=ALU.mult), [bpy, B_gt], [B_oo])
            V(lambda e: e.scalar_tensor_tensor(out=x1t[:, :], in0=x1t[:, :], scalar=ALPHA, in1=oo[:, :], op0=ALU.mult, op1=ALU.add), [B_x1t, B_oo], [B_x1t])
            A(lambda e: e.activation(out=oo[:, :], in_=x1t[:, :], func=AF.Copy, accum_out=st[:, 0:1]), [B_x1t], [B_st, B_oo])
            A(lambda e: e.activation(out=oo[:, :], in_=x1t[:, :], func=AF.Square, accum_out=st[:, 1:2]), [B_x1t], [B_st, B_oo])
            V(lambda e: e.tensor_scalar(out=st[:, 2:3], in0=st[:, 0:1], scalar1=1.0 / D, scalar2=None, op0=ALU.mult), [B_st], [B_st])
            V(lambda e: e.tensor_tensor(out=st[:, 3:4], in0=st[:, 2:3], in1=st[:, 2:3], op=ALU.mult), [B_st], [B_st])
            V(lambda e: e.scalar_tensor_tensor(out=st[:, 4:5], in0=st[:, 1:2], scalar=1.0 / D, in1=st[:, 3:4], op0=ALU.mult, op1=ALU.subtract), [B_st], [B_st])
            V(lambda e: e.tensor_scalar(out=st[:, 4:5], in0=st[:, 4:5], scalar1=1e-5, scalar2=None, op0=ALU.add), [B_st], [B_st])
            A(lambda e: e.activation(out=st[:, 5:6], in_=st[:, 4:5], func=AF.Sqrt), [B_st], [B_st])
            V(lambda e: e.reciprocal(out=st[:, 5:6], in_=st[:, 5:6]), [B_st], [B_st])
            V(lambda e: e.scalar_tensor_tensor(out=st[:, 6:7], in0=st[:, 2:3], scalar=-1.0, in1=st[:, 5:6], op0=ALU.mult, op1=ALU.mult), [B_st], [B_st])
            A(lambda e: e.activation(out=oo[:, :], in_=x1t[:, :], func=AF.Identity, scale=st[:, 5:6], bias=st[:, 6:7]), [B_x1t, B_st], [B_oo])
            G(lambda e: e.tensor_tensor(out=oo[:, :], in0=oo[:, :], in1=lnbc[:, 0, :], op=ALU.mult), [B_oo, B_ln], [B_oo])
            G(lambda e: e.tensor_tensor(out=oo[:, :], in0=oo[:, :], in1=lnbc[:, 1, :], op=ALU.add), [B_oo, B_ln], [B_oo])
            sc.dma("sp", out_v[:, n, :], oo[:, :], reads=[B_oo])
    ar.release(mE)


def make_e0(L, NB, bank):
    sc, ar, ps, B_ps = L["sc"], L["ar"], L["ps"], L["B_ps"]
    identb, B_ident = L["identb"], L["B_ident"]
    A = lambda fn, r=(), w=(): sc.op("act", fn, r, w)
    P = lambda fn, r=(), w=(): sc.op("pe", fn, r, w)
    G = lambda fn, r=(), w=(): sc.op("pool", fn, r, w)
    NF = 3
    stf = [ar.alloc([128, D], F32, "e0f%d" % i) for i in range(NF)]
    o16 = [ar.alloc([128, D], BF16, "e0h%d" % i) for i in range(NF)]
    uT = [ar.alloc([128, D], BF16, "e0t%d" % i) for i in range(2)]
    B_f = [Buf("e0f%d" % i) for i in range(NF)]
    B_o = [Buf("e0h%d" % i) for i in range(NF)]
    B_t = [Buf("e0t0"), Buf("e0t1")]
    pu_v = L["pu_d"].rearrange("(i1 i2) d -> i2 i1 d", i2=128)
    pv_v = L["pv_d"].rearrange("(i1 i2) d -> i2 i1 d", i2=128)
    NI = 256

    def load(i):
        c, isv = i // 2, i % 2
        sc.dma("sp", stf[i % NF][:, :], (pv_v if isv else pu_v)[c, :, :], writes=[B_f[i % NF]])

    def cast(i):
        G(lambda e: e.tensor_copy(out=o16[i % NF][:, :], in_=stf[i % NF][:, :]), [B_f[i % NF]], [B_o[i % NF]])

    def finish(i):
        c, isv = i // 2, i % 2
        if isv:
            sc.dma("sp", L["uv_d"][c, :, 1024:2048], o16[i % NF][:, :], reads=[B_o[i % NF]])
        else:
            pT = ps[bank][:, :].bitcast(BF16)
            for kc in range(8):
                P(lambda e: e.transpose(pT[:, kc * 128:(kc + 1) * 128], o16[i % NF][:, kc * 128:(kc + 1) * 128], identb[:, :]), [B_o[i % NF], B_ident], [B_ps[bank]])
            A(lambda e: e.activation(out=uT[c % 2][:, :], in_=pT[:, :], func=AF.Copy), [B_ps[bank]], [B_t[c % 2]])
            sc.dma("sp", L["uv_d"][c, :, 0:1024], uT[c % 2][:, :], reads=[B_t[c % 2]])
    load(0)
    load(1)
    cast(0)
    for k in range(NI):
        if k + 2 < NI:
            load(k + 2)
        if k + 1 < NI:
            cast(k + 1)
        finish(k)
        yield
```

```python
import numpy as np
import ml_dtypes
import concourse.bass as bass
import concourse.mybir as mybir
from concourse.bass_utils import run_bass_kernel_spmd

F32 = mybir.dt.float32
BF16 = mybir.dt.bfloat16
I32 = mybir.dt.int32
U32 = mybir.dt.uint32
AF = mybir.ActivationFunctionType
ALU = mybir.AluOpType
AX = mybir.AxisListType

S = 4096
D = 1024
NT = S // 128
IN_COLS = 5316
DBG = {}
STOP_AFTER = None


class Buf:
    __slots__ = ("name", "lw", "rd")

    def __init__(self, name):
        self.name = name
        self.lw = None
        self.rd = []


class _Rec:
    def __init__(self):
        self.call = None

    def __getattr__(self, name):
        def f(*args, **kwargs):
            self.call = (name, args, kwargs)
            return self
        return f


class Sched:
    ENGS = ("pe", "act", "dve", "pool", "sp")

    def __init__(self, nc, n_dma_sems=40):
        self.nc = nc
        self.ops = {e: [] for e in self.ENGS}
        self.sem = {e: nc.alloc_semaphore("c_" + e) for e in self.ENGS}
        self.cnt = {e: 0 for e in self.ENGS}
        self.seen = {e: {} for e in self.ENGS}
        self.dsem = [nc.alloc_semaphore("d%d" % i) for i in range(n_dma_sems)]
        self.dval = [0] * n_dma_sems
        self.drr = 0
        self.drr_sw = 0
        self.NSW = 8
        self.NHW = n_dma_sems - 8
        self.all_events = []

    def _waits(self, eng, reads, writes):
        deps = []
        for b in reads:
            if b.lw is not None:
                deps.append(b.lw)
        for b in writes:
            if b.lw is not None:
                deps.append(b.lw)
            deps.extend(b.rd)
        out = {}
        for (sem, val, src) in deps:
            if src == "pe" and eng == "pe":
                continue
            k = sem.num
            if self.seen[eng].get(k, 0) >= val:
                continue
            if out.get(k, (None, 0))[1] < val:
                out[k] = (sem, val)
        for k, (sem, val) in out.items():
            self.seen[eng][k] = val
        return list(out.values())

    def op(self, eng, fn, reads=(), writes=()):
        waits = self._waits(eng, reads, writes)
        self.cnt[eng] += 1
        sem = self.sem[eng]
        val = self.cnt[eng]
        ev = (sem, val, eng)

        rec = _Rec()
        fn(rec)
        name, args, kwargs = rec.call

        def emit(e, waits=waits, sem=sem, name=name, args=args, kwargs=kwargs):
            for (s, v) in waits:
                e.wait_ge(s, v)
            getattr(e, name)(*args, **kwargs).then_inc(sem, 1)
        self.ops[eng].append(emit)
        for b in reads:
            b.rd.append(ev)
        for b in writes:
            b.lw = ev
            b.rd = []
        return ev

    def dma(self, eng, out, in_, reads=(), writes=(), fn=None, **kw):
        if fn is not None:
            rec = _Rec()
            fn(rec)
            mname, margs, mkw = rec.call
        else:
            mname, margs, mkw = "dma_start", (), dict(out=out, in_=in_, **kw)
        if eng == "pool":
            i = self.NHW + (self.drr_sw % self.NSW)
            self.drr_sw += 1
        else:
            i = self.drr
            self.drr = (self.drr + 1) % self.NHW
        sem = self.dsem[i]
        waits = self._waits(eng, reads, writes)
        prev = self.dval[i]
        if prev > 0 and self.seen[eng].get(sem.num, 0) < prev:
            waits.append((sem, prev))
            self.seen[eng][sem.num] = prev
        self.dval[i] += 16
        val = self.dval[i]
        ev = (sem, val, "dma")

        def emit(e, waits=waits, sem=sem, mname=mname, margs=margs, mkw=mkw):
            for (s, v) in waits:
                e.wait_ge(s, v)
            getattr(e, mname)(*margs, **mkw).then_inc(sem, 16)
        self.ops[eng].append(emit)
        for b in reads:
            b.rd.append(ev)
        for b in writes:
            b.lw = ev
            b.rd = []
        self.all_events.append(ev)
        return ev

    def raw(self, eng, fn):
        self.ops[eng].append(fn)

    def barrier(self):
        targets = []
        for en in self.ENGS:
            if self.cnt[en] > 0:
                targets.append((self.sem[en], self.cnt[en], en))
        for i, s in enumerate(self.dsem):
            if self.dval[i] > 0:
                targets.append((s, self.dval[i], "dma"))
        for eng in self.ENGS:
            waits = []
            for (s, v, src) in targets:
                if src == eng:
                    continue
                if self.seen[eng].get(s.num, 0) >= v:
                    continue
                self.seen[eng][s.num] = v
                waits.append((s, v))

            def emit(e, waits=waits):
                for (s, v) in waits:
                    e.wait_ge(s, v)
            self.ops[eng].append(emit)

    def finish(self, final_events):
        nc = self.nc
        with nc.Block() as block:
            def run(name):
                def f(e):
                    for emit in self.ops[name]:
                        emit(e)
                    if name == "sp":
                        for (s, v, _) in final_events:
                            e.wait_ge(s, v)
                        for i, s in enumerate(self.dsem):
                            if self.dval[i] > 0:
                                e.wait_ge(s, self.dval[i])
                        for en in ("pe", "act", "dve", "pool"):
                            if self.cnt[en] > 0:
                                e.wait_ge(self.sem[en], self.cnt[en])
                return f
            block.tensor(run("pe"))
            block.scalar(run("act"))
            block.vector(run("dve"))
            block.gpsimd(run("pool"))
            block.sync(run("sp"))


class Arena:
    def __init__(self, nc, base=0, top=192 * 1024):
        self.nc = nc
        self.off = base
        self.top = top
        self.n = 0
        self.sc = None

    def mark(self):
        return self.off

    def release(self, m):
        self.off = m
        if self.sc is not None:
            self.sc.barrier()

    def alloc(self, shape, dtype, name="t"):
        esz = {F32: 4, BF16: 2, I32: 4, U32: 4}[dtype]
        per = esz
        for s in shape[1:]:
            per *= s
        per = (per + 63) // 64 * 64
        self.n += 1
        t = self.nc.alloc_sbuf_tensor_at("%s_%d" % (name, self.n), list(shape), dtype, offset=self.off)
        self.off += per
        assert self.off <= self.top, ("SBUF overflow", name, self.off)
        return t


def build(dbg=None, stop_after=None, phases=None, feed=(), nblk=8):
    dbg = dbg or {}
    phases = phases or {'0', 'A', 'B', 'C', 'D', 'E', 'F'}
    nc = bass.Bass("TRN2", target_bir_lowering=False)
    sc = Sched(nc)
    ar = Arena(nc, base=(nc.sbuf_base + 63) // 64 * 64, top=nc.sbuf_top // 64 * 64)
    ar.sc = sc

    def din(name, shape, dt=F32):
        return nc.dram_tensor(name, list(shape), dt, kind="ExternalInput").ap()

    def dscratch(name, shape, dt=F32):
        kind = "ExternalOutput" if name in dbg else ("ExternalInput" if name in feed else "Internal")
        return nc.dram_tensor(name, list(shape), dt, kind=kind).ap()

    x_d = din("x", [S, D])
    c_d = din("c_col", [128, 8])
    wada_d = din("w_ada", [D, 6 * D])
    bada_col_d = din("b_ada_col", [128, 48])
    bada_bc_d = din("b_ada_bc", [128, 6 * D])
    win_d = din("w_in", [D, IN_COLS])
    ident_d = din("ident", [128, 128])
    out_d = nc.dram_tensor("out", [S, D], F32, kind="ExternalOutput").ap()

    mu_d = din("mu_col", [128, 14])
    rwvec_d = din("rwvec", [128, 20])
    w2a2_d = din("w2a2", [128, 512])
    g2_d = din("g2", [128, 512])
    gnbc_d = din("gn_bc", [128, 2, 256])
    cst_d = din("cst", [128, 1024])
    wbra_d = din("w_br_a", [512, D])
    wbrb_d = din("w_br_b", [D, D])
    wout_d = din("w_out", [D, D])
    lnbc_d = din("ln_bc", [128, 4, D])
    wq_d = din("peer_wq", [D, D])
    pkeys_d = din("peer_keysT", [128, 8, 128])
    pu_d = din("peer_u", [16384, D])
    pv_d = din("peer_v", [16384, D])
    cvec_d = din("cvec", [128, 256])
    biasT_d = din("biasT", [128, 3, 1024])
    negm_d = din("negm", [128, 128])
    zrw_d = dscratch("zrw", [1792, S], F32)
    zq_d = dscratch("zq", [1024, S], BF16)
    zqi_d = dscratch("zqi", [256, S], BF16)
    zg_d = dscratch("zg", [2048, S], BF16)
    ztok_d = dscratch("ztok", [S, 196], F32)
    rwo_d = dscratch("rwo", [512, S], BF16)
    dsao_d = dscratch("dsao", [1024, S], BF16)
    x1_d = dscratch("x1dbg", [S, D], F32)
    y2_d = dscratch("y2dbg", [S, D], F32)
    x1s_d = dscratch("x1s", [S, D], F32)
    h2T_d = dscratch("h2T", [D, S], BF16)
    selT_d = dscratch("selT", [3, 128, S], F32)
    uv_d = dscratch("uv16", [128, 128, 2048], BF16)
    iota_d = din("iota128", [128, 128])
    iota128 = ar.alloc([128, 128], F32, "iota128")
    B_iota = Buf("iota")
    iota16 = iota128[:, 0:16]

    ident = ar.alloc([128, 128], F32, "ident")
    identb = ar.alloc([128, 128], BF16, "identb")
    modcol = ar.alloc([128, 48], F32, "modcol")
    onep1 = ar.alloc([128, 8], F32, "onep1")
    onep2 = ar.alloc([128, 8], F32, "onep2")
    gt_bc = ar.alloc([128, 4, D], F32, "gt_bc")
    B_ident = Buf("ident")
    B_mod = Buf("mod")
    B_gt = Buf("gt")

    ps = [nc.alloc_psum_tensor("ps%d" % i, [128, 512], F32) for i in range(8)]
    B_ps = [Buf("ps%d" % i) for i in range(8)]

    sc.dma("sp", ident[:, :], ident_d[:, :], writes=[B_ident])
    sc.dma("sp", iota128[:, :], iota_d[:, :], writes=[B_iota])

    mark_stg = ar.mark()
    STG = 1024
    stg = [ar.alloc([128, STG], F32, "stg%d" % i) for i in range(3)]
    B_stg = [Buf("stg%d" % i) for i in range(3)]
    stg_i = [0]

    def load_bf16(dst, src, n, bdst, eng="pool"):
        P = dst.shape[0]
        for o in range(0, n, STG):
            w = min(STG, n - o)
            k = stg_i[0] % 3
            stg_i[0] += 1
            sc.dma("sp", stg[k][0:P, 0:w], src[:, o:o + w], writes=[B_stg[k]])
            sc.op(eng, lambda e, k=k, o=o, w=w, P=P, dst=dst: e.tensor_copy(out=dst[:, o:o + w], in_=stg[k][0:P, 0:w]),
                  reads=[B_stg[k]], writes=[bdst])
    sc.op("dve", lambda e: e.tensor_copy(out=identb[:, :], in_=ident[:, :]), reads=[B_ident], writes=[B_ident])

    if '0' in phases:
        m0 = ar.mark()
        c_sb = ar.alloc([128, 8], F32, "c_sb")
        sil = ar.alloc([128, 8], F32, "sil")
        silbc = ar.alloc([128, 8, 128], F32, "silbc")
        bcol = ar.alloc([128, 48], F32, "bcol")
        bbc = ar.alloc([128, 4, D], F32, "bbc")
        wa = [ar.alloc([128, 8, 1024], F32, "wa%d" % i) for i in range(4)]
        B_c = Buf("c")
        B_wa = [Buf("wa0"), Buf("wa1"), Buf("wa2"), Buf("wa3")]
        B_b = Buf("bcol")
        sc.dma("sp", c_sb[:, :], c_d[:, :], writes=[B_c])
        sc.dma("sp", bcol[:, :], bada_col_d[:, :], writes=[B_b])
        for gi_, g_ in enumerate((2, 3, 4, 5)):
            sc.dma("sp", bbc[:, gi_, :], bada_bc_d[:, g_ * D:(g_ + 1) * D], writes=[B_b])
        sc.op("act", lambda e: e.activation(out=sil[:, :], in_=c_sb[:, :], func=AF.Silu), reads=[B_c], writes=[B_c])
        for kc in range(8):
            sc.op("dve", lambda e, kc=kc: e.tensor_copy(out=silbc[:, kc, :], in_=sil[:, kc:kc + 1].to_broadcast([128, 128])),
                  reads=[B_c], writes=[B_c])
        wada_v = wada_d.rearrange("(kc p) n -> p kc n", p=128)
        for g in range(6):
            w = wa[g % 4]
            bw = B_wa[g % 4]
            for kc in range(8):
                sc.dma("sp", w[:, kc, :], wada_v[:, kc, g * 1024:(g + 1) * 1024], writes=[bw])
            if g in (2, 3, 4, 5):
                gi = g - 2
                for half in range(2):
                    p = ps[half]
                    for kc in range(8):
                        sc.op("pe", lambda e, p=p, w=w, kc=kc, half=half: e.matmul(
                            p[:, :], lhsT=silbc[:, kc, :], rhs=w[:, kc, half * 512:(half + 1) * 512],
                            start=(kc == 0), stop=(kc == 7)), reads=[bw, B_c], writes=[B_ps[half]])
                    sc.op("dve", lambda e, p=p, gi=gi, half=half: e.tensor_tensor(
                        out=gt_bc[:, gi, half * 512:(half + 1) * 512], in0=p[:, :],
                        in1=bbc[:, gi, half * 512:(half + 1) * 512], op=ALU.add),
                        reads=[B_ps[half], B_b], writes=[B_gt])
            if g in (0, 1, 3, 4):
                p = ps[2]
                for fc in range(8):
                    for kc in range(8):
                        sc.op("pe", lambda e, p=p, w=w, kc=kc, fc=fc: e.matmul(
                            p[:, fc:fc + 1], lhsT=w[:, kc, fc * 128:(fc + 1) * 128], rhs=sil[:, kc:kc + 1],
                            start=(kc == 0), stop=(kc == 7)), reads=[bw, B_c], writes=[B_ps[2]])
                sc.op("dve", lambda e, p=p, g=g: e.tensor_tensor(
                    out=modcol[:, g * 8:(g + 1) * 8], in0=p[:, 0:8], in1=bcol[:, g * 8:(g + 1) * 8], op=ALU.add),
                    reads=[B_ps[2], B_b], writes=[B_mod])
        sc.op("dve", lambda e: e.tensor_scalar(out=onep1[:, :], in0=modcol[:, 8:16], scalar1=1.0, scalar2=None, op0=ALU.add),
              reads=[B_mod], writes=[B_mod])
        sc.op("dve", lambda e: e.tensor_scalar(out=onep2[:, :], in0=modcol[:, 32:40], scalar1=1.0, scalar2=None, op0=ALU.add),
              reads=[B_mod], writes=[B_mod])
        sc.op("dve", lambda e: e.tensor_scalar(out=gt_bc[:, 2, :], in0=gt_bc[:, 2, :], scalar1=1.0, scalar2=None, op0=ALU.add),
              reads=[B_gt], writes=[B_gt])
        ar.release(m0)
        if "modcol" in dbg:
            dd = nc.dram_tensor("modcol_o", [128, 48], F32, kind="ExternalOutput").ap()
            sc.dma("sp", dd[:, :], modcol[:, :], reads=[B_mod])
            dd2 = nc.dram_tensor("gt_o", [128, 4 * D], F32, kind="ExternalOutput").ap()
            sc.dma("sp", dd2[:, :], gt_bc[:, :, :].rearrange("p a b -> p (a b)"), reads=[B_gt])

    if 'A' in phases:
        mA = ar.mark()
        fm_chunks = []
        for i in range(14):
            fm_chunks.append((i * 128, "rw", i))
        for i in range(8):
            fm_chunks.append((1792 + i * 128, "q", i))
        for i in range(2):
            fm_chunks.append((2944 + i * 128, "qi", i))
        for i in range(16):
            fm_chunks.append((3268 + i * 128, "g", i))
        winb = ar.alloc([128, 8, IN_COLS], BF16, "winb")
        B_win = Buf("win")
        win_v = win_d.rearrange("(kc p) n -> p kc n", p=128)
        for kc in range(8):
            load_bf16(winb[:, kc, :], win_v[:, kc, :], IN_COLS, B_win)
        xt = [ar.alloc([128, 4, D], F32, "xt%d" % i) for i in range(2)]
        B_xt = [Buf("xt0"), Buf("xt1")]
        hT = [ar.alloc([128, 8, 512], BF16, "hT%d" % i) for i in range(2)]
        B_hT = [Buf("hT0"), Buf("hT1")]
        NEV = 8
        ev32 = [ar.alloc([128, 512], F32, "ev32_%d" % i) for i in range(NEV)]
        ev16 = [ar.alloc([128, 512], BF16, "ev16_%d" % i) for i in range(NEV)]
        B_ev32 = [Buf("ev32_%d" % i) for i in range(NEV)]
        B_ev16 = [Buf("ev16_%d" % i) for i in range(NEV)]
        ztk = [ar.alloc([128, 196], F32, "ztk%d" % i) for i in range(2)]
        B_ztk = [Buf("ztk0"), Buf("ztk1")]
        x_v = x_d.rearrange("(n p) m -> p n m", p=128)
        pi = 0
        evi = 0
        tok_cols = [(2816, 128, 0), (3200, 68, 128)]
        for tb in range(8):
            xb = xt[tb % 2]
            bx = B_xt[tb % 2]
            hb = hT[tb % 2]
            bh = B_hT[tb % 2]
            for j in range(4):
                sc.dma("sp", xb[:, j, :], x_v[:, tb * 4 + j, :], writes=[bx])
            for kc in range(8):
                p = ps[pi % 8]
                bp = B_ps[pi % 8]
                pi += 1
                for j in range(4):
                    sc.op("pe", lambda e, p=p, xb=xb, j=j, kc=kc: e.transpose(
                        p[:, j * 128:(j + 1) * 128], xb[:, j, kc * 128:(kc + 1) * 128], ident[:, :]),
                        reads=[bx, B_ident], writes=[bp])
                sc.op("act", lambda e, p=p, hb=hb, kc=kc: e.activation(
                    out=hb[:, kc, :], in_=p[:, :], func=AF.Identity,
                    scale=onep1[:, kc:kc + 1], bias=modcol[:, kc:kc + 1]),
                    reads=[bp, B_mod], writes=[bh])
            for ci, (col0, kind, idx) in enumerate(fm_chunks):
                p = ps[pi % 8]
                bp = B_ps[pi % 8]
                pi += 1
                for kc in range(8):
                    sc.op("pe", lambda e, p=p, kc=kc, col0=col0, hb=hb: e.matmul(
                        p[:, :], lhsT=winb[:, kc, col0:col0 + 128], rhs=hb[:, kc, :],
                        start=(kc == 0), stop=(kc == 7)), reads=[B_win, bh], writes=[bp])
                k = evi % NEV
                evi += 1
                eng = "dve" if (ci % 2 == 0) else "act"
                tsl = slice(tb * 512, (tb + 1) * 512)
                if kind == "rw":
                    dst = ev32[k]
                    bd = B_ev32[k]
                    if eng == "dve":
                        sc.op("dve", lambda e, p=p, dst=dst: e.tensor_copy(out=dst[:, :], in_=p[:, :]), reads=[bp], writes=[bd])
                    else:
                        sc.op("act", lambda e, p=p, dst=dst: e.activation(out=dst[:, :], in_=p[:, :], func=AF.Copy), reads=[bp], writes=[bd])
                    sc.dma("sp", zrw_d[idx * 128:(idx + 1) * 128, tsl], dst[:, :], reads=[bd])
                elif kind in ("q", "qi"):
                    dst = ev16[k]
                    bd = B_ev16[k]
                    if eng == "dve":
                        sc.op("dve", lambda e, p=p, dst=dst: e.tensor_copy(out=dst[:, :], in_=p[:, :]), reads=[bp], writes=[bd])
                    else:
                        sc.op("act", lambda e, p=p, dst=dst: e.activation(out=dst[:, :], in_=p[:, :], func=AF.Copy), reads=[bp], writes=[bd])
                    dd = zq_d if kind == "q" else zqi_d
                    sc.dma("sp", dd[idx * 128:(idx + 1) * 128, tsl], dst[:, :], reads=[bd])
                else:
                    dst = ev16[k]
                    bd = B_ev16[k]
                    sc.op("act", lambda e, p=p, dst=dst: e.activation(out=dst[:, :], in_=p[:, :], func=AF.Sigmoid), reads=[bp], writes=[bd])
                    sc.dma("sp", zg_d[idx * 128:(idx + 1) * 128, tsl], dst[:, :], reads=[bd])
            for j in range(4):
                p = ps[pi % 8]
                bp = B_ps[pi % 8]
                pi += 1
                for (c0, ncol, o0) in tok_cols:
                    for kc in range(8):
                        sc.op("pe", lambda e, p=p, kc=kc, j=j, c0=c0, ncol=ncol, o0=o0, hb=hb: e.matmul(
                            p[:, o0:o0 + ncol], lhsT=hb[:, kc, j * 128:(j + 1) * 128], rhs=winb[:, kc, c0:c0 + ncol],
                            start=(kc == 0), stop=(kc == 7)), reads=[B_win, bh], writes=[bp])
                zt = ztk[j % 2]
                bz = B_ztk[j % 2]
                sc.op("dve", lambda e, p=p, zt=zt: e.tensor_copy(out=zt[:, :], in_=p[:, 0:196]), reads=[bp], writes=[bz])
                r0 = (tb * 4 + j) * 128
                sc.dma("sp", ztok_d[r0:r0 + 128, :], zt[:, :], reads=[bz])
        ar.release(mA)

    e0_done_flag = [False]
    if 'B' in phases:
        phase_B(locals())
    if 'C' in phases:
        phase_C(locals())
    if 'D' in phases:
        phase_D(locals())
    if 'E' in phases:
        phase_E(locals())

    sc.finish([])
    return nc


def _prep_inputs(inputs):
    f = lambda a: np.ascontiguousarray(np.asarray(a, dtype=np.float32))
    x = f(inputs["x"])
    c = f(inputs["c"])
    b_ada = f(inputs["b_ada"])[0]
    shared = {
        "w_ada": f(inputs["w_ada"])[0],
        "b_ada_col": np.ascontiguousarray(b_ada.reshape(48, 128).T),
        "b_ada_bc": np.ascontiguousarray(np.broadcast_to(b_ada[None, :], (128, 6 * D))),
        "w_in": f(inputs["w_in"])[0],
        "ident": np.eye(128, dtype=np.float32),
        "iota128": np.ascontiguousarray(np.broadcast_to(np.arange(128, dtype=np.float32)[None], (128, 128))),
    }
    col = lambda v, n: np.ascontiguousarray(f(v).reshape(n, 128).T)
    shared["mu_col"] = col(inputs["rw_mu"][0], 14)
    shared["rwvec"] = np.ascontiguousarray(np.concatenate([col(inputs["rw_w0"][0], 4), col(inputs["rw_a0"][0], 4), col(inputs["rw_k_k"][0], 4),
                                                            col(inputs["rw_k_a"][0], 4), col(f(inputs["rw_r_k"])[0].reshape(-1), 4)], axis=1))
    shared["w2a2"] = np.ascontiguousarray(np.concatenate([f(inputs["rw_w2"])[0], f(inputs["rw_a2"])[0]], axis=0))
    shared["g2"] = f(inputs["rw_g2"])[0]
    gn2 = np.stack([f(inputs["rw_gn_g"])[0], f(inputs["rw_gn_b"])[0]]).reshape(2, 4, 2, 64)
    gnl = np.zeros((128, 2, 4, 64), np.float32)
    gnl[0:64] = gn2[:, :, 0, :][None]
    gnl[64:128] = gn2[:, :, 1, :][None]
    shared["gn_bc"] = np.ascontiguousarray(gnl.reshape(128, 2, 256))
    cst = np.zeros((128, 1024), np.float32)
    iu = np.triu(np.ones((64, 64), np.float32), 1)
    il = np.triu(np.ones((64, 64), np.float32), 0)
    cst[:, 0:128] = np.block([[iu, il], [iu, il]])
    cst[0:64, 128:192] = iu.T
    cst[64:128, 128:192] = iu.T
    cst[0:64, 192:256] = 1.0
    cst[64:128, 256:320] = 1.0
    sm = np.ones(512, np.float32); sm[::64] = 0.0
    cst[:, 320:832] = sm[None, :]
    cst[0:64, 832:896] = np.eye(64, dtype=np.float32)
    cst[64:128, 832:896] = np.eye(64, dtype=np.float32)
    cst[:, 896] = 1.0
    shared["cst"] = cst
    shared["w_br_a"] = f(inputs["w_br_a"])[0]
    shared["w_br_b"] = f(inputs["w_br_b"])[0]
    shared["w_out"] = f(inputs["w_out"])[0]
    lnr = np.stack([f(inputs["ln1_g"])[0], f(inputs["ln1_b"])[0], f(inputs["ln2_g"])[0], f(inputs["ln2_b"])[0]])
    shared["ln_bc"] = np.ascontiguousarray(np.broadcast_to(lnr[None], (128, 4, D)))
    shared["peer_wq"] = f(inputs["peer_wq"])[0]
    pk = f(inputs["peer_keys"])[0]
    shared["peer_keysT"] = np.ascontiguousarray(pk.transpose(1, 3, 0, 2).reshape(128, 8, 128))
    shared["peer_u"] = f(inputs["peer_u"])[0]
    cv = np.concatenate([f(inputs["dsa_kv_g"])[0], f(inputs["idx_k_g"])[0], f(inputs["idx_k_b"])[0]])
    shared["cvec"] = np.ascontiguousarray(np.broadcast_to(cv[None], (128, 256)))
    rb = f(inputs["rel_bias"])
    nn_ = np.arange(0, 256)
    nf = np.maximum(nn_, 1).astype(np.float32)
    large = 16 + (np.log(nf / np.float32(16)) / np.float32(np.log(8.0)) * np.float32(16)).astype(np.int32)
    bucket = np.where(nn_ < 16, nn_, np.minimum(large, 31))
    sI = np.arange(128)[:, None]
    qI = np.arange(128)[None, :]
    bd = bucket[np.clip(qI - sI, 0, 255)]
    bp = bucket[np.clip(qI + 128 - sI, 0, 255)]
    bT = np.zeros((128, 3, 8, 128), np.float32)
    bT[:, 0] = rb[bd].transpose(0, 2, 1)
    bT[:, 1] = rb[bp].transpose(0, 2, 1)
    bT[:, 2] = rb[31][None, :, None]
    shared["biasT"] = np.ascontiguousarray(bT.reshape(128, 3, 1024))
    shared["negm"] = np.where(np.arange(128)[None, :] <= np.arange(128)[:, None], 0.0, -1e30).astype(np.float32)
    shared["peer_v"] = f(inputs["peer_v"])[0]
    maps = []
    for b in range(8):
        m = dict(shared)
        m["x"] = x[b]
        m["c_col"] = np.ascontiguousarray(c[b].reshape(8, 128).T)
        maps.append(m)
    return maps


def kernel(**inputs):
    nc = build()
    maps = _prep_inputs(inputs)
    res = run_bass_kernel_spmd(nc, maps, core_ids=list(range(8)))
    out = np.stack([np.asarray(r["out"], dtype=np.float32) for r in res.results], axis=0)
    return out


def _bc_mid(ap, n):
    sh = list(ap.shape)
    return ap.unsqueeze(1).to_broadcast([sh[0], n] + sh[1:])


def phase_B(L):
    nc, sc, ar, ps, B_ps = L["nc"], L["sc"], L["ar"], L["ps"], L["B_ps"]
    identb, B_ident, load_bf16 = L["identb"], L["B_ident"], L["load_bf16"]
    zrw_d, rwo_d = L["zrw_d"], L["rwo_d"]
    V = lambda fn, r=(), w=(): sc.op("dve", fn, r, w)
    A = lambda fn, r=(), w=(): sc.op("act", fn, r, w)
    P = lambda fn, r=(), w=(): sc.op("pe", fn, r, w)
    mB = ar.mark()
    cst = ar.alloc([128, 1024], F32, "cst")
    mu = ar.alloc([128, 14], F32, "mu")
    rwvec = ar.alloc([128, 20], F32, "rwvec")
    omka = ar.alloc([128, 4], F32, "omka")
    w2a2b = ar.alloc([128, 512], BF16, "w2a2b")
    g2b = ar.alloc([128, 512], BF16, "g2b")
    gnbc = ar.alloc([128, 2, 256], F32, "gnbc")
    bones = ar.alloc([128, 128], BF16, "bones")
    onesb = ar.alloc([128, 1], BF16, "onesb")
    B_c = Buf("cstB")
    sc.dma("sp", cst[:, :], L["cst_d"][:, :], writes=[B_c])
    sc.dma("sp", mu[:, :], L["mu_d"][:, :], writes=[B_c])
    sc.dma("sp", rwvec[:, :], L["rwvec_d"][:, :], writes=[B_c])
    sc.dma("sp", gnbc[:, :, :], L["gnbc_d"][:, :, :], writes=[B_c])
    load_bf16(w2a2b[:, :], L["w2a2_d"][:, :], 512, B_c)
    load_bf16(g2b[:, :], L["g2_d"][:, :], 512, B_c)
    V(lambda e: e.tensor_scalar(out=omka[:, :], in0=rwvec[:, 12:16], scalar1=-1.0, scalar2=1.0, op0=ALU.mult, op1=ALU.add), [B_c], [B_c])
    V(lambda e: e.tensor_copy(out=bones[:, :], in_=cst[:, 192:320]), [B_c], [B_c])
    V(lambda e: e.tensor_copy(out=onesb[:, :], in_=cst[:, 896:897]), [B_c], [B_c])
    maskA = cst[:, 0:128]
    maskT = cst[:, 128:192]
    scanmask = cst[:, 320:832]
    eye64 = cst[:, 832:896]
    w0c, a0c, kkc, kac, rkc = (rwvec[:, 0:4], rwvec[:, 4:8], rwvec[:, 8:12], rwvec[:, 12:16], rwvec[:, 16:20])

    zb = ar.alloc([128, 14, 513], F32, "zb")
    zs = ar.alloc([128, 14, 512], F32, "zs")
    tmp = [ar.alloc([128, 512], F32, "tmpB%d" % i) for i in range(3)]
    B_tmp = [Buf("tmpB%d" % i) for i in range(3)]
    th = ar.alloc([128, 512], BF16, "th")
    al16 = ar.alloc([128, 512], BF16, "al16")
    sgl = ar.alloc([128, 512], BF16, "sgl")
    sq16 = ar.alloc([128, 512], BF16, "sq16")
    asg = ar.alloc([128, 512], F32, "asg")
    kk = ar.alloc([128, 512], F32, "kk")
    kp = ar.alloc([128, 512], F32, "kp")
    bv = ar.alloc([128, 512], F32, "bv")
    lw = ar.alloc([128, 512], F32, "lw")
    cs = ar.alloc([128, 512], F32, "cs")
    E = [ar.alloc([128, 512], F32, "E%d" % i) for i in range(4)]
    E5 = ar.alloc([128, 4, 8], F32, "E5")
    AR = ar.alloc([128, 4, 8, 2, 64], BF16, "AR")
    BK = ar.alloc([128, 4, 8, 2, 64], BF16, "BK")
    KH = ar.alloc([128, 4, 512], BF16, "KH")
    BH = ar.alloc([128, 4, 512], BF16, "BH")
    Vb = ar.alloc([128, 4, 512], BF16, "Vb")
    rkr = ar.alloc([128, 4, 512], BF16, "rkr")
    rwoT = ar.alloc([128, 4, 512], BF16, "rwoT")
    B_zb, B_zs, B_pre, B_blk, B_rwoT = Buf("zb"), Buf("zs"), Buf("pre"), Buf("blk"), Buf("rwoT")
    Vt2p = [ar.alloc([128, 512], BF16, "Vt2_%d" % i) for i in range(2)]
    KHt2p = [ar.alloc([128, 512], BF16, "KHt2_%d" % i) for i in range(2)]
    BHt2p = [ar.alloc([128, 512], BF16, "BHt2_%d" % i) for i in range(2)]
    sABp = [ar.alloc([128, 4, 128], BF16, "sAB%d" % i) for i in range(2)]
    sAKp = [ar.alloc([128, 4, 128], BF16, "sAK%d" % i) for i in range(2)]
    Xfp = [ar.alloc([128, 4, 64], BF16, "Xf%d" % i) for i in range(2)]
    B_Vtp = [Buf("Vt0"), Buf("Vt1")]
    B_sAp = [Buf("sA0"), Buf("sA1")]
    B_Xfp = [Buf("Xf0"), Buf("Xf1")]
    Mx = [ar.alloc([128, 4, 64], BF16, "Mx%d" % i) for i in range(2)]
    MT = [ar.alloc([128, 4, 64], BF16, "MT%d" % i) for i in range(2)]
    X = [ar.alloc([128, 4, 64], BF16, "X%d" % i) for i in range(2)]
    RHSs = ar.alloc([128, 4, 64], BF16, "RHSs")
    SAs = ar.alloc([128, 4, 64], BF16, "SAs")
    ST = ar.alloc([128, 4, 64], BF16, "ST")
    STf = ar.alloc([128, 4, 64], F32, "STf")
    sqy = ar.alloc([128, 256], F32, "sqy")
    yn = ar.alloc([128, 256], F32, "yn")
    bon = ar.alloc([128, 256], F32, "bon")
    O16 = ar.alloc([128, 256], BF16, "O16")
    st8 = ar.alloc([128, 4, 8], F32, "st8")
    B_Vt, B_sA, B_M, B_MT, B_X, B_R, B_SA, B_ST, B_ep, B_st8, B_O = (Buf("Vt"), Buf("sA"), [Buf("M0"), Buf("M1")], [Buf("MT0"), Buf("MT1")],
                                                                    [Buf("X0"), Buf("X1")], Buf("R"), Buf("SA"), Buf("ST"), Buf("ep"), Buf("st8"), Buf("O"))
    e0 = make_e0(L, 2, 7) if ('E' in L["phases"] or 'E0' in L["phases"]) else iter(())
    V(lambda e: e.memset(STf[:, :, :], 0.0), [], [B_ST])
    V(lambda e: e.memset(ST[:, :, :], 0.0), [], [B_ST])
    V(lambda e: e.memset(zb[:, :, 0:1], 0.0), [], [B_zb])

    def v3(ap2):
        return ap2.rearrange("p (c t) -> p c t", t=64)

    for tb in range(L['nblk']):
        for i in range(14):
            if tb == 0:
                sc.dma("sp", zb[:, i, 1:513], zrw_d[i * 128:(i + 1) * 128, 0:512], writes=[B_zb])
            else:
                sc.dma("sp", zb[:, i, 0:513], zrw_d[i * 128:(i + 1) * 128, tb * 512 - 1:tb * 512 + 512], writes=[B_zb])
        for i in range(14):
            t0 = tmp[i % 2]
            bt0 = B_tmp[i % 2]
            V(lambda e, i=i, t0=t0: e.tensor_tensor(out=t0[:, :], in0=zb[:, i, 0:512], in1=zb[:, i, 1:513], op=ALU.subtract), [B_zb], [bt0])
            V(lambda e, i=i, t0=t0: e.scalar_tensor_tensor(out=zs[:, i, :], in0=t0[:, :], scalar=mu[:, i:i + 1], in1=zb[:, i, 1:513],
                                                           op0=ALU.mult, op1=ALU.add), [bt0, B_zb, B_c], [B_zs])
        A(lambda e: e.activation(out=th[:, :], in_=zs[:, 12, :], func=AF.Tanh), [B_zs], [B_pre])
        V(lambda e: e.tensor_copy(out=al16[:, :], in_=zs[:, 12, :]), [B_zs], [B_pre])
        A(lambda e: e.activation(out=sgl[:, :], in_=zs[:, 13, :], func=AF.Sigmoid), [B_zs], [B_blk])
        for j in range(4):
            pW, bW = ps[0], B_ps[0]
            pA, bA = ps[1], B_ps[1]
            pQ, bQ = ps[2], B_ps[2]
            P(lambda e, j=j: e.matmul(pW[:, :], lhsT=w2a2b[0:64, j * 128:(j + 1) * 128], rhs=th[0:64, :], start=True, stop=True), [B_c, B_pre], [bW])
            P(lambda e, j=j: e.matmul(pA[:, :], lhsT=w2a2b[64:128, j * 128:(j + 1) * 128], rhs=al16[64:128, :], start=True, stop=True), [B_c, B_pre], [bA])
            A(lambda e, j=j: e.activation(out=lw[:, :], in_=pW[:, :], func=AF.Sigmoid, bias=w0c[:, j:j + 1]), [bW, B_c], [B_pre])
            A(lambda e, j=j: e.activation(out=asg[:, :], in_=pA[:, :], func=AF.Sigmoid, bias=a0c[:, j:j + 1]), [bA, B_c], [B_pre])
            A(lambda e, j=j: e.activation(out=sq16[:, :], in_=zs[:, 4 + j, :], func=AF.Square, scale=kkc[:, j:j + 1]), [B_zs, B_c], [B_pre])
            P(lambda e: e.matmul(pQ[:, :], lhsT=bones[:, :], rhs=sq16[:, :], start=True, stop=True), [B_c, B_pre], [bQ])
            V(lambda e: e.tensor_scalar(out=tmp[2][:, :], in0=pQ[:, :], scalar1=1e-24, scalar2=None, op0=ALU.max), [bQ], [B_tmp[2]])
            A(lambda e: e.activation(out=tmp[2][:, :], in_=tmp[2][:, :], func=AF.Sqrt), [B_tmp[2]], [B_tmp[2]])
            V(lambda e: e.reciprocal(out=tmp[2][:, :], in_=tmp[2][:, :]), [B_tmp[2]], [B_tmp[2]])
            V(lambda e, j=j: e.scalar_tensor_tensor(out=kk[:, :], in0=zs[:, 4 + j, :], scalar=kkc[:, j:j + 1], in1=tmp[2][:, :],
                                                    op0=ALU.mult, op1=ALU.mult), [B_zs, B_tmp[2], B_c], [B_pre])
            V(lambda e, j=j: e.tensor_scalar(out=tmp[0][:, :], in0=asg[:, :], scalar1=kac[:, j:j + 1], scalar2=omka[:, j:j + 1],
                                             op0=ALU.mult, op1=ALU.add), [B_pre, B_c], [B_tmp[0]])
            V(lambda e, j=j: e.tensor_tensor(out=kp[:, :], in0=zs[:, 4 + j, :], in1=tmp[0][:, :], op=ALU.mult), [B_zs, B_tmp[0]], [B_pre])
            V(lambda e: e.tensor_tensor(out=bv[:, :], in0=kk[:, :], in1=asg[:, :], op=ALU.mult), [B_pre], [B_pre])
            V(lambda e: e.tensor_scalar(out=lw[:, :], in0=lw[:, :], scalar1=-0.6065306597126334, scalar2=None, op0=ALU.mult), [B_pre], [B_pre])
            V(lambda e: e.tensor_tensor_scan(out=cs[:, :], data0=scanmask, data1=lw[:, :], initial=0.0, op0=ALU.mult, op1=ALU.add), [B_pre, B_c], [B_pre])
            V(lambda e: e.tensor_tensor(out=tmp[0][:, :], in0=cs[:, :], in1=lw[:, :], op=ALU.subtract), [B_pre], [B_tmp[0]])
            V(lambda e: e.tensor_tensor(out=v3(tmp[1][:, :]), in0=v3(cs[:, :])[:, :, 63:64].to_broadcast([128, 8, 64]), in1=v3(cs[:, :]),
                                        op=ALU.subtract), [B_pre], [B_tmp[1]])
            A(lambda e: e.activation(out=E[0][:, :], in_=cs[:, :], func=AF.Exp), [B_pre], [B_pre])
            A(lambda e: e.activation(out=E[1][:, :], in_=cs[:, :], func=AF.Exp, scale=-1.0), [B_pre], [B_pre])
            A(lambda e: e.activation(out=E[2][:, :], in_=tmp[0][:, :], func=AF.Exp), [B_tmp[0]], [B_pre])
            A(lambda e: e.activation(out=E[3][:, :], in_=tmp[1][:, :], func=AF.Exp), [B_tmp[1]], [B_pre])
            V(lambda e, j=j: e.tensor_copy(out=E5[:, j, :], in_=v3(E[0][:, :])[:, :, 63]), [B_pre], [B_blk])
            V(lambda e, j=j: e.tensor_tensor(out=AR[:, j, :, 1, :], in0=v3(zs[:, j, :]), in1=v3(E[0][:, :]), op=ALU.mult), [B_zs, B_pre], [B_blk])
            V(lambda e, j=j: e.tensor_tensor(out=BK[:, j, :, 1, :], in0=v3(kp[:, :]), in1=v3(E[1][:, :]), op=ALU.mult), [B_pre], [B_blk])
            V(lambda e, j=j: e.tensor_tensor(out=BK[:, j, :, 0, :], in0=v3(bv[:, :]), in1=v3(E[1][:, :]), op=ALU.mult), [B_pre], [B_blk])
            V(lambda e, j=j: e.scalar_tensor_tensor(out=AR[:, j, :, 0, :], in0=v3(kk[:, :]), scalar=-1.0, in1=v3(E[2][:, :]),
                                                    op0=ALU.mult, op1=ALU.mult), [B_pre], [B_blk])
            V(lambda e, j=j: e.tensor_tensor(out=KH[:, j, :], in0=kp[:, :], in1=E[3][:, :], op=ALU.mult), [B_pre], [B_blk])
            V(lambda e, j=j: e.tensor_tensor(out=BH[:, j, :], in0=bv[:, :], in1=E[3][:, :], op=ALU.mult), [B_pre], [B_blk])
            A(lambda e, j=j: e.activation(out=Vb[:, j, :], in_=zs[:, 8 + j, :], func=AF.Copy), [B_zs], [B_blk])
            V(lambda e, j=j: e.scalar_tensor_tensor(out=rkr[:, j, :], in0=zs[:, j, :], scalar=rkc[:, j:j + 1], in1=kp[:, :],
                                                    op0=ALU.mult, op1=ALU.mult), [B_zs, B_pre, B_c], [B_blk])

        def v4(ap2):
            return ap2.rearrange("p (j t) -> p j t", t=64)
        hl = [(h // 2, slice((h % 2) * 64, (h % 2) * 64 + 64), slice((h // 2) * 64, (h // 2) * 64 + 64), slice(h * 64, (h + 1) * 64)) for h in range(8)]

        def pre(c):
            q = c % 2
            csl = slice(c * 64, (c + 1) * 64)
            Vt2, KHt2, BHt2, sAB, sAK = Vt2p[q], KHt2p[q], BHt2p[q], sABp[q], sAKp[q]
            B_Vt, B_sA = B_Vtp[q], B_sAp[q]
            pT = ps[3][:, :].bitcast(BF16)
            pT2 = ps[4][:, :].bitcast(BF16)
            for half in range(2):
                hp = slice(half * 64, half * 64 + 64)
                for j in range(4):
                    P(lambda e: e.transpose(pT[hp, j * 128:(j + 1) * 128], Vb[:, j, csl], identb[:, :]), [B_blk, B_ident], [B_ps[3]])
                    P(lambda e: e.transpose(pT[hp, 512 + j * 128:512 + (j + 1) * 128], KH[:, j, csl], identb[:, :]), [B_blk, B_ident], [B_ps[3]])
                    P(lambda e: e.transpose(pT2[hp, j * 128:(j + 1) * 128], BH[:, j, csl], identb[:, :]), [B_blk, B_ident], [B_ps[4]])
            yield
            V(lambda e: e.tensor_copy(out=Vt2[:, :], in_=pT[:, 0:512]), [B_ps[3]], [B_Vt])
            A(lambda e: e.activation(out=KHt2[:, :], in_=pT[:, 512:1024], func=AF.Copy), [B_ps[3]], [B_Vt])
            A(lambda e: e.activation(out=BHt2[:, :], in_=pT2[:, 0:512], func=AF.Copy), [B_ps[4]], [B_Vt])
            for (j, pp, js, hs) in hl:
                P(lambda e: e.matmul(ps[0][pp, j * 128:(j + 1) * 128], lhsT=BK[pp, j, c, 0, :], rhs=AR[pp, j, c, :, :], start=True, stop=True), [B_blk], [B_ps[0]])
                P(lambda e: e.matmul(ps[1][pp, j * 128:(j + 1) * 128], lhsT=BK[pp, j, c, 1, :], rhs=AR[pp, j, c, :, :], start=True, stop=True), [B_blk], [B_ps[1]])
                P(lambda e: e.matmul(ps[2][pp, j * 64:(j + 1) * 64], lhsT=AR[pp, j, c, 0, :], rhs=BK[pp, j, c, 0, :], start=True, stop=True), [B_blk], [B_ps[2]])
            yield
            V(lambda e: e.tensor_tensor(out=sAB[:, :, :], in0=ps[0][:, :].rearrange("p (j t) -> p j t", t=128), in1=_bc_mid(maskA, 4), op=ALU.mult), [B_ps[0], B_c], [B_sA])
            V(lambda e: e.tensor_tensor(out=sAK[:, :, :], in0=ps[1][:, :].rearrange("p (j t) -> p j t", t=128), in1=_bc_mid(maskA, 4), op=ALU.mult), [B_ps[1], B_c], [B_sA])
            V(lambda e: e.tensor_tensor(out=MT[0][:, :, :], in0=v4(ps[2][:, 0:256]), in1=_bc_mid(maskT, 4), op=ALU.mult), [B_ps[2], B_c], [B_MT[0]])
            V(lambda e: e.tensor_copy(out=Mx[0][:, :, :], in_=sAB[:, :, 0:64]), [B_sA], [B_M[0]])
            V(lambda e: e.tensor_tensor(out=X[0][:, :, :], in0=sAB[:, :, 0:64], in1=_bc_mid(eye64, 4), op=ALU.add), [B_sA, B_c], [B_X[0]])
            yield
            cur = 0
            pa, pb, pc = ps[2], ps[3], ps[4]
            for rd in range(5):
                nxt = 1 - cur
                for (j, pp, js, hs) in hl:
                    P(lambda e: e.matmul(pa[pp, js], lhsT=MT[cur][pp, j, :], rhs=Mx[cur][pp, j, :], start=True, stop=True), [B_MT[cur], B_M[cur]], [B_ps[2]])
                    P(lambda e: e.matmul(pb[pp, js], lhsT=Mx[cur][pp, j, :], rhs=MT[cur][pp, j, :], start=True, stop=True), [B_MT[cur], B_M[cur]], [B_ps[3]])
                yield
                V(lambda e: e.tensor_copy(out=Mx[nxt][:, :, :], in_=v4(pa[:, 0:256])), [B_ps[2]], [B_M[nxt]])
                A(lambda e: e.activation(out=MT[nxt][:, :, :], in_=v4(pb[:, 0:256]), func=AF.Copy), [B_ps[3]], [B_MT[nxt]])
                for (j, pp, js, hs) in hl:
                    P(lambda e: e.matmul(pc[pp, js], lhsT=MT[nxt][pp, j, :], rhs=X[cur][pp, j, :], start=True, stop=True), [B_MT[nxt], B_X[cur]], [B_ps[4]])
                yield
                if rd < 4:
                    V(lambda e: e.tensor_tensor(out=X[nxt][:, :, :], in0=X[cur][:, :, :], in1=v4(pc[:, 0:256]), op=ALU.add), [B_X[cur], B_ps[4]], [B_X[nxt]])
                else:
                    V(lambda e: e.tensor_tensor(out=Xfp[q][:, :, :], in0=X[cur][:, :, :], in1=v4(pc[:, 0:256]), op=ALU.add), [B_X[cur], B_ps[4]], [B_Xfp[q]])
                cur = nxt
                yield

        def post(c):
            q = c % 2
            csl = slice(c * 64, (c + 1) * 64)
            Vt2, KHt2, BHt2, sAB, sAK, Xf = Vt2p[q], KHt2p[q], BHt2p[q], sABp[q], sAKp[q], Xfp[q]
            B_Vt, B_sA, bXf = B_Vtp[q], B_sAp[q], B_Xfp[q]
            pR, bR = ps[5], B_ps[5]
            pY, bY = ps[6], B_ps[6]
            pU, bU = ps[7], B_ps[7]
            for (j, pp, js, hs) in hl:
                P(lambda e: e.matmul(pR[pp, js], lhsT=AR[pp, j, c, 0, :], rhs=ST[pp, j, :], start=True, stop=False), [B_blk, B_ST], [bR])
                P(lambda e: e.matmul(pR[pp, js], lhsT=sAK[pp, j, 0:64], rhs=Vt2[pp, hs], start=False, stop=True), [B_sA, B_Vt], [bR])
            yield
            V(lambda e: e.tensor_copy(out=RHSs[:, :, :], in_=v4(pR[:, 0:256])), [bR], [B_R])
            for (j, pp, js, hs) in hl:
                P(lambda e: e.matmul(pR[pp, js], lhsT=Xf[pp, j, :], rhs=RHSs[pp, j, :], start=True, stop=True), [bXf, B_R], [bR])
            yield
            V(lambda e: e.tensor_copy(out=SAs[:, :, :], in_=v4(pR[:, 0:256])), [bR], [B_SA])
            for (j, pp, js, hs) in hl:
                P(lambda e: e.matmul(pY[pp, js], lhsT=AR[pp, j, c, 1, :], rhs=ST[pp, j, :], start=True, stop=False), [B_blk, B_ST], [bY])
                P(lambda e: e.matmul(pY[pp, js], lhsT=sAK[pp, j, 64:128], rhs=Vt2[pp, hs], start=False, stop=False), [B_sA, B_Vt], [bY])
                P(lambda e: e.matmul(pY[pp, js], lhsT=sAB[pp, j, 64:128], rhs=SAs[pp, j, :], start=False, stop=True), [B_sA, B_SA], [bY])
            for (j, pp, js, hs) in hl:
                P(lambda e: e.matmul(pU[pp, js], lhsT=KHt2[pp, hs], rhs=Vt2[pp, hs], start=True, stop=False), [B_Vt], [bU])
                P(lambda e: e.matmul(pU[pp, js], lhsT=BHt2[pp, hs], rhs=SAs[pp, j, :], start=False, stop=True), [B_Vt, B_SA], [bU])
            yield
            V(lambda e: e.tensor_tensor(out=STf[:, :, :], in0=STf[:, :, :], in1=E5[:, :, c:c + 1].to_broadcast([128, 4, 64]), op=ALU.mult), [B_blk, B_ST, bY, bR], [B_ST])
            V(lambda e: e.tensor_tensor(out=STf[:, :, :], in0=STf[:, :, :], in1=v4(pU[:, 0:256]), op=ALU.add), [bU, B_ST], [B_ST])
            V(lambda e: e.tensor_copy(out=ST[:, :, :], in_=STf[:, :, :]), [B_ST], [B_ST])
            pG, bG = ps[5], B_ps[5]
            for (j, pp, js, hs) in hl:
                P(lambda e: e.matmul(pG[pp, 256 + j:256 + j + 1], lhsT=rkr[pp, j, csl], rhs=onesb[pp, 0:1], start=True, stop=True), [B_blk, B_c, B_SA], [bG])
                P(lambda e: e.matmul(pG[pp, js], lhsT=sgl[:, csl], rhs=g2b[:, hs], start=True, stop=True), [B_blk, B_c, B_SA, B_R], [bG])
            y3 = v4(pY[:, 0:256])
            V(lambda e: e.tensor_reduce(out=st8[:, :, 0], in_=y3, axis=AX.X, op=ALU.add), [bY], [B_st8])
            A(lambda e: e.activation(out=sqy[:, :], in_=pY[:, 0:256], func=AF.Square), [bY], [B_ep])
            yield
            V(lambda e: e.tensor_reduce(out=st8[:, :, 1], in_=v4(sqy[:, :]), axis=AX.X, op=ALU.add), [B_ep], [B_st8])
            V(lambda e: e.tensor_scalar(out=st8[:, :, 2], in0=st8[:, :, 0], scalar1=1.0 / 64, scalar2=None, op0=ALU.mult), [B_st8], [B_st8])
            V(lambda e: e.tensor_tensor(out=st8[:, :, 3], in0=st8[:, :, 2], in1=st8[:, :, 2], op=ALU.mult), [B_st8], [B_st8])
            V(lambda e: e.scalar_tensor_tensor(out=st8[:, :, 4], in0=st8[:, :, 1], scalar=1.0 / 64, in1=st8[:, :, 3], op0=ALU.mult, op1=ALU.subtract), [B_st8], [B_st8])
            V(lambda e: e.tensor_scalar(out=st8[:, :, 4], in0=st8[:, :, 4], scalar1=64e-5, scalar2=None, op0=ALU.add), [B_st8], [B_st8])
            A(lambda e: e.activation(out=st8[:, :, 5], in_=st8[:, :, 4], func=AF.Sqrt), [B_st8], [B_st8])
            yield
            V(lambda e: e.reciprocal(out=st8[:, :, 5], in_=st8[:, :, 5]), [B_st8], [B_st8])
            yn3 = v4(yn[:, :])
            V(lambda e: e.tensor_tensor(out=yn3, in0=y3, in1=st8[:, :, 2:3].to_broadcast([128, 4, 64]), op=ALU.subtract), [bY, B_st8], [B_ep])
            V(lambda e: e.tensor_tensor(out=yn3, in0=yn3, in1=st8[:, :, 5:6].to_broadcast([128, 4, 64]), op=ALU.mult), [B_ep, B_st8], [B_ep])
            V(lambda e: e.tensor_tensor(out=yn[:, :], in0=yn[:, :], in1=gnbc[:, 0, :], op=ALU.mult), [B_ep, B_c], [B_ep])
            V(lambda e: e.tensor_tensor(out=yn[:, :], in0=yn[:, :], in1=gnbc[:, 1, :], op=ALU.add), [B_ep, B_c], [B_ep])
            V(lambda e: e.tensor_copy(out=st8[:, :, 6], in_=pG[:, 256:260]), [bG], [B_st8])
            for half in range(2):
                hp = slice(half * 64, half * 64 + 64)
                V(lambda e: e.tensor_tensor(out=v4(bon[hp, :]), in0=Vt2[hp, :].rearrange("p (j q t) -> p j q t", q=2, t=64)[:, :, half, :],
                                            in1=st8[hp, :, 6:7].to_broadcast([64, 4, 64]), op=ALU.mult), [B_Vt, B_st8], [B_ep])
            V(lambda e: e.tensor_tensor(out=yn[:, :], in0=yn[:, :], in1=bon[:, :], op=ALU.add), [B_ep], [B_ep])
            V(lambda e: e.tensor_tensor(out=O16[:, :], in0=yn[:, :], in1=pG[:, 0:256], op=ALU.mult), [B_ep, bG], [B_O])
            pO = pU[:, 384:512].bitcast(BF16)
            for (j, pp, js, hs) in hl:
                P(lambda e: e.transpose(pO[pp, js], O16[pp, js], identb[pp, pp]), [B_O, B_ident, B_ST], [bU])
            yield
            V(lambda e: e.tensor_copy(out=rwoT[:, :, csl], in_=v4(pO[:, 0:256])), [bU], [B_rwoT])

        def run_both(ga, gb):
            alive_a, alive_b = ga is not None, gb is not None
            while alive_a or alive_b:
                if alive_a:
                    try:
                        next(ga)
                    except StopIteration:
                        alive_a = False
                if alive_b:
                    try:
                        next(gb)
                    except StopIteration:
                        alive_b = False

        run_both(pre(0), None)
        for c in range(8):
            for _ in range(4):
                next(e0, None)
            run_both(post(c), pre(c + 1) if c + 1 < 8 else None)
        for j in range(4):
            sc.dma("sp", rwo_d[j * 128:(j + 1) * 128, tb * 512:(tb + 1) * 512], rwoT[:, j, :], reads=[B_rwoT])
    for _ in e0:
        pass
    if 'E' in L["phases"]:
        L["e0_done_flag"][0] = True
    ar.release(mB)


def phase_D(L):
    nc, sc, ar, ps, B_ps = L["nc"], L["sc"], L["ar"], L["ps"], L["B_ps"]
    ident, identb, B_ident, load_bf16 = L["ident"], L["identb"], L["B_ident"], L["load_bf16"]
    gt_bc, B_gt = L["gt_bc"], L["B_gt"]
    dbg = L["dbg"]
    V = lambda fn, r=(), w=(): sc.op("dve", fn, r, w)
    A = lambda fn, r=(), w=(): sc.op("act", fn, r, w)
    P = lambda fn, r=(), w=(): sc.op("pe", fn, r, w)
    G = lambda fn, r=(), w=(): sc.op("pool", fn, r, w)
    ALPHA = 2.0 ** 0.25
    mD = ar.mark()
    wbra = ar.alloc([128, 4, D], BF16, "wbra")
    wbrb = ar.alloc([128, 8, D], BF16, "wbrb")
    wout = ar.alloc([128, 8, D], BF16, "wout")
    wqb = ar.alloc([128, 8, D], BF16, "wqb")
    keysT = ar.alloc([128, 8, 128], BF16, "keysT")
    lnbc = ar.alloc([128, 2, D], F32, "lnbc")
    B_w = Buf("wD")
    for kc in range(4):
        load_bf16(wbra[:, kc, :], L["wbra_d"][kc * 128:(kc + 1) * 128, :], D, B_w)
    for kc in range(8):
        load_bf16(wbrb[:, kc, :], L["wbrb_d"][kc * 128:(kc + 1) * 128, :], D, B_w)
        load_bf16(wout[:, kc, :], L["wout_d"][kc * 128:(kc + 1) * 128, :], D, B_w)
        load_bf16(wqb[:, kc, :], L["wq_d"][kc * 128:(kc + 1) * 128, :], D, B_w)
    load_bf16(keysT[:, :, :].rearrange("p a b -> p (a b)"), L["pkeys_d"][:, :, :].rearrange("p a b -> p (a b)"), 1024, B_w)
    sc.dma("sp", lnbc[:, :, :], L["lnbc_d"][:, 0:2, :], writes=[B_w])
    rwoB = ar.alloc([128, 4, 512], BF16, "rwoB")
    dsaB = ar.alloc([128, 8, 512], BF16, "dsaB")
    zgr = [ar.alloc([128, 2, 512], BF16, "zgr%d" % i) for i in range(2)]
    B_zgr = [Buf("zgr0"), Buf("zgr1")]
    mg = ar.alloc([128, 8, 512], BF16, "mg")
    t1 = ar.alloc([128, 512], F32, "t1")
    t2 = ar.alloc([128, 512], F32, "t2")
    B_in, B_mg, B_t = Buf("inD"), Buf("mg"), Buf("tD")
    xt = ar.alloc([128, D], F32, "xtD")
    u = ar.alloc([128, D], F32, "uD")
    x1 = ar.alloc([128, D], F32, "x1")
    h2 = ar.alloc([128, D], F32, "h2")
    st = ar.alloc([128, 8], F32, "stD")
    h2T = ar.alloc([128, 8, 128], BF16, "h2T")
    qT = ar.alloc([128, 8, 128], BF16, "qT")
    ssb = ar.alloc([128, 16, 128], F32, "ssb")
    stmp = ar.alloc([128, 256], F32, "stmp")
    tv = ar.alloc([128, 16, 16], F32, "tv")
    ti = ar.alloc([128, 16, 16], U32, "ti")
    tif = ar.alloc([128, 16, 16], F32, "tif")
    cand = ar.alloc([128, 8, 256], F32, "cand")
    mv = ar.alloc([128, 8, 16], F32, "mv")
    posu = ar.alloc([128, 8, 16], U32, "posu")
    au = ar.alloc([128, 8, 16], U32, "au")
    bu = ar.alloc([128, 8, 16], U32, "bu")
    abf = ar.alloc([128, 2, 128], F32, "abf")
    oh16 = ar.alloc([128, 128, 16], F32, "oh16")
    junk = oh16[:, 0:64, :].rearrange("p a b -> p (a b)")
    sel = ar.alloc([128, 3, 128], F32, "sel")
    selT = ar.alloc([128, 3, 128], F32, "selT")
    gate = ar.alloc([128, 8, 16], F32, "gate")
    gs = ar.alloc([128, 8], F32, "gs")
    iota16 = L["iota16"]
    B_oh, B_sel, B_selT = Buf("oh"), Buf("sel"), Buf("selT")
    B_x, B_u, B_x1, B_h2, B_st, B_h2T, B_qT, B_s, B_tk, B_c, B_e, B_hu, B_acc, B_j = [Buf(n) for n in
        ("x", "u", "x1", "h2", "st", "h2T", "qT", "s", "tk", "cand", "eid", "hu", "acc", "junk")]
    x_v = L["x_d"].rearrange("(n p) m -> p n m", p=128)
    out_v = L["out_d"].rearrange("(n p) m -> p n m", p=128)

    def layer_norm(src, bsrc, dst, bdst, gi):
        A(lambda e: e.activation(out=junk, in_=src[:, :], func=AF.Copy, accum_out=st[:, 0:1]), [bsrc], [B_st, B_oh])
        A(lambda e: e.activation(out=junk, in_=src[:, :], func=AF.Square, accum_out=st[:, 1:2]), [bsrc], [B_st, B_oh])
        V(lambda e: e.tensor_scalar(out=st[:, 2:3], in0=st[:, 0:1], scalar1=1.0 / D, scalar2=None, op0=ALU.mult), [B_st], [B_st])
        V(lambda e: e.tensor_tensor(out=st[:, 3:4], in0=st[:, 2:3], in1=st[:, 2:3], op=ALU.mult), [B_st], [B_st])
        V(lambda e: e.scalar_tensor_tensor(out=st[:, 4:5], in0=st[:, 1:2], scalar=1.0 / D, in1=st[:, 3:4], op0=ALU.mult, op1=ALU.subtract), [B_st], [B_st])
        V(lambda e: e.tensor_scalar(out=st[:, 4:5], in0=st[:, 4:5], scalar1=1e-5, scalar2=None, op0=ALU.add), [B_st], [B_st])
        A(lambda e: e.activation(out=st[:, 5:6], in_=st[:, 4:5], func=AF.Sqrt), [B_st], [B_st])
        V(lambda e: e.reciprocal(out=st[:, 5:6], in_=st[:, 5:6]), [B_st], [B_st])
        V(lambda e: e.scalar_tensor_tensor(out=st[:, 6:7], in0=st[:, 2:3], scalar=-1.0, in1=st[:, 5:6], op0=ALU.mult, op1=ALU.mult), [B_st], [B_st])
        A(lambda e: e.activation(out=dst[:, :], in_=src[:, :], func=AF.Identity, scale=st[:, 5:6], bias=st[:, 6:7]), [bsrc, B_st], [bdst])
        G(lambda e: e.tensor_tensor(out=dst[:, :], in0=dst[:, :], in1=lnbc[:, gi, :], op=ALU.mult), [bdst, B_w], [bdst])
        G(lambda e: e.tensor_tensor(out=dst[:, :], in0=dst[:, :], in1=lnbc[:, gi + 1, :], op=ALU.add), [bdst, B_w], [bdst])

    x1p = [x1, ar.alloc([128, D], F32, "x1b")]
    B_x1p = [B_x1, Buf("x1b")]
    h2Tp_ = [h2T, ar.alloc([128, 8, 128], BF16, "h2Tb")]
    B_h2Tp_ = [B_h2T, Buf("h2Tb")]
    ssbp = [ssb, ar.alloc([128, 16, 128], F32, "ssbb")]
    B_sp = [B_s, Buf("ssbb")]
    prevn = [None]
    B_ohh = [Buf("ohh%d" % i) for i in range(8)]
    stmp16 = ar.alloc([128, 16, 128], F32, "stmp16")
    stmp8 = stmp16[:, :, :].rearrange("p (h a) b -> p h (a b)", a=2)
    B_tkg = [Buf("tkg%d" % i) for i in range(16)]
    B_tkg2 = [Buf("tkgb%d" % i) for i in range(16)]
    B_stg16 = [Buf("stg16_%d" % i) for i in range(16)]
    B_tig = [Buf("tig%d" % i) for i in range(16)]
    B_tig2 = [Buf("tigb%d" % i) for i in range(16)]
    B_mvh = [Buf("mvh%d" % i) for i in range(8)]
    B_mvh2 = [Buf("mvhb%d" % i) for i in range(8)]
    B_st8h = [B_stg16[2 * i] for i in range(8)]
    B_posh = [Buf("posh%d" % i) for i in range(8)]
    B_posh2 = [Buf("poshb%d" % i) for i in range(8)]

    def stageA(n, jt):
        q = n % 2
        x1_, bx1_, h2T_, bh2T_, ssb_, bs_ = x1p[q], B_x1p[q], h2Tp_[q], B_h2Tp_[q], ssbp[q], B_sp[q]
        sc.dma("sp", xt[:, :], x_v[:, n, :], writes=[B_x])
        for half in range(2):
            pM, bM = ps[4 + half], B_ps[4 + half]
            hsl = slice(half * 512, (half + 1) * 512)
            for dc in range(8):
                P(lambda e: e.matmul(pM[:, :], lhsT=mg[:, dc, jt * 128:(jt + 1) * 128], rhs=wout[:, dc, hsl], start=(dc == 0), stop=(dc == 7)), [B_mg, B_w], [bM])
            V(lambda e: e.tensor_tensor(out=u[:, hsl], in0=pM[:, :], in1=gt_bc[:, 0, hsl], op=ALU.mult), [bM, B_gt], [B_u])
            V(lambda e: e.scalar_tensor_tensor(out=u[:, hsl], in0=xt[:, hsl], scalar=ALPHA, in1=u[:, hsl], op0=ALU.mult, op1=ALU.add), [B_x, B_u], [B_u])
        layer_norm(u, B_u, x1_, bx1_, 0)
        if "x1dbg" in dbg:
            sc.dma("sp", L["x1_d"][n * 128:(n + 1) * 128, :], x1_[:, :], reads=[bx1_])
        sc.dma("sp", L["x1s_d"][n * 128:(n + 1) * 128, :], x1_[:, :], reads=[bx1_])
        G(lambda e: e.tensor_tensor(out=h2[:, :], in0=x1_[:, :], in1=gt_bc[:, 2, :], op=ALU.mult), [bx1_, B_gt], [B_h2])
        G(lambda e: e.tensor_tensor(out=h2[:, :], in0=h2[:, :], in1=gt_bc[:, 1, :], op=ALU.add), [B_h2, B_gt], [B_h2])
        for kc in range(8):
            pp_, bp_ = ps[kc // 4], B_ps[kc // 4]
            P(lambda e: e.transpose(pp_[:, (kc % 4) * 128:(kc % 4 + 1) * 128], h2[:, kc * 128:(kc + 1) * 128], ident[:, :]), [B_h2, B_ident], [bp_])
        for k2 in range(2):
            A(lambda e: e.activation(out=h2T_[:, k2 * 4:(k2 + 1) * 4, :], in_=ps[k2][:, :].rearrange("p (a b) -> p a b", b=128), func=AF.Copy), [B_ps[k2]], [bh2T_])
        for kc in range(8):
            sc.dma("sp", L["h2T_d"][kc * 128:(kc + 1) * 128, n * 128:(n + 1) * 128], h2T_[:, kc, :], reads=[bh2T_])
        for hh in range(8):
            pq, bq = ps[2 + hh // 4], B_ps[2 + hh // 4]
            for kc in range(8):
                P(lambda e: e.matmul(pq[:, (hh % 4) * 128:(hh % 4 + 1) * 128], lhsT=wqb[:, kc, hh * 128:(hh + 1) * 128], rhs=h2T_[:, kc, :],
                                     start=(kc == 0), stop=(kc == 7)), [B_w, bh2T_], [bq])
        for k2 in range(2):
            A(lambda e: e.activation(out=qT[:, k2 * 4:(k2 + 1) * 4, :], in_=ps[2 + k2][:, :].rearrange("p (a b) -> p a b", b=128), func=AF.Copy), [B_ps[2 + k2]], [B_qT])
        for g in range(16):
            hh, cc = g % 8, g // 8
            pS, bS = ps[4 + g // 4], B_ps[4 + g // 4]
            P(lambda e: e.matmul(pS[:, (g % 4) * 128:(g % 4 + 1) * 128], lhsT=qT[cc * 64:(cc + 1) * 64, hh, :], rhs=keysT[cc * 64:(cc + 1) * 64, hh, :],
                                 start=True, stop=True), [B_qT, B_w], [bS])
        for k4 in range(4):
            A(lambda e: e.activation(out=ssb_[:, k4 * 4:(k4 + 1) * 4, :], in_=ps[4 + k4][:, :].rearrange("p (a b) -> p a b", b=128), func=AF.Copy), [B_ps[4 + k4]], [bs_])

    def stageB(n):
        q = n % 2
        ssb_, bs_ = ssbp[q], B_sp[q]
        for g in range(16):
            V(lambda e: e.max(out=tv[:, g, 0:8], in_=ssb_[:, g, :]), [bs_], [B_tkg[g]])
        for g in range(16):
            V(lambda e: e.match_replace(out=stmp16[:, g, :], in_to_replace=tv[:, g, 0:8], in_values=ssb_[:, g, :], imm_value=-1e30), [bs_, B_tkg[g]], [B_stg16[g]])
        for g in range(16):
            V(lambda e: e.max(out=tv[:, g, 8:16], in_=stmp16[:, g, :]), [B_stg16[g]], [B_tkg2[g]])
        for g in range(16):
            V(lambda e: e.max_index(out=ti[:, g, 0:8], in_max=tv[:, g, 0:8], in_values=ssb_[:, g, :]), [bs_, B_tkg[g]], [B_tig[g]])
        for g in range(16):
            V(lambda e: e.max_index(out=ti[:, g, 8:16], in_max=tv[:, g, 8:16], in_values=ssb_[:, g, :]), [bs_, B_tkg2[g]], [B_tig2[g]])
        V(lambda e: e.tensor_copy(out=tif[:, :, :], in_=ti[:, :, :]), B_tig + B_tig2, [B_tk])
        V(lambda e: e.tensor_copy(out=tv[:, 0:1, 0:1], in_=tv[:, 0:1, 0:1]), B_tkg + B_tkg2, [B_tk])
        tvv = tv[:, :, :].rearrange("p (c h) k -> p h c k", c=2)
        tfv = tif[:, :, :].rearrange("p (c h) k -> p h c k", c=2)
        c4 = cand[:, :, :].rearrange("p h (a b) -> p h a b", b=16)
        for hh in range(8):
            V(lambda e: e.tensor_tensor(out=c4[:, hh, :, :], in0=tvv[:, hh, 0, :].unsqueeze(2).to_broadcast([128, 16, 16]),
                                        in1=tvv[:, hh, 1, :].unsqueeze(1).to_broadcast([128, 16, 16]), op=ALU.add), [B_tk], [B_c])
        for hh in range(8):
            V(lambda e: e.max(out=mv[:, hh, 0:8], in_=cand[:, hh, :]), [B_c], [B_mvh[hh]])
        for hh in range(8):
            V(lambda e: e.match_replace(out=stmp8[:, hh, :], in_to_replace=mv[:, hh, 0:8], in_values=cand[:, hh, :], imm_value=-1e30), [B_c, B_mvh[hh]], [B_stg16[2 * hh], B_stg16[2 * hh + 1]])
        for hh in range(8):
            V(lambda e: e.max(out=mv[:, hh, 8:16], in_=stmp8[:, hh, :]), [B_stg16[2 * hh], B_stg16[2 * hh + 1]], [B_mvh2[hh]])
        for hh in range(8):
            V(lambda e: e.max_index(out=posu[:, hh, 0:8], in_max=mv[:, hh, 0:8], in_values=cand[:, hh, :]), [B_c, B_mvh[hh]], [B_posh[hh]])
        for hh in range(8):
            V(lambda e: e.max_index(out=posu[:, hh, 8:16], in_max=mv[:, hh, 8:16], in_values=cand[:, hh, :]), [B_c, B_mvh2[hh]], [B_posh2[hh]])
        V(lambda e: e.tensor_copy(out=mv[:, 0:1, 0:1], in_=mv[:, 0:1, 0:1]), B_mvh + B_mvh2 + B_posh + B_posh2, [B_e])
        V(lambda e: e.tensor_scalar(out=au[:, :, :], in0=posu[:, :, :], scalar1=4, scalar2=None, op0=ALU.logical_shift_right), [B_e], [B_e])
        V(lambda e: e.tensor_scalar(out=bu[:, :, :], in0=posu[:, :, :], scalar1=15, scalar2=None, op0=ALU.bitwise_and), [B_e], [B_e])
        V(lambda e: e.tensor_copy(out=abf[:, 0, :], in_=au[:, :, :].rearrange("p a b -> p (a b)")), [B_e], [B_e])
        V(lambda e: e.tensor_copy(out=abf[:, 1, :], in_=bu[:, :, :].rearrange("p a b -> p (a b)")), [B_e], [B_e])
        for cc in range(2):
            V(lambda e: e.tensor_tensor(out=oh16[:, :, :], in0=abf[:, cc, :].unsqueeze(2).to_broadcast([128, 128, 16]),
                                        in1=iota16.unsqueeze(1).to_broadcast([128, 128, 16]), op=ALU.is_equal), [B_e, L["B_iota"]], [B_oh] + B_ohh)
            for hh in range(8):
                V(lambda e: e.tensor_tensor(out=oh16[:, hh * 16:(hh + 1) * 16, :], in0=oh16[:, hh * 16:(hh + 1) * 16, :],
                                            in1=tfv[:, hh, cc, :].unsqueeze(1).to_broadcast([128, 16, 16]), op=ALU.mult), [B_oh, B_tk], [B_ohh[hh]])
            V(lambda e: e.tensor_reduce(out=sel[:, cc, :], in_=oh16[:, :, :], axis=AX.X, op=ALU.add), B_ohh, [B_sel, B_oh])
        V(lambda e: e.tensor_tensor(out=gate[:, :, :], in0=mv[:, :, :], in1=mv[:, :, 0:1].to_broadcast([128, 8, 16]), op=ALU.subtract), [B_e], [B_hu])
        A(lambda e: e.activation(out=gate[:, :, :], in_=gate[:, :, :], func=AF.Exp), [B_hu], [B_hu])
        V(lambda e: e.tensor_reduce(out=gs[:, :], in_=gate[:, :, :], axis=AX.X, op=ALU.add), [B_hu], [B_hu])
        V(lambda e: e.reciprocal(out=gs[:, :], in_=gs[:, :]), [B_hu], [B_hu])
        V(lambda e: e.tensor_tensor(out=sel[:, 2, :].rearrange("p (a b) -> p a b", b=16), in0=gate[:, :, :], in1=gs[:, :].unsqueeze(2).to_broadcast([128, 8, 16]), op=ALU.mult),
          [B_hu], [B_sel])
        pI, bI = ps[6], B_ps[6]
        for q3 in range(3):
            P(lambda e: e.transpose(pI[:, q3 * 128:(q3 + 1) * 128], sel[:, q3, :], ident[:, :]), [B_sel, B_ident], [bI])
        A(lambda e: e.activation(out=selT[:, :, :], in_=pI[:, 0:384].rearrange("p (a b) -> p a b", b=128), func=AF.Copy), [bI], [B_selT])
        for q3 in range(3):
            sc.dma("sp", L["selT_d"][q3, :, n * 128:(n + 1) * 128], selT[:, q3, :], reads=[B_selT])

    for tb in range(L["nblk"]):
        tsl = slice(tb * 512, (tb + 1) * 512)
        for kc in range(4):
            sc.dma("sp", rwoB[:, kc, :], L["rwo_d"][kc * 128:(kc + 1) * 128, tsl], writes=[B_in])
        for kc in range(8):
            sc.dma("sp", dsaB[:, kc, :], L["dsao_d"][kc * 128:(kc + 1) * 128, tsl], writes=[B_in])
        for dc in range(8):
            pA, bA = ps[dc % 2], B_ps[dc % 2]
            pB, bB = ps[2 + dc % 2], B_ps[2 + dc % 2]
            zg_, bz_ = zgr[dc % 2], B_zgr[dc % 2]
            sc.dma("sp", zg_[:, 0, :], L["zg_d"][dc * 128:(dc + 1) * 128, tsl], writes=[bz_])
            sc.dma("sp", zg_[:, 1, :], L["zg_d"][(8 + dc) * 128:(9 + dc) * 128, tsl], writes=[bz_])
            for kc in range(4):
                P(lambda e: e.matmul(pA[:, :], lhsT=wbra[:, kc, dc * 128:(dc + 1) * 128], rhs=rwoB[:, kc, :], start=(kc == 0), stop=(kc == 3)), [B_w, B_in], [bA])
            for kc in range(8):
                P(lambda e: e.matmul(pB[:, :], lhsT=wbrb[:, kc, dc * 128:(dc + 1) * 128], rhs=dsaB[:, kc, :], start=(kc == 0), stop=(kc == 7)), [B_w, B_in], [bB])
            V(lambda e: e.tensor_tensor(out=t1[:, :], in0=pA[:, :], in1=zg_[:, 0, :], op=ALU.mult), [bA, bz_], [B_t])
            V(lambda e: e.tensor_tensor(out=t2[:, :], in0=pB[:, :], in1=zg_[:, 1, :], op=ALU.mult), [bB, bz_], [B_t])
            V(lambda e: e.tensor_tensor(out=mg[:, dc, :], in0=t1[:, :], in1=t2[:, :], op=ALU.add), [B_t], [B_mg])
        for jt in range(4):
            n = tb * 4 + jt
            stageA(n, jt)
            if prevn[0] is not None:
                stageB(prevn[0])
            prevn[0] = n
    stageB(prevn[0])
    ar.release(mD)


def phase_C(L):
    nc, sc, ar, ps, B_ps = L["nc"], L["sc"], L["ar"], L["ps"], L["B_ps"]
    identb, B_ident = L["identb"], L["B_ident"]
    V = lambda fn, r=(), w=(): sc.op("dve", fn, r, w)
    A = lambda fn, r=(), w=(): sc.op("act", fn, r, w)
    P = lambda fn, r=(), w=(): sc.op("pe", fn, r, w)
    G = lambda fn, r=(), w=(): sc.op("pool", fn, r, w)
    NQB = L["nblk"] * 4
    mC = ar.mark()
    cvec = ar.alloc([128, 256], F32, "cvec")
    biasT = ar.alloc([128, 3, 1024], F32, "biasT")
    negm = ar.alloc([128, 128], F32, "negm")
    ckv_tok = ar.alloc([128, 32, 129], BF16, "ckv_tok")
    ckvT = ar.alloc([128, S], BF16, "ckvT")
    kiT2 = ar.alloc([128, S], BF16, "kiT2")
    wall = ar.alloc([128, 32, 4], F32, "wall")
    B_cc, B_kv, B_ki, B_wl = Buf("cc"), Buf("kv"), Buf("ki"), Buf("wl")
    sc.dma("sp", cvec[:, :], L["cvec_d"][:, :], writes=[B_cc])
    sc.dma("sp", biasT[:, :, :], L["biasT_d"][:, :, :], writes=[B_cc])
    sc.dma("sp", negm[:, :], L["negm_d"][:, :], writes=[B_cc])
    V(lambda e: e.memset(ckv_tok[:, :, 128:129], 1.0), [], [B_kv])
    for bi_ in range(2):
        V(lambda e: e.tensor_tensor(out=biasT[:, bi_, :], in0=biasT[:, bi_, :], in1=biasT[:, 2, :], op=ALU.subtract), [B_cc], [B_cc])
    zt = [ar.alloc([128, 196], F32, "ztC%d" % i) for i in range(2)]
    B_zt = [Buf("ztC0"), Buf("ztC1")]
    sq = ar.alloc([128, 128], F32, "sqC")
    c16 = ar.alloc([128, 128], BF16, "c16")
    k32 = ar.alloc([128, 64], F32, "k32")
    k16 = ar.alloc([128, 128], BF16, "k16")
    stc = ar.alloc([128, 8], F32, "stc")
    B_sq, B_c16, B_k, B_stc = Buf("sqC"), Buf("c16"), Buf("k"), Buf("stc")
    for n in range(NQB):
        z, bz = zt[n % 2], B_zt[n % 2]
        sc.dma("sp", z[:, :], L["ztok_d"][n * 128:(n + 1) * 128, :], writes=[bz])
        A(lambda e: e.activation(out=sq[:, :], in_=z[:, 0:128], func=AF.Square, accum_out=stc[:, 0:1]), [bz], [B_sq, B_stc])
        V(lambda e: e.tensor_scalar(out=stc[:, 1:2], in0=stc[:, 0:1], scalar1=1.0 / 128, scalar2=1e-5, op0=ALU.mult, op1=ALU.add), [B_stc], [B_stc])
        A(lambda e: e.activation(out=stc[:, 1:2], in_=stc[:, 1:2], func=AF.Sqrt), [B_stc], [B_stc])
        V(lambda e: e.reciprocal(out=stc[:, 1:2], in_=stc[:, 1:2]), [B_stc], [B_stc])
        V(lambda e: e.scalar_tensor_tensor(out=ckv_tok[:, n, 0:128], in0=z[:, 0:128], scalar=stc[:, 1:2], in1=cvec[:, 0:128], op0=ALU.mult, op1=ALU.mult),
          [bz, B_stc, B_cc], [B_kv])
        V(lambda e: e.tensor_reduce(out=stc[:, 2:3], in_=z[:, 128:192], axis=AX.X, op=ALU.add), [bz], [B_stc])
        A(lambda e: e.activation(out=sq[:, 0:64], in_=z[:, 128:192], func=AF.Square, accum_out=stc[:, 3:4]), [bz], [B_sq, B_stc])
        V(lambda e: e.tensor_scalar(out=stc[:, 4:5], in0=stc[:, 2:3], scalar1=1.0 / 64, scalar2=None, op0=ALU.mult), [B_stc], [B_stc])
        V(lambda e: e.tensor_tensor(out=stc[:, 5:6], in0=stc[:, 4:5], in1=stc[:, 4:5], op=ALU.mult), [B_stc], [B_stc])
        V(lambda e: e.scalar_tensor_tensor(out=stc[:, 6:7], in0=stc[:, 3:4], scalar=1.0 / 64, in1=stc[:, 5:6], op0=ALU.mult, op1=ALU.subtract), [B_stc], [B_stc])
        V(lambda e: e.tensor_scalar(out=stc[:, 6:7], in0=stc[:, 6:7], scalar1=1e-5, scalar2=None, op0=ALU.add), [B_stc], [B_stc])
        A(lambda e: e.activation(out=stc[:, 6:7], in_=stc[:, 6:7], func=AF.Sqrt), [B_stc], [B_stc])
        V(lambda e: e.reciprocal(out=stc[:, 6:7], in_=stc[:, 6:7]), [B_stc], [B_stc])
        V(lambda e: e.tensor_scalar(out=k32[:, :], in0=z[:, 128:192], scalar1=stc[:, 4:5], scalar2=stc[:, 6:7], op0=ALU.subtract, op1=ALU.mult), [bz, B_stc], [B_k])
        V(lambda e: e.tensor_tensor(out=k32[:, :], in0=k32[:, :], in1=cvec[:, 128:192], op=ALU.mult), [B_k, B_cc], [B_k])
        V(lambda e: e.tensor_tensor(out=k16[:, 0:64], in0=k32[:, :], in1=cvec[:, 192:256], op=ALU.add), [B_k, B_cc], [B_k])
        V(lambda e: e.tensor_copy(out=k16[:, 64:128], in_=k16[:, 0:64]), [B_k], [B_k])
        V(lambda e: e.tensor_scalar(out=wall[:, n, :], in0=z[:, 192:196], scalar1=0.0625, scalar2=None, op0=ALU.mult), [bz], [B_wl])
        pT = ps[n % 2][:, :].bitcast(BF16)
        bT = B_ps[n % 2]
        P(lambda e: e.transpose(pT[:, 0:128], ckv_tok[:, n, 0:128], identb[:, :]), [B_kv, B_ident], [bT])
        P(lambda e: e.transpose(pT[:, 128:256], k16[:, :], identb[:, :]), [B_k, B_ident], [bT])
        A(lambda e: e.activation(out=ckvT[:, n * 128:(n + 1) * 128], in_=pT[:, 0:128], func=AF.Copy, scale=128.0 ** -0.5), [bT], [B_kv])
        V(lambda e: e.tensor_copy(out=kiT2[:, n * 128:(n + 1) * 128], in_=pT[:, 128:256]), [bT], [B_ki])
    qiB = [ar.alloc([128, 2, 128], BF16, "qiB%d" % i) for i in range(2)]
    zqB = [ar.alloc([128, 8, 128], BF16, "zqB%d" % i) for i in range(2)]
    B_qi = [Buf("qi0"), Buf("qi1")]
    B_zq = [Buf("zq0"), Buf("zq1")]
    score2 = [ar.alloc([128, S], F32, "score%d" % i) for i in range(2)]
    maskb2 = [ar.alloc([128, S], BF16, "maskb%d" % i) for i in range(2)]
    maskT2 = [ar.alloc([128, 32, 128], BF16, "maskT%d" % i) for i in range(2)]
    bis2 = [ar.alloc([128, 8], F32, "bis%d" % i) for i in range(2)]
    junk = ar.alloc([128, S], BF16, "junkC")
    junkA = ar.alloc([128, S // 2], BF16, "junkA")
    rl = [ar.alloc([128, 512], F32, "rl%d" % i) for i in range(2)]
    B_rl = [Buf("rl0"), Buf("rl1")]
    lg = ar.alloc([128, 1024], F32, "lg")
    PT2 = [ar.alloc([128, 8, 128], BF16, "PT%d" % i) for i in range(2)]
    B_PT2 = [Buf("PT0"), Buf("PT1")]
    Oacc = ar.alloc([128, 8, 129], F32, "Oacc")
    rec = ar.alloc([128, 8], F32, "rec")
    Oo = ar.alloc([128, 8, 128], BF16, "Oo")
    dsT = ar.alloc([128, 8, 128], BF16, "dsT")
    B_sc2 = [Buf("score0"), Buf("score1")]
    B_mb2 = [Buf("maskb0"), Buf("maskb1")]
    B_mT2 = [Buf("maskT0"), Buf("maskT1")]
    B_bisA = [Buf("bisA0"), Buf("bisA1")]
    B_bisB = [Buf("bisB0"), Buf("bisB1")]
    B_bisC = [Buf("bisC0"), Buf("bisC1")]
    B_j, B_jA, B_lg, B_O, B_Oo, B_dsT = [Buf(n_) for n_ in ("junkC", "junkA", "lg", "Oacc", "Oo", "dsT")]

    def stage1(j):
        q = j % 2
        Lk = (j + 1) * 128
        qi, bqi, zq, bzq = qiB[q], B_qi[q], zqB[q], B_zq[q]
        score, B_sc = score2[q], B_sc2[q]
        qsl = slice(j * 128, (j + 1) * 128)
        for cch in range(2):
            sc.dma("sp", qi[:, cch, :], L["zqi_d"][cch * 128:(cch + 1) * 128, qsl], writes=[bqi])
        for h in range(8):
            sc.dma("sp", zq[:, h, :], L["zq_d"][h * 128:(h + 1) * 128, qsl], writes=[bzq])
        nkc = (Lk + 511) // 512
        ri = 0
        for kc in range(nkc):
            wd = min(512, Lk - kc * 512)
            ksl = slice(kc * 512, kc * 512 + wd)
            for hi in range(4):
                po = (hi % 2) * 64
                pD, bD = ps[hi], B_ps[hi]
                P(lambda e: e.matmul(pD[:, 0:wd], lhsT=qi[po:po + 64, hi // 2, :], rhs=kiT2[po:po + 64, ksl], start=True, stop=True), [bqi, B_ki], [bD])
                r_, br_ = rl[ri % 2], B_rl[ri % 2]
                ri += 1
                A(lambda e: e.activation(out=r_[:, 0:wd], in_=pD[:, 0:wd], func=AF.Relu), [bD], [br_])
                if hi == 0:
                    V(lambda e: e.tensor_scalar(out=score[:, ksl], in0=r_[:, 0:wd], scalar1=wall[:, j, 0:1], scalar2=None, op0=ALU.mult), [br_, B_wl], [B_sc])
                else:
                    V(lambda e: e.scalar_tensor_tensor(out=score[:, ksl], in0=r_[:, 0:wd], scalar=wall[:, j, hi:hi + 1], in1=score[:, ksl], op0=ALU.mult, op1=ALU.add),
                      [br_, B_wl, B_sc], [B_sc])
        V(lambda e: e.tensor_tensor(out=score[:, qsl], in0=score[:, qsl], in1=negm[:, :], op=ALU.add), [B_sc, B_cc], [B_sc])

    def bisect_iters(j):
        q = j % 2
        Lk = (j + 1) * 128
        score, B_sc, bis = score2[q], B_sc2[q], bis2[q]
        bA, bB, bC = B_bisA[q], B_bisB[q], B_bisC[q]
        if Lk > 256:
            na = (Lk // 2) // 128 * 128
            V(lambda e: e.memset(bis[:, 6:7], 0.0), [bA], [bA])
            step = 32.0
            for it in range(21):
                V(lambda e: e.tensor_scalar(out=bis[:, 7:8], in0=bis[:, 6:7], scalar1=-1.0, scalar2=None, op0=ALU.mult), [bA], [bB])
                A(lambda e: e.activation(out=junkA[:, 0:na], in_=score[:, 0:na], func=AF.Sign, bias=bis[:, 7:8], accum_out=bis[:, 2:3]), [B_sc, bB], [B_jA, bC])
                V(lambda e: e.tensor_scalar(out=junk[:, na:Lk], in0=score[:, na:Lk], scalar1=bis[:, 6:7], scalar2=None, op0=ALU.is_ge, op1=ALU.add, accum_out=bis[:, 3:4]),
                  [B_sc, bA], [B_j, bA])
                V(lambda e: e.scalar_tensor_tensor(out=bis[:, 4:5], in0=bis[:, 2:3], scalar=0.5, in1=bis[:, 3:4], op0=ALU.mult, op1=ALU.add), [bA, bC], [bA])
                V(lambda e: e.tensor_scalar(out=bis[:, 5:6], in0=bis[:, 4:5], scalar1=255.5 - na / 2.0, scalar2=2.0 * step, op0=ALU.is_ge, op1=ALU.mult), [bA], [bA])
                V(lambda e: e.scalar_tensor_tensor(out=bis[:, 6:7], in0=bis[:, 5:6], scalar=-step, in1=bis[:, 6:7], op0=ALU.add, op1=ALU.add), [bA, bB], [bA])
                step *= 0.5
                yield
            V(lambda e: e.tensor_scalar(out=bis[:, 6:7], in0=bis[:, 6:7], scalar1=-4.0 * step, scalar2=None, op0=ALU.add), [bA], [bA])
        else:
            V(lambda e: e.memset(bis[:, 6:7], -1e29), [bA], [bA])

    def stage3(j):
        q = j % 2
        Lk = (j + 1) * 128
        score, B_sc, bis, maskb, B_mb, maskT, B_mT = score2[q], B_sc2[q], bis2[q], maskb2[q], B_mb2[q], maskT2[q], B_mT2[q]
        V(lambda e: e.tensor_scalar(out=maskb[:, 0:Lk], in0=score[:, 0:Lk], scalar1=bis[:, 6:7], scalar2=None, op0=ALU.is_ge), [B_sc, B_bisA[q]], [B_mb])
        for k8 in range((j + 8) // 8):
            nn = min(8, j + 1 - k8 * 8)
            pM = ps[4][:, :].bitcast(BF16)
            for kk_ in range(nn):
                kt = k8 * 8 + kk_
                P(lambda e: e.transpose(pM[:, kk_ * 128:(kk_ + 1) * 128], maskb[:, kt * 128:(kt + 1) * 128], identb[:, :]), [B_mb, B_ident], [B_ps[4]])
            A(lambda e: e.activation(out=maskT[:, k8 * 8:k8 * 8 + nn, :], in_=pM[:, 0:nn * 128].rearrange("p (a b) -> p a b", b=128), func=AF.Copy, scale=30000.0, bias=-30000.0),
              [B_ps[4]], [B_mT])

    def stage4(j, side):
        q = j % 2
        zq, bzq, maskT, B_mT = zqB[q], B_zq[q], maskT2[q], B_mT2[q]
        qsl = slice(j * 128, (j + 1) * 128)
        for kt in range(j + 1):
            near = kt >= j - 1
            bsel = 0 if kt == j else 1
            PTk, bPT = PT2[kt % 2], B_PT2[kt % 2]
            for half in range(2):
                pL, bL = ps[(kt % 2) * 2 + half], B_ps[(kt % 2) * 2 + half]
                P(lambda e: e.matmul(pL[:, :], lhsT=ckvT[:, kt * 128:(kt + 1) * 128], rhs=zq[:, half * 4:(half + 1) * 4, :], start=True, stop=False), [B_kv, bzq], [bL])
                for h4 in range(4):
                    P(lambda e: e.matmul(pL[:, h4 * 128:(h4 + 1) * 128], lhsT=identb[:, :], rhs=maskT[:, kt, :], start=False, stop=(h4 == 3)), [B_ident, B_mT], [bL])
                if near:
                    V(lambda e: e.tensor_tensor(out=lg[:, half * 512:(half + 1) * 512], in0=pL[:, :], in1=biasT[:, bsel, half * 512:(half + 1) * 512], op=ALU.add),
                      [bL, B_cc], [B_lg])
                    A(lambda e: e.activation(out=PTk[:, half * 4:(half + 1) * 4, :], in_=lg[:, half * 512:(half + 1) * 512].rearrange("p (h q) -> p h q", q=128), func=AF.Exp),
                      [B_lg], [bPT])
                else:
                    A(lambda e: e.activation(out=PTk[:, half * 4:(half + 1) * 4, :], in_=pL[:, :].rearrange("p (h q) -> p h q", q=128), func=AF.Exp), [bL], [bPT])
            for h in range(8):
                pO, bO = ps[5 + h // 3], B_ps[5 + h // 3]
                P(lambda e: e.matmul(pO[:, (h % 3) * 129:(h % 3 + 1) * 129], lhsT=PTk[:, h, :], rhs=ckv_tok[:, kt, :], start=(kt == 0 and h % 3 == 0), stop=(kt == j),
                                     skip_group_check=True), [bPT, B_kv], [bO])
            if side is not None:
                next(side, None)
        for b3 in range(3):
            nh = 3 if b3 < 2 else 2
            V(lambda e: e.tensor_copy(out=Oacc[:, b3 * 3:b3 * 3 + nh, :], in_=ps[5 + b3][:, 0:nh * 129].rearrange("p (h d) -> p h d", d=129)), [B_ps[5 + b3]], [B_O])
        V(lambda e: e.reciprocal(out=rec[:, :], in_=Oacc[:, :, 128]), [B_O], [B_Oo])
        V(lambda e: e.tensor_tensor(out=Oo[:, :, :], in0=Oacc[:, :, 0:128], in1=rec[:, :].unsqueeze(2).to_broadcast([128, 8, 128]), op=ALU.mult), [B_O, B_Oo], [B_Oo])
        pX = ps[4][:, :].bitcast(BF16)
        for h in range(8):
            P(lambda e: e.transpose(pX[:, h * 128:(h + 1) * 128], Oo[:, h, :], identb[:, :]), [B_Oo, B_ident], [B_ps[4]])
        V(lambda e: e.tensor_copy(out=dsT[:, :, :], in_=pX[:, :].rearrange("p (a b) -> p a b", b=128)), [B_ps[4]], [B_dsT])
        for h in range(8):
            sc.dma("sp", L["dsao_d"][h * 128:(h + 1) * 128, qsl], dsT[:, h, :], reads=[B_dsT])

    stage1(0)
    for _ in bisect_iters(0):
        pass
    stage3(0)
    for j in range(NQB):
        side = None
        if j + 1 < NQB:
            stage1(j + 1)
            side = bisect_iters(j + 1)
        stage4(j, side)
        if side is not None:
            for _ in side:
                pass
            stage3(j + 1)
    ar.release(mC)


def phase_E(L):
    nc, sc, ar, ps, B_ps = L["nc"], L["sc"], L["ar"], L["ps"], L["B_ps"]
    identb, B_ident = L["identb"], L["B_ident"]
    gt_bc, B_gt = L["gt_bc"], L["B_gt"]
    iota128, B_iota = L["iota128"], L["B_iota"]
    dbg = L["dbg"]
    V = lambda fn, r=(), w=(): sc.op("dve", fn, r, w)
    A = lambda fn, r=(), w=(): sc.op("act", fn, r, w)
    P = lambda fn, r=(), w=(): sc.op("pe", fn, r, w)
    G = lambda fn, r=(), w=(): sc.op("pool", fn, r, w)
    ALPHA = 2.0 ** 0.25
    if not L["e0_done_flag"][0]:
        m0 = ar.mark()
        for _ in make_e0(L, 3, 0):
            pass
        ar.release(m0)
    ar.release(L["mark_stg"])
    mE = ar.mark()
    TP = 256
    lnbc = ar.alloc([128, 2, D], F32, "lnbcE")
    B_ln = Buf("lnE")
    sc.dma("sp", lnbc[:, :, :], L["lnbc_d"][:, 2:4, :], writes=[B_ln])
    Gs2 = [ar.alloc([128, 128, TP], BF16, "Gs%d" % i) for i in range(2)]
    h2Tp2 = [ar.alloc([128, 8, TP], BF16, "h2Tp%d" % i) for i in range(2)]
    IT1 = ar.alloc([128, 3, TP], F32, "IT1")
    IT2 = [IT1, IT1]
    ITb2 = [ar.alloc([128, 3, TP], BF16, "ITb%d" % i) for i in range(2)]
    iotab = ar.alloc([128, 128], BF16, "iotab")
    NBT = 8
    eqb = [ar.alloc([128, NBT, 128], BF16, "eqb%d" % i) for i in range(1)]
    Lb = [ar.alloc([128, NBT, 128], BF16, "Lb%d" % i) for i in range(2)]
    Rb = [ar.alloc([128, NBT, 128], BF16, "Rb%d" % i) for i in range(2)]
    B_Gs2 = [Buf("Gs0"), Buf("Gs1")]
    B_h2Tp2 = [Buf("h2Tp0"), Buf("h2Tp1")]
    B_IT2 = [Buf("IT0"), Buf("IT1")]
    B_ITf1 = Buf("ITf")
    B_ITf2 = [B_ITf1, B_ITf1]
    B_eq = [Buf("eqb0")]
    B_eqh = [Buf("eqh0"), Buf("eqh1")]
    V(lambda e: e.tensor_copy(out=iotab[:, :], in_=iota128[:, :]), [B_iota], [B_iota])
    B_Lb = [Buf("Lb0"), Buf("Lb1")]
    B_Rb = [Buf("Rb0"), Buf("Rb1")]
    NS = 4
    uTc = [ar.alloc([128, 1024], BF16, "uTc%d" % i) for i in range(NS)]
    vcb = [ar.alloc([128, 1024], BF16, "vcb%d" % i) for i in range(NS)]
    B_uTc = [Buf("uTc%d" % i) for i in range(NS)]
    B_vcb = [Buf("vcb%d" % i) for i in range(NS)]
    gl = [ar.alloc([128, TP], BF16, "gl%d" % i) for i in range(3)]
    AT = [ar.alloc([128, TP], BF16, "AT%d" % i) for i in range(3)]
    B_gl = [Buf("gl0"), Buf("gl1"), Buf("gl2")]
    B_AT = [Buf("AT0"), Buf("AT1"), Buf("AT2")]
    x1t = ar.alloc([128, D], F32, "x1t")
    oo = ar.alloc([128, D], F32, "ooE")
    jk = oo
    st = ar.alloc([128, 8], F32, "stE")
    B_x1t, B_oo, B_st = Buf("x1t"), Buf("ooE"), Buf("stE")
    B_jk = B_oo
    out_v = L["out_d"].rearrange("(n p) m -> p n m", p=128)
    iota3 = iotab[:, :].unsqueeze(1).to_broadcast([128, NBT, 128])
    npass = L["nblk"] * 2
    NBATCH = TP // NBT

    def emit_loads(p_):
        q = p_ % 2
        tsl = slice(p_ * TP, (p_ + 1) * TP)
        for kc in range(8):
            sc.dma("sp", h2Tp2[q][:, kc, :], L["h2T_d"][kc * 128:(kc + 1) * 128, tsl], writes=[B_h2Tp2[q]])
        for q3 in range(3):
            sc.dma("sp", IT2[q][:, q3, :], L["selT_d"][q3, :, tsl], writes=[B_ITf2[q]])
        A(lambda e: e.activation(out=ITb2[q][:, :, :], in_=IT2[q][:, :, :], func=AF.Copy), [B_ITf2[q]], [B_IT2[q]])

    gcount = [0]

    def gbatch_gen(p_, b):
        q = p_ % 2
        Gs, B_Gs, ITb, B_IT = Gs2[q], B_Gs2[q], ITb2[q], B_IT2[q]
        gi = gcount[0]
        gcount[0] += 1
        Lk, bLk = Lb[gi % 2], B_Lb[gi % 2]
        Rk, bRk = Rb[gi % 2], B_Rb[gi % 2]
        eq_ = eqb[0]
        H = NBT // 2
        for hf in range(2):
            hs_ = slice(hf * H, (hf + 1) * H)
            bs_ = slice(b * NBT + hf * H, b * NBT + (hf + 1) * H)
            io_ = iotab[:, :].unsqueeze(1).to_broadcast([128, H, 128])
            V(lambda e: e.tensor_tensor(out=eq_[:, hs_, :], in0=io_, in1=ITb[:, 0, bs_].unsqueeze(2).to_broadcast([128, H, 128]), op=ALU.is_equal), [B_IT, B_iota], [B_eqh[hf]])
            yield
            V(lambda e: e.tensor_tensor(out=Lk[:, hs_, :], in0=eq_[:, hs_, :], in1=ITb[:, 2, bs_].unsqueeze(2).to_broadcast([128, H, 128]), op=ALU.mult), [B_eqh[hf], B_IT], [bLk])
            yield
            V(lambda e: e.tensor_tensor(out=Rk[:, hs_, :], in0=io_, in1=ITb[:, 1, bs_].unsqueeze(2).to_broadcast([128, H, 128]), op=ALU.is_equal), [B_IT, B_iota], [bRk])
            yield
        yield
        yield
        for t4 in range(NBT // 4):
            pg, bpg = ps[7], B_ps[7]
            for tt in range(4):
                t = t4 * 4 + tt
                P(lambda e: e.matmul(pg[:, tt * 128:(tt + 1) * 128], lhsT=Lk[:, t, :], rhs=Rk[:, t, :], start=True, stop=True), [bLk, bRk], [bpg])
            t0 = b * NBT + t4 * 4
            src = pg[:, :].rearrange("p (t i) -> p i t", i=128)
            A(lambda e: e.activation(out=Gs[:, :, t0:t0 + 4], in_=src, func=AF.Copy), [bpg], [B_Gs])
            yield

    def gall_gen(p_):
        for b in range(NBATCH):
            for _ in gbatch_gen(p_, b):
                yield

    emit_loads(0)
    for _ in gall_gen(0):
        pass
    for p_ in range(npass):
        q = p_ % 2
        Gs, B_Gs, h2Tp, B_h2Tp = Gs2[q], B_Gs2[q], h2Tp2[q], B_h2Tp2[q]
        if p_ + 1 < npass:
            emit_loads(p_ + 1)

        def load_u(c):
            sc.dma("sp", uTc[c % NS][:, :], L["uv_d"][c, :, 0:1024], writes=[B_uTc[c % NS]])

        def emit_hu(c):
            k = c % NS
            if c == 0:
                load_u(0)
                load_u(1)
            if c + 2 < 128:
                load_u(c + 2)
            sc.dma("sp", vcb[k][:, :], L["uv_d"][c, :, 1024:2048], writes=[B_vcb[k]])
            pH, bH = ps[4 + c % 3], B_ps[4 + c % 3]
            for kc in range(8):
                P(lambda e: e.matmul(pH[:, 0:TP], lhsT=uTc[k][:, kc * 128:(kc + 1) * 128], rhs=h2Tp[:, kc, :], start=(kc == 0), stop=(kc == 7)), [B_uTc[k], B_h2Tp], [bH])

        def emit_y2(c):
            k = c % NS
            pH, bH = ps[4 + c % 3], B_ps[4 + c % 3]
            g_, bg_ = gl[c % 3], B_gl[c % 3]
            a_, ba_ = AT[c % 3], B_AT[c % 3]
            A(lambda e: e.activation(out=g_[:, :], in_=pH[:, 0:TP], func=AF.Gelu), [bH], [bg_])
            V(lambda e: e.tensor_tensor(out=a_[:, :], in0=g_[:, :], in1=Gs[:, c, :], op=ALU.mult), [bg_, B_Gs], [ba_])
            for tt in range(2):
                for half in range(2):
                    py, bpy = ps[tt * 2 + half], B_ps[tt * 2 + half]
                    P(lambda e: e.matmul(py[:, :], lhsT=a_[:, tt * 128:(tt + 1) * 128], rhs=vcb[k][:, half * 512:(half + 1) * 512], start=(c == 0), stop=(c == 127)),
                      [ba_, B_vcb[k]], [bpy])
        emit_hu(0)
        emit_hu(1)
        gg = gall_gen(p_ + 1) if p_ + 1 < npass else None
        steps_per_chunk = (NBATCH * 10 + 127) // 128
        for c in range(128):
            if c + 2 < 128:
                emit_hu(c + 2)
            emit_y2(c)
            if gg is not None:
                for _ in range(steps_per_chunk):
                    next(gg, None)
        if gg is not None:
            for _ in gg:
                pass
        for tt in range(2):
            n = p_ * 2 + tt
            sc.dma("sp", x1t[:, :], L["x1s_d"][n * 128:(n + 1) * 128, :], writes=[B_x1t])
            for half in range(2):
                hsl = slice(half * 512, (half + 1) * 512)
                py, bpy = ps[tt * 2 + half], B_ps[tt * 2 + half]
                if "y2dbg" in dbg:
                    V(lambda e: e.tensor_copy(out=oo[:, hsl], in_=py[:, :]), [bpy], [B_oo])
                    sc.dma("sp", L["y2_d"][n * 128:(n + 1) * 128, hsl], oo[:, hsl], reads=[B_oo])
                V(lambda e: e.tensor_tensor(out=oo[:, hsl], in0=py[:, :], in1=gt_bc[:, 3, hsl], op=ALU.mult), [bpy, B_gt], [B_oo])
            V(lambda e: e.scalar_tensor_tensor(out=x1t[:, :], in0=x1t[:, :], scalar=ALPHA, in1=oo[:, :], op0=ALU.mult, op1=ALU.add), [B_x1t, B_oo], [B_x1t])
            A(lambda e: e.activation(out=oo[:, :], in_=x1t[:, :], func=AF.Copy, accum_out=st[:, 0:1]), [B_x1t], [B_st, B_oo])
            A(lambda e: e.activation(out=oo[:, :], in_=x1t[:, :], func=AF.Square, accum_out=st[:, 1:2]), [B_x1t], [B_st, B_oo])
            V(lambda e: e.tensor_scalar(out=st[:, 2:3], in0=st[:, 0:1], scalar1=1.0 / D, scalar2=None, op0=ALU.mult), [B_st], [B_st])
            V(lambda e: e.tensor_tensor(out=st[:, 3:4], in0=st[:, 2:3], in1=st[:, 2:3], op=ALU.mult), [B_st], [B_st])
            V(lambda e: e.scalar_tensor_tensor(out=st[:, 4:5], in0=st[:, 1:2], scalar=1.0 / D, in1=st[:, 3:4], op0=ALU.mult, op1=ALU.subtract), [B_st], [B_st])
            V(lambda e: e.tensor_scalar(out=st[:, 4:5], in0=st[:, 4:5], scalar1=1e-5, scalar2=None, op0=ALU.add), [B_st], [B_st])
            A(lambda e: e.activation(out=st[:, 5:6], in_=st[:, 4:5], func=AF.Sqrt), [B_st], [B_st])
            V(lambda e: e.reciprocal(out=st[:, 5:6], in_=st[:, 5:6]), [B_st], [B_st])
            V(lambda e: e.scalar_tensor_tensor(out=st[:, 6:7], in0=st[:, 2:3], scalar=-1.0, in1=st[:, 5:6], op0=ALU.mult, op1=ALU.mult), [B_st], [B_st])
            A(lambda e: e.activation(out=oo[:, :], in_=x1t[:, :], func=AF.Identity, scale=st[:, 5:6], bias=st[:, 6:7]), [B_x1t, B_st], [B_oo])
            G(lambda e: e.tensor_tensor(out=oo[:, :], in0=oo[:, :], in1=lnbc[:, 0, :], op=ALU.mult), [B_oo, B_ln], [B_oo])
            G(lambda e: e.tensor_tensor(out=oo[:, :], in0=oo[:, :], in1=lnbc[:, 1, :], op=ALU.add), [B_oo, B_ln], [B_oo])
            sc.dma("sp", out_v[:, n, :], oo[:, :], reads=[B_oo])
    ar.release(mE)


def make_e0(L, NB, bank):
    sc, ar, ps, B_ps = L["sc"], L["ar"], L["ps"], L["B_ps"]
    identb, B_ident = L["identb"], L["B_ident"]
    A = lambda fn, r=(), w=(): sc.op("act", fn, r, w)
    P = lambda fn, r=(), w=(): sc.op("pe", fn, r, w)
    G = lambda fn, r=(), w=(): sc.op("pool", fn, r, w)
    NF = 3
    stf = [ar.alloc([128, D], F32, "e0f%d" % i) for i in range(NF)]
    o16 = [ar.alloc([128, D], BF16, "e0h%d" % i) for i in range(NF)]
    uT = [ar.alloc([128, D], BF16, "e0t%d" % i) for i in range(2)]
    B_f = [Buf("e0f%d" % i) for i in range(NF)]
    B_o = [Buf("e0h%d" % i) for i in range(NF)]
    B_t = [Buf("e0t0"), Buf("e0t1")]
    pu_v = L["pu_d"].rearrange("(i1 i2) d -> i2 i1 d", i2=128)
    pv_v = L["pv_d"].rearrange("(i1 i2) d -> i2 i1 d", i2=128)
    NI = 256

    def load(i):
        c, isv = i // 2, i % 2
        sc.dma("sp", stf[i % NF][:, :], (pv_v if isv else pu_v)[c, :, :], writes=[B_f[i % NF]])

    def cast(i):
        G(lambda e: e.tensor_copy(out=o16[i % NF][:, :], in_=stf[i % NF][:, :]), [B_f[i % NF]], [B_o[i % NF]])

    def finish(i):
        c, isv = i // 2, i % 2
        if isv:
            sc.dma("sp", L["uv_d"][c, :, 1024:2048], o16[i % NF][:, :], reads=[B_o[i % NF]])
        else:
            pT = ps[bank][:, :].bitcast(BF16)
            for kc in range(8):
                P(lambda e: e.transpose(pT[:, kc * 128:(kc + 1) * 128], o16[i % NF][:, kc * 128:(kc + 1) * 128], identb[:, :]), [B_o[i % NF], B_ident], [B_ps[bank]])
            A(lambda e: e.activation(out=uT[c % 2][:, :], in_=pT[:, :], func=AF.Copy), [B_ps[bank]], [B_t[c % 2]])
            sc.dma("sp", L["uv_d"][c, :, 0:1024], uT[c % 2][:, :], reads=[B_t[c % 2]])
    load(0)
    load(1)
    cast(0)
    for k in range(NI):
        if k + 2 < NI:
            load(k + 2)
        if k + 1 < NI:
            cast(k + 1)
        finish(k)
        yield
```

```python
import numpy as np
import ml_dtypes
import concourse.bass as bass
import concourse.mybir as mybir
from concourse.bass_utils import run_bass_kernel_spmd

F32 = mybir.dt.float32
BF16 = mybir.dt.bfloat16
I32 = mybir.dt.int32
U32 = mybir.dt.uint32
AF = mybir.ActivationFunctionType
ALU = mybir.AluOpType
AX = mybir.AxisListType

S = 4096
D = 1024
NT = S // 128
IN_COLS = 5316
DBG = {}
STOP_AFTER = None


class Buf:
    __slots__ = ("name", "lw", "rd")

    def __init__(self, name):
        self.name = name
        self.lw = None
        self.rd = []


class _Rec:
    def __init__(self):
        self.call = None

    def __getattr__(self, name):
        def f(*args, **kwargs):
            self.call = (name, args, kwargs)
            return self
        return f


class Sched:
    ENGS = ("pe", "act", "dve", "pool", "sp")

    def __init__(self, nc, n_dma_sems=40):
        self.nc = nc
        self.ops = {e: [] for e in self.ENGS}
        self.sem = {e: nc.alloc_semaphore("c_" + e) for e in self.ENGS}
        self.cnt = {e: 0 for e in self.ENGS}
        self.seen = {e: {} for e in self.ENGS}
        self.dsem = [nc.alloc_semaphore("d%d" % i) for i in range(n_dma_sems)]
        self.dval = [0] * n_dma_sems
        self.drr = 0
        self.drr_sw = 0
        self.NSW = 8
        self.NHW = n_dma_sems - 8
        self.all_events = []

    def _waits(self, eng, reads, writes):
        deps = []
        for b in reads:
            if b.lw is not None:
                deps.append(b.lw)
        for b in writes:
            if b.lw is not None:
                deps.append(b.lw)
            deps.extend(b.rd)
        out = {}
        for (sem, val, src) in deps:
            if src == "pe" and eng == "pe":
                continue
            k = sem.num
            if self.seen[eng].get(k, 0) >= val:
                continue
            if out.get(k, (None, 0))[1] < val:
                out[k] = (sem, val)
        for k, (sem, val) in out.items():
            self.seen[eng][k] = val
        return list(out.values())

    def op(self, eng, fn, reads=(), writes=()):
        waits = self._waits(eng, reads, writes)
        self.cnt[eng] += 1
        sem = self.sem[eng]
        val = self.cnt[eng]
        ev = (sem, val, eng)

        rec = _Rec()
        fn(rec)
        name, args, kwargs = rec.call

        def emit(e, waits=waits, sem=sem, name=name, args=args, kwargs=kwargs):
            for (s, v) in waits:
                e.wait_ge(s, v)
            getattr(e, name)(*args, **kwargs).then_inc(sem, 1)
        self.ops[eng].append(emit)
        for b in reads:
            b.rd.append(ev)
        for b in writes:
            b.lw = ev
            b.rd = []
        return ev

    def dma(self, eng, out, in_, reads=(), writes=(), fn=None, **kw):
        if fn is not None:
            rec = _Rec()
            fn(rec)
            mname, margs, mkw = rec.call
        else:
            mname, margs, mkw = "dma_start", (), dict(out=out, in_=in_, **kw)
        if eng == "pool":
            i = self.NHW + (self.drr_sw % self.NSW)
            self.drr_sw += 1
        else:
            i = self.drr
            self.drr = (self.drr + 1) % self.NHW
        sem = self.dsem[i]
        waits = self._waits(eng, reads, writes)
        prev = self.dval[i]
        if prev > 0 and self.seen[eng].get(sem.num, 0) < prev:
            waits.append((sem, prev))
            self.seen[eng][sem.num] = prev
        self.dval[i] += 16
        val = self.dval[i]
        ev = (sem, val, "dma")

        def emit(e, waits=waits, sem=sem, mname=mname, margs=margs, mkw=mkw):
            for (s, v) in waits:
                e.wait_ge(s, v)
            getattr(e, mname)(*margs, **mkw).then_inc(sem, 16)
        self.ops[eng].append(emit)
        for b in reads:
            b.rd.append(ev)
        for b in writes:
            b.lw = ev
            b.rd = []
        self.all_events.append(ev)
        return ev

    def raw(self, eng, fn):
        self.ops[eng].append(fn)

    def barrier(self):
        targets = []
        for en in self.ENGS:
            if self.cnt[en] > 0:
                targets.append((self.sem[en], self.cnt[en], en))
        for i, s in enumerate(self.dsem):
            if self.dval[i] > 0:
                targets.append((s, self.dval[i], "dma"))
        for eng in self.ENGS:
            waits = []
            for (s, v, src) in targets:
                if src == eng:
                    continue
                if self.seen[eng].get(s.num, 0) >= v:
                    continue
                self.seen[eng][s.num] = v
                waits.append((s, v))

            def emit(e, waits=waits):
                for (s, v) in waits:
                    e.wait_ge(s, v)
            self.ops[eng].append(emit)

    def finish(self, final_events):
        nc = self.nc
        with nc.Block() as block:
            def run(name):
                def f(e):
                    for emit in self.ops[name]:
                        emit(e)
                    if name == "sp":
                        for (s, v, _) in final_events:
                            e.wait_ge(s, v)
                        for i, s in enumerate(self.dsem):
                            if self.dval[i] > 0:
                                e.wait_ge(s, self.dval[i])
                        for en in ("pe", "act", "dve", "pool"):
                            if self.cnt[en] > 0:
                                e.wait_ge(self.sem[en], self.cnt[en])
                return f
            block.tensor(run("pe"))
            block.scalar(run("act"))
            block.vector(run("dve"))
            block.gpsimd(run("pool"))
            block.sync(run("sp"))


class Arena:
    def __init__(self, nc, base=0, top=192 * 1024):
        self.nc = nc
        self.off = base
        self.top = top
        self.n = 0
        self.sc = None

    def mark(self):
        return self.off

    def release(self, m):
        self.off = m
        if self.sc is not None:
            self.sc.barrier()

    def alloc(self, shape, dtype, name="t"):
        esz = {F32: 4, BF16: 2, I32: 4, U32: 4}[dtype]
        per = esz
        for s in shape[1:]:
            per *= s
        per = (per + 63) // 64 * 64
        self.n += 1
        t = self.nc.alloc_sbuf_tensor_at("%s_%d" % (name, self.n), list(shape), dtype, offset=self.off)
        self.off += per
        assert self.off <= self.top, ("SBUF overflow", name, self.off)
        return t


def build(dbg=None, stop_after=None, phases=None, feed=(), nblk=8):
    dbg = dbg or {}
    phases = phases or {'0', 'A', 'B', 'C', 'D', 'E', 'F'}
    nc = bass.Bass("TRN2", target_bir_lowering=False)
    sc = Sched(nc)
    ar = Arena(nc, base=(nc.sbuf_base + 63) // 64 * 64, top=nc.sbuf_top // 64 * 64)
    ar.sc = sc

    def din(name, shape, dt=F32):
        return nc.dram_tensor(name, list(shape), dt, kind="ExternalInput").ap()

    def dscratch(name, shape, dt=F32):
        kind = "ExternalOutput" if name in dbg else ("ExternalInput" if name in feed else "Internal")
        return nc.dram_tensor(name, list(shape), dt, kind=kind).ap()

    x_d = din("x", [S, D])
    c_d = din("c_col", [128, 8])
    wada_d = din("w_ada", [D, 6 * D])
    bada_col_d = din("b_ada_col", [128, 48])
    bada_bc_d = din("b_ada_bc", [128, 6 * D])
    win_d = din("w_in", [D, IN_COLS])
    ident_d = din("ident", [128, 128])
    out_d = nc.dram_tensor("out", [S, D], F32, kind="ExternalOutput").ap()

    mu_d = din("mu_col", [128, 14])
    rwvec_d = din("rwvec", [128, 20])
    w2a2_d = din("w2a2", [128, 512])
    g2_d = din("g2", [128, 512])
    gnbc_d = din("gn_bc", [128, 2, 256])
    cst_d = din("cst", [128, 1024])
    wbra_d = din("w_br_a", [512, D])
    wbrb_d = din("w_br_b", [D, D])
    wout_d = din("w_out", [D, D])
    lnbc_d = din("ln_bc", [128, 4, D])
    wq_d = din("peer_wq", [D, D])
    pkeys_d = din("peer_keysT", [128, 8, 128])
    pu_d = din("peer_u", [16384, D])
    pv_d = din("peer_v", [16384, D])
    cvec_d = din("cvec", [128, 256])
    biasT_d = din("biasT", [128, 3, 1024])
    negm_d = din("negm", [128, 128])
    zrw_d = dscratch("zrw", [1792, S], F32)
    zq_d = dscratch("zq", [1024, S], BF16)
    zqi_d = dscratch("zqi", [256, S], BF16)
    zg_d = dscratch("zg", [2048, S], BF16)
    ztok_d = dscratch("ztok", [S, 196], F32)
    rwo_d = dscratch("rwo", [512, S], BF16)
    dsao_d = dscratch("dsao", [1024, S], BF16)
    x1_d = dscratch("x1dbg", [S, D], F32)
    y2_d = dscratch("y2dbg", [S, D], F32)
    x1s_d = dscratch("x1s", [S, D], F32)
    h2T_d = dscratch("h2T", [D, S], BF16)
    selT_d = dscratch("selT", [3, 128, S], F32)
    uv_d = dscratch("uv16", [128, 128, 2048], BF16)
    iota_d = din("iota128", [128, 128])
    iota128 = ar.alloc([128, 128], F32, "iota128")
    B_iota = Buf("iota")
    iota16 = iota128[:, 0:16]

    ident = ar.alloc([128, 128], F32, "ident")
    identb = ar.alloc([128, 128], BF16, "identb")
    modcol = ar.alloc([128, 48], F32, "modcol")
    onep1 = ar.alloc([128, 8], F32, "onep1")
    onep2 = ar.alloc([128, 8], F32, "onep2")
    gt_bc = ar.alloc([128, 4, D], F32, "gt_bc")
    B_ident = Buf("ident")
    B_mod = Buf("mod")
    B_gt = Buf("gt")

    ps = [nc.alloc_psum_tensor("ps%d" % i, [128, 512], F32) for i in range(8)]
    B_ps = [Buf("ps%d" % i) for i in range(8)]

    sc.dma("sp", ident[:, :], ident_d[:, :], writes=[B_ident])
    sc.dma("sp", iota128[:, :], iota_d[:, :], writes=[B_iota])

    mark_stg = ar.mark()
    STG = 1024
    stg = [ar.alloc([128, STG], F32, "stg%d" % i) for i in range(3)]
    B_stg = [Buf("stg%d" % i) for i in range(3)]
    stg_i = [0]

    def load_bf16(dst, src, n, bdst, eng="pool"):
        P = dst.shape[0]
        for o in range(0, n, STG):
            w = min(STG, n - o)
            k = stg_i[0] % 3
            stg_i[0] += 1
            sc.dma("sp", stg[k][0:P, 0:w], src[:, o:o + w], writes=[B_stg[k]])
            sc.op(eng, lambda e, k=k, o=o, w=w, P=P, dst=dst: e.tensor_copy(out=dst[:, o:o + w], in_=stg[k][0:P, 0:w]),
                  reads=[B_stg[k]], writes=[bdst])
    sc.op("dve", lambda e: e.tensor_copy(out=identb[:, :], in_=ident[:, :]), reads=[B_ident], writes=[B_ident])

    if '0' in phases:
        m0 = ar.mark()
        c_sb = ar.alloc([128, 8], F32, "c_sb")
        sil = ar.alloc([128, 8], F32, "sil")
        silbc = ar.alloc([128, 8, 128], F32, "silbc")
        bcol = ar.alloc([128, 48], F32, "bcol")
        bbc = ar.alloc([128, 4, D], F32, "bbc")
        wa = [ar.alloc([128, 8, 1024], F32, "wa%d" % i) for i in range(4)]
        B_c = Buf("c")
        B_wa = [Buf("wa0"), Buf("wa1"), Buf("wa2"), Buf("wa3")]
        B_b = Buf("bcol")
        sc.dma("sp", c_sb[:, :], c_d[:, :], writes=[B_c])
        sc.dma("sp", bcol[:, :], bada_col_d[:, :], writes=[B_b])
        for gi_, g_ in enumerate((2, 3, 4, 5)):
            sc.dma("sp", bbc[:, gi_, :], bada_bc_d[:, g_ * D:(g_ + 1) * D], writes=[B_b])
        sc.op("act", lambda e: e.activation(out=sil[:, :], in_=c_sb[:, :], func=AF.Silu), reads=[B_c], writes=[B_c])
        for kc in range(8):
            sc.op("dve", lambda e, kc=kc: e.tensor_copy(out=silbc[:, kc, :], in_=sil[:, kc:kc + 1].to_broadcast([128, 128])),
                  reads=[B_c], writes=[B_c])
        wada_v = wada_d.rearrange("(kc p) n -> p kc n", p=128)
        for g in range(6):
            w = wa[g % 4]
            bw = B_wa[g % 4]
            for kc in range(8):
                sc.dma("sp", w[:, kc, :], wada_v[:, kc, g * 1024:(g + 1) * 1024], writes=[bw])
            if g in (2, 3, 4, 5):
                gi = g - 2
                for half in range(2):
                    p = ps[half]
                    for kc in range(8):
                        sc.op("pe", lambda e, p=p, w=w, kc=kc, half=half: e.matmul(
                            p[:, :], lhsT=silbc[:, kc, :], rhs=w[:, kc, half * 512:(half + 1) * 512],
                            start=(kc == 0), stop=(kc == 7)), reads=[bw, B_c], writes=[B_ps[half]])
                    sc.op("dve", lambda e, p=p, gi=gi, half=half: e.tensor_tensor(
                        out=gt_bc[:, gi, half * 512:(half + 1) * 512], in0=p[:, :],
                        in1=bbc[:, gi, half * 512:(half + 1) * 512], op=ALU.add),
                        reads=[B_ps[half], B_b], writes=[B_gt])
            if g in (0, 1, 3, 4):
                p = ps[2]
                for fc in range(8):
                    for kc in range(8):
                        sc.op("pe", lambda e, p=p, w=w, kc=kc, fc=fc: e.matmul(
                            p[:, fc:fc + 1], lhsT=w[:, kc, fc * 128:(fc + 1) * 128], rhs=sil[:, kc:kc + 1],
                            start=(kc == 0), stop=(kc == 7)), reads=[bw, B_c], writes=[B_ps[2]])
                sc.op("dve", lambda e, p=p, g=g: e.tensor_tensor(
                    out=modcol[:, g * 8:(g + 1) * 8], in0=p[:, 0:8], in1=bcol[:, g * 8:(g + 1) * 8], op=ALU.add),
                    reads=[B_ps[2], B_b], writes=[B_mod])
        sc.op("dve", lambda e: e.tensor_scalar(out=onep1[:, :], in0=modcol[:, 8:16], scalar1=1.0, scalar2=None, op0=ALU.add),
              reads=[B_mod], writes=[B_mod])
        sc.op("dve", lambda e: e.tensor_scalar(out=onep2[:, :], in0=modcol[:, 32:40], scalar1=1.0, scalar2=None, op0=ALU.add),
              reads=[B_mod], writes=[B_mod])
        sc.op("dve", lambda e: e.tensor_scalar(out=gt_bc[:, 2, :], in0=gt_bc[:, 2, :], scalar1=1.0, scalar2=None, op0=ALU.add),
              reads=[B_gt], writes=[B_gt])
        ar.release(m0)
        if "modcol" in dbg:
            dd = nc.dram_tensor("modcol_o", [128, 48], F32, kind="ExternalOutput").ap()
            sc.dma("sp", dd[:, :], modcol[:, :], reads=[B_mod])
            dd2 = nc.dram_tensor("gt_o", [128, 4 * D], F32, kind="ExternalOutput").ap()
            sc.dma("sp", dd2[:, :], gt_bc[:, :, :].rearrange("p a b -> p (a b)"), reads=[B_gt])

    if 'A' in phases:
        mA = ar.mark()
        fm_chunks = []
        for i in range(14):
            fm_chunks.append((i * 128, "rw", i))
        for i in range(8):
            fm_chunks.append((1792 + i * 128, "q", i))
        for i in range(2):
            fm_chunks.append((2944 + i * 128, "qi", i))
        for i in range(16):
            fm_chunks.append((3268 + i * 128, "g", i))
        winb = ar.alloc([128, 8, IN_COLS], BF16, "winb")
        B_win = Buf("win")
        win_v = win_d.rearrange("(kc p) n -> p kc n", p=128)
        for kc in range(8):
            load_bf16(winb[:, kc, :], win_v[:, kc, :], IN_COLS, B_win)
        xt = [ar.alloc([128, 4, D], F32, "xt%d" % i) for i in range(2)]
        B_xt = [Buf("xt0"), Buf("xt1")]
        hT = [ar.alloc([128, 8, 512], BF16, "hT%d" % i) for i in range(2)]
        B_hT = [Buf("hT0"), Buf("hT1")]
        NEV = 8
        ev32 = [ar.alloc([128, 512], F32, "ev32_%d" % i) for i in range(NEV)]
        ev16 = [ar.alloc([128, 512], BF16, "ev16_%d" % i) for i in range(NEV)]
        B_ev32 = [Buf("ev32_%d" % i) for i in range(NEV)]
        B_ev16 = [Buf("ev16_%d" % i) for i in range(NEV)]
        ztk = [ar.alloc([128, 196], F32, "ztk%d" % i) for i in range(2)]
        B_ztk = [Buf("ztk0"), Buf("ztk1")]
        x_v = x_d.rearrange("(n p) m -> p n m", p=128)
        pi = 0
        evi = 0
        tok_cols = [(2816, 128, 0), (3200, 68, 128)]
        for tb in range(8):
            xb = xt[tb % 2]
            bx = B_xt[tb % 2]
            hb = hT[tb % 2]
            bh = B_hT[tb % 2]
            for j in range(4):
                sc.dma("sp", xb[:, j, :], x_v[:, tb * 4 + j, :], writes=[bx])
            for kc in range(8):
                p = ps[pi % 8]
                bp = B_ps[pi % 8]
                pi += 1
                for j in range(4):
                    sc.op("pe", lambda e, p=p, xb=xb, j=j, kc=kc: e.transpose(
                        p[:, j * 128:(j + 1) * 128], xb[:, j, kc * 128:(kc + 1) * 128], ident[:, :]),
                        reads=[bx, B_ident], writes=[bp])
                sc.op("act", lambda e, p=p, hb=hb, kc=kc: e.activation(
                    out=hb[:, kc, :], in_=p[:, :], func=AF.Identity,
                    scale=onep1[:, kc:kc + 1], bias=modcol[:, kc:kc + 1]),
                    reads=[bp, B_mod], writes=[bh])
            for ci, (col0, kind, idx) in enumerate(fm_chunks):
                p = ps[pi % 8]
                bp = B_ps[pi % 8]
                pi += 1
                for kc in range(8):
                    sc.op("pe", lambda e, p=p, kc=kc, col0=col0, hb=hb: e.matmul(
                        p[:, :], lhsT=winb[:, kc, col0:col0 + 128], rhs=hb[:, kc, :],
                        start=(kc == 0), stop=(kc == 7)), reads=[B_win, bh], writes=[bp])
                k = evi % NEV
                evi += 1
                eng = "dve" if (ci % 2 == 0) else "act"
                tsl = slice(tb * 512, (tb + 1) * 512)
                if kind == "rw":
                    dst = ev32[k]
                    bd = B_ev32[k]
                    if eng == "dve":
                        sc.op("dve", lambda e, p=p, dst=dst: e.tensor_copy(out=dst[:, :], in_=p[:, :]), reads=[bp], writes=[bd])
                    else:
                        sc.op("act", lambda e, p=p, dst=dst: e.activation(out=dst[:, :], in_=p[:, :], func=AF.Copy), reads=[bp], writes=[bd])
                    sc.dma("sp", zrw_d[idx * 128:(idx + 1) * 128, tsl], dst[:, :], reads=[bd])
                elif kind in ("q", "qi"):
                    dst = ev16[k]
                    bd = B_ev16[k]
                    if eng == "dve":
                        sc.op("dve", lambda e, p=p, dst=dst: e.tensor_copy(out=dst[:, :], in_=p[:, :]), reads=[bp], writes=[bd])
                    else:
                        sc.op("act", lambda e, p=p, dst=dst: e.activation(out=dst[:, :], in_=p[:, :], func=AF.Copy), reads=[bp], writes=[bd])
                    dd = zq_d if kind == "q" else zqi_d
                    sc.dma("sp", dd[idx * 128:(idx + 1) * 128, tsl], dst[:, :], reads=[bd])
                else:
                    dst = ev16[k]
                    bd = B_ev16[k]
                    sc.op("act", lambda e, p=p, dst=dst: e.activation(out=dst[:, :], in_=p[:, :], func=AF.Sigmoid), reads=[bp], writes=[bd])
                    sc.dma("sp", zg_d[idx * 128:(idx + 1) * 128, tsl], dst[:, :], reads=[bd])
            for j in range(4):
                p = ps[pi % 8]
                bp = B_ps[pi % 8]
                pi += 1
                for (c0, ncol, o0) in tok_cols:
                    for kc in range(8):
                        sc.op("pe", lambda e, p=p, kc=kc, j=j, c0=c0, ncol=ncol, o0=o0, hb=hb: e.matmul(
                            p[:, o0:o0 + ncol], lhsT=hb[:, kc, j * 128:(j + 1) * 128], rhs=winb[:, kc, c0:c0 + ncol],
                            start=(kc == 0), stop=(kc == 7)), reads=[B_win, bh], writes=[bp])
                zt = ztk[j % 2]
                bz = B_ztk[j % 2]
                sc.op("dve", lambda e, p=p, zt=zt: e.tensor_copy(out=zt[:, :], in_=p[:, 0:196]), reads=[bp], writes=[bz])
                r0 = (tb * 4 + j) * 128
                sc.dma("sp", ztok_d[r0:r0 + 128, :], zt[:, :], reads=[bz])
        ar.release(mA)

    e0_done_flag = [False]
    if 'B' in phases:
        phase_B(locals())
    if 'C' in phases:
        phase_C(locals())
    if 'D' in phases:
        phase_D(locals())
    if 'E' in phases:
        phase_E(locals())

    sc.finish([])
    return nc


def _prep_inputs(inputs):
    f = lambda a: np.ascontiguousarray(np.asarray(a, dtype=np.float32))
    x = f(inputs["x"])
    c = f(inputs["c"])
    b_ada = f(inputs["b_ada"])[0]
    shared = {
        "w_ada": f(inputs["w_ada"])[0],
        "b_ada_col": np.ascontiguousarray(b_ada.reshape(48, 128).T),
        "b_ada_bc": np.ascontiguousarray(np.broadcast_to(b_ada[None, :], (128, 6 * D))),
        "w_in": f(inputs["w_in"])[0],
        "ident": np.eye(128, dtype=np.float32),
        "iota128": np.ascontiguousarray(np.broadcast_to(np.arange(128, dtype=np.float32)[None], (128, 128))),
    }
    col = lambda v, n: np.ascontiguousarray(f(v).reshape(n, 128).T)
    shared["mu_col"] = col(inputs["rw_mu"][0], 14)
    shared["rwvec"] = np.ascontiguousarray(np.concatenate([col(inputs["rw_w0"][0], 4), col(inputs["rw_a0"][0], 4), col(inputs["rw_k_k"][0], 4),
                                                            col(inputs["rw_k_a"][0], 4), col(f(inputs["rw_r_k"])[0].reshape(-1), 4)], axis=1))
    shared["w2a2"] = np.ascontiguousarray(np.concatenate([f(inputs["rw_w2"])[0], f(inputs["rw_a2"])[0]], axis=0))
    shared["g2"] = f(inputs["rw_g2"])[0]
    gn2 = np.stack([f(inputs["rw_gn_g"])[0], f(inputs["rw_gn_b"])[0]]).reshape(2, 4, 2, 64)
    gnl = np.zeros((128, 2, 4, 64), np.float32)
    gnl[0:64] = gn2[:, :, 0, :][None]
    gnl[64:128] = gn2[:, :, 1, :][None]
    shared["gn_bc"] = np.ascontiguousarray(gnl.reshape(128, 2, 256))
    cst = np.zeros((128, 1024), np.float32)
    iu = np.triu(np.ones((64, 64), np.float32), 1)
    il = np.triu(np.ones((64, 64), np.float32), 0)
    cst[:, 0:128] = np.block([[iu, il], [iu, il]])
    cst[0:64, 128:192] = iu.T
    cst[64:128, 128:192] = iu.T
    cst[0:64, 192:256] = 1.0
    cst[64:128, 256:320] = 1.0
    sm = np.ones(512, np.float32); sm[::64] = 0.0
    cst[:, 320:832] = sm[None, :]
    cst[0:64, 832:896] = np.eye(64, dtype=np.float32)
    cst[64:128, 832:896] = np.eye(64, dtype=np.float32)
    cst[:, 896] = 1.0
    shared["cst"] = cst
    shared["w_br_a"] = f(inputs["w_br_a"])[0]
    shared["w_br_b"] = f(inputs["w_br_b"])[0]
    shared["w_out"] = f(inputs["w_out"])[0]
    lnr = np.stack([f(inputs["ln1_g"])[0], f(inputs["ln1_b"])[0], f(inputs["ln2_g"])[0], f(inputs["ln2_b"])[0]])
    shared["ln_bc"] = np.ascontiguousarray(np.broadcast_to(lnr[None], (128, 4, D)))
    shared["peer_wq"] = f(inputs["peer_wq"])[0]
    pk = f(inputs["peer_keys"])[0]
    shared["peer_keysT"] = np.ascontiguousarray(pk.transpose(1, 3, 0, 2).reshape(128, 8, 128))
    shared["peer_u"] = f(inputs["peer_u"])[0]
    cv = np.concatenate([f(inputs["dsa_kv_g"])[0], f(inputs["idx_k_g"])[0], f(inputs["idx_k_b"])[0]])
    shared["cvec"] = np.ascontiguousarray(np.broadcast_to(cv[None], (128, 256)))
    rb = f(inputs["rel_bias"])
    nn_ = np.arange(0, 256)
    nf = np.maximum(nn_, 1).astype(np.float32)
    large = 16 + (np.log(nf / np.float32(16)) / np.float32(np.log(8.0)) * np.float32(16)).astype(np.int32)
    bucket = np.where(nn_ < 16, nn_, np.minimum(large, 31))
    sI = np.arange(128)[:, None]
    qI = np.arange(128)[None, :]
    bd = bucket[np.clip(qI - sI, 0, 255)]
    bp = bucket[np.clip(qI + 128 - sI, 0, 255)]
    bT = np.zeros((128, 3, 8, 128), np.float32)
    bT[:, 0] = rb[bd].transpose(0, 2, 1)
    bT[:, 1] = rb[bp].transpose(0, 2, 1)
    bT[:, 2] = rb[31][None, :, None]
    shared["biasT"] = np.ascontiguousarray(bT.reshape(128, 3, 1024))
    shared["negm"] = np.where(np.arange(128)[None, :] <= np.arange(128)[:, None], 0.0, -1e30).astype(np.float32)
    shared["peer_v"] = f(inputs["peer_v"])[0]
    maps = []
    for b in range(8):
        m = dict(shared)
        m["x"] = x[b]
        m["c_col"] = np.ascontiguousarray(c[b].reshape(8, 128).T)
        maps.append(m)
    return maps


def kernel(**inputs):
    nc = build()
    maps = _prep_inputs(inputs)
    res = run_bass_kernel_spmd(nc, maps, core_ids=list(range(8)))
    out = np.stack([np.asarray(r["out"], dtype=np.float32) for r in res.results], axis=0)
    return out


def _bc_mid(ap, n):
    sh = list(ap.shape)
    return ap.unsqueeze(1).to_broadcast([sh[0], n] + sh[1:])


def phase_B(L):
    nc, sc, ar, ps, B_ps = L["nc"], L["sc"], L["ar"], L["ps"], L["B_ps"]
    identb, B_ident, load_bf16 = L["identb"], L["B_ident"], L["load_bf16"]
    zrw_d, rwo_d = L["zrw_d"], L["rwo_d"]
    V = lambda fn, r=(), w=(): sc.op("dve", fn, r, w)
    A = lambda fn, r=(), w=(): sc.op("act", fn, r, w)
    P = lambda fn, r=(), w=(): sc.op("pe", fn, r, w)
    mB = ar.mark()
    cst = ar.alloc([128, 1024], F32, "cst")
    mu = ar.alloc([128, 14], F32, "mu")
    rwvec = ar.alloc([128, 20], F32, "rwvec")
    omka = ar.alloc([128, 4], F32, "omka")
    w2a2b = ar.alloc([128, 512], BF16, "w2a2b")
    g2b = ar.alloc([128, 512], BF16, "g2b")
    gnbc = ar.alloc([128, 2, 256], F32, "gnbc")
    bones = ar.alloc([128, 128], BF16, "bones")
    onesb = ar.alloc([128, 1], BF16, "onesb")
    B_c = Buf("cstB")
    sc.dma("sp", cst[:, :], L["cst_d"][:, :], writes=[B_c])
    sc.dma("sp", mu[:, :], L["mu_d"][:, :], writes=[B_c])
    sc.dma("sp", rwvec[:, :], L["rwvec_d"][:, :], writes=[B_c])
    sc.dma("sp", gnbc[:, :, :], L["gnbc_d"][:, :, :], writes=[B_c])
    load_bf16(w2a2b[:, :], L["w2a2_d"][:, :], 512, B_c)
    load_bf16(g2b[:, :], L["g2_d"][:, :], 512, B_c)
    V(lambda e: e.tensor_scalar(out=omka[:, :], in0=rwvec[:, 12:16], scalar1=-1.0, scalar2=1.0, op0=ALU.mult, op1=ALU.add), [B_c], [B_c])
    V(lambda e: e.tensor_copy(out=bones[:, :], in_=cst[:, 192:320]), [B_c], [B_c])
    V(lambda e: e.tensor_copy(out=onesb[:, :], in_=cst[:, 896:897]), [B_c], [B_c])
    maskA = cst[:, 0:128]
    maskT = cst[:, 128:192]
    scanmask = cst[:, 320:832]
    eye64 = cst[:, 832:896]
    w0c, a0c, kkc, kac, rkc = (rwvec[:, 0:4], rwvec[:, 4:8], rwvec[:, 8:12], rwvec[:, 12:16], rwvec[:, 16:20])

    zb = ar.alloc([128, 14, 513], F32, "zb")
    zs = ar.alloc([128, 14, 512], F32, "zs")
    tmp = [ar.alloc([128, 512], F32, "tmpB%d" % i) for i in range(3)]
    B_tmp = [Buf("tmpB%d" % i) for i in range(3)]
    th = ar.alloc([128, 512], BF16, "th")
    al16 = ar.alloc([128, 512], BF16, "al16")
    sgl = ar.alloc([128, 512], BF16, "sgl")
    sq16 = ar.alloc([128, 512], BF16, "sq16")
    asg = ar.alloc([128, 512], F32, "asg")
    kk = ar.alloc([128, 512], F32, "kk")
    kp = ar.alloc([128, 512], F32, "kp")
    bv = ar.alloc([128, 512], F32, "bv")
    lw = ar.alloc([128, 512], F32, "lw")
    cs = ar.alloc([128, 512], F32, "cs")
    E = [ar.alloc([128, 512], F32, "E%d" % i) for i in range(4)]
    E5 = ar.alloc([128, 4, 8], F32, "E5")
    AR = ar.alloc([128, 4, 8, 2, 64], BF16, "AR")
    BK = ar.alloc([128, 4, 8, 2, 64], BF16, "BK")
    KH = ar.alloc([128, 4, 512], BF16, "KH")
    BH = ar.alloc([128, 4, 512], BF16, "BH")
    Vb = ar.alloc([128, 4, 512], BF16, "Vb")
    rkr = ar.alloc([128, 4, 512], BF16, "rkr")
    rwoT = ar.alloc([128, 4, 512], BF16, "rwoT")
    B_zb, B_zs, B_pre, B_blk, B_rwoT = Buf("zb"), Buf("zs"), Buf("pre"), Buf("blk"), Buf("rwoT")
    Vt2p = [ar.alloc([128, 512], BF16, "Vt2_%d" % i) for i in range(2)]
    KHt2p = [ar.alloc([128, 512], BF16, "KHt2_%d" % i) for i in range(2)]
    BHt2p = [ar.alloc([128, 512], BF16, "BHt2_%d" % i) for i in range(2)]
    sABp = [ar.alloc([128, 4, 128], BF16, "sAB%d" % i) for i in range(2)]
    sAKp = [ar.alloc([128, 4, 128], BF16, "sAK%d" % i) for i in range(2)]
    Xfp = [ar.alloc([128, 4, 64], BF16, "Xf%d" % i) for i in range(2)]
    B_Vtp = [Buf("Vt0"), Buf("Vt1")]
    B_sAp = [Buf("sA0"), Buf("sA1")]
    B_Xfp = [Buf("Xf0"), Buf("Xf1")]
    Mx = [ar.alloc([128, 4, 64], BF16, "Mx%d" % i) for i in range(2)]
    MT = [ar.alloc([128, 4, 64], BF16, "MT%d" % i) for i in range(2)]
    X = [ar.alloc([128, 4, 64], BF16, "X%d" % i) for i in range(2)]
    RHSs = ar.alloc([128, 4, 64], BF16, "RHSs")
    SAs = ar.alloc([128, 4, 64], BF16, "SAs")
    ST = ar.alloc([128, 4, 64], BF16, "ST")
    STf = ar.alloc([128, 4, 64], F32, "STf")
    sqy = ar.alloc([128, 256], F32, "sqy")
    yn = ar.alloc([128, 256], F32, "yn")
    bon = ar.alloc([128, 256], F32, "bon")
    O16 = ar.alloc([128, 256], BF16, "O16")
    st8 = ar.alloc([128, 4, 8], F32, "st8")
    B_Vt, B_sA, B_M, B_MT, B_X, B_R, B_SA, B_ST, B_ep, B_st8, B_O = (Buf("Vt"), Buf("sA"), [Buf("M0"), Buf("M1")], [Buf("MT0"), Buf("MT1")],
                                                                    [Buf("X0"), Buf("X1")], Buf("R"), Buf("SA"), Buf("ST"), Buf("ep"), Buf("st8"), Buf("O"))
    e0 = make_e0(L, 2, 7) if ('E' in L["phases"] or 'E0' in L["phases"]) else iter(())
    V(lambda e: e.memset(STf[:, :, :], 0.0), [], [B_ST])
    V(lambda e: e.memset(ST[:, :, :], 0.0), [], [B_ST])
    V(lambda e: e.memset(zb[:, :, 0:1], 0.0), [], [B_zb])

    def v3(ap2):
        return ap2.rearrange("p (c t) -> p c t", t=64)

    for tb in range(L['nblk']):
        for i in range(14):
            if tb == 0:
                sc.dma("sp", zb[:, i, 1:513], zrw_d[i * 128:(i + 1) * 128, 0:512], writes=[B_zb])
            else:
                sc.dma("sp", zb[:, i, 0:513], zrw_d[i * 128:(i + 1) * 128, tb * 512 - 1:tb * 512 + 512], writes=[B_zb])
        for i in range(14):
            t0 = tmp[i % 2]
            bt0 = B_tmp[i % 2]
            V(lambda e, i=i, t0=t0: e.tensor_tensor(out=t0[:, :], in0=zb[:, i, 0:512], in1=zb[:, i, 1:513], op=ALU.subtract), [B_zb], [bt0])
            V(lambda e, i=i, t0=t0: e.scalar_tensor_tensor(out=zs[:, i, :], in0=t0[:, :], scalar=mu[:, i:i + 1], in1=zb[:, i, 1:513],
                                                           op0=ALU.mult, op1=ALU.add), [bt0, B_zb, B_c], [B_zs])
        A(lambda e: e.activation(out=th[:, :], in_=zs[:, 12, :], func=AF.Tanh), [B_zs], [B_pre])
        V(lambda e: e.tensor_copy(out=al16[:, :], in_=zs[:, 12, :]), [B_zs], [B_pre])
        A(lambda e: e.activation(out=sgl[:, :], in_=zs[:, 13, :], func=AF.Sigmoid), [B_zs], [B_blk])
        for j in range(4):
            pW, bW = ps[0], B_ps[0]
            pA, bA = ps[1], B_ps[1]
            pQ, bQ = ps[2], B_ps[2]
            P(lambda e, j=j: e.matmul(pW[:, :], lhsT=w2a2b[0:64, j * 128:(j + 1) * 128], rhs=th[0:64, :], start=True, stop=True), [B_c, B_pre], [bW])
            P(lambda e, j=j: e.matmul(pA[:, :], lhsT=w2a2b[64:128, j * 128:(j + 1) * 128], rhs=al16[64:128, :], start=True, stop=True), [B_c, B_pre], [bA])
            A(lambda e, j=j: e.activation(out=lw[:, :], in_=pW[:, :], func=AF.Sigmoid, bias=w0c[:, j:j + 1]), [bW, B_c], [B_pre])
            A(lambda e, j=j: e.activation(out=asg[:, :], in_=pA[:, :], func=AF.Sigmoid, bias=a0c[:, j:j + 1]), [bA, B_c], [B_pre])
            A(lambda e, j=j: e.activation(out=sq16[:, :], in_=zs[:, 4 + j, :], func=AF.Square, scale=kkc[:, j:j + 1]), [B_zs, B_c], [B_pre])
            P(lambda e: e.matmul(pQ[:, :], lhsT=bones[:, :], rhs=sq16[:, :], start=True, stop=True), [B_c, B_pre], [bQ])
            V(lambda e: e.tensor_scalar(out=tmp[2][:, :], in0=pQ[:, :], scalar1=1e-24, scalar2=None, op0=ALU.max), [bQ], [B_tmp[2]])
            A(lambda e: e.activation(out=tmp[2][:, :], in_=tmp[2][:, :], func=AF.Sqrt), [B_tmp[2]], [B_tmp[2]])
            V(lambda e: e.reciprocal(out=tmp[2][:, :], in_=tmp[2][:, :]), [B_tmp[2]], [B_tmp[2]])
            V(lambda e, j=j: e.scalar_tensor_tensor(out=kk[:, :], in0=zs[:, 4 + j, :], scalar=kkc[:, j:j + 1], in1=tmp[2][:, :],
                                                    op0=ALU.mult, op1=ALU.mult), [B_zs, B_tmp[2], B_c], [B_pre])
            V(lambda e, j=j: e.tensor_scalar(out=tmp[0][:, :], in0=asg[:, :], scalar1=kac[:, j:j + 1], scalar2=omka[:, j:j + 1],
                                             op0=ALU.mult, op1=ALU.add), [B_pre, B_c], [B_tmp[0]])
            V(lambda e, j=j: e.tensor_tensor(out=kp[:, :], in0=zs[:, 4 + j, :], in1=tmp[0][:, :], op=ALU.mult), [B_zs, B_tmp[0]], [B_pre])
            V(lambda e: e.tensor_tensor(out=bv[:, :], in0=kk[:, :], in1=asg[:, :], op=ALU.mult), [B_pre], [B_pre])
            V(lambda e: e.tensor_scalar(out=lw[:, :], in0=lw[:, :], scalar1=-0.6065306597126334, scalar2=None, op0=ALU.mult), [B_pre], [B_pre])
            V(lambda e: e.tensor_tensor_scan(out=cs[:, :], data0=scanmask, data1=lw[:, :], initial=0.0, op0=ALU.mult, op1=ALU.add), [B_pre, B_c], [B_pre])
            V(lambda e: e.tensor_tensor(out=tmp[0][:, :], in0=cs[:, :], in1=lw[:, :], op=ALU.subtract), [B_pre], [B_tmp[0]])
            V(lambda e: e.tensor_tensor(out=v3(tmp[1][:, :]), in0=v3(cs[:, :])[:, :, 63:64].to_broadcast([128, 8, 64]), in1=v3(cs[:, :]),
                                        op=ALU.subtract), [B_pre], [B_tmp[1]])
            A(lambda e: e.activation(out=E[0][:, :], in_=cs[:, :], func=AF.Exp), [B_pre], [B_pre])
            A(lambda e: e.activation(out=E[1][:, :], in_=cs[:, :], func=AF.Exp, scale=-1.0), [B_pre], [B_pre])
            A(lambda e: e.activation(out=E[2][:, :], in_=tmp[0][:, :], func=AF.Exp), [B_tmp[0]], [B_pre])
            A(lambda e: e.activation(out=E[3][:, :], in_=tmp[1][:, :], func=AF.Exp), [B_tmp[1]], [B_pre])
            V(lambda e, j=j: e.tensor_copy(out=E5[:, j, :], in_=v3(E[0][:, :])[:, :, 63]), [B_pre], [B_blk])
            V(lambda e, j=j: e.tensor_tensor(out=AR[:, j, :, 1, :], in0=v3(zs[:, j, :]), in1=v3(E[0][:, :]), op=ALU.mult), [B_zs, B_pre], [B_blk])
            V(lambda e, j=j: e.tensor_tensor(out=BK[:, j, :, 1, :], in0=v3(kp[:, :]), in1=v3(E[1][:, :]), op=ALU.mult), [B_pre], [B_blk])
            V(lambda e, j=j: e.tensor_tensor(out=BK[:, j, :, 0, :], in0=v3(bv[:, :]), in1=v3(E[1][:, :]), op=ALU.mult), [B_pre], [B_blk])
            V(lambda e, j=j: e.scalar_tensor_tensor(out=AR[:, j, :, 0, :], in0=v3(kk[:, :]), scalar=-1.0, in1=v3(E[2][:, :]),
                                                    op0=ALU.mult, op1=ALU.mult), [B_pre], [B_blk])
            V(lambda e, j=j: e.tensor_tensor(out=KH[:, j, :], in0=kp[:, :], in1=E[3][:, :], op=ALU.mult), [B_pre], [B_blk])
            V(lambda e, j=j: e.tensor_tensor(out=BH[:, j, :], in0=bv[:, :], in1=E[3][:, :], op=ALU.mult), [B_pre], [B_blk])
            A(lambda e, j=j: e.activation(out=Vb[:, j, :], in_=zs[:, 8 + j, :], func=AF.Copy), [B_zs], [B_blk])
            V(lambda e, j=j: e.scalar_tensor_tensor(out=rkr[:, j, :], in0=zs[:, j, :], scalar=rkc[:, j:j + 1], in1=kp[:, :],
                                                    op0=ALU.mult, op1=ALU.mult), [B_zs, B_pre, B_c], [B_blk])

        def v4(ap2):
            return ap2.rearrange("p (j t) -> p j t", t=64)
        hl = [(h // 2, slice((h % 2) * 64, (h % 2) * 64 + 64), slice((h // 2) * 64, (h // 2) * 64 + 64), slice(h * 64, (h + 1) * 64)) for h in range(8)]

        def pre(c):
            q = c % 2
            csl = slice(c * 64, (c + 1) * 64)
            Vt2, KHt2, BHt2, sAB, sAK = Vt2p[q], KHt2p[q], BHt2p[q], sABp[q], sAKp[q]
            B_Vt, B_sA = B_Vtp[q], B_sAp[q]
            pT = ps[3][:, :].bitcast(BF16)
            pT2 = ps[4][:, :].bitcast(BF16)
            for half in range(2):
                hp = slice(half * 64, half * 64 + 64)
                for j in range(4):
                    P(lambda e: e.transpose(pT[hp, j * 128:(j + 1) * 128], Vb[:, j, csl], identb[:, :]), [B_blk, B_ident], [B_ps[3]])
                    P(lambda e: e.transpose(pT[hp, 512 + j * 128:512 + (j + 1) * 128], KH[:, j, csl], identb[:, :]), [B_blk, B_ident], [B_ps[3]])
                    P(lambda e: e.transpose(pT2[hp, j * 128:(j + 1) * 128], BH[:, j, csl], identb[:, :]), [B_blk, B_ident], [B_ps[4]])
            yield
            V(lambda e: e.tensor_copy(out=Vt2[:, :], in_=pT[:, 0:512]), [B_ps[3]], [B_Vt])
            A(lambda e: e.activation(out=KHt2[:, :], in_=pT[:, 512:1024], func=AF.Copy), [B_ps[3]], [B_Vt])
            A(lambda e: e.activation(out=BHt2[:, :], in_=pT2[:, 0:512], func=AF.Copy), [B_ps[4]], [B_Vt])
            for (j, pp, js, hs) in hl:
                P(lambda e: e.matmul(ps[0][pp, j * 128:(j + 1) * 128], lhsT=BK[pp, j, c, 0, :], rhs=AR[pp, j, c, :, :], start=True, stop=True), [B_blk], [B_ps[0]])
                P(lambda e: e.matmul(ps[1][pp, j * 128:(j + 1) * 128], lhsT=BK[pp, j, c, 1, :], rhs=AR[pp, j, c, :, :], start=True, stop=True), [B_blk], [B_ps[1]])
                P(lambda e: e.matmul(ps[2][pp, j * 64:(j + 1) * 64], lhsT=AR[pp, j, c, 0, :], rhs=BK[pp, j, c, 0, :], start=True, stop=True), [B_blk], [B_ps[2]])
            yield
            V(lambda e: e.tensor_tensor(out=sAB[:, :, :], in0=ps[0][:, :].rearrange("p (j t) -> p j t", t=128), in1=_bc_mid(maskA, 4), op=ALU.mult), [B_ps[0], B_c], [B_sA])
            V(lambda e: e.tensor_tensor(out=sAK[:, :, :], in0=ps[1][:, :].rearrange("p (j t) -> p j t", t=128), in1=_bc_mid(maskA, 4), op=ALU.mult), [B_ps[1], B_c], [B_sA])
            V(lambda e: e.tensor_tensor(out=MT[0][:, :, :], in0=v4(ps[2][:, 0:256]), in1=_bc_mid(maskT, 4), op=ALU.mult), [B_ps[2], B_c], [B_MT[0]])
            V(lambda e: e.tensor_copy(out=Mx[0][:, :, :], in_=sAB[:, :, 0:64]), [B_sA], [B_M[0]])
            V(lambda e: e.tensor_tensor(out=X[0][:, :, :], in0=sAB[:, :, 0:64], in1=_bc_mid(eye64, 4), op=ALU.add), [B_sA, B_c], [B_X[0]])
            yield
            cur = 0
            pa, pb, pc = ps[2], ps[3], ps[4]
            for rd in range(5):
                nxt = 1 - cur
                for (j, pp, js, hs) in hl:
                    P(lambda e: e.matmul(pa[pp, js], lhsT=MT[cur][pp, j, :], rhs=Mx[cur][pp, j, :], start=True, stop=True), [B_MT[cur], B_M[cur]], [B_ps[2]])
                    P(lambda e: e.matmul(pb[pp, js], lhsT=Mx[cur][pp, j, :], rhs=MT[cur][pp, j, :], start=True, stop=True), [B_MT[cur], B_M[cur]], [B_ps[3]])
                yield
                V(lambda e: e.tensor_copy(out=Mx[nxt][:, :, :], in_=v4(pa[:, 0:256])), [B_ps[2]], [B_M[nxt]])
                A(lambda e: e.activation(out=MT[nxt][:, :, :], in_=v4(pb[:, 0:256]), func=AF.Copy), [B_ps[3]], [B_MT[nxt]])
                for (j, pp, js, hs) in hl:
                    P(lambda e: e.matmul(pc[pp, js], lhsT=MT[nxt][pp, j, :], rhs=X[cur][pp, j, :], start=True, stop=True), [B_MT[nxt], B_X[cur]], [B_ps[4]])
                yield
                if rd < 4:
                    V(lambda e: e.tensor_tensor(out=X[nxt][:, :, :], in0=X[cur][:, :, :], in1=v4(pc[:, 0:256]), op=ALU.add), [B_X[cur], B_ps[4]], [B_X[nxt]])
                else:
                    V(lambda e: e.tensor_tensor(out=Xfp[q][:, :, :], in0=X[cur][:, :, :], in1=v4(pc[:, 0:256]), op=ALU.add), [B_X[cur], B_ps[4]], [B_Xfp[q]])
                cur = nxt
                yield

        def post(c):
            q = c % 2
            csl = slice(c * 64, (c + 1) * 64)
            Vt2, KHt2, BHt2, sAB, sAK, Xf = Vt2p[q], KHt2p[q], BHt2p[q], sABp[q], sAKp[q], Xfp[q]
            B_Vt, B_sA, bXf = B_Vtp[q], B_sAp[q], B_Xfp[q]
            pR, bR = ps[5], B_ps[5]
            pY, bY = ps[6], B_ps[6]
            pU, bU = ps[7], B_ps[7]
            for (j, pp, js, hs) in hl:
                P(lambda e: e.matmul(pR[pp, js], lhsT=AR[pp, j, c, 0, :], rhs=ST[pp, j, :], start=True, stop=False), [B_blk, B_ST], [bR])
                P(lambda e: e.matmul(pR[pp, js], lhsT=sAK[pp, j, 0:64], rhs=Vt2[pp, hs], start=False, stop=True), [B_sA, B_Vt], [bR])
            yield
            V(lambda e: e.tensor_copy(out=RHSs[:, :, :], in_=v4(pR[:, 0:256])), [bR], [B_R])
            for (j, pp, js, hs) in hl:
                P(lambda e: e.matmul(pR[pp, js], lhsT=Xf[pp, j, :], rhs=RHSs[pp, j, :], start=True, stop=True), [bXf, B_R], [bR])
            yield
            V(lambda e: e.tensor_copy(out=SAs[:, :, :], in_=v4(pR[:, 0:256])), [bR], [B_SA])
            for (j, pp, js, hs) in hl:
                P(lambda e: e.matmul(pY[pp, js], lhsT=AR[pp, j, c, 1, :], rhs=ST[pp, j, :], start=True, stop=False), [B_blk, B_ST], [bY])
                P(lambda e: e.matmul(pY[pp, js], lhsT=sAK[pp, j, 64:128], rhs=Vt2[pp, hs], start=False, stop=False), [B_sA, B_Vt], [bY])
                P(lambda e: e.matmul(pY[pp, js], lhsT=sAB[pp, j, 64:128], rhs=SAs[pp, j, :], start=False, stop=True), [B_sA, B_SA], [bY])
            for (j, pp, js, hs) in hl:
                P(lambda e: e.matmul(pU[pp, js], lhsT=KHt2[pp, hs], rhs=Vt2[pp, hs], start=True, stop=False), [B_Vt], [bU])
                P(lambda e: e.matmul(pU[pp, js], lhsT=BHt2[pp, hs], rhs=SAs[pp, j, :], start=False, stop=True), [B_Vt, B_SA], [bU])
            yield
            V(lambda e: e.tensor_tensor(out=STf[:, :, :], in0=STf[:, :, :], in1=E5[:, :, c:c + 1].to_broadcast([128, 4, 64]), op=ALU.mult), [B_blk, B_ST, bY, bR], [B_ST])
            V(lambda e: e.tensor_tensor(out=STf[:, :, :], in0=STf[:, :, :], in1=v4(pU[:, 0:256]), op=ALU.add), [bU, B_ST], [B_ST])
            V(lambda e: e.tensor_copy(out=ST[:, :, :], in_=STf[:, :, :]), [B_ST], [B_ST])
            pG, bG = ps[5], B_ps[5]
            for (j, pp, js, hs) in hl:
                P(lambda e: e.matmul(pG[pp, 256 + j:256 + j + 1], lhsT=rkr[pp, j, csl], rhs=onesb[pp, 0:1], start=True, stop=True), [B_blk, B_c, B_SA], [bG])
                P(lambda e: e.matmul(pG[pp, js], lhsT=sgl[:, csl], rhs=g2b[:, hs], start=True, stop=True), [B_blk, B_c, B_SA, B_R], [bG])
            y3 = v4(pY[:, 0:256])
            V(lambda e: e.tensor_reduce(out=st8[:, :, 0], in_=y3, axis=AX.X, op=ALU.add), [bY], [B_st8])
            A(lambda e: e.activation(out=sqy[:, :], in_=pY[:, 0:256], func=AF.Square), [bY], [B_ep])
            yield
            V(lambda e: e.tensor_reduce(out=st8[:, :, 1], in_=v4(sqy[:, :]), axis=AX.X, op=ALU.add), [B_ep], [B_st8])
            V(lambda e: e.tensor_scalar(out=st8[:, :, 2], in0=st8[:, :, 0], scalar1=1.0 / 64, scalar2=None, op0=ALU.mult), [B_st8], [B_st8])
            V(lambda e: e.tensor_tensor(out=st8[:, :, 3], in0=st8[:, :, 2], in1=st8[:, :, 2], op=ALU.mult), [B_st8], [B_st8])
            V(lambda e: e.scalar_tensor_tensor(out=st8[:, :, 4], in0=st8[:, :, 1], scalar=1.0 / 64, in1=st8[:, :, 3], op0=ALU.mult, op1=ALU.subtract), [B_st8], [B_st8])
            V(lambda e: e.tensor_scalar(out=st8[:, :, 4], in0=st8[:, :, 4], scalar1=64e-5, scalar2=None, op0=ALU.add), [B_st8], [B_st8])
            A(lambda e: e.activation(out=st8[:, :, 5], in_=st8[:, :, 4], func=AF.Sqrt), [B_st8], [B_st8])
            yield
            V(lambda e: e.reciprocal(out=st8[:, :, 5], in_=st8[:, :, 5]), [B_st8], [B_st8])
            yn3 = v4(yn[:, :])
            V(lambda e: e.tensor_tensor(out=yn3, in0=y3, in1=st8[:, :, 2:3].to_broadcast([128, 4, 64]), op=ALU.subtract), [bY, B_st8], [B_ep])
            V(lambda e: e.tensor_tensor(out=yn3, in0=yn3, in1=st8[:, :, 5:6].to_broadcast([128, 4, 64]), op=ALU.mult), [B_ep, B_st8], [B_ep])
            V(lambda e: e.tensor_tensor(out=yn[:, :], in0=yn[:, :], in1=gnbc[:, 0, :], op=ALU.mult), [B_ep, B_c], [B_ep])
            V(lambda e: e.tensor_tensor(out=yn[:, :], in0=yn[:, :], in1=gnbc[:, 1, :], op=ALU.add), [B_ep, B_c], [B_ep])
            V(lambda e: e.tensor_copy(out=st8[:, :, 6], in_=pG[:, 256:260]), [bG], [B_st8])
            for half in range(2):
                hp = slice(half * 64, half * 64 + 64)
                V(lambda e: e.tensor_tensor(out=v4(bon[hp, :]), in0=Vt2[hp, :].rearrange("p (j q t) -> p j q t", q=2, t=64)[:, :, half, :],
                                            in1=st8[hp, :, 6:7].to_broadcast([64, 4, 64]), op=ALU.mult), [B_Vt, B_st8], [B_ep])
            V(lambda e: e.tensor_tensor(out=yn[:, :], in0=yn[:, :], in1=bon[:, :], op=ALU.add), [B_ep], [B_ep])
            V(lambda e: e.tensor_tensor(out=O16[:, :], in0=yn[:, :], in1=pG[:, 0:256], op=ALU.mult), [B_ep, bG], [B_O])
            pO = pU[:, 384:512].bitcast(BF16)
            for (j, pp, js, hs) in hl:
                P(lambda e: e.transpose(pO[pp, js], O16[pp, js], identb[pp, pp]), [B_O, B_ident, B_ST], [bU])
            yield
            V(lambda e: e.tensor_copy(out=rwoT[:, :, csl], in_=v4(pO[:, 0:256])), [bU], [B_rwoT])

        def run_both(ga, gb):
            alive_a, alive_b = ga is not None, gb is not None
            while alive_a or alive_b:
                if alive_a:
                    try:
                        next(ga)
                    except StopIteration:
                        alive_a = False
                if alive_b:
                    try:
                        next(gb)
                    except StopIteration:
                        alive_b = False

        run_both(pre(0), None)
        for c in range(8):
            for _ in range(4):
                next(e0, None)
            run_both(post(c), pre(c + 1) if c + 1 < 8 else None)
        for j in range(4):
            sc.dma("sp", rwo_d[j * 128:(j + 1) * 128, tb * 512:(tb + 1) * 512], rwoT[:, j, :], reads=[B_rwoT])
    for _ in e0:
        pass
    if 'E' in L["phases"]:
        L["e0_done_flag"][0] = True
    ar.release(mB)


def phase_D(L):
    nc, sc, ar, ps, B_ps = L["nc"], L["sc"], L["ar"], L["ps"], L["B_ps"]
    ident, identb, B_ident, load_bf16 = L["ident"], L["identb"], L["B_ident"], L["load_bf16"]
    gt_bc, B_gt = L["gt_bc"], L["B_gt"]
    dbg = L["dbg"]
    V = lambda fn, r=(), w=(): sc.op("dve", fn, r, w)
    A = lambda fn, r=(), w=(): sc.op("act", fn, r, w)
    P = lambda fn, r=(), w=(): sc.op("pe", fn, r, w)
    G = lambda fn, r=(), w=(): sc.op("pool", fn, r, w)
    ALPHA = 2.0 ** 0.25
    mD = ar.mark()
    wbra = ar.alloc([128, 4, D], BF16, "wbra")
    wbrb = ar.alloc([128, 8, D], BF16, "wbrb")
    wout = ar.alloc([128, 8, D], BF16, "wout")
    wqb = ar.alloc([128, 8, D], BF16, "wqb")
    keysT = ar.alloc([128, 8, 128], BF16, "keysT")
    lnbc = ar.alloc([128, 2, D], F32, "lnbc")
    B_w = Buf("wD")
    for kc in range(4):
        load_bf16(wbra[:, kc, :], L["wbra_d"][kc * 128:(kc + 1) * 128, :], D, B_w)
    for kc in range(8):
        load_bf16(wbrb[:, kc, :], L["wbrb_d"][kc * 128:(kc + 1) * 128, :], D, B_w)
        load_bf16(wout[:, kc, :], L["wout_d"][kc * 128:(kc + 1) * 128, :], D, B_w)
        load_bf16(wqb[:, kc, :], L["wq_d"][kc * 128:(kc + 1) * 128, :], D, B_w)
    load_bf16(keysT[:, :, :].rearrange("p a b -> p (a b)"), L["pkeys_d"][:, :, :].rearrange("p a b -> p (a b)"), 1024, B_w)
    sc.dma("sp", lnbc[:, :, :], L["lnbc_d"][:, 0:2, :], writes=[B_w])
    rwoB = ar.alloc([128, 4, 512], BF16, "rwoB")
    dsaB = ar.alloc([128, 8, 512], BF16, "dsaB")
    zgr = [ar.alloc([128, 2, 512], BF16, "zgr%d" % i) for i in range(2)]
    B_zgr = [Buf("zgr0"), Buf("zgr1")]
    mg = ar.alloc([128, 8, 512], BF16, "mg")
    t1 = ar.alloc([128, 512], F32, "t1")
    t2 = ar.alloc([128, 512], F32, "t2")
    B_in, B_mg, B_t = Buf("inD"), Buf("mg"), Buf("tD")
    xt = ar.alloc([128, D], F32, "xtD")
    u = ar.alloc([128, D], F32, "uD")
    x1 = ar.alloc([128, D], F32, "x1")
    h2 = ar.alloc([128, D], F32, "h2")
    st = ar.alloc([128, 8], F32, "stD")
    h2T = ar.alloc([128, 8, 128], BF16, "h2T")
    qT = ar.alloc([128, 8, 128], BF16, "qT")
    ssb = ar.alloc([128, 16, 128], F32, "ssb")
    stmp = ar.alloc([128, 256], F32, "stmp")
    tv = ar.alloc([128, 16, 16], F32, "tv")
    ti = ar.alloc([128, 16, 16], U32, "ti")
    tif = ar.alloc([128, 16, 16], F32, "tif")
    cand = ar.alloc([128, 8, 256], F32, "cand")
    mv = ar.alloc([128, 8, 16], F32, "mv")
    posu = ar.alloc([128, 8, 16], U32, "posu")
    au = ar.alloc([128, 8, 16], U32, "au")
    bu = ar.alloc([128, 8, 16], U32, "bu")
    abf = ar.alloc([128, 2, 128], F32, "abf")
    oh16 = ar.alloc([128, 128, 16], F32, "oh16")
    junk = oh16[:, 0:64, :].rearrange("p a b -> p (a b)")
    sel = ar.alloc([128, 3, 128], F32, "sel")
    selT = ar.alloc([128, 3, 128], F32, "selT")
    gate = ar.alloc([128, 8, 16], F32, "gate")
    gs = ar.alloc([128, 8], F32, "gs")
    iota16 = L["iota16"]
    B_oh, B_sel, B_selT = Buf("oh"), Buf("sel"), Buf("selT")
    B_x, B_u, B_x1, B_h2, B_st, B_h2T, B_qT, B_s, B_tk, B_c, B_e, B_hu, B_acc, B_j = [Buf(n) for n in
        ("x", "u", "x1", "h2", "st", "h2T", "qT", "s", "tk", "cand", "eid", "hu", "acc", "junk")]
    x_v = L["x_d"].rearrange("(n p) m -> p n m", p=128)
    out_v = L["out_d"].rearrange("(n p) m -> p n m", p=128)

    def layer_norm(src, bsrc, dst, bdst, gi):
        A(lambda e: e.activation(out=junk, in_=src[:, :], func=AF.Copy, accum_out=st[:, 0:1]), [bsrc], [B_st, B_oh])
        A(lambda e: e.activation(out=junk, in_=src[:, :], func=AF.Square, accum_out=st[:, 1:2]), [bsrc], [B_st, B_oh])
        V(lambda e: e.tensor_scalar(out=st[:, 2:3], in0=st[:, 0:1], scalar1=1.0 / D, scalar2=None, op0=ALU.mult), [B_st], [B_st])
        V(lambda e: e.tensor_tensor(out=st[:, 3:4], in0=st[:, 2:3], in1=st[:, 2:3], op=ALU.mult), [B_st], [B_st])
        V(lambda e: e.scalar_tensor_tensor(out=st[:, 4:5], in0=st[:, 1:2], scalar=1.0 / D, in1=st[:, 3:4], op0=ALU.mult, op1=ALU.subtract), [B_st], [B_st])
        V(lambda e: e.tensor_scalar(out=st[:, 4:5], in0=st[:, 4:5], scalar1=1e-5, scalar2=None, op0=ALU.add), [B_st], [B_st])
        A(lambda e: e.activation(out=st[:, 5:6], in_=st[:, 4:5], func=AF.Sqrt), [B_st], [B_st])
        V(lambda e: e.reciprocal(out=st[:, 5:6], in_=st[:, 5:6]), [B_st], [B_st])
        V(lambda e: e.scalar_tensor_tensor(out=st[:, 6:7], in0=st[:, 2:3], scalar=-1.0, in1=st[:, 5:6], op0=ALU.mult, op1=ALU.mult), [B_st], [B_st])
        A(lambda e: e.activation(out=dst[:, :], in_=src[:, :], func=AF.Identity, scale=st[:, 5:6], bias=st[:, 6:7]), [bsrc, B_st], [bdst])
        G(lambda e: e.tensor_tensor(out=dst[:, :], in0=dst[:, :], in1=lnbc[:, gi, :], op=ALU.mult), [bdst, B_w], [bdst])
        G(lambda e: e.tensor_tensor(out=dst[:, :], in0=dst[:, :], in1=lnbc[:, gi + 1, :], op=ALU.add), [bdst, B_w], [bdst])

    x1p = [x1, ar.alloc([128, D], F32, "x1b")]
    B_x1p = [B_x1, Buf("x1b")]
    h2Tp_ = [h2T, ar.alloc([128, 8, 128], BF16, "h2Tb")]
    B_h2Tp_ = [B_h2T, Buf("h2Tb")]
    ssbp = [ssb, ar.alloc([128, 16, 128], F32, "ssbb")]
    B_sp = [B_s, Buf("ssbb")]
    prevn = [None]
    B_ohh = [Buf("ohh%d" % i) for i in range(8)]
    stmp16 = ar.alloc([128, 16, 128], F32, "stmp16")
    stmp8 = stmp16[:, :, :].rearrange("p (h a) b -> p h (a b)", a=2)
    B_tkg = [Buf("tkg%d" % i) for i in range(16)]
    B_tkg2 = [Buf("tkgb%d" % i) for i in range(16)]
    B_stg16 = [Buf("stg16_%d" % i) for i in range(16)]
    B_tig = [Buf("tig%d" % i) for i in range(16)]
    B_tig2 = [Buf("tigb%d" % i) for i in range(16)]
    B_mvh = [Buf("mvh%d" % i) for i in range(8)]
    B_mvh2 = [Buf("mvhb%d" % i) for i in range(8)]
    B_st8h = [B_stg16[2 * i] for i in range(8)]
    B_posh = [Buf("posh%d" % i) for i in range(8)]
    B_posh2 = [Buf("poshb%d" % i) for i in range(8)]

    def stageA(n, jt):
        q = n % 2
        x1_, bx1_, h2T_, bh2T_, ssb_, bs_ = x1p[q], B_x1p[q], h2Tp_[q], B_h2Tp_[q], ssbp[q], B_sp[q]
        sc.dma("sp", xt[:, :], x_v[:, n, :], writes=[B_x])
        for half in range(2):
            pM, bM = ps[4 + half], B_ps[4 + half]
            hsl = slice(half * 512, (half + 1) * 512)
            for dc in range(8):
                P(lambda e: e.matmul(pM[:, :], lhsT=mg[:, dc, jt * 128:(jt + 1) * 128], rhs=wout[:, dc, hsl], start=(dc == 0), stop=(dc == 7)), [B_mg, B_w], [bM])
            V(lambda e: e.tensor_tensor(out=u[:, hsl], in0=pM[:, :], in1=gt_bc[:, 0, hsl], op=ALU.mult), [bM, B_gt], [B_u])
            V(lambda e: e.scalar_tensor_tensor(out=u[:, hsl], in0=xt[:, hsl], scalar=ALPHA, in1=u[:, hsl], op0=ALU.mult, op1=ALU.add), [B_x, B_u], [B_u])
        layer_norm(u, B_u, x1_, bx1_, 0)
        if "x1dbg" in dbg:
            sc.dma("sp", L["x1_d"][n * 128:(n + 1) * 128, :], x1_[:, :], reads=[bx1_])
        sc.dma("sp", L["x1s_d"][n * 128:(n + 1) * 128, :], x1_[:, :], reads=[bx1_])
        G(lambda e: e.tensor_tensor(out=h2[:, :], in0=x1_[:, :], in1=gt_bc[:, 2, :], op=ALU.mult), [bx1_, B_gt], [B_h2])
        G(lambda e: e.tensor_tensor(out=h2[:, :], in0=h2[:, :], in1=gt_bc[:, 1, :], op=ALU.add), [B_h2, B_gt], [B_h2])
        for kc in range(8):
            pp_, bp_ = ps[kc // 4], B_ps[kc // 4]
            P(lambda e: e.transpose(pp_[:, (kc % 4) * 128:(kc % 4 + 1) * 128], h2[:, kc * 128:(kc + 1) * 128], ident[:, :]), [B_h2, B_ident], [bp_])
        for k2 in range(2):
            A(lambda e: e.activation(out=h2T_[:, k2 * 4:(k2 + 1) * 4, :], in_=ps[k2][:, :].rearrange("p (a b) -> p a b", b=128), func=AF.Copy), [B_ps[k2]], [bh2T_])
        for kc in range(8):
            sc.dma("sp", L["h2T_d"][kc * 128:(kc + 1) * 128, n * 128:(n + 1) * 128], h2T_[:, kc, :], reads=[bh2T_])
        for hh in range(8):
            pq, bq = ps[2 + hh // 4], B_ps[2 + hh // 4]
            for kc in range(8):
                P(lambda e: e.matmul(pq[:, (hh % 4) * 128:(hh % 4 + 1) * 128], lhsT=wqb[:, kc, hh * 128:(hh + 1) * 128], rhs=h2T_[:, kc, :],
                                     start=(kc == 0), stop=(kc == 7)), [B_w, bh2T_], [bq])
        for k2 in range(2):
            A(lambda e: e.activation(out=qT[:, k2 * 4:(k2 + 1) * 4, :], in_=ps[2 + k2][:, :].rearrange("p (a b) -> p a b", b=128), func=AF.Copy), [B_ps[2 + k2]], [B_qT])
        for g in range(16):
            hh, cc = g % 8, g // 8
            pS, bS = ps[4 + g // 4], B_ps[4 + g // 4]
            P(lambda e: e.matmul(pS[:, (g % 4) * 128:(g % 4 + 1) * 128], lhsT=qT[cc * 64:(cc + 1) * 64, hh, :], rhs=keysT[cc * 64:(cc + 1) * 64, hh, :],
                                 start=True, stop=True), [B_qT, B_w], [bS])
        for k4 in range(4):
            A(lambda e: e.activation(out=ssb_[:, k4 * 4:(k4 + 1) * 4, :], in_=ps[4 + k4][:, :].rearrange("p (a b) -> p a b", b=128), func=AF.Copy), [B_ps[4 + k4]], [bs_])

    def stageB(n):
        q = n % 2
        ssb_, bs_ = ssbp[q], B_sp[q]
        for g in range(16):
            V(lambda e: e.max(out=tv[:, g, 0:8], in_=ssb_[:, g, :]), [bs_], [B_tkg[g]])
        for g in range(16):
            V(lambda e: e.match_replace(out=stmp16[:, g, :], in_to_replace=tv[:, g, 0:8], in_values=ssb_[:, g, :], imm_value=-1e30), [bs_, B_tkg[g]], [B_stg16[g]])
        for g in range(16):
            V(lambda e: e.max(out=tv[:, g, 8:16], in_=stmp16[:, g, :]), [B_stg16[g]], [B_tkg2[g]])
        for g in range(16):
            V(lambda e: e.max_index(out=ti[:, g, 0:8], in_max=tv[:, g, 0:8], in_values=ssb_[:, g, :]), [bs_, B_tkg[g]], [B_tig[g]])
        for g in range(16):
            V(lambda e: e.max_index(out=ti[:, g, 8:16], in_max=tv[:, g, 8:16], in_values=ssb_[:, g, :]), [bs_, B_tkg2[g]], [B_tig2[g]])
        V(lambda e: e.tensor_copy(out=tif[:, :, :], in_=ti[:, :, :]), B_tig + B_tig2, [B_tk])
        V(lambda e: e.tensor_copy(out=tv[:, 0:1, 0:1], in_=tv[:, 0:1, 0:1]), B_tkg + B_tkg2, [B_tk])
        tvv = tv[:, :, :].rearrange("p (c h) k -> p h c k", c=2)
        tfv = tif[:, :, :].rearrange("p (c h) k -> p h c k", c=2)
        c4 = cand[:, :, :].rearrange("p h (a b) -> p h a b", b=16)
        for hh in range(8):
            V(lambda e: e.tensor_tensor(out=c4[:, hh, :, :], in0=tvv[:, hh, 0, :].unsqueeze(2).to_broadcast([128, 16, 16]),
                                        in1=tvv[:, hh, 1, :].unsqueeze(1).to_broadcast([128, 16, 16]), op=ALU.add), [B_tk], [B_c])
        for hh in range(8):
            V(lambda e: e.max(out=mv[:, hh, 0:8], in_=cand[:, hh, :]), [B_c], [B_mvh[hh]])
        for hh in range(8):
            V(lambda e: e.match_replace(out=stmp8[:, hh, :], in_to_replace=mv[:, hh, 0:8], in_values=cand[:, hh, :], imm_value=-1e30), [B_c, B_mvh[hh]], [B_stg16[2 * hh], B_stg16[2 * hh + 1]])
        for hh in range(8):
            V(lambda e: e.max(out=mv[:, hh, 8:16], in_=stmp8[:, hh, :]), [B_stg16[2 * hh], B_stg16[2 * hh + 1]], [B_mvh2[hh]])
        for hh in range(8):
            V(lambda e: e.max_index(out=posu[:, hh, 0:8], in_max=mv[:, hh, 0:8], in_values=cand[:, hh, :]), [B_c, B_mvh[hh]], [B_posh[hh]])
        for hh in range(8):
            V(lambda e: e.max_index(out=posu[:, hh, 8:16], in_max=mv[:, hh, 8:16], in_values=cand[:, hh, :]), [B_c, B_mvh2[hh]], [B_posh2[hh]])
        V(lambda e: e.tensor_copy(out=mv[:, 0:1, 0:1], in_=mv[:, 0:1, 0:1]), B_mvh + B_mvh2 + B_posh + B_posh2, [B_e])
        V(lambda e: e.tensor_scalar(out=au[:, :, :], in0=posu[:, :, :], scalar1=4, scalar2=None, op0=ALU.logical_shift_right), [B_e], [B_e])
        V(lambda e: e.tensor_scalar(out=bu[:, :, :], in0=posu[:, :, :], scalar1=15, scalar2=None, op0=ALU.bitwise_and), [B_e], [B_e])
        V(lambda e: e.tensor_copy(out=abf[:, 0, :], in_=au[:, :, :].rearrange("p a b -> p (a b)")), [B_e], [B_e])
        V(lambda e: e.tensor_copy(out=abf[:, 1, :], in_=bu[:, :, :].rearrange("p a b -> p (a b)")), [B_e], [B_e])
        for cc in range(2):
            V(lambda e: e.tensor_tensor(out=oh16[:, :, :], in0=abf[:, cc, :].unsqueeze(2).to_broadcast([128, 128, 16]),
                                        in1=iota16.unsqueeze(1).to_broadcast([128, 128, 16]), op=ALU.is_equal), [B_e, L["B_iota"]], [B_oh] + B_ohh)
            for hh in range(8):
                V(lambda e: e.tensor_tensor(out=oh16[:, hh * 16:(hh + 1) * 16, :], in0=oh16[:, hh * 16:(hh + 1) * 16, :],
                                            in1=tfv[:, hh, cc, :].unsqueeze(1).to_broadcast([128, 16, 16]), op=ALU.mult), [B_oh, B_tk], [B_ohh[hh]])
            V(lambda e: e.tensor_reduce(out=sel[:, cc, :], in_=oh16[:, :, :], axis=AX.X, op=ALU.add), B_ohh, [B_sel, B_oh])
        V(lambda e: e.tensor_tensor(out=gate[:, :, :], in0=mv[:, :, :], in1=mv[:, :, 0:1].to_broadcast([128, 8, 16]), op=ALU.subtract), [B_e], [B_hu])
        A(lambda e: e.activation(out=gate[:, :, :], in_=gate[:, :, :], func=AF.Exp), [B_hu], [B_hu])
        V(lambda e: e.tensor_reduce(out=gs[:, :], in_=gate[:, :, :], axis=AX.X, op=ALU.add), [B_hu], [B_hu])
        V(lambda e: e.reciprocal(out=gs[:, :], in_=gs[:, :]), [B_hu], [B_hu])
        V(lambda e: e.tensor_tensor(out=sel[:, 2, :].rearrange("p (a b) -> p a b", b=16), in0=gate[:, :, :], in1=gs[:, :].unsqueeze(2).to_broadcast([128, 8, 16]), op=ALU.mult),
          [B_hu], [B_sel])
        pI, bI = ps[6], B_ps[6]
        for q3 in range(3):
            P(lambda e: e.transpose(pI[:, q3 * 128:(q3 + 1) * 128], sel[:, q3, :], ident[:, :]), [B_sel, B_ident], [bI])
        A(lambda e: e.activation(out=selT[:, :, :], in_=pI[:, 0:384].rearrange("p (a b) -> p a b", b=128), func=AF.Copy), [bI], [B_selT])
        for q3 in range(3):
            sc.dma("sp", L["selT_d"][q3, :, n * 128:(n + 1) * 128], selT[:, q3, :], reads=[B_selT])

    for tb in range(L["nblk"]):
        tsl = slice(tb * 512, (tb + 1) * 512)
        for kc in range(4):
            sc.dma("sp", rwoB[:, kc, :], L["rwo_d"][kc * 128:(kc + 1) * 128, tsl], writes=[B_in])
        for kc in range(8):
            sc.dma("sp", dsaB[:, kc, :], L["dsao_d"][kc * 128:(kc + 1) * 128, tsl], writes=[B_in])
        for dc in range(8):
            pA, bA = ps[dc % 2], B_ps[dc % 2]
            pB, bB = ps[2 + dc % 2], B_ps[2 + dc % 2]
            zg_, bz_ = zgr[dc % 2], B_zgr[dc % 2]
            sc.dma("sp", zg_[:, 0, :], L["zg_d"][dc * 128:(dc + 1) * 128, tsl], writes=[bz_])
            sc.dma("sp", zg_[:, 1, :], L["zg_d"][(8 + dc) * 128:(9 + dc) * 128, tsl], writes=[bz_])
            for kc in range(4):
                P(lambda e: e.matmul(pA[:, :], lhsT=wbra[:, kc, dc * 128:(dc + 1) * 128], rhs=rwoB[:, kc, :], start=(kc == 0), stop=(kc == 3)), [B_w, B_in], [bA])
            for kc in range(8):
                P(lambda e: e.matmul(pB[:, :], lhsT=wbrb[:, kc, dc * 128:(dc + 1) * 128], rhs=dsaB[:, kc, :], start=(kc == 0), stop=(kc == 7)), [B_w, B_in], [bB])
            V(lambda e: e.tensor_tensor(out=t1[:, :], in0=pA[:, :], in1=zg_[:, 0, :], op=ALU.mult), [bA, bz_], [B_t])
            V(lambda e: e.tensor_tensor(out=t2[:, :], in0=pB[:, :], in1=zg_[:, 1, :], op=ALU.mult), [bB, bz_], [B_t])
            V(lambda e: e.tensor_tensor(out=mg[:, dc, :], in0=t1[:, :], in1=t2[:, :], op=ALU.add), [B_t], [B_mg])
        for jt in range(4):
            n = tb * 4 + jt
            stageA(n, jt)
            if prevn[0] is not None:
                stageB(prevn[0])
            prevn[0] = n
    stageB(prevn[0])
    ar.release(mD)


def phase_C(L):
    nc, sc, ar, ps, B_ps = L["nc"], L["sc"], L["ar"], L["ps"], L["B_ps"]
    identb, B_ident = L["identb"], L["B_ident"]
    V = lambda fn, r=(), w=(): sc.op("dve", fn, r, w)
    A = lambda fn, r=(), w=(): sc.op("act", fn, r, w)
    P = lambda fn, r=(), w=(): sc.op("pe", fn, r, w)
    G = lambda fn, r=(), w=(): sc.op("pool", fn, r, w)
    NQB = L["nblk"] * 4
    mC = ar.mark()
    cvec = ar.alloc([128, 256], F32, "cvec")
    biasT = ar.alloc([128, 3, 1024], F32, "biasT")
    negm = ar.alloc([128, 128], F32, "negm")
    ckv_tok = ar.alloc([128, 32, 129], BF16, "ckv_tok")
    ckvT = ar.alloc([128, S], BF16, "ckvT")
    kiT2 = ar.alloc([128, S], BF16, "kiT2")
    wall = ar.alloc([128, 32, 4], F32, "wall")
    B_cc, B_kv, B_ki, B_wl = Buf("cc"), Buf("kv"), Buf("ki"), Buf("wl")
    sc.dma("sp", cvec[:, :], L["cvec_d"][:, :], writes=[B_cc])
    sc.dma("sp", biasT[:, :, :], L["biasT_d"][:, :, :], writes=[B_cc])
    sc.dma("sp", negm[:, :], L["negm_d"][:, :], writes=[B_cc])
    V(lambda e: e.memset(ckv_tok[:, :, 128:129], 1.0), [], [B_kv])
    for bi_ in range(2):
        V(lambda e: e.tensor_tensor(out=biasT[:, bi_, :], in0=biasT[:, bi_, :], in1=biasT[:, 2, :], op=ALU.subtract), [B_cc], [B_cc])
    zt = [ar.alloc([128, 196], F32, "ztC%d" % i) for i in range(2)]
    B_zt = [Buf("ztC0"), Buf("ztC1")]
    sq = ar.alloc([128, 128], F32, "sqC")
    c16 = ar.alloc([128, 128], BF16, "c16")
    k32 = ar.alloc([128, 64], F32, "k32")
    k16 = ar.alloc([128, 128], BF16, "k16")
    stc = ar.alloc([128, 8], F32, "stc")
    B_sq, B_c16, B_k, B_stc = Buf("sqC"), Buf("c16"), Buf("k"), Buf("stc")
    for n in range(NQB):
        z, bz = zt[n % 2], B_zt[n % 2]
        sc.dma("sp", z[:, :], L["ztok_d"][n * 128:(n + 1) * 128, :], writes=[bz])
        A(lambda e: e.activation(out=sq[:, :], in_=z[:, 0:128], func=AF.Square, accum_out=stc[:, 0:1]), [bz], [B_sq, B_stc])
        V(lambda e: e.tensor_scalar(out=stc[:, 1:2], in0=stc[:, 0:1], scalar1=1.0 / 128, scalar2=1e-5, op0=ALU.mult, op1=ALU.add), [B_stc], [B_stc])
        A(lambda e: e.activation(out=stc[:, 1:2], in_=stc[:, 1:2], func=AF.Sqrt), [B_stc], [B_stc])
        V(lambda e: e.reciprocal(out=stc[:, 1:2], in_=stc[:, 1:2]), [B_stc], [B_stc])
        V(lambda e: e.scalar_tensor_tensor(out=ckv_tok[:, n, 0:128], in0=z[:, 0:128], scalar=stc[:, 1:2], in1=cvec[:, 0:128], op0=ALU.mult, op1=ALU.mult),
          [bz, B_stc, B_cc], [B_kv])
        V(lambda e: e.tensor_reduce(out=stc[:, 2:3], in_=z[:, 128:192], axis=AX.X, op=ALU.add), [bz], [B_stc])
        A(lambda e: e.activation(out=sq[:, 0:64], in_=z[:, 128:192], func=AF.Square, accum_out=stc[:, 3:4]), [bz], [B_sq, B_stc])
        V(lambda e: e.tensor_scalar(out=stc[:, 4:5], in0=stc[:, 2:3], scalar1=1.0 / 64, scalar2=None, op0=ALU.mult), [B_stc], [B_stc])
        V(lambda e: e.tensor_tensor(out=stc[:, 5:6], in0=stc[:, 4:5], in1=stc[:, 4:5], op=ALU.mult), [B_stc], [B_stc])
        V(lambda e: e.scalar_tensor_tensor(out=stc[:, 6:7], in0=stc[:, 3:4], scalar=1.0 / 64, in1=stc[:, 5:6], op0=ALU.mult, op1=ALU.subtract), [B_stc], [B_stc])
        V(lambda e: e.tensor_scalar(out=stc[:, 6:7], in0=stc[:, 6:7], scalar1=1e-5, scalar2=None, op0=ALU.add), [B_stc], [B_stc])
        A(lambda e: e.activation(out=stc[:, 6:7], in_=stc[:, 6:7], func=AF.Sqrt), [B_stc], [B_stc])
        V(lambda e: e.reciprocal(out=stc[:, 6:7], in_=stc[:, 6:7]), [B_stc], [B_stc])
        V(lambda e: e.tensor_scalar(out=k32[:, :], in0=z[:, 128:192], scalar1=stc[:, 4:5], scalar2=stc[:, 6:7], op0=ALU.subtract, op1=ALU.mult), [bz, B_stc], [B_k])
        V(lambda e: e.tensor_tensor(out=k32[:, :], in0=k32[:, :], in1=cvec[:, 128:192], op=ALU.mult), [B_k, B_cc], [B_k])
        V(lambda e: e.tensor_tensor(out=k16[:, 0:64], in0=k32[:, :], in1=cvec[:, 192:256], op=ALU.add), [B_k, B_cc], [B_k])
        V(lambda e: e.tensor_copy(out=k16[:, 64:128], in_=k16[:, 0:64]), [B_k], [B_k])
        V(lambda e: e.tensor_scalar(out=wall[:, n, :], in0=z[:, 192:196], scalar1=0.0625, scalar2=None, op0=ALU.mult), [bz], [B_wl])
        pT = ps[n % 2][:, :].bitcast(BF16)
        bT = B_ps[n % 2]
        P(lambda e: e.transpose(pT[:, 0:128], ckv_tok[:, n, 0:128], identb[:, :]), [B_kv, B_ident], [bT])
        P(lambda e: e.transpose(pT[:, 128:256], k16[:, :], identb[:, :]), [B_k, B_ident], [bT])
        A(lambda e: e.activation(out=ckvT[:, n * 128:(n + 1) * 128], in_=pT[:, 0:128], func=AF.Copy, scale=128.0 ** -0.5), [bT], [B_kv])
        V(lambda e: e.tensor_copy(out=kiT2[:, n * 128:(n + 1) * 128], in_=pT[:, 128:256]), [bT], [B_ki])
    qiB = [ar.alloc([128, 2, 128], BF16, "qiB%d" % i) for i in range(2)]
    zqB = [ar.alloc([128, 8, 128], BF16, "zqB%d" % i) for i in range(2)]
    B_qi = [Buf("qi0"), Buf("qi1")]
    B_zq = [Buf("zq0"), Buf("zq1")]
    score2 = [ar.alloc([128, S], F32, "score%d" % i) for i in range(2)]
    maskb2 = [ar.alloc([128, S], BF16, "maskb%d" % i) for i in range(2)]
    maskT2 = [ar.alloc([128, 32, 128], BF16, "maskT%d" % i) for i in range(2)]
    bis2 = [ar.alloc([128, 8], F32, "bis%d" % i) for i in range(2)]
    junk = ar.alloc([128, S], BF16, "junkC")
    junkA = ar.alloc([128, S // 2], BF16, "junkA")
    rl = [ar.alloc([128, 512], F32, "rl%d" % i) for i in range(2)]
    B_rl = [Buf("rl0"), Buf("rl1")]
    lg = ar.alloc([128, 1024], F32, "lg")
    PT2 = [ar.alloc([128, 8, 128], BF16, "PT%d" % i) for i in range(2)]
    B_PT2 = [Buf("PT0"), Buf("PT1")]
    Oacc = ar.alloc([128, 8, 129], F32, "Oacc")
    rec = ar.alloc([128, 8], F32, "rec")
    Oo = ar.alloc([128, 8, 128], BF16, "Oo")
    dsT = ar.alloc([128, 8, 128], BF16, "dsT")
    B_sc2 = [Buf("score0"), Buf("score1")]
    B_mb2 = [Buf("maskb0"), Buf("maskb1")]
    B_mT2 = [Buf("maskT0"), Buf("maskT1")]
    B_bisA = [Buf("bisA0"), Buf("bisA1")]
    B_bisB = [Buf("bisB0"), Buf("bisB1")]
    B_bisC = [Buf("bisC0"), Buf("bisC1")]
    B_j, B_jA, B_lg, B_O, B_Oo, B_dsT = [Buf(n_) for n_ in ("junkC", "junkA", "lg", "Oacc", "Oo", "dsT")]

    def stage1(j):
        q = j % 2
        Lk = (j + 1) * 128
        qi, bqi, zq, bzq = qiB[q], B_qi[q], zqB[q], B_zq[q]
        score, B_sc = score2[q], B_sc2[q]
        qsl = slice(j * 128, (j + 1) * 128)
        for cch in range(2):
            sc.dma("sp", qi[:, cch, :], L["zqi_d"][cch * 128:(cch + 1) * 128, qsl], writes=[bqi])
        for h in range(8):
            sc.dma("sp", zq[:, h, :], L["zq_d"][h * 128:(h + 1) * 128, qsl], writes=[bzq])
        nkc = (Lk + 511) // 512
        ri = 0
        for kc in range(nkc):
            wd = min(512, Lk - kc * 512)
            ksl = slice(kc * 512, kc * 512 + wd)
            for hi in range(4):
                po = (hi % 2) * 64
                pD, bD = ps[hi], B_ps[hi]
                P(lambda e: e.matmul(pD[:, 0:wd], lhsT=qi[po:po + 64, hi // 2, :], rhs=kiT2[po:po + 64, ksl], start=True, stop=True), [bqi, B_ki], [bD])
                r_, br_ = rl[ri % 2], B_rl[ri % 2]
                ri += 1
                A(lambda e: e.activation(out=r_[:, 0:wd], in_=pD[:, 0:wd], func=AF.Relu), [bD], [br_])
                if hi == 0:
                    V(lambda e: e.tensor_scalar(out=score[:, ksl], in0=r_[:, 0:wd], scalar1=wall[:, j, 0:1], scalar2=None, op0=ALU.mult), [br_, B_wl], [B_sc])
                else:
                    V(lambda e: e.scalar_tensor_tensor(out=score[:, ksl], in0=r_[:, 0:wd], scalar=wall[:, j, hi:hi + 1], in1=score[:, ksl], op0=ALU.mult, op1=ALU.add),
                      [br_, B_wl, B_sc], [B_sc])
        V(lambda e: e.tensor_tensor(out=score[:, qsl], in0=score[:, qsl], in1=negm[:, :], op=ALU.add), [B_sc, B_cc], [B_sc])

    def bisect_iters(j):
        q = j % 2
        Lk = (j + 1) * 128
        score, B_sc, bis = score2[q], B_sc2[q], bis2[q]
        bA, bB, bC = B_bisA[q], B_bisB[q], B_bisC[q]
        if Lk > 256:
            na = (Lk // 2) // 128 * 128
            V(lambda e: e.memset(bis[:, 6:7], 0.0), [bA], [bA])
            step = 16.0
            for it in range(20):
                V(lambda e: e.tensor_scalar(out=bis[:, 7:8], in0=bis[:, 6:7], scalar1=-1.0, scalar2=None, op0=ALU.mult), [bA], [bB])
                A(lambda e: e.activation(out=junkA[:, 0:na], in_=score[:, 0:na], func=AF.Sign, bias=bis[:, 7:8], accum_out=bis[:, 2:3]), [B_sc, bB], [B_jA, bC])
                V(lambda e: e.tensor_scalar(out=junk[:, na:Lk], in0=score[:, na:Lk], scalar1=bis[:, 6:7], scalar2=None, op0=ALU.is_ge, op1=ALU.add, accum_out=bis[:, 3:4]),
                  [B_sc, bA], [B_j, bA])
                V(lambda e: e.scalar_tensor_tensor(out=bis[:, 4:5], in0=bis[:, 2:3], scalar=0.5, in1=bis[:, 3:4], op0=ALU.mult, op1=ALU.add), [bA, bC], [bA])
                V(lambda e: e.tensor_scalar(out=bis[:, 5:6], in0=bis[:, 4:5], scalar1=255.5 - na / 2.0, scalar2=2.0 * step, op0=ALU.is_ge, op1=ALU.mult), [bA], [bA])
                V(lambda e: e.scalar_tensor_tensor(out=bis[:, 6:7], in0=bis[:, 5:6], scalar=-step, in1=bis[:, 6:7], op0=ALU.add, op1=ALU.add), [bA, bB], [bA])
                step *= 0.5
                yield
            V(lambda e: e.tensor_scalar(out=bis[:, 6:7], in0=bis[:, 6:7], scalar1=-4.0 * step, scalar2=None, op0=ALU.add), [bA], [bA])
        else:
            V(lambda e: e.memset(bis[:, 6:7], -1e29), [bA], [bA])

    def stage3(j):
        q = j % 2
        Lk = (j + 1) * 128
        score, B_sc, bis, maskb, B_mb, maskT, B_mT = score2[q], B_sc2[q], bis2[q], maskb2[q], B_mb2[q], maskT2[q], B_mT2[q]
        V(lambda e: e.tensor_scalar(out=maskb[:, 0:Lk], in0=score[:, 0:Lk], scalar1=bis[:, 6:7], scalar2=None, op0=ALU.is_ge), [B_sc, B_bisA[q]], [B_mb])
        for k8 in range((j + 8) // 8):
            nn = min(8, j + 1 - k8 * 8)
            pM = ps[4][:, :].bitcast(BF16)
            for kk_ in range(nn):
                kt = k8 * 8 + kk_
                P(lambda e: e.transpose(pM[:, kk_ * 128:(kk_ + 1) * 128], maskb[:, kt * 128:(kt + 1) * 128], identb[:, :]), [B_mb, B_ident], [B_ps[4]])
            A(lambda e: e.activation(out=maskT[:, k8 * 8:k8 * 8 + nn, :], in_=pM[:, 0:nn * 128].rearrange("p (a b) -> p a b", b=128), func=AF.Copy, scale=30000.0, bias=-30000.0),
              [B_ps[4]], [B_mT])

    def stage4(j, side):
        q = j % 2
        zq, bzq, maskT, B_mT = zqB[q], B_zq[q], maskT2[q], B_mT2[q]
        qsl = slice(j * 128, (j + 1) * 128)
        for kt in range(j + 1):
            near = kt >= j - 1
            bsel = 0 if kt == j else 1
            PTk, bPT = PT2[kt % 2], B_PT2[kt % 2]
            for half in range(2):
                pL, bL = ps[(kt % 2) * 2 + half], B_ps[(kt % 2) * 2 + half]
                P(lambda e: e.matmul(pL[:, :], lhsT=ckvT[:, kt * 128:(kt + 1) * 128], rhs=zq[:, half * 4:(half + 1) * 4, :], start=True, stop=False), [B_kv, bzq], [bL])
                for h4 in range(4):
                    P(lambda e: e.matmul(pL[:, h4 * 128:(h4 + 1) * 128], lhsT=identb[:, :], rhs=maskT[:, kt, :], start=False, stop=(h4 == 3)), [B_ident, B_mT], [bL])
                if near:
                    V(lambda e: e.tensor_tensor(out=lg[:, half * 512:(half + 1) * 512], in0=pL[:, :], in1=biasT[:, bsel, half * 512:(half + 1) * 512], op=ALU.add),
                      [bL, B_cc], [B_lg])
                    A(lambda e: e.activation(out=PTk[:, half * 4:(half + 1) * 4, :], in_=lg[:, half * 512:(half + 1) * 512].rearrange("p (h q) -> p h q", q=128), func=AF.Exp),
                      [B_lg], [bPT])
                else:
                    A(lambda e: e.activation(out=PTk[:, half * 4:(half + 1) * 4, :], in_=pL[:, :].rearrange("p (h q) -> p h q", q=128), func=AF.Exp), [bL], [bPT])
            for h in range(8):
                pO, bO = ps[5 + h // 3], B_ps[5 + h // 3]
                P(lambda e: e.matmul(pO[:, (h % 3) * 129:(h % 3 + 1) * 129], lhsT=PTk[:, h, :], rhs=ckv_tok[:, kt, :], start=(kt == 0 and h % 3 == 0), stop=(kt == j),
                                     skip_group_check=True), [bPT, B_kv], [bO])
            if side is not None:
                next(side, None)
        for b3 in range(3):
            nh = 3 if b3 < 2 else 2
            V(lambda e: e.tensor_copy(out=Oacc[:, b3 * 3:b3 * 3 + nh, :], in_=ps[5 + b3][:, 0:nh * 129].rearrange("p (h d) -> p h d", d=129)), [B_ps[5 + b3]], [B_O])
        V(lambda e: e.reciprocal(out=rec[:, :], in_=Oacc[:, :, 128]), [B_O], [B_Oo])
        V(lambda e: e.tensor_tensor(out=Oo[:, :, :], in0=Oacc[:, :, 0:128], in1=rec[:, :].unsqueeze(2).to_broadcast([128, 8, 128]), op=ALU.mult), [B_O, B_Oo], [B_Oo])
        pX = ps[4][:, :].bitcast(BF16)
        for h in range(8):
            P(lambda e: e.transpose(pX[:, h * 128:(h + 1) * 128], Oo[:, h, :], identb[:, :]), [B_Oo, B_ident], [B_ps[4]])
        V(lambda e: e.tensor_copy(out=dsT[:, :, :], in_=pX[:, :].rearrange("p (a b) -> p a b", b=128)), [B_ps[4]], [B_dsT])
        for h in range(8):
            sc.dma("sp", L["dsao_d"][h * 128:(h + 1) * 128, qsl], dsT[:, h, :], reads=[B_dsT])

    stage1(0)
    for _ in bisect_iters(0):
        pass
    stage3(0)
    for j in range(NQB):
        side = None
        if j + 1 < NQB:
            stage1(j + 1)
            side = bisect_iters(j + 1)
        stage4(j, side)
        if side is not None:
            for _ in side:
                pass
            stage3(j + 1)
    ar.release(mC)


def phase_E(L):
    nc, sc, ar, ps, B_ps = L["nc"], L["sc"], L["ar"], L["ps"], L["B_ps"]
    identb, B_ident = L["identb"], L["B_ident"]
    gt_bc, B_gt = L["gt_bc"], L["B_gt"]
    iota128, B_iota = L["iota128"], L["B_iota"]
    dbg = L["dbg"]
    V = lambda fn, r=(), w=(): sc.op("dve", fn, r, w)
    A = lambda fn, r=(), w=(): sc.op("act", fn, r, w)
    P = lambda fn, r=(), w=(): sc.op("pe", fn, r, w)
    G = lambda fn, r=(), w=(): sc.op("pool", fn, r, w)
    ALPHA = 2.0 ** 0.25
    if not L["e0_done_flag"][0]:
        m0 = ar.mark()
        for _ in make_e0(L, 3, 0):
            pass
        ar.release(m0)
    ar.release(L["mark_stg"])
    mE = ar.mark()
    TP = 256
    lnbc = ar.alloc([128, 2, D], F32, "lnbcE")
    B_ln = Buf("lnE")
    sc.dma("sp", lnbc[:, :, :], L["lnbc_d"][:, 2:4, :], writes=[B_ln])
    Gs2 = [ar.alloc([128, 128, TP], BF16, "Gs%d" % i) for i in range(2)]
    h2Tp2 = [ar.alloc([128, 8, TP], BF16, "h2Tp%d" % i) for i in range(2)]
    IT1 = ar.alloc([128, 3, TP], F32, "IT1")
    IT2 = [IT1, IT1]
    ITb2 = [ar.alloc([128, 3, TP], BF16, "ITb%d" % i) for i in range(2)]
    iotab = ar.alloc([128, 128], BF16, "iotab")
    NBT = 8
    eqb = [ar.alloc([128, NBT, 128], BF16, "eqb%d" % i) for i in range(1)]
    Lb = [ar.alloc([128, NBT, 128], BF16, "Lb%d" % i) for i in range(2)]
    Rb = [ar.alloc([128, NBT, 128], BF16, "Rb%d" % i) for i in range(2)]
    B_Gs2 = [Buf("Gs0"), Buf("Gs1")]
    B_h2Tp2 = [Buf("h2Tp0"), Buf("h2Tp1")]
    B_IT2 = [Buf("IT0"), Buf("IT1")]
    B_ITf1 = Buf("ITf")
    B_ITf2 = [B_ITf1, B_ITf1]
    B_eq = [Buf("eqb0")]
    B_eqh = [Buf("eqh0"), Buf("eqh1")]
    V(lambda e: e.tensor_copy(out=iotab[:, :], in_=iota128[:, :]), [B_iota], [B_iota])
    B_Lb = [Buf("Lb0"), Buf("Lb1")]
    B_Rb = [Buf("Rb0"), Buf("Rb1")]
    NS = 4
    uTc = [ar.alloc([128, 1024], BF16, "uTc%d" % i) for i in range(NS)]
    vcb = [ar.alloc([128, 1024], BF16, "vcb%d" % i) for i in range(NS)]
    B_uTc = [Buf("uTc%d" % i) for i in range(NS)]
    B_vcb = [Buf("vcb%d" % i) for i in range(NS)]
    gl = [ar.alloc([128, TP], BF16, "gl%d" % i) for i in range(3)]
    AT = [ar.alloc([128, TP], BF16, "AT%d" % i) for i in range(3)]
    B_gl = [Buf("gl0"), Buf("gl1"), Buf("gl2")]
    B_AT = [Buf("AT0"), Buf("AT1"), Buf("AT2")]
    x1t = ar.alloc([128, D], F32, "x1t")
    oo = ar.alloc([128, D], F32, "ooE")
    jk = oo
    st = ar.alloc([128, 8], F32, "stE")
    B_x1t, B_oo, B_st = Buf("x1t"), Buf("ooE"), Buf("stE")
    B_jk = B_oo
    out_v = L["out_d"].rearrange("(n p) m -> p n m", p=128)
    iota3 = iotab[:, :].unsqueeze(1).to_broadcast([128, NBT, 128])
    npass = L["nblk"] * 2
    NBATCH = TP // NBT

    def emit_loads(p_):
        q = p_ % 2
        tsl = slice(p_ * TP, (p_ + 1) * TP)
        for kc in range(8):
            sc.dma("sp", h2Tp2[q][:, kc, :], L["h2T_d"][kc * 128:(kc + 1) * 128, tsl], writes=[B_h2Tp2[q]])
        for q3 in range(3):
            sc.dma("sp", IT2[q][:, q3, :], L["selT_d"][q3, :, tsl], writes=[B_ITf2[q]])
        A(lambda e: e.activation(out=ITb2[q][:, :, :], in_=IT2[q][:, :, :], func=AF.Copy), [B_ITf2[q]], [B_IT2[q]])

    gcount = [0]

    def gbatch_gen(p_, b):
        q = p_ % 2
        Gs, B_Gs, ITb, B_IT = Gs2[q], B_Gs2[q], ITb2[q], B_IT2[q]
        gi = gcount[0]
        gcount[0] += 1
        Lk, bLk = Lb[gi % 2], B_Lb[gi % 2]
        Rk, bRk = Rb[gi % 2], B_Rb[gi % 2]
        eq_ = eqb[0]
        H = NBT // 2
        for hf in range(2):
            hs_ = slice(hf * H, (hf + 1) * H)
            bs_ = slice(b * NBT + hf * H, b * NBT + (hf + 1) * H)
            io_ = iotab[:, :].unsqueeze(1).to_broadcast([128, H, 128])
            V(lambda e: e.tensor_tensor(out=eq_[:, hs_, :], in0=io_, in1=ITb[:, 0, bs_].unsqueeze(2).to_broadcast([128, H, 128]), op=ALU.is_equal), [B_IT, B_iota], [B_eqh[hf]])
            yield
            V(lambda e: e.tensor_tensor(out=Lk[:, hs_, :], in0=eq_[:, hs_, :], in1=ITb[:, 2, bs_].unsqueeze(2).to_broadcast([128, H, 128]), op=ALU.mult), [B_eqh[hf], B_IT], [bLk])
            yield
            V(lambda e: e.tensor_tensor(out=Rk[:, hs_, :], in0=io_, in1=ITb[:, 1, bs_].unsqueeze(2).to_broadcast([128, H, 128]), op=ALU.is_equal), [B_IT, B_iota], [bRk])
            yield
        yield
        yield
        for t4 in range(NBT // 4):
            pg, bpg = ps[7], B_ps[7]
            for tt in range(4):
                t = t4 * 4 + tt
                P(lambda e: e.matmul(pg[:, tt * 128:(tt + 1) * 128], lhsT=Lk[:, t, :], rhs=Rk[:, t, :], start=True, stop=True), [bLk, bRk], [bpg])
            t0 = b * NBT + t4 * 4
            src = pg[:, :].rearrange("p (t i) -> p i t", i=128)
            A(lambda e: e.activation(out=Gs[:, :, t0:t0 + 4], in_=src, func=AF.Copy), [bpg], [B_Gs])
            yield

    def gall_gen(p_):
        for b in range(NBATCH):
            for _ in gbatch_gen(p_, b):
                yield

    emit_loads(0)
    for _ in gall_gen(0):
        pass
    for p_ in range(npass):
        q = p_ % 2
        Gs, B_Gs, h2Tp, B_h2Tp = Gs2[q], B_Gs2[q], h2Tp2[q], B_h2Tp2[q]
        if p_ + 1 < npass:
            emit_loads(p_ + 1)

        def load_u(c):
            sc.dma("sp", uTc[c % NS][:, :], L["uv_d"][c, :, 0:1024], writes=[B_uTc[c % NS]])

        def emit_hu(c):
            k = c % NS
            if c == 0:
                load_u(0)
                load_u(1)
            if c + 2 < 128:
                load_u(c + 2)
            sc.dma("sp", vcb[k][:, :], L["uv_d"][c, :, 1024:2048], writes=[B_vcb[k]])
            pH, bH = ps[4 + c % 3], B_ps[4 + c % 3]
            for kc in range(8):
                P(lambda e: e.matmul(pH[:, 0:TP], lhsT=uTc[k][:, kc * 128:(kc + 1) * 128], rhs=h2Tp[:, kc, :], start=(kc == 0), stop=(kc == 7)), [B_uTc[k], B_h2Tp], [bH])

        def emit_y2(c):
            k = c % NS
            pH, bH = ps[4 + c % 3], B_ps[4 + c % 3]
            g_, bg_ = gl[c % 3], B_gl[c % 3]
            a_, ba_ = AT[c % 3], B_AT[c % 3]
            A(lambda e: e.activation(out=g_[:, :], in_=pH[:, 0:TP], func=AF.Gelu), [bH], [bg_])
            V(lambda e: e.tensor_tensor(out=a_[:, :], in0=g_[:, :], in1=Gs[:, c, :], op=ALU.mult), [bg_, B_Gs], [ba_])
            for tt in range(2):
                for half in range(2):
                    py, bpy = ps[tt * 2 + half], B_ps[tt * 2 + half]
                    P(lambda e: e.matmul(py[:, :], lhsT=a_[:, tt * 128:(tt + 1) * 128], rhs=vcb[k][:, half * 512:(half + 1) * 512], start=(c == 0), stop=(c == 127)),
                      [ba_, B_vcb[k]], [bpy])
        emit_hu(0)
        emit_hu(1)
        gg = gall_gen(p_ + 1) if p_ + 1 < npass else None
        steps_per_chunk = (NBATCH * 10 + 127) // 128
        for c in range(128):
            if c + 2 < 128:
                emit_hu(c + 2)
            emit_y2(c)
            if gg is not None:
                for _ in range(steps_per_chunk):
                    next(gg, None)
        if gg is not None:
            for _ in gg:
                pass
        for tt in range(2):
            n = p_ * 2 + tt
            sc.dma("sp", x1t[:, :], L["x1s_d"][n * 128:(n + 1) * 128, :], writes=[B_x1t])
            for half in range(2):
                hsl = slice(half * 512, (half + 1) * 512)
                py, bpy = ps[tt * 2 + half], B_ps[tt * 2 + half]
                if "y2dbg" in dbg:
                    V(lambda e: e.tensor_copy(out=oo[:, hsl], in_=py[:, :]), [bpy], [B_oo])
                    sc.dma("sp", L["y2_d"][n * 128:(n + 1) * 128, hsl], oo[:, hsl], reads=[B_oo])
                V(lambda e: e.tensor_tensor(out=oo[:, hsl], in0=py[:, :], in1=gt_bc[:, 3, hsl], op=ALU.mult), [bpy, B_gt], [B_oo])
            V(lambda e: e.scalar_tensor_tensor(out=x1t[:, :], in0=x1t[:, :], scalar=ALPHA, in1=oo[:, :], op0=ALU.mult, op1=ALU.add), [B_x1t, B_oo], [B_x1t])
            A(lambda e: e.activation(out=oo[:, :], in_=x1t[:, :], func=AF.Copy, accum_out=st[:, 0:1]), [B_x1t], [B_st, B_oo])
            A(lambda e: e.activation(out=oo[:, :], in_=x1t[:, :], func=AF.Square, accum_out=st[:, 1:2]), [B_x1t], [B_st, B_oo])
            V(lambda e: e.tensor_scalar(out=st[:, 2:3], in0=st[:, 0:1], scalar1=1.0 / D, scalar2=None, op0=ALU.mult), [B_st], [B_st])
            V(lambda e: e.tensor_tensor(out=st[:, 3:4], in0=st[:, 2:3], in1=st[:, 2:3], op=ALU.mult), [B_st], [B_st])
            V(lambda e: e.scalar_tensor_tensor(out=st[:, 4:5], in0=st[:, 1:2], scalar=1.0 / D, in1=st[:, 3:4], op0=ALU.mult, op1=ALU.subtract), [B_st], [B_st])
            V(lambda e: e.tensor_scalar(out=st[:, 4:5], in0=st[:, 4:5], scalar1=1e-5, scalar2=None, op0=ALU.add), [B_st], [B_st])
            A(lambda e: e.activation(out=st[:, 5:6], in_=st[:, 4:5], func=AF.Sqrt), [B_st], [B_st])
            V(lambda e: e.reciprocal(out=st[:, 5:6], in_=st[:, 5:6]), [B_st], [B_st])
            V(lambda e: e.scalar_tensor_tensor(out=st[:, 6:7], in0=st[:, 2:3], scalar=-1.0, in1=st[:, 5:6], op0=ALU.mult, op1=ALU.mult), [B_st], [B_st])
            A(lambda e: e.activation(out=oo[:, :], in_=x1t[:, :], func=AF.Identity, scale=st[:, 5:6], bias=st[:, 6:7]), [B_x1t, B_st], [B_oo])
            G(lambda e: e.tensor_tensor(out=oo[:, :], in0=oo[:, :], in1=lnbc[:, 0, :], op=ALU.mult), [B_oo, B_ln], [B_oo])
            G(lambda e: e.tensor_tensor(out=oo[:, :], in0=oo[:, :], in1=lnbc[:, 1, :], op=ALU.add), [B_oo, B_ln], [B_oo])
            sc.dma("sp", out_v[:, n, :], oo[:, :], reads=[B_oo])
    ar.release(mE)


def make_e0(L, NB, bank):
    sc, ar, ps, B_ps = L["sc"], L["ar"], L["ps"], L["B_ps"]
    identb, B_ident = L["identb"], L["B_ident"]
    A = lambda fn, r=(), w=(): sc.op("act", fn, r, w)
    P = lambda fn, r=(), w=(): sc.op("pe", fn, r, w)
    G = lambda fn, r=(), w=(): sc.op("pool", fn, r, w)
    NF = 3
    stf = [ar.alloc([128, D], F32, "e0f%d" % i) for i in range(NF)]
    o16 = [ar.alloc([128, D], BF16, "e0h%d" % i) for i in range(NF)]
    uT = [ar.alloc([128, D], BF16, "e0t%d" % i) for i in range(2)]
    B_f = [Buf("e0f%d" % i) for i in range(NF)]
    B_o = [Buf("e0h%d" % i) for i in range(NF)]
    B_t = [Buf("e0t0"), Buf("e0t1")]
    pu_v = L["pu_d"].rearrange("(i1 i2) d -> i2 i1 d", i2=128)
    pv_v = L["pv_d"].rearrange("(i1 i2) d -> i2 i1 d", i2=128)
    NI = 256

    def load(i):
        c, isv = i // 2, i % 2
        sc.dma("sp", stf[i % NF][:, :], (pv_v if isv else pu_v)[c, :, :], writes=[B_f[i % NF]])

    def cast(i):
        G(lambda e: e.tensor_copy(out=o16[i % NF][:, :], in_=stf[i % NF][:, :]), [B_f[i % NF]], [B_o[i % NF]])

    def finish(i):
        c, isv = i // 2, i % 2
        if isv:
            sc.dma("sp", L["uv_d"][c, :, 1024:2048], o16[i % NF][:, :], reads=[B_o[i % NF]])
        else:
            pT = ps[bank][:, :].bitcast(BF16)
            for kc in range(8):
                P(lambda e: e.transpose(pT[:, kc * 128:(kc + 1) * 128], o16[i % NF][:, kc * 128:(kc + 1) * 128], identb[:, :]), [B_o[i % NF], B_ident], [B_ps[bank]])
            A(lambda e: e.activation(out=uT[c % 2][:, :], in_=pT[:, :], func=AF.Copy), [B_ps[bank]], [B_t[c % 2]])
            sc.dma("sp", L["uv_d"][c, :, 0:1024], uT[c % 2][:, :], reads=[B_t[c % 2]])
    load(0)
    load(1)
    cast(0)
    for k in range(NI):
        if k + 2 < NI:
            load(k + 2)
        if k + 1 < NI:
            cast(k + 1)
        finish(k)
        yield
```

```python
import numpy as np
import ml_dtypes
import concourse.bass as bass
import concourse.mybir as mybir
from concourse.bass_utils import run_bass_kernel_spmd

F32 = mybir.dt.float32
BF16 = mybir.dt.bfloat16
I32 = mybir.dt.int32
U32 = mybir.dt.uint32
AF = mybir.ActivationFunctionType
ALU = mybir.AluOpType
AX = mybir.AxisListType

S = 4096
D = 1024
NT = S // 128
IN_COLS = 5316
DBG = {}
STOP_AFTER = None


class Buf:
    __slots__ = ("name", "lw", "rd")

    def __init__(self, name):
        self.name = name
        self.lw = None
        self.rd = []


class _Rec:
    def __init__(self):
        self.call = None

    def __getattr__(self, name):
        def f(*args, **kwargs):
            self.call = (name, args, kwargs)
            return self
        return f


class Sched:
    ENGS = ("pe", "act", "dve", "pool", "sp")

    def __init__(self, nc, n_dma_sems=40):
        self.nc = nc
        self.ops = {e: [] for e in self.ENGS}
        self.sem = {e: nc.alloc_semaphore("c_" + e) for e in self.ENGS}
        self.cnt = {e: 0 for e in self.ENGS}
        self.seen = {e: {} for e in self.ENGS}
        self.dsem = [nc.alloc_semaphore("d%d" % i) for i in range(n_dma_sems)]
        self.dval = [0] * n_dma_sems
        self.drr = 0
        self.drr_sw = 0
        self.NSW = 8
        self.NHW = n_dma_sems - 8
        self.all_events = []

    def _waits(self, eng, reads, writes):
        deps = []
        for b in reads:
            if b.lw is not None:
                deps.append(b.lw)
        for b in writes:
            if b.lw is not None:
                deps.append(b.lw)
            deps.extend(b.rd)
        out = {}
        for (sem, val, src) in deps:
            if src == "pe" and eng == "pe":
                continue
            k = sem.num
            if self.seen[eng].get(k, 0) >= val:
                continue
            if out.get(k, (None, 0))[1] < val:
                out[k] = (sem, val)
        for k, (sem, val) in out.items():
            self.seen[eng][k] = val
        return list(out.values())

    def op(self, eng, fn, reads=(), writes=()):
        waits = self._waits(eng, reads, writes)
        self.cnt[eng] += 1
        sem = self.sem[eng]
        val = self.cnt[eng]
        ev = (sem, val, eng)

        rec = _Rec()
        fn(rec)
        name, args, kwargs = rec.call

        def emit(e, waits=waits, sem=sem, name=name, args=args, kwargs=kwargs):
            for (s, v) in waits:
                e.wait_ge(s, v)
            getattr(e, name)(*args, **kwargs).then_inc(sem, 1)
        self.ops[eng].append(emit)
        for b in reads:
            b.rd.append(ev)
        for b in writes:
            b.lw = ev
            b.rd = []
        return ev

    def dma(self, eng, out, in_, reads=(), writes=(), fn=None, **kw):
        if fn is not None:
            rec = _Rec()
            fn(rec)
            mname, margs, mkw = rec.call
        else:
            mname, margs, mkw = "dma_start", (), dict(out=out, in_=in_, **kw)
        if eng == "pool":
            i = self.NHW + (self.drr_sw % self.NSW)
            self.drr_sw += 1
        else:
            i = self.drr
            self.drr = (self.drr + 1) % self.NHW
        sem = self.dsem[i]
        waits = self._waits(eng, reads, writes)
        prev = self.dval[i]
        if prev > 0 and self.seen[eng].get(sem.num, 0) < prev:
            waits.append((sem, prev))
            self.seen[eng][sem.num] = prev
        self.dval[i] += 16
        val = self.dval[i]
        ev = (sem, val, "dma")

        def emit(e, waits=waits, sem=sem, mname=mname, margs=margs, mkw=mkw):
            for (s, v) in waits:
                e.wait_ge(s, v)
            getattr(e, mname)(*margs, **mkw).then_inc(sem, 16)
        self.ops[eng].append(emit)
        for b in reads:
            b.rd.append(ev)
        for b in writes:
            b.lw = ev
            b.rd = []
        self.all_events.append(ev)
        return ev

    def raw(self, eng, fn):
        self.ops[eng].append(fn)

    def barrier(self):
        targets = []
        for en in self.ENGS:
            if self.cnt[en] > 0:
                targets.append((self.sem[en], self.cnt[en], en))
        for i, s in enumerate(self.dsem):
            if self.dval[i] > 0:
                targets.append((s, self.dval[i], "dma"))
        for eng in self.ENGS:
            waits = []
            for (s, v, src) in targets:
                if src == eng:
                    continue
                if self.seen[eng].get(s.num, 0) >= v:
                    continue
                self.seen[eng][s.num] = v
                waits.append((s, v))

            def emit(e, waits=waits):
                for (s, v) in waits:
                    e.wait_ge(s, v)
            self.ops[eng].append(emit)

    def finish(self, final_events):
        nc = self.nc
        with nc.Block() as block:
            def run(name):
                def f(e):
                    for emit in self.ops[name]:
                        emit(e)
                    if name == "sp":
                        for (s, v, _) in final_events:
                            e.wait_ge(s, v)
                        for i, s in enumerate(self.dsem):
                            if self.dval[i] > 0:
                                e.wait_ge(s, self.dval[i])
                        for en in ("pe", "act", "dve", "pool"):
                            if self.cnt[en] > 0:
                                e.wait_ge(self.sem[en], self.cnt[en])
                return f
            block.tensor(run("pe"))
            block.scalar(run("act"))
            block.vector(run("dve"))
            block.gpsimd(run("pool"))
            block.sync(run("sp"))


class Arena:
    def __init__(self, nc, base=0, top=192 * 1024):
        self.nc = nc
        self.off = base
        self.top = top
        self.n = 0
        self.sc = None

    def mark(self):
        return self.off

    def release(self, m):
        self.off = m
        if self.sc is not None:
            self.sc.barrier()

    def alloc(self, shape, dtype, name="t"):
        esz = {F32: 4, BF16: 2, I32: 4, U32: 4}[dtype]
        per = esz
        for s in shape[1:]:
            per *= s
        per = (per + 63) // 64 * 64
        self.n += 1
        t = self.nc.alloc_sbuf_tensor_at("%s_%d" % (name, self.n), list(shape), dtype, offset=self.off)
        self.off += per
        assert self.off <= self.top, ("SBUF overflow", name, self.off)
        return t


def build(dbg=None, stop_after=None, phases=None, feed=(), nblk=8):
    dbg = dbg or {}
    phases = phases or {'0', 'A', 'B', 'C', 'D', 'E', 'F'}
    nc = bass.Bass("TRN2", target_bir_lowering=False)
    sc = Sched(nc)
    ar = Arena(nc, base=(nc.sbuf_base + 63) // 64 * 64, top=nc.sbuf_top // 64 * 64)
    ar.sc = sc

    def din(name, shape, dt=F32):
        return nc.dram_tensor(name, list(shape), dt, kind="ExternalInput").ap()

    def dscratch(name, shape, dt=F32):
        kind = "ExternalOutput" if name in dbg else ("ExternalInput" if name in feed else "Internal")
        return nc.dram_tensor(name, list(shape), dt, kind=kind).ap()

    x_d = din("x", [S, D])
    c_d = din("c_col", [128, 8])
    wada_d = din("w_ada", [D, 6 * D])
    bada_col_d = din("b_ada_col", [128, 48])
    bada_bc_d = din("b_ada_bc", [128, 6 * D])
    win_d = din("w_in", [D, IN_COLS])
    ident_d = din("ident", [128, 128])
    out_d = nc.dram_tensor("out", [S, D], F32, kind="ExternalOutput").ap()

    mu_d = din("mu_col", [128, 14])
    rwvec_d = din("rwvec", [128, 20])
    w2a2_d = din("w2a2", [128, 512])
    g2_d = din("g2", [128, 512])
    gnbc_d = din("gn_bc", [128, 2, 256])
    cst_d = din("cst", [128, 1024])
    wbra_d = din("w_br_a", [512, D])
    wbrb_d = din("w_br_b", [D, D])
    wout_d = din("w_out", [D, D])
    lnbc_d = din("ln_bc", [128, 4, D])
    wq_d = din("peer_wq", [D, D])
    pkeys_d = din("peer_keysT", [128, 8, 128])
    pu_d = din("peer_u", [16384, D])
    pv_d = din("peer_v", [16384, D])
    cvec_d = din("cvec", [128, 256])
    biasT_d = din("biasT", [128, 3, 1024])
    negm_d = din("negm", [128, 128])
    zrw_d = dscratch("zrw", [1792, S], F32)
    zq_d = dscratch("zq", [1024, S], BF16)
    zqi_d = dscratch("zqi", [256, S], BF16)
    zg_d = dscratch("zg", [2048, S], BF16)
    ztok_d = dscratch("ztok", [S, 196], F32)
    rwo_d = dscratch("rwo", [512, S], BF16)
    dsao_d = dscratch("dsao", [1024, S], BF16)
    x1_d = dscratch("x1dbg", [S, D], F32)
    y2_d = dscratch("y2dbg", [S, D], F32)
    x1s_d = dscratch("x1s", [S, D], F32)
    h2T_d = dscratch("h2T", [D, S], BF16)
    selT_d = dscratch("selT", [3, 128, S], F32)
    uv_d = dscratch("uv16", [128, 128, 2048], BF16)
    iota_d = din("iota128", [128, 128])
    iota128 = ar.alloc([128, 128], F32, "iota128")
    B_iota = Buf("iota")
    iota16 = iota128[:, 0:16]

    ident = ar.alloc([128, 128], F32, "ident")
    identb = ar.alloc([128, 128], BF16, "identb")
    modcol = ar.alloc([128, 48], F32, "modcol")
    onep1 = ar.alloc([128, 8], F32, "onep1")
    onep2 = ar.alloc([128, 8], F32, "onep2")
    gt_bc = ar.alloc([128, 4, D], F32, "gt_bc")
    B_ident = Buf("ident")
    B_mod = Buf("mod")
    B_gt = Buf("gt")

    ps = [nc.alloc_psum_tensor("ps%d" % i, [128, 512], F32) for i in range(8)]
    B_ps = [Buf("ps%d" % i) for i in range(8)]

    sc.dma("sp", ident[:, :], ident_d[:, :], writes=[B_ident])
    sc.dma("sp", iota128[:, :], iota_d[:, :], writes=[B_iota])

    mark_stg = ar.mark()
    STG = 1024
    stg = [ar.alloc([128, STG], F32, "stg%d" % i) for i in range(3)]
    B_stg = [Buf("stg%d" % i) for i in range(3)]
    stg_i = [0]

    def load_bf16(dst, src, n, bdst, eng="pool"):
        P = dst.shape[0]
        for o in range(0, n, STG):
            w = min(STG, n - o)
            k = stg_i[0] % 3
            stg_i[0] += 1
            sc.dma("sp", stg[k][0:P, 0:w], src[:, o:o + w], writes=[B_stg[k]])
            sc.op(eng, lambda e, k=k, o=o, w=w, P=P, dst=dst: e.tensor_copy(out=dst[:, o:o + w], in_=stg[k][0:P, 0:w]),
                  reads=[B_stg[k]], writes=[bdst])
    sc.op("dve", lambda e: e.tensor_copy(out=identb[:, :], in_=ident[:, :]), reads=[B_ident], writes=[B_ident])

    if '0' in phases:
        m0 = ar.mark()
        c_sb = ar.alloc([128, 8], F32, "c_sb")
        sil = ar.alloc([128, 8], F32, "sil")
        silbc = ar.alloc([128, 8, 128], F32, "silbc")
        bcol = ar.alloc([128, 48], F32, "bcol")
        bbc = ar.alloc([128, 4, D], F32, "bbc")
        wa = [ar.alloc([128, 8, 1024], F32, "wa%d" % i) for i in range(4)]
        B_c = Buf("c")
        B_wa = [Buf("wa0"), Buf("wa1"), Buf("wa2"), Buf("wa3")]
        B_b = Buf("bcol")
        sc.dma("sp", c_sb[:, :], c_d[:, :], writes=[B_c])
        sc.dma("sp", bcol[:, :], bada_col_d[:, :], writes=[B_b])
        for gi_, g_ in enumerate((2, 3, 4, 5)):
            sc.dma("sp", bbc[:, gi_, :], bada_bc_d[:, g_ * D:(g_ + 1) * D], writes=[B_b])
        sc.op("act", lambda e: e.activation(out=sil[:, :], in_=c_sb[:, :], func=AF.Silu), reads=[B_c], writes=[B_c])
        for kc in range(8):
            sc.op("dve", lambda e, kc=kc: e.tensor_copy(out=silbc[:, kc, :], in_=sil[:, kc:kc + 1].to_broadcast([128, 128])),
                  reads=[B_c], writes=[B_c])
        wada_v = wada_d.rearrange("(kc p) n -> p kc n", p=128)
        for g in range(6):
            w = wa[g % 4]
            bw = B_wa[g % 4]
            for kc in range(8):
                sc.dma("sp", w[:, kc, :], wada_v[:, kc, g * 1024:(g + 1) * 1024], writes=[bw])
            if g in (2, 3, 4, 5):
                gi = g - 2
                for half in range(2):
                    p = ps[half]
                    for kc in range(8):
                        sc.op("pe", lambda e, p=p, w=w, kc=kc, half=half: e.matmul(
                            p[:, :], lhsT=silbc[:, kc, :], rhs=w[:, kc, half * 512:(half + 1) * 512],
                            start=(kc == 0), stop=(kc == 7)), reads=[bw, B_c], writes=[B_ps[half]])
                    sc.op("dve", lambda e, p=p, gi=gi, half=half: e.tensor_tensor(
                        out=gt_bc[:, gi, half * 512:(half + 1) * 512], in0=p[:, :],
                        in1=bbc[:, gi, half * 512:(half + 1) * 512], op=ALU.add),
                        reads=[B_ps[half], B_b], writes=[B_gt])
            if g in (0, 1, 3, 4):
                p = ps[2]
                for fc in range(8):
                    for kc in range(8):
                        sc.op("pe", lambda e, p=p, w=w, kc=kc, fc=fc: e.matmul(
                            p[:, fc:fc + 1], lhsT=w[:, kc, fc * 128:(fc + 1) * 128], rhs=sil[:, kc:kc + 1],
                            start=(kc == 0), stop=(kc == 7)), reads=[bw, B_c], writes=[B_ps[2]])
                sc.op("dve", lambda e, p=p, g=g: e.tensor_tensor(
                    out=modcol[:, g * 8:(g + 1) * 8], in0=p[:, 0:8], in1=bcol[:, g * 8:(g + 1) * 8], op=ALU.add),
                    reads=[B_ps[2], B_b], writes=[B_mod])
        sc.op("dve", lambda e: e.tensor_scalar(out=onep1[:, :], in0=modcol[:, 8:16], scalar1=1.0, scalar2=None, op0=ALU.add),
              reads=[B_mod], writes=[B_mod])
        sc.op("dve", lambda e: e.tensor_scalar(out=onep2[:, :], in0=modcol[:, 32:40], scalar1=1.0, scalar2=None, op0=ALU.add),
              reads=[B_mod], writes=[B_mod])
        sc.op("dve", lambda e: e.tensor_scalar(out=gt_bc[:, 2, :], in0=gt_bc[:, 2, :], scalar1=1.0, scalar2=None, op0=ALU.add),
              reads=[B_gt], writes=[B_gt])
        ar.release(m0)
        if "modcol" in dbg:
            dd = nc.dram_tensor("modcol_o", [128, 48], F32, kind="ExternalOutput").ap()
            sc.dma("sp", dd[:, :], modcol[:, :], reads=[B_mod])
            dd2 = nc.dram_tensor("gt_o", [128, 4 * D], F32, kind="ExternalOutput").ap()
            sc.dma("sp", dd2[:, :], gt_bc[:, :, :].rearrange("p a b -> p (a b)"), reads=[B_gt])

    if 'A' in phases:
        mA = ar.mark()
        fm_chunks = []
        for i in range(14):
            fm_chunks.append((i * 128, "rw", i))
        for i in range(8):
            fm_chunks.append((1792 + i * 128, "q", i))
        for i in range(2):
            fm_chunks.append((2944 + i * 128, "qi", i))
        for i in range(16):
            fm_chunks.append((3268 + i * 128, "g", i))
        winb = ar.alloc([128, 8, IN_COLS], BF16, "winb")
        B_win = Buf("win")
        win_v = win_d.rearrange("(kc p) n -> p kc n", p=128)
        for kc in range(8):
            load_bf16(winb[:, kc, :], win_v[:, kc, :], IN_COLS, B_win)
        xt = [ar.alloc([128, 4, D], F32, "xt%d" % i) for i in range(2)]
        B_xt = [Buf("xt0"), Buf("xt1")]
        hT = [ar.alloc([128, 8, 512], BF16, "hT%d" % i) for i in range(2)]
        B_hT = [Buf("hT0"), Buf("hT1")]
        NEV = 8
        ev32 = [ar.alloc([128, 512], F32, "ev32_%d" % i) for i in range(NEV)]
        ev16 = [ar.alloc([128, 512], BF16, "ev16_%d" % i) for i in range(NEV)]
        B_ev32 = [Buf("ev32_%d" % i) for i in range(NEV)]
        B_ev16 = [Buf("ev16_%d" % i) for i in range(NEV)]
        ztk = [ar.alloc([128, 196], F32, "ztk%d" % i) for i in range(2)]
        B_ztk = [Buf("ztk0"), Buf("ztk1")]
        x_v = x_d.rearrange("(n p) m -> p n m", p=128)
        pi = 0
        evi = 0
        tok_cols = [(2816, 128, 0), (3200, 68, 128)]
        for tb in range(8):
            xb = xt[tb % 2]
            bx = B_xt[tb % 2]
            hb = hT[tb % 2]
            bh = B_hT[tb % 2]
            for j in range(4):
                sc.dma("sp", xb[:, j, :], x_v[:, tb * 4 + j, :], writes=[bx])
            for kc in range(8):
                p = ps[pi % 8]
                bp = B_ps[pi % 8]
                pi += 1
                for j in range(4):
                    sc.op("pe", lambda e, p=p, xb=xb, j=j, kc=kc: e.transpose(
                        p[:, j * 128:(j + 1) * 128], xb[:, j, kc * 128:(kc + 1) * 128], ident[:, :]),
                        reads=[bx, B_ident], writes=[bp])
                sc.op("act", lambda e, p=p, hb=hb, kc=kc: e.activation(
                    out=hb[:, kc, :], in_=p[:, :], func=AF.Identity,
                    scale=onep1[:, kc:kc + 1], bias=modcol[:, kc:kc + 1]),
                    reads=[bp, B_mod], writes=[bh])
            for ci, (col0, kind, idx) in enumerate(fm_chunks):
                p = ps[pi % 8]
                bp = B_ps[pi % 8]
                pi += 1
                for kc in range(8):
                    sc.op("pe", lambda e, p=p, kc=kc, col0=col0, hb=hb: e.matmul(
                        p[:, :], lhsT=winb[:, kc, col0:col0 + 128], rhs=hb[:, kc, :],
                        start=(kc == 0), stop=(kc == 7)), reads=[B_win, bh], writes=[bp])
                k = evi % NEV
                evi += 1
                eng = "dve" if (ci % 2 == 0) else "act"
                tsl = slice(tb * 512, (tb + 1) * 512)
                if kind == "rw":
                    dst = ev32[k]
                    bd = B_ev32[k]
                    if eng == "dve":
                        sc.op("dve", lambda e, p=p, dst=dst: e.tensor_copy(out=dst[:, :], in_=p[:, :]), reads=[bp], writes=[bd])
                    else:
                        sc.op("act", lambda e, p=p, dst=dst: e.activation(out=dst[:, :], in_=p[:, :], func=AF.Copy), reads=[bp], writes=[bd])
                    sc.dma("sp", zrw_d[idx * 128:(idx + 1) * 128, tsl], dst[:, :], reads=[bd])
                elif kind in ("q", "qi"):
                    dst = ev16[k]
                    bd = B_ev16[k]
                    if eng == "dve":
                        sc.op("dve", lambda e, p=p, dst=dst: e.tensor_copy(out=dst[:, :], in_=p[:, :]), reads=[bp], writes=[bd])
                    else:
                        sc.op("act", lambda e, p=p, dst=dst: e.activation(out=dst[:, :], in_=p[:, :], func=AF.Copy), reads=[bp], writes=[bd])
                    dd = zq_d if kind == "q" else zqi_d
                    sc.dma("sp", dd[idx * 128:(idx + 1) * 128, tsl], dst[:, :], reads=[bd])
                else:
                    dst = ev16[k]
                    bd = B_ev16[k]
                    sc.op("act", lambda e, p=p, dst=dst: e.activation(out=dst[:, :], in_=p[:, :], func=AF.Sigmoid), reads=[bp], writes=[bd])
                    sc.dma("sp", zg_d[idx * 128:(idx + 1) * 128, tsl], dst[:, :], reads=[bd])
            for j in range(4):
                p = ps[pi % 8]
                bp = B_ps[pi % 8]
                pi += 1
                for (c0, ncol, o0) in tok_cols:
                    for kc in range(8):
                        sc.op("pe", lambda e, p=p, kc=kc, j=j, c0=c0, ncol=ncol, o0=o0, hb=hb: e.matmul(
                            p[:, o0:o0 + ncol], lhsT=hb[:, kc, j * 128:(j + 1) * 128], rhs=winb[:, kc, c0:c0 + ncol],
                            start=(kc == 0), stop=(kc == 7)), reads=[B_win, bh], writes=[bp])
                zt = ztk[j % 2]
                bz = B_ztk[j % 2]
                sc.op("dve", lambda e, p=p, zt=zt: e.tensor_copy(out=zt[:, :], in_=p[:, 0:196]), reads=[bp], writes=[bz])
                r0 = (tb * 4 + j) * 128
                sc.dma("sp", ztok_d[r0:r0 + 128, :], zt[:, :], reads=[bz])
        ar.release(mA)

    e0_done_flag = [False]
    if 'B' in phases:
        phase_B(locals())
    if 'C' in phases:
        phase_C(locals())
    if 'D' in phases:
        phase_D(locals())
    if 'E' in phases:
        phase_E(locals())

    sc.finish([])
    return nc


def _prep_inputs(inputs):
    f = lambda a: np.ascontiguousarray(np.asarray(a, dtype=np.float32))
    x = f(inputs["x"])
    c = f(inputs["c"])
    b_ada = f(inputs["b_ada"])[0]
    shared = {
        "w_ada": f(inputs["w_ada"])[0],
        "b_ada_col": np.ascontiguousarray(b_ada.reshape(48, 128).T),
        "b_ada_bc": np.ascontiguousarray(np.broadcast_to(b_ada[None, :], (128, 6 * D))),
        "w_in": f(inputs["w_in"])[0],
        "ident": np.eye(128, dtype=np.float32),
        "iota128": np.ascontiguousarray(np.broadcast_to(np.arange(128, dtype=np.float32)[None], (128, 128))),
    }
    col = lambda v, n: np.ascontiguousarray(f(v).reshape(n, 128).T)
    shared["mu_col"] = col(inputs["rw_mu"][0], 14)
    shared["rwvec"] = np.ascontiguousarray(np.concatenate([col(inputs["rw_w0"][0], 4), col(inputs["rw_a0"][0], 4), col(inputs["rw_k_k"][0], 4),
                                                            col(inputs["rw_k_a"][0], 4), col(f(inputs["rw_r_k"])[0].reshape(-1), 4)], axis=1))
    shared["w2a2"] = np.ascontiguousarray(np.concatenate([f(inputs["rw_w2"])[0], f(inputs["rw_a2"])[0]], axis=0))
    shared["g2"] = f(inputs["rw_g2"])[0]
    gn2 = np.stack([f(inputs["rw_gn_g"])[0], f(inputs["rw_gn_b"])[0]]).reshape(2, 4, 2, 64)
    gnl = np.zeros((128, 2, 4, 64), np.float32)
    gnl[0:64] = gn2[:, :, 0, :][None]
    gnl[64:128] = gn2[:, :, 1, :][None]
    shared["gn_bc"] = np.ascontiguousarray(gnl.reshape(128, 2, 256))
    cst = np.zeros((128, 1024), np.float32)
    iu = np.triu(np.ones((64, 64), np.float32), 1)
    il = np.triu(np.ones((64, 64), np.float32), 0)
    cst[:, 0:128] = np.block([[iu, il], [iu, il]])
    cst[0:64, 128:192] = iu.T
    cst[64:128, 128:192] = iu.T
    cst[0:64, 192:256] = 1.0
    cst[64:128, 256:320] = 1.0
    sm = np.ones(512, np.float32); sm[::64] = 0.0
    cst[:, 320:832] = sm[None, :]
    cst[0:64, 832:896] = np.eye(64, dtype=np.float32)
    cst[64:128, 832:896] = np.eye(64, dtype=np.float32)
    cst[:, 896] = 1.0
    shared["cst"] = cst
    shared["w_br_a"] = f(inputs["w_br_a"])[0]
    shared["w_br_b"] = f(inputs["w_br_b"])[0]
    shared["w_out"] = f(inputs["w_out"])[0]
    lnr = np.stack([f(inputs["ln1_g"])[0], f(inputs["ln1_b"])[0], f(inputs["ln2_g"])[0], f(inputs["ln2_b"])[0]])
    shared["ln_bc"] = np.ascontiguousarray(np.broadcast_to(lnr[None], (128, 4, D)))
    shared["peer_wq"] = f(inputs["peer_wq"])[0]
    pk = f(inputs["peer_keys"])[0]
    shared["peer_keysT"] = np.ascontiguousarray(pk.transpose(1, 3, 0, 2).reshape(128, 8, 128))
    shared["peer_u"] = f(inputs["peer_u"])[0]
    cv = np.concatenate([f(inputs["dsa_kv_g"])[0], f(inputs["idx_k_g"])[0], f(inputs["idx_k_b"])[0]])
    shared["cvec"] = np.ascontiguousarray(np.broadcast_to(cv[None], (128, 256)))
    rb = f(inputs["rel_bias"])
    nn_ = np.arange(0, 256)
    nf = np.maximum(nn_, 1).astype(np.float32)
    large = 16 + (np.log(nf / np.float32(16)) / np.float32(np.log(8.0)) * np.float32(16)).astype(np.int32)
    bucket = np.where(nn_ < 16, nn_, np.minimum(large, 31))
    sI = np.arange(128)[:, None]
    qI = np.arange(128)[None, :]
    bd = bucket[np.clip(qI - sI, 0, 255)]
    bp = bucket[np.clip(qI + 128 - sI, 0, 255)]
    bT = np.zeros((128, 3, 8, 128), np.float32)
    bT[:, 0] = rb[bd].transpose(0, 2, 1)
    bT[:, 1] = rb[bp].transpose(0, 2, 1)
    bT[:, 2] = rb[31][None, :, None]
    shared["biasT"] = np.ascontiguousarray(bT.reshape(128, 3, 1024))
    shared["negm"] = np.where(np.arange(128)[None, :] <= np.arange(128)[:, None], 0.0, -1e30).astype(np.float32)
    shared["peer_v"] = f(inputs["peer_v"])[0]
    maps = []
    for b in range(8):
        m = dict(shared)
        m["x"] = x[b]
        m["c_col"] = np.ascontiguousarray(c[b].reshape(8, 128).T)
        maps.append(m)
    return maps


def kernel(**inputs):
    nc = build()
    maps = _prep_inputs(inputs)
    res = run_bass_kernel_spmd(nc, maps, core_ids=list(range(8)))
    out = np.stack([np.asarray(r["out"], dtype=np.float32) for r in res.results], axis=0)
    return out


def _bc_mid(ap, n):
    sh = list(ap.shape)
    return ap.unsqueeze(1).to_broadcast([sh[0], n] + sh[1:])


def phase_B(L):
    nc, sc, ar, ps, B_ps = L["nc"], L["sc"], L["ar"], L["ps"], L["B_ps"]
    identb, B_ident, load_bf16 = L["identb"], L["B_ident"], L["load_bf16"]
    zrw_d, rwo_d = L["zrw_d"], L["rwo_d"]
    V = lambda fn, r=(), w=(): sc.op("dve", fn, r, w)
    A = lambda fn, r=(), w=(): sc.op("act", fn, r, w)
    P = lambda fn, r=(), w=(): sc.op("pe", fn, r, w)
    mB = ar.mark()
    cst = ar.alloc([128, 1024], F32, "cst")
    mu = ar.alloc([128, 14], F32, "mu")
    rwvec = ar.alloc([128, 20], F32, "rwvec")
    omka = ar.alloc([128, 4], F32, "omka")
    w2a2b = ar.alloc([128, 512], BF16, "w2a2b")
    g2b = ar.alloc([128, 512], BF16, "g2b")
    gnbc = ar.alloc([128, 2, 256], F32, "gnbc")
    bones = ar.alloc([128, 128], BF16, "bones")
    onesb = ar.alloc([128, 1], BF16, "onesb")
    B_c = Buf("cstB")
    sc.dma("sp", cst[:, :], L["cst_d"][:, :], writes=[B_c])
    sc.dma("sp", mu[:, :], L["mu_d"][:, :], writes=[B_c])
    sc.dma("sp", rwvec[:, :], L["rwvec_d"][:, :], writes=[B_c])
    sc.dma("sp", gnbc[:, :, :], L["gnbc_d"][:, :, :], writes=[B_c])
    load_bf16(w2a2b[:, :], L["w2a2_d"][:, :], 512, B_c)
    load_bf16(g2b[:, :], L["g2_d"][:, :], 512, B_c)
    V(lambda e: e.tensor_scalar(out=omka[:, :], in0=rwvec[:, 12:16], scalar1=-1.0, scalar2=1.0, op0=ALU.mult, op1=ALU.add), [B_c], [B_c])
    V(lambda e: e.tensor_copy(out=bones[:, :], in_=cst[:, 192:320]), [B_c], [B_c])
    V(lambda e: e.tensor_copy(out=onesb[:, :], in_=cst[:, 896:897]), [B_c], [B_c])
    maskA = cst[:, 0:128]
    maskT = cst[:, 128:192]
    scanmask = cst[:, 320:832]
    eye64 = cst[:, 832:896]
    w0c, a0c, kkc, kac, rkc = (rwvec[:, 0:4], rwvec[:, 4:8], rwvec[:, 8:12], rwvec[:, 12:16], rwvec[:, 16:20])

    zb = ar.alloc([128, 14, 513], F32, "zb")
    zs = ar.alloc([128, 14, 512], F32, "zs")
    tmp = [ar.alloc([128, 512], F32, "tmpB%d" % i) for i in range(3)]
    B_tmp = [Buf("tmpB%d" % i) for i in range(3)]
    th = ar.alloc([128, 512], BF16, "th")
    al16 = ar.alloc([128, 512], BF16, "al16")
    sgl = ar.alloc([128, 512], BF16, "sgl")
    sq16 = ar.alloc([128, 512], BF16, "sq16")
    asg = ar.alloc([128, 512], F32, "asg")
    kk = ar.alloc([128, 512], F32, "kk")
    kp = ar.alloc([128, 512], F32, "kp")
    bv = ar.alloc([128, 512], F32, "bv")
    lw = ar.alloc([128, 512], F32, "lw")
    cs = ar.alloc([128, 512], F32, "cs")
    E = [ar.alloc([128, 512], F32, "E%d" % i) for i in range(4)]
    E5 = ar.alloc([128, 4, 8], F32, "E5")
    AR = ar.alloc([128, 4, 8, 2, 64], BF16, "AR")
    BK = ar.alloc([128, 4, 8, 2, 64], BF16, "BK")
    KH = ar.alloc([128, 4, 512], BF16, "KH")
    BH = ar.alloc([128, 4, 512], BF16, "BH")
    Vb = ar.alloc([128, 4, 512], BF16, "Vb")
    rkr = ar.alloc([128, 4, 512], BF16, "rkr")
    rwoT = ar.alloc([128, 4, 512], BF16, "rwoT")
    B_zb, B_zs, B_pre, B_blk, B_rwoT = Buf("zb"), Buf("zs"), Buf("pre"), Buf("blk"), Buf("rwoT")
    Vt2p = [ar.alloc([128, 512], BF16, "Vt2_%d" % i) for i in range(2)]
    KHt2p = [ar.alloc([128, 512], BF16, "KHt2_%d" % i) for i in range(2)]
    BHt2p = [ar.alloc([128, 512], BF16, "BHt2_%d" % i) for i in range(2)]
    sABp = [ar.alloc([128, 4, 128], BF16, "sAB%d" % i) for i in range(2)]
    sAKp = [ar.alloc([128, 4, 128], BF16, "sAK%d" % i) for i in range(2)]
    Xfp = [ar.alloc([128, 4, 64], BF16, "Xf%d" % i) for i in range(2)]
    B_Vtp = [Buf("Vt0"), Buf("Vt1")]
    B_sAp = [Buf("sA0"), Buf("sA1")]
    B_Xfp = [Buf("Xf0"), Buf("Xf1")]
    Mx = [ar.alloc([128, 4, 64], BF16, "Mx%d" % i) for i in range(2)]
    MT = [ar.alloc([128, 4, 64], BF16, "MT%d" % i) for i in range(2)]
    X = [ar.alloc([128, 4, 64], BF16, "X%d" % i) for i in range(2)]
    RHSs = ar.alloc([128, 4, 64], BF16, "RHSs")
    SAs = ar.alloc([128, 4, 64], BF16, "SAs")
    ST = ar.alloc([128, 4, 64], BF16, "ST")
    STf = ar.alloc([128, 4, 64], F32, "STf")
    sqy = ar.alloc([128, 256], F32, "sqy")
    yn = ar.alloc([128, 256], F32, "yn")
    bon = ar.alloc([128, 256], F32, "bon")
    O16 = ar.alloc([128, 256], BF16, "O16")
    st8 = ar.alloc([128, 4, 8], F32, "st8")
    B_Vt, B_sA, B_M, B_MT, B_X, B_R, B_SA, B_ST, B_ep, B_st8, B_O = (Buf("Vt"), Buf("sA"), [Buf("M0"), Buf("M1")], [Buf("MT0"), Buf("MT1")],
                                                                    [Buf("X0"), Buf("X1")], Buf("R"), Buf("SA"), Buf("ST"), Buf("ep"), Buf("st8"), Buf("O"))
    e0 = make_e0(L, 2, 7) if ('E' in L["phases"] or 'E0' in L["phases"]) else iter(())
    V(lambda e: e.memset(STf[:, :, :], 0.0), [], [B_ST])
    V(lambda e: e.memset(ST[:, :, :], 0.0), [], [B_ST])
    V(lambda e: e.memset(zb[:, :, 0:1], 0.0), [], [B_zb])

    def v3(ap2):
        return ap2.rearrange("p (c t) -> p c t", t=64)

    for tb in range(L['nblk']):
        for i in range(14):
            if tb == 0:
                sc.dma("sp", zb[:, i, 1:513], zrw_d[i * 128:(i + 1) * 128, 0:512], writes=[B_zb])
            else:
                sc.dma("sp", zb[:, i, 0:513], zrw_d[i * 128:(i + 1) * 128, tb * 512 - 1:tb * 512 + 512], writes=[B_zb])
        for i in range(14):
            t0 = tmp[i % 2]
            bt0 = B_tmp[i % 2]
            V(lambda e, i=i, t0=t0: e.tensor_tensor(out=t0[:, :], in0=zb[:, i, 0:512], in1=zb[:, i, 1:513], op=ALU.subtract), [B_zb], [bt0])
            V(lambda e, i=i, t0=t0: e.scalar_tensor_tensor(out=zs[:, i, :], in0=t0[:, :], scalar=mu[:, i:i + 1], in1=zb[:, i, 1:513],
                                                           op0=ALU.mult, op1=ALU.add), [bt0, B_zb, B_c], [B_zs])
        A(lambda e: e.activation(out=th[:, :], in_=zs[:, 12, :], func=AF.Tanh), [B_zs], [B_pre])
        V(lambda e: e.tensor_copy(out=al16[:, :], in_=zs[:, 12, :]), [B_zs], [B_pre])
        A(lambda e: e.activation(out=sgl[:, :], in_=zs[:, 13, :], func=AF.Sigmoid), [B_zs], [B_blk])
        for j in range(4):
            pW, bW = ps[0], B_ps[0]
            pA, bA = ps[1], B_ps[1]
            pQ, bQ = ps[2], B_ps[2]
            P(lambda e, j=j: e.matmul(pW[:, :], lhsT=w2a2b[0:64, j * 128:(j + 1) * 128], rhs=th[0:64, :], start=True, stop=True), [B_c, B_pre], [bW])
            P(lambda e, j=j: e.matmul(pA[:, :], lhsT=w2a2b[64:128, j * 128:(j + 1) * 128], rhs=al16[64:128, :], start=True, stop=True), [B_c, B_pre], [bA])
            A(lambda e, j=j: e.activation(out=lw[:, :], in_=pW[:, :], func=AF.Sigmoid, bias=w0c[:, j:j + 1]), [bW, B_c], [B_pre])
            A(lambda e, j=j: e.activation(out=asg[:, :], in_=pA[:, :], func=AF.Sigmoid, bias=a0c[:, j:j + 1]), [bA, B_c], [B_pre])
            A(lambda e, j=j: e.activation(out=sq16[:, :], in_=zs[:, 4 + j, :], func=AF.Square, scale=kkc[:, j:j + 1]), [B_zs, B_c], [B_pre])
            P(lambda e: e.matmul(pQ[:, :], lhsT=bones[:, :], rhs=sq16[:, :], start=True, stop=True), [B_c, B_pre], [bQ])
            V(lambda e: e.tensor_scalar(out=tmp[2][:, :], in0=pQ[:, :], scalar1=1e-24, scalar2=None, op0=ALU.max), [bQ], [B_tmp[2]])
            A(lambda e: e.activation(out=tmp[2][:, :], in_=tmp[2][:, :], func=AF.Sqrt), [B_tmp[2]], [B_tmp[2]])
            V(lambda e: e.reciprocal(out=tmp[2][:, :], in_=tmp[2][:, :]), [B_tmp[2]], [B_tmp[2]])
            V(lambda e, j=j: e.scalar_tensor_tensor(out=kk[:, :], in0=zs[:, 4 + j, :], scalar=kkc[:, j:j + 1], in1=tmp[2][:, :],
                                                    op0=ALU.mult, op1=ALU.mult), [B_zs, B_tmp[2], B_c], [B_pre])
            V(lambda e, j=j: e.tensor_scalar(out=tmp[0][:, :], in0=asg[:, :], scalar1=kac[:, j:j + 1], scalar2=omka[:, j:j + 1],
                                             op0=ALU.mult, op1=ALU.add), [B_pre, B_c], [B_tmp[0]])
            V(lambda e, j=j: e.tensor_tensor(out=kp[:, :], in0=zs[:, 4 + j, :], in1=tmp[0][:, :], op=ALU.mult), [B_zs, B_tmp[0]], [B_pre])
            V(lambda e: e.tensor_tensor(out=bv[:, :], in0=kk[:, :], in1=asg[:, :], op=ALU.mult), [B_pre], [B_pre])
            V(lambda e: e.tensor_scalar(out=lw[:, :], in0=lw[:, :], scalar1=-0.6065306597126334, scalar2=None, op0=ALU.mult), [B_pre], [B_pre])
            V(lambda e: e.tensor_tensor_scan(out=cs[:, :], data0=scanmask, data1=lw[:, :], initial=0.0, op0=ALU.mult, op1=ALU.add), [B_pre, B_c], [B_pre])
            V(lambda e: e.tensor_tensor(out=tmp[0][:, :], in0=cs[:, :], in1=lw[:, :], op=ALU.subtract), [B_pre], [B_tmp[0]])
            V(lambda e: e.tensor_tensor(out=v3(tmp[1][:, :]), in0=v3(cs[:, :])[:, :, 63:64].to_broadcast([128, 8, 64]), in1=v3(cs[:, :]),
                                        op=ALU.subtract), [B_pre], [B_tmp[1]])
            A(lambda e: e.activation(out=E[0][:, :], in_=cs[:, :], func=AF.Exp), [B_pre], [B_pre])
            A(lambda e: e.activation(out=E[1][:, :], in_=cs[:, :], func=AF.Exp, scale=-1.0), [B_pre], [B_pre])
            A(lambda e: e.activation(out=E[2][:, :], in_=tmp[0][:, :], func=AF.Exp), [B_tmp[0]], [B_pre])
            A(lambda e: e.activation(out=E[3][:, :], in_=tmp[1][:, :], func=AF.Exp), [B_tmp[1]], [B_pre])
            V(lambda e, j=j: e.tensor_copy(out=E5[:, j, :], in_=v3(E[0][:, :])[:, :, 63]), [B_pre], [B_blk])
            V(lambda e, j=j: e.tensor_tensor(out=AR[:, j, :, 1, :], in0=v3(zs[:, j, :]), in1=v3(E[0][:, :]), op=ALU.mult), [B_zs, B_pre], [B_blk])
            V(lambda e, j=j: e.tensor_tensor(out=BK[:, j, :, 1, :], in0=v3(kp[:, :]), in1=v3(E[1][:, :]), op=ALU.mult), [B_pre], [B_blk])
            V(lambda e, j=j: e.tensor_tensor(out=BK[:, j, :, 0, :], in0=v3(bv[:, :]), in1=v3(E[1][:, :]), op=ALU.mult), [B_pre], [B_blk])
            V(lambda e, j=j: e.scalar_tensor_tensor(out=AR[:, j, :, 0, :], in0=v3(kk[:, :]), scalar=-1.0, in1=v3(E[2][:, :]),
                                                    op0=ALU.mult, op1=ALU.mult), [B_pre], [B_blk])
            V(lambda e, j=j: e.tensor_tensor(out=KH[:, j, :], in0=kp[:, :], in1=E[3][:, :], op=ALU.mult), [B_pre], [B_blk])
            V(lambda e, j=j: e.tensor_tensor(out=BH[:, j, :], in0=bv[:, :], in1=E[3][:, :], op=ALU.mult), [B_pre], [B_blk])
            A(lambda e, j=j: e.activation(out=Vb[:, j, :], in_=zs[:, 8 + j, :], func=AF.Copy), [B_zs], [B_blk])
            V(lambda e, j=j: e.scalar_tensor_tensor(out=rkr[:, j, :], in0=zs[:, j, :], scalar=rkc[:, j:j + 1], in1=kp[:, :],
                                                    op0=ALU.mult, op1=ALU.mult), [B_zs, B_pre, B_c], [B_blk])

        def v4(ap2):
            return ap2.rearrange("p (j t) -> p j t", t=64)
        hl = [(h // 2, slice((h % 2) * 64, (h % 2) * 64 + 64), slice((h // 2) * 64, (h // 2) * 64 + 64), slice(h * 64, (h + 1) * 64)) for h in range(8)]

        def pre(c):
            q = c % 2
            csl = slice(c * 64, (c + 1) * 64)
            Vt2, KHt2, BHt2, sAB, sAK = Vt2p[q], KHt2p[q], BHt2p[q], sABp[q], sAKp[q]
            B_Vt, B_sA = B_Vtp[q], B_sAp[q]
            pT = ps[3][:, :].bitcast(BF16)
            pT2 = ps[4][:, :].bitcast(BF16)
            for half in range(2):
                hp = slice(half * 64, half * 64 + 64)
                for j in range(4):
                    P(lambda e: e.transpose(pT[hp, j * 128:(j + 1) * 128], Vb[:, j, csl], identb[:, :]), [B_blk, B_ident], [B_ps[3]])
                    P(lambda e: e.transpose(pT[hp, 512 + j * 128:512 + (j + 1) * 128], KH[:, j, csl], identb[:, :]), [B_blk, B_ident], [B_ps[3]])
                    P(lambda e: e.transpose(pT2[hp, j * 128:(j + 1) * 128], BH[:, j, csl], identb[:, :]), [B_blk, B_ident], [B_ps[4]])
            yield
            V(lambda e: e.tensor_copy(out=Vt2[:, :], in_=pT[:, 0:512]), [B_ps[3]], [B_Vt])
            A(lambda e: e.activation(out=KHt2[:, :], in_=pT[:, 512:1024], func=AF.Copy), [B_ps[3]], [B_Vt])
            A(lambda e: e.activation(out=BHt2[:, :], in_=pT2[:, 0:512], func=AF.Copy), [B_ps[4]], [B_Vt])
            for (j, pp, js, hs) in hl:
                P(lambda e: e.matmul(ps[0][pp, j * 128:(j + 1) * 128], lhsT=BK[pp, j, c, 0, :], rhs=AR[pp, j, c, :, :], start=True, stop=True), [B_blk], [B_ps[0]])
                P(lambda e: e.matmul(ps[1][pp, j * 128:(j + 1) * 128], lhsT=BK[pp, j, c, 1, :], rhs=AR[pp, j, c, :, :], start=True, stop=True), [B_blk], [B_ps[1]])
                P(lambda e: e.matmul(ps[2][pp, j * 64:(j + 1) * 64], lhsT=AR[pp, j, c, 0, :], rhs=BK[pp, j, c, 0, :], start=True, stop=True), [B_blk], [B_ps[2]])
            yield
            V(lambda e: e.tensor_tensor(out=sAB[:, :, :], in0=ps[0][:, :].rearrange("p (j t) -> p j t", t=128), in1=_bc_mid(maskA, 4), op=ALU.mult), [B_ps[0], B_c], [B_sA])
            V(lambda e: e.tensor_tensor(out=sAK[:, :, :], in0=ps[1][:, :].rearrange("p (j t) -> p j t", t=128), in1=_bc_mid(maskA, 4), op=ALU.mult), [B_ps[1], B_c], [B_sA])
            V(lambda e: e.tensor_tensor(out=MT[0][:, :, :], in0=v4(ps[2][:, 0:256]), in1=_bc_mid(maskT, 4), op=ALU.mult), [B_ps[2], B_c], [B_MT[0]])
            V(lambda e: e.tensor_copy(out=Mx[0][:, :, :], in_=sAB[:, :, 0:64]), [B_sA], [B_M[0]])
            V(lambda e: e.tensor_tensor(out=X[0][:, :, :], in0=sAB[:, :, 0:64], in1=_bc_mid(eye64, 4), op=ALU.add), [B_sA, B_c], [B_X[0]])
            yield
            cur = 0
            pa, pb, pc = ps[2], ps[3], ps[4]
            for rd in range(5):
                nxt = 1 - cur
                for (j, pp, js, hs) in hl:
                    P(lambda e: e.matmul(pa[pp, js], lhsT=MT[cur][pp, j, :], rhs=Mx[cur][pp, j, :], start=True, stop=True), [B_MT[cur], B_M[cur]], [B_ps[2]])
                    P(lambda e: e.matmul(pb[pp, js], lhsT=Mx[cur][pp, j, :], rhs=MT[cur][pp, j, :], start=True, stop=True), [B_MT[cur], B_M[cur]], [B_ps[3]])
                yield
                V(lambda e: e.tensor_copy(out=Mx[nxt][:, :, :], in_=v4(pa[:, 0:256])), [B_ps[2]], [B_M[nxt]])
                A(lambda e: e.activation(out=MT[nxt][:, :, :], in_=v4(pb[:, 0:256]), func=AF.Copy), [B_ps[3]], [B_MT[nxt]])
                for (j, pp, js, hs) in hl:
                    P(lambda e: e.matmul(pc[pp, js], lhsT=MT[nxt][pp, j, :], rhs=X[cur][pp, j, :], start=True, stop=True), [B_MT[nxt], B_X[cur]], [B_ps[4]])
                yield
                if rd < 4:
                    V(lambda e: e.tensor_tensor(out=X[nxt][:, :, :], in0=X[cur][:, :, :], in1=v4(pc[:, 0:256]), op=ALU.add), [B_X[cur], B_ps[4]], [B_X[nxt]])
                else:
                    V(lambda e: e.tensor_tensor(out=Xfp[q][:, :, :], in0=X[cur][:, :, :], in1=v4(pc[:, 0:256]), op=ALU.add), [B_X[cur], B_ps[4]], [B_Xfp[q]])
                cur = nxt
                yield

        def post(c):
            q = c % 2
            csl = slice(c * 64, (c + 1) * 64)
            Vt2, KHt2, BHt2, sAB, sAK, Xf = Vt2p[q], KHt2p[q], BHt2p[q], sABp[q], sAKp[q], Xfp[q]
            B_Vt, B_sA, bXf = B_Vtp[q], B_sAp[q], B_Xfp[q]
            pR, bR = ps[5], B_ps[5]
            pY, bY = ps[6], B_ps[6]
            pU, bU = ps[7], B_ps[7]
            for (j, pp, js, hs) in hl:
                P(lambda e: e.matmul(pR[pp, js], lhsT=AR[pp, j, c, 0, :], rhs=ST[pp, j, :], start=True, stop=False), [B_blk, B_ST], [bR])
                P(lambda e: e.matmul(pR[pp, js], lhsT=sAK[pp, j, 0:64], rhs=Vt2[pp, hs], start=False, stop=True), [B_sA, B_Vt], [bR])
            yield
            V(lambda e: e.tensor_copy(out=RHSs[:, :, :], in_=v4(pR[:, 0:256])), [bR], [B_R])
            for (j, pp, js, hs) in hl:
                P(lambda e: e.matmul(pR[pp, js], lhsT=Xf[pp, j, :], rhs=RHSs[pp, j, :], start=True, stop=True), [bXf, B_R], [bR])
            yield
            V(lambda e: e.tensor_copy(out=SAs[:, :, :], in_=v4(pR[:, 0:256])), [bR], [B_SA])
            for (j, pp, js, hs) in hl:
                P(lambda e: e.matmul(pY[pp, js], lhsT=AR[pp, j, c, 1, :], rhs=ST[pp, j, :], start=True, stop=False), [B_blk, B_ST], [bY])
                P(lambda e: e.matmul(pY[pp, js], lhsT=sAK[pp, j, 64:128], rhs=Vt2[pp, hs], start=False, stop=False), [B_sA, B_Vt], [bY])
                P(lambda e: e.matmul(pY[pp, js], lhsT=sAB[pp, j, 64:128], rhs=SAs[pp, j, :], start=False, stop=True), [B_sA, B_SA], [bY])
            for (j, pp, js, hs) in hl:
                P(lambda e: e.matmul(pU[pp, js], lhsT=KHt2[pp, hs], rhs=Vt2[pp, hs], start=True, stop=False), [B_Vt], [bU])
                P(lambda e: e.matmul(pU[pp, js], lhsT=BHt2[pp, hs], rhs=SAs[pp, j, :], start=False, stop=True), [B_Vt, B_SA], [bU])
            yield
            V(lambda e: e.tensor_tensor(out=STf[:, :, :], in0=STf[:, :, :], in1=E5[:, :, c:c + 1].to_broadcast([128, 4, 64]), op=ALU.mult), [B_blk, B_ST, bY, bR], [B_ST])
            V(lambda e: e.tensor_tensor(out=STf[:, :, :], in0=STf[:, :, :], in1=v4(pU[:, 0:256]), op=ALU.add), [bU, B_ST], [B_ST])
            V(lambda e: e.tensor_copy(out=ST[:, :, :], in_=STf[:, :, :]), [B_ST], [B_ST])
            pG, bG = ps[5], B_ps[5]
            for (j, pp, js, hs) in hl:
                P(lambda e: e.matmul(pG[pp, 256 + j:256 + j + 1], lhsT=rkr[pp, j, csl], rhs=onesb[pp, 0:1], start=True, stop=True), [B_blk, B_c, B_SA], [bG])
                P(lambda e: e.matmul(pG[pp, js], lhsT=sgl[:, csl], rhs=g2b[:, hs], start=True, stop=True), [B_blk, B_c, B_SA, B_R], [bG])
            y3 = v4(pY[:, 0:256])
            V(lambda e: e.tensor_reduce(out=st8[:, :, 0], in_=y3, axis=AX.X, op=ALU.add), [bY], [B_st8])
            A(lambda e: e.activation(out=sqy[:, :], in_=pY[:, 0:256], func=AF.Square), [bY], [B_ep])
            yield
            V(lambda e: e.tensor_reduce(out=st8[:, :, 1], in_=v4(sqy[:, :]), axis=AX.X, op=ALU.add), [B_ep], [B_st8])
            V(lambda e: e.tensor_scalar(out=st8[:, :, 2], in0=st8[:, :, 0], scalar1=1.0 / 64, scalar2=None, op0=ALU.mult), [B_st8], [B_st8])
            V(lambda e: e.tensor_tensor(out=st8[:, :, 3], in0=st8[:, :, 2], in1=st8[:, :, 2], op=ALU.mult), [B_st8], [B_st8])
            V(lambda e: e.scalar_tensor_tensor(out=st8[:, :, 4], in0=st8[:, :, 1], scalar=1.0 / 64, in1=st8[:, :, 3], op0=ALU.mult, op1=ALU.subtract), [B_st8], [B_st8])
            V(lambda e: e.tensor_scalar(out=st8[:, :, 4], in0=st8[:, :, 4], scalar1=64e-5, scalar2=None, op0=ALU.add), [B_st8], [B_st8])
            A(lambda e: e.activation(out=st8[:, :, 5], in_=st8[:, :, 4], func=AF.Sqrt), [B_st8], [B_st8])
            yield
            V(lambda e: e.reciprocal(out=st8[:, :, 5], in_=st8[:, :, 5]), [B_st8], [B_st8])
            yn3 = v4(yn[:, :])
            V(lambda e: e.tensor_tensor(out=yn3, in0=y3, in1=st8[:, :, 2:3].to_broadcast([128, 4, 64]), op=ALU.subtract), [bY, B_st8], [B_ep])
            V(lambda e: e.tensor_tensor(out=yn3, in0=yn3, in1=st8[:, :, 5:6].to_broadcast([128, 4, 64]), op=ALU.mult), [B_ep, B_st8], [B_ep])
            V(lambda e: e.tensor_tensor(out=yn[:, :], in0=yn[:, :], in1=gnbc[:, 0, :], op=ALU.mult), [B_ep, B_c], [B_ep])
            V(lambda e: e.tensor_tensor(out=yn[:, :], in0=yn[:, :], in1=gnbc[:, 1, :], op=ALU.add), [B_ep, B_c], [B_ep])
            V(lambda e: e.tensor_copy(out=st8[:, :, 6], in_=pG[:, 256:260]), [bG], [B_st8])
            for half in range(2):
                hp = slice(half * 64, half * 64 + 64)
                V(lambda e: e.tensor_tensor(out=v4(bon[hp, :]), in0=Vt2[hp, :].rearrange("p (j q t) -> p j q t", q=2, t=64)[:, :, half, :],
                                            in1=st8[hp, :, 6:7].to_broadcast([64, 4, 64]), op=ALU.mult), [B_Vt, B_st8], [B_ep])
            V(lambda e: e.tensor_tensor(out=yn[:, :], in0=yn[:, :], in1=bon[:, :], op=ALU.add), [B_ep], [B_ep])
            V(lambda e: e.tensor_tensor(out=O16[:, :], in0=yn[:, :], in1=pG[:, 0:256], op=ALU.mult), [B_ep, bG], [B_O])
            pO = pU[:, 384:512].bitcast(BF16)
            for (j, pp, js, hs) in hl:
                P(lambda e: e.transpose(pO[pp, js], O16[pp, js], identb[pp, pp]), [B_O, B_ident, B_ST], [bU])
            yield
            V(lambda e: e.tensor_copy(out=rwoT[:, :, csl], in_=v4(pO[:, 0:256])), [bU], [B_rwoT])

        def run_both(ga, gb):
            alive_a, alive_b = ga is not None, gb is not None
            while alive_a or alive_b:
                if alive_a:
                    try:
                        next(ga)
                    except StopIteration:
                        alive_a = False
                if alive_b:
                    try:
                        next(gb)
                    except StopIteration:
                        alive_b = False

        run_both(pre(0), None)
        for c in range(8):
            for _ in range(4):
                next(e0, None)
            run_both(post(c), pre(c + 1) if c + 1 < 8 else None)
        for j in range(4):
            sc.dma("sp", rwo_d[j * 128:(j + 1) * 128, tb * 512:(tb + 1) * 512], rwoT[:, j, :], reads=[B_rwoT])
    for _ in e0:
        pass
    if 'E' in L["phases"]:
        L["e0_done_flag"][0] = True
    ar.release(mB)


def phase_D(L):
    nc, sc, ar, ps, B_ps = L["nc"], L["sc"], L["ar"], L["ps"], L["B_ps"]
    ident, identb, B_ident, load_bf16 = L["ident"], L["identb"], L["B_ident"], L["load_bf16"]
    gt_bc, B_gt = L["gt_bc"], L["B_gt"]
    dbg = L["dbg"]
    V = lambda fn, r=(), w=(): sc.op("dve", fn, r, w)
    A = lambda fn, r=(), w=(): sc.op("act", fn, r, w)
    P = lambda fn, r=(), w=(): sc.op("pe", fn, r, w)
    G = lambda fn, r=(), w=(): sc.op("pool", fn, r, w)
    ALPHA = 2.0 ** 0.25
    mD = ar.mark()
    wbra = ar.alloc([128, 4, D], BF16, "wbra")
    wbrb = ar.alloc([128, 8, D], BF16, "wbrb")
    wout = ar.alloc([128, 8, D], BF16, "wout")
    wqb = ar.alloc([128, 8, D], BF16, "wqb")
    keysT = ar.alloc([128, 8, 128], BF16, "keysT")
    lnbc = ar.alloc([128, 2, D], F32, "lnbc")
    B_w = Buf("wD")
    for kc in range(4):
        load_bf16(wbra[:, kc, :], L["wbra_d"][kc * 128:(kc + 1) * 128, :], D, B_w)
    for kc in range(8):
        load_bf16(wbrb[:, kc, :], L["wbrb_d"][kc * 128:(kc + 1) * 128, :], D, B_w)
        load_bf16(wout[:, kc, :], L["wout_d"][kc * 128:(kc + 1) * 128, :], D, B_w)
        load_bf16(wqb[:, kc, :], L["wq_d"][kc * 128:(kc + 1) * 128, :], D, B_w)
    load_bf16(keysT[:, :, :].rearrange("p a b -> p (a b)"), L["pkeys_d"][:, :, :].rearrange("p a b -> p (a b)"), 1024, B_w)
    sc.dma("sp", lnbc[:, :, :], L["lnbc_d"][:, 0:2, :], writes=[B_w])
    rwoB = ar.alloc([128, 4, 512], BF16, "rwoB")
    dsaB = ar.alloc([128, 8, 512], BF16, "dsaB")
    zgr = [ar.alloc([128, 2, 512], BF16, "zgr%d" % i) for i in range(2)]
    B_zgr = [Buf("zgr0"), Buf("zgr1")]
    mg = ar.alloc([128, 8, 512], BF16, "mg")
    t1 = ar.alloc([128, 512], F32, "t1")
    t2 = ar.alloc([128, 512], F32, "t2")
    B_in, B_mg, B_t = Buf("inD"), Buf("mg"), Buf("tD")
    xt = ar.alloc([128, D], F32, "xtD")
    u = ar.alloc([128, D], F32, "uD")
    x1 = ar.alloc([128, D], F32, "x1")
    h2 = ar.alloc([128, D], F32, "h2")
    st = ar.alloc([128, 8], F32, "stD")
    h2T = ar.alloc([128, 8, 128], BF16, "h2T")
    qT = ar.alloc([128, 8, 128], BF16, "qT")
    ssb = ar.alloc([128, 16, 128], F32, "ssb")
    stmp = ar.alloc([128, 256], F32, "stmp")
    tv = ar.alloc([128, 16, 16], F32, "tv")
    ti = ar.alloc([128, 16, 16], U32, "ti")
    tif = ar.alloc([128, 16, 16], F32, "tif")
    cand = ar.alloc([128, 8, 256], F32, "cand")
    mv = ar.alloc([128, 8, 16], F32, "mv")
    posu = ar.alloc([128, 8, 16], U32, "posu")
    au = ar.alloc([128, 8, 16], U32, "au")
    bu = ar.alloc([128, 8, 16], U32, "bu")
    abf = ar.alloc([128, 2, 128], F32, "abf")
    oh16 = ar.alloc([128, 128, 16], F32, "oh16")
    junk = oh16[:, 0:64, :].rearrange("p a b -> p (a b)")
    sel = ar.alloc([128, 3, 128], F32, "sel")
    selT = ar.alloc([128, 3, 128], F32, "selT")
    gate = ar.alloc([128, 8, 16], F32, "gate")
    gs = ar.alloc([128, 8], F32, "gs")
    iota16 = L["iota16"]
    B_oh, B_sel, B_selT = Buf("oh"), Buf("sel"), Buf("selT")
    B_x, B_u, B_x1, B_h2, B_st, B_h2T, B_qT, B_s, B_tk, B_c, B_e, B_hu, B_acc, B_j = [Buf(n) for n in
        ("x", "u", "x1", "h2", "st", "h2T", "qT", "s", "tk", "cand", "eid", "hu", "acc", "junk")]
    x_v = L["x_d"].rearrange("(n p) m -> p n m", p=128)
    out_v = L["out_d"].rearrange("(n p) m -> p n m", p=128)

    def layer_norm(src, bsrc, dst, bdst, gi):
        A(lambda e: e.activation(out=junk, in_=src[:, :], func=AF.Copy, accum_out=st[:, 0:1]), [bsrc], [B_st, B_oh])
        A(lambda e: e.activation(out=junk, in_=src[:, :], func=AF.Square, accum_out=st[:, 1:2]), [bsrc], [B_st, B_oh])
        V(lambda e: e.tensor_scalar(out=st[:, 2:3], in0=st[:, 0:1], scalar1=1.0 / D, scalar2=None, op0=ALU.mult), [B_st], [B_st])
        V(lambda e: e.tensor_tensor(out=st[:, 3:4], in0=st[:, 2:3], in1=st[:, 2:3], op=ALU.mult), [B_st], [B_st])
        V(lambda e: e.scalar_tensor_tensor(out=st[:, 4:5], in0=st[:, 1:2], scalar=1.0 / D, in1=st[:, 3:4], op0=ALU.mult, op1=ALU.subtract), [B_st], [B_st])
        V(lambda e: e.tensor_scalar(out=st[:, 4:5], in0=st[:, 4:5], scalar1=1e-5, scalar2=None, op0=ALU.add), [B_st], [B_st])
        A(lambda e: e.activation(out=st[:, 5:6], in_=st[:, 4:5], func=AF.Sqrt), [B_st], [B_st])
        V(lambda e: e.reciprocal(out=st[:, 5:6], in_=st[:, 5:6]), [B_st], [B_st])
        V(lambda e: e.scalar_tensor_tensor(out=st[:, 6:7], in0=st[:, 2:3], scalar=-1.0, in1=st[:, 5:6], op0=ALU.mult, op1=ALU.mult), [B_st], [B_st])
        A(lambda e: e.activation(out=dst[:, :], in_=src[:, :], func=AF.Identity, scale=st[:, 5:6], bias=st[:, 6:7]), [bsrc, B_st], [bdst])
        G(lambda e: e.tensor_tensor(out=dst[:, :], in0=dst[:, :], in1=lnbc[:, gi, :], op=ALU.mult), [bdst, B_w], [bdst])
        G(lambda e: e.tensor_tensor(out=dst[:, :], in0=dst[:, :], in1=lnbc[:, gi + 1, :], op=ALU.add), [bdst, B_w], [bdst])

    x1p = [x1, ar.alloc([128, D], F32, "x1b")]
    B_x1p = [B_x1, Buf("x1b")]
    h2Tp_ = [h2T, ar.alloc([128, 8, 128], BF16, "h2Tb")]
    B_h2Tp_ = [B_h2T, Buf("h2Tb")]
    ssbp = [ssb, ar.alloc([128, 16, 128], F32, "ssbb")]
    B_sp = [B_s, Buf("ssbb")]
    prevn = [None]
    B_ohh = [Buf("ohh%d" % i) for i in range(8)]
    stmp16 = ar.alloc([128, 16, 128], F32, "stmp16")
    stmp8 = stmp16[:, :, :].rearrange("p (h a) b -> p h (a b)", a=2)
    B_tkg = [Buf("tkg%d" % i) for i in range(16)]
    B_tkg2 = [Buf("tkgb%d" % i) for i in range(16)]
    B_stg16 = [Buf("stg16_%d" % i) for i in range(16)]
    B_tig = [Buf("tig%d" % i) for i in range(16)]
    B_tig2 = [Buf("tigb%d" % i) for i in range(16)]
    B_mvh = [Buf("mvh%d" % i) for i in range(8)]
    B_mvh2 = [Buf("mvhb%d" % i) for i in range(8)]
    B_st8h = [B_stg16[2 * i] for i in range(8)]
    B_posh = [Buf("posh%d" % i) for i in range(8)]
    B_posh2 = [Buf("poshb%d" % i) for i in range(8)]

    def stageA(n, jt):
        q = n % 2
        x1_, bx1_, h2T_, bh2T_, ssb_, bs_ = x1p[q], B_x1p[q], h2Tp_[q], B_h2Tp_[q], ssbp[q], B_sp[q]
        sc.dma("sp", xt[:, :], x_v[:, n, :], writes=[B_x])
        for half in range(2):
            pM, bM = ps[4 + half], B_ps[4 + half]
            hsl = slice(half * 512, (half + 1) * 512)
            for dc in range(8):
                P(lambda e: e.matmul(pM[:, :], lhsT=mg[:, dc, jt * 128:(jt + 1) * 128], rhs=wout[:, dc, hsl], start=(dc == 0), stop=(dc == 7)), [B_mg, B_w], [bM])
            V(lambda e: e.tensor_tensor(out=u[:, hsl], in0=pM[:, :], in1=gt_bc[:, 0, hsl], op=ALU.mult), [bM, B_gt], [B_u])
            V(lambda e: e.scalar_tensor_tensor(out=u[:, hsl], in0=xt[:, hsl], scalar=ALPHA, in1=u[:, hsl], op0=ALU.mult, op1=ALU.add), [B_x, B_u], [B_u])
        layer_norm(u, B_u, x1_, bx1_, 0)
        if "x1dbg" in dbg:
            sc.dma("sp", L["x1_d"][n * 128:(n + 1) * 128, :], x1_[:, :], reads=[bx1_])
        sc.dma("sp", L["x1s_d"][n * 128:(n + 1) * 128, :], x1_[:, :], reads=[bx1_])
        G(lambda e: e.tensor_tensor(out=h2[:, :], in0=x1_[:, :], in1=gt_bc[:, 2, :], op=ALU.mult), [bx1_, B_gt], [B_h2])
        G(lambda e: e.tensor_tensor(out=h2[:, :], in0=h2[:, :], in1=gt_bc[:, 1, :], op=ALU.add), [B_h2, B_gt], [B_h2])
        for kc in range(8):
            pp_, bp_ = ps[kc // 4], B_ps[kc // 4]
            P(lambda e: e.transpose(pp_[:, (kc % 4) * 128:(kc % 4 + 1) * 128], h2[:, kc * 128:(kc + 1) * 128], ident[:, :]), [B_h2, B_ident], [bp_])
        for k2 in range(2):
            A(lambda e: e.activation(out=h2T_[:, k2 * 4:(k2 + 1) * 4, :], in_=ps[k2][:, :].rearrange("p (a b) -> p a b", b=128), func=AF.Copy), [B_ps[k2]], [bh2T_])
        for kc in range(8):
            sc.dma("sp", L["h2T_d"][kc * 128:(kc + 1) * 128, n * 128:(n + 1) * 128], h2T_[:, kc, :], reads=[bh2T_])
        for hh in range(8):
            pq, bq = ps[2 + hh // 4], B_ps[2 + hh // 4]
            for kc in range(8):
                P(lambda e: e.matmul(pq[:, (hh % 4) * 128:(hh % 4 + 1) * 128], lhsT=wqb[:, kc, hh * 128:(hh + 1) * 128], rhs=h2T_[:, kc, :],
                                     start=(kc == 0), stop=(kc == 7)), [B_w, bh2T_], [bq])
        for k2 in range(2):
            A(lambda e: e.activation(out=qT[:, k2 * 4:(k2 + 1) * 4, :], in_=ps[2 + k2][:, :].rearrange("p (a b) -> p a b", b=128), func=AF.Copy), [B_ps[2 + k2]], [B_qT])
        for g in range(16):
            hh, cc = g % 8, g // 8
            pS, bS = ps[4 + g // 4], B_ps[4 + g // 4]
            P(lambda e: e.matmul(pS[:, (g % 4) * 128:(g % 4 + 1) * 128], lhsT=qT[cc * 64:(cc + 1) * 64, hh, :], rhs=keysT[cc * 64:(cc + 1) * 64, hh, :],
                                 start=True, stop=True), [B_qT, B_w], [bS])
        for k4 in range(4):
            A(lambda e: e.activation(out=ssb_[:, k4 * 4:(k4 + 1) * 4, :], in_=ps[4 + k4][:, :].rearrange("p (a b) -> p a b", b=128), func=AF.Copy), [B_ps[4 + k4]], [bs_])

    def stageB(n):
        q = n % 2
        ssb_, bs_ = ssbp[q], B_sp[q]
        for g in range(16):
            V(lambda e: e.max(out=tv[:, g, 0:8], in_=ssb_[:, g, :]), [bs_], [B_tkg[g]])
        for g in range(16):
            V(lambda e: e.match_replace(out=stmp16[:, g, :], in_to_replace=tv[:, g, 0:8], in_values=ssb_[:, g, :], imm_value=-1e30), [bs_, B_tkg[g]], [B_stg16[g]])
        for g in range(16):
            V(lambda e: e.max(out=tv[:, g, 8:16], in_=stmp16[:, g, :]), [B_stg16[g]], [B_tkg2[g]])
        for g in range(16):
            V(lambda e: e.max_index(out=ti[:, g, 0:8], in_max=tv[:, g, 0:8], in_values=ssb_[:, g, :]), [bs_, B_tkg[g]], [B_tig[g]])
        for g in range(16):
            V(lambda e: e.max_index(out=ti[:, g, 8:16], in_max=tv[:, g, 8:16], in_values=ssb_[:, g, :]), [bs_, B_tkg2[g]], [B_tig2[g]])
        V(lambda e: e.tensor_copy(out=tif[:, :, :], in_=ti[:, :, :]), B_tig + B_tig2, [B_tk])
        V(lambda e: e.tensor_copy(out=tv[:, 0:1, 0:1], in_=tv[:, 0:1, 0:1]), B_tkg + B_tkg2, [B_tk])
        tvv = tv[:, :, :].rearrange("p (c h) k -> p h c k", c=2)
        tfv = tif[:, :, :].rearrange("p (c h) k -> p h c k", c=2)
        c4 = cand[:, :, :].rearrange("p h (a b) -> p h a b", b=16)
        for hh in range(8):
            V(lambda e: e.tensor_tensor(out=c4[:, hh, :, :], in0=tvv[:, hh, 0, :].unsqueeze(2).to_broadcast([128, 16, 16]),
                                        in1=tvv[:, hh, 1, :].unsqueeze(1).to_broadcast([128, 16, 16]), op=ALU.add), [B_tk], [B_c])
        for hh in range(8):
            V(lambda e: e.max(out=mv[:, hh, 0:8], in_=cand[:, hh, :]), [B_c], [B_mvh[hh]])
        for hh in range(8):
            V(lambda e: e.match_replace(out=stmp8[:, hh, :], in_to_replace=mv[:, hh, 0:8], in_values=cand[:, hh, :], imm_value=-1e30), [B_c, B_mvh[hh]], [B_stg16[2 * hh], B_stg16[2 * hh + 1]])
        for hh in range(8):
            V(lambda e: e.max(out=mv[:, hh, 8:16], in_=stmp8[:, hh, :]), [B_stg16[2 * hh], B_stg16[2 * hh + 1]], [B_mvh2[hh]])
        for hh in range(8):
            V(lambda e: e.max_index(out=posu[:, hh, 0:8], in_max=mv[:, hh, 0:8], in_values=cand[:, hh, :]), [B_c, B_mvh[hh]], [B_posh[hh]])
        for hh in range(8):
            V(lambda e: e.max_index(out=posu[:, hh, 8:16], in_max=mv[:, hh, 8:16], in_values=cand[:, hh, :]), [B_c, B_mvh2[hh]], [B_posh2[hh]])
        V(lambda e: e.tensor_copy(out=mv[:, 0:1, 0:1], in_=mv[:, 0:1, 0:1]), B_mvh + B_mvh2 + B_posh + B_posh2, [B_e])
        V(lambda e: e.tensor_scalar(out=au[:, :, :], in0=posu[:, :, :], scalar1=4, scalar2=None, op0=ALU.logical_shift_right), [B_e], [B_e])
        V(lambda e: e.tensor_scalar(out=bu[:, :, :], in0=posu[:, :, :], scalar1=15, scalar2=None, op0=ALU.bitwise_and), [B_e], [B_e])
        V(lambda e: e.tensor_copy(out=abf[:, 0, :], in_=au[:, :, :].rearrange("p a b -> p (a b)")), [B_e], [B_e])
        V(lambda e: e.tensor_copy(out=abf[:, 1, :], in_=bu[:, :, :].rearrange("p a b -> p (a b)")), [B_e], [B_e])
        for cc in range(2):
            V(lambda e: e.tensor_tensor(out=oh16[:, :, :], in0=abf[:, cc, :].unsqueeze(2).to_broadcast([128, 128, 16]),
                                        in1=iota16.unsqueeze(1).to_broadcast([128, 128, 16]), op=ALU.is_equal), [B_e, L["B_iota"]], [B_oh] + B_ohh)
            for hh in range(8):
                V(lambda e: e.tensor_tensor(out=oh16[:, hh * 16:(hh + 1) * 16, :], in0=oh16[:, hh * 16:(hh + 1) * 16, :],
                                            in1=tfv[:, hh, cc, :].unsqueeze(1).to_broadcast([128, 16, 16]), op=ALU.mult), [B_oh, B_tk], [B_ohh[hh]])
            V(lambda e: e.tensor_reduce(out=sel[:, cc, :], in_=oh16[:, :, :], axis=AX.X, op=ALU.add), B_ohh, [B_sel, B_oh])
        V(lambda e: e.tensor_tensor(out=gate[:, :, :], in0=mv[:, :, :], in1=mv[:, :, 0:1].to_broadcast([128, 8, 16]), op=ALU.subtract), [B_e], [B_hu])
        A(lambda e: e.activation(out=gate[:, :, :], in_=gate[:, :, :], func=AF.Exp), [B_hu], [B_hu])
        V(lambda e: e.tensor_reduce(out=gs[:, :], in_=gate[:, :, :], axis=AX.X, op=ALU.add), [B_hu], [B_hu])
        V(lambda e: e.reciprocal(out=gs[:, :], in_=gs[:, :]), [B_hu], [B_hu])
        V(lambda e: e.tensor_tensor(out=sel[:, 2, :].rearrange("p (a b) -> p a b", b=16), in0=gate[:, :, :], in1=gs[:, :].unsqueeze(2).to_broadcast([128, 8, 16]), op=ALU.mult),
          [B_hu], [B_sel])
        pI, bI = ps[6], B_ps[6]
        for q3 in range(3):
            P(lambda e: e.transpose(pI[:, q3 * 128:(q3 + 1) * 128], sel[:, q3, :], ident[:, :]), [B_sel, B_ident], [bI])
        A(lambda e: e.activation(out=selT[:, :, :], in_=pI[:, 0:384].rearrange("p (a b) -> p a b", b=128), func=AF.Copy), [bI], [B_selT])
        for q3 in range(3):
            sc.dma("sp", L["selT_d"][q3, :, n * 128:(n + 1) * 128], selT[:, q3, :], reads=[B_selT])

    for tb in range(L["nblk"]):
        tsl = slice(tb * 512, (tb + 1) * 512)
        for kc in range(4):
            sc.dma("sp", rwoB[:, kc, :], L["rwo_d"][kc * 128:(kc + 1) * 128, tsl], writes=[B_in])
        for kc in range(8):
            sc.dma("sp", dsaB[:, kc, :], L["dsao_d"][kc * 128:(kc + 1) * 128, tsl], writes=[B_in])
        for dc in range(8):
            pA, bA = ps[dc % 2], B_ps[dc % 2]
            pB, bB = ps[2 + dc % 2], B_ps[2 + dc % 2]
            zg_, bz_ = zgr[dc % 2], B_zgr[dc % 2]
            sc.dma("sp", zg_[:, 0, :], L["zg_d"][dc * 128:(dc + 1) * 128, tsl], writes=[bz_])
            sc.dma("sp", zg_[:, 1, :], L["zg_d"][(8 + dc) * 128:(9 + dc) * 128, tsl], writes=[bz_])
            for kc in range(4):
                P(lambda e: e.matmul(pA[:, :], lhsT=wbra[:, kc, dc * 128:(dc + 1) * 128], rhs=rwoB[:, kc, :], start=(kc == 0), stop=(kc == 3)), [B_w, B_in], [bA])
            for kc in range(8):
                P(lambda e: e.matmul(pB[:, :], lhsT=wbrb[:, kc, dc * 128:(dc + 1) * 128], rhs=dsaB[:, kc, :], start=(kc == 0), stop=(kc == 7)), [B_w, B_in], [bB])
            V(lambda e: e.tensor_tensor(out=t1[:, :], in0=pA[:, :], in1=zg_[:, 0, :], op=ALU.mult), [bA, bz_], [B_t])
            V(lambda e: e.tensor_tensor(out=t2[:, :], in0=pB[:, :], in1=zg_[:, 1, :], op=ALU.mult), [bB, bz_], [B_t])
            V(lambda e: e.tensor_tensor(out=mg[:, dc, :], in0=t1[:, :], in1=t2[:, :], op=ALU.add), [B_t], [B_mg])
        for jt in range(4):
            n = tb * 4 + jt
            stageA(n, jt)
            if prevn[0] is not None:
                stageB(prevn[0])
            prevn[0] = n
    stageB(prevn[0])
    ar.release(mD)


def phase_C(L):
    nc, sc, ar, ps, B_ps = L["nc"], L["sc"], L["ar"], L["ps"], L["B_ps"]
    identb, B_ident = L["identb"], L["B_ident"]
    V = lambda fn, r=(), w=(): sc.op("dve", fn, r, w)
    A = lambda fn, r=(), w=(): sc.op("act", fn, r, w)
    P = lambda fn, r=(), w=(): sc.op("pe", fn, r, w)
    G = lambda fn, r=(), w=(): sc.op("pool", fn, r, w)
    NQB = L["nblk"] * 4
    mC = ar.mark()
    cvec = ar.alloc([128, 256], F32, "cvec")
    biasT = ar.alloc([128, 3, 1024], F32, "biasT")
    negm = ar.alloc([128, 128], F32, "negm")
    ckv_tok = ar.alloc([128, 32, 129], BF16, "ckv_tok")
    ckvT = ar.alloc([128, S], BF16, "ckvT")
    kiT2 = ar.alloc([128, S], BF16, "kiT2")
    wall = ar.alloc([128, 32, 4], F32, "wall")
    B_cc, B_kv, B_ki, B_wl = Buf("cc"), Buf("kv"), Buf("ki"), Buf("wl")
    sc.dma("sp", cvec[:, :], L["cvec_d"][:, :], writes=[B_cc])
    sc.dma("sp", biasT[:, :, :], L["biasT_d"][:, :, :], writes=[B_cc])
    sc.dma("sp", negm[:, :], L["negm_d"][:, :], writes=[B_cc])
    V(lambda e: e.memset(ckv_tok[:, :, 128:129], 1.0), [], [B_kv])
    biasTb = ar.alloc([128, 2, 1024], BF16, "biasTb")
    for bi_ in range(2):
        V(lambda e: e.tensor_tensor(out=biasT[:, bi_, :], in0=biasT[:, bi_, :], in1=biasT[:, 2, :], op=ALU.subtract), [B_cc], [B_cc])
        V(lambda e: e.tensor_copy(out=biasTb[:, bi_, :], in_=biasT[:, bi_, :]), [B_cc], [B_cc])
    zt = [ar.alloc([128, 196], F32, "ztC%d" % i) for i in range(2)]
    B_zt = [Buf("ztC0"), Buf("ztC1")]
    sq = ar.alloc([128, 128], F32, "sqC")
    c16 = ar.alloc([128, 128], BF16, "c16")
    k32 = ar.alloc([128, 64], F32, "k32")
    k16 = ar.alloc([128, 128], BF16, "k16")
    stc = ar.alloc([128, 8], F32, "stc")
    B_sq, B_c16, B_k, B_stc = Buf("sqC"), Buf("c16"), Buf("k"), Buf("stc")
    for n in range(NQB):
        z, bz = zt[n % 2], B_zt[n % 2]
        sc.dma("sp", z[:, :], L["ztok_d"][n * 128:(n + 1) * 128, :], writes=[bz])
        A(lambda e: e.activation(out=sq[:, :], in_=z[:, 0:128], func=AF.Square, accum_out=stc[:, 0:1]), [bz], [B_sq, B_stc])
        V(lambda e: e.tensor_scalar(out=stc[:, 1:2], in0=stc[:, 0:1], scalar1=1.0 / 128, scalar2=1e-5, op0=ALU.mult, op1=ALU.add), [B_stc], [B_stc])
        A(lambda e: e.activation(out=stc[:, 1:2], in_=stc[:, 1:2], func=AF.Sqrt), [B_stc], [B_stc])
        V(lambda e: e.reciprocal(out=stc[:, 1:2], in_=stc[:, 1:2]), [B_stc], [B_stc])
        V(lambda e: e.scalar_tensor_tensor(out=ckv_tok[:, n, 0:128], in0=z[:, 0:128], scalar=stc[:, 1:2], in1=cvec[:, 0:128], op0=ALU.mult, op1=ALU.mult),
          [bz, B_stc, B_cc], [B_kv])
        V(lambda e: e.tensor_reduce(out=stc[:, 2:3], in_=z[:, 128:192], axis=AX.X, op=ALU.add), [bz], [B_stc])
        A(lambda e: e.activation(out=sq[:, 0:64], in_=z[:, 128:192], func=AF.Square, accum_out=stc[:, 3:4]), [bz], [B_sq, B_stc])
        V(lambda e: e.tensor_scalar(out=stc[:, 4:5], in0=stc[:, 2:3], scalar1=1.0 / 64, scalar2=None, op0=ALU.mult), [B_stc], [B_stc])
        V(lambda e: e.tensor_tensor(out=stc[:, 5:6], in0=stc[:, 4:5], in1=stc[:, 4:5], op=ALU.mult), [B_stc], [B_stc])
        V(lambda e: e.scalar_tensor_tensor(out=stc[:, 6:7], in0=stc[:, 3:4], scalar=1.0 / 64, in1=stc[:, 5:6], op0=ALU.mult, op1=ALU.subtract), [B_stc], [B_stc])
        V(lambda e: e.tensor_scalar(out=stc[:, 6:7], in0=stc[:, 6:7], scalar1=1e-5, scalar2=None, op0=ALU.add), [B_stc], [B_stc])
        A(lambda e: e.activation(out=stc[:, 6:7], in_=stc[:, 6:7], func=AF.Sqrt), [B_stc], [B_stc])
        V(lambda e: e.reciprocal(out=stc[:, 6:7], in_=stc[:, 6:7]), [B_stc], [B_stc])
        V(lambda e: e.tensor_scalar(out=k32[:, :], in0=z[:, 128:192], scalar1=stc[:, 4:5], scalar2=stc[:, 6:7], op0=ALU.subtract, op1=ALU.mult), [bz, B_stc], [B_k])
        V(lambda e: e.tensor_tensor(out=k32[:, :], in0=k32[:, :], in1=cvec[:, 128:192], op=ALU.mult), [B_k, B_cc], [B_k])
        V(lambda e: e.tensor_tensor(out=k16[:, 0:64], in0=k32[:, :], in1=cvec[:, 192:256], op=ALU.add), [B_k, B_cc], [B_k])
        V(lambda e: e.tensor_copy(out=k16[:, 64:128], in_=k16[:, 0:64]), [B_k], [B_k])
        V(lambda e: e.tensor_scalar(out=wall[:, n, :], in0=z[:, 192:196], scalar1=0.0625, scalar2=None, op0=ALU.mult), [bz], [B_wl])
        pT = ps[n % 2][:, :].bitcast(BF16)
        bT = B_ps[n % 2]
        P(lambda e: e.transpose(pT[:, 0:128], ckv_tok[:, n, 0:128], identb[:, :]), [B_kv, B_ident], [bT])
        P(lambda e: e.transpose(pT[:, 128:256], k16[:, :], identb[:, :]), [B_k, B_ident], [bT])
        A(lambda e: e.activation(out=ckvT[:, n * 128:(n + 1) * 128], in_=pT[:, 0:128], func=AF.Copy, scale=128.0 ** -0.5), [bT], [B_kv])
        V(lambda e: e.tensor_copy(out=kiT2[:, n * 128:(n + 1) * 128], in_=pT[:, 128:256]), [bT], [B_ki])
    qiB = [ar.alloc([128, 2, 128], BF16, "qiB%d" % i) for i in range(2)]
    zqB = [ar.alloc([128, 8, 128], BF16, "zqB%d" % i) for i in range(2)]
    B_qi = [Buf("qi0"), Buf("qi1")]
    B_zq = [Buf("zq0"), Buf("zq1")]
    score2 = [ar.alloc([128, S], F32, "score%d" % i) for i in range(2)]
    maskb2 = [ar.alloc([128, S], BF16, "maskb%d" % i) for i in range(2)]
    maskT2 = [ar.alloc([128, 32, 128], BF16, "maskT%d" % i) for i in range(2)]
    bis2 = [ar.alloc([128, 8], F32, "bis%d" % i) for i in range(2)]
    junk = ar.alloc([128, S], BF16, "junkC")
    junkA = ar.alloc([128, S // 2], BF16, "junkA")
    rl = [ar.alloc([128, 512], F32, "rl%d" % i) for i in range(2)]
    B_rl = [Buf("rl0"), Buf("rl1")]
    lg = ar.alloc([128, 1024], F32, "lg")
    PT2 = [ar.alloc([128, 8, 128], BF16, "PT%d" % i) for i in range(2)]
    B_PT2 = [Buf("PT0"), Buf("PT1")]
    Oacc = ar.alloc([128, 8, 129], F32, "Oacc")
    rec = ar.alloc([128, 8], F32, "rec")
    Oo = ar.alloc([128, 8, 128], BF16, "Oo")
    dsT = ar.alloc([128, 8, 128], BF16, "dsT")
    B_sc2 = [Buf("score0"), Buf("score1")]
    B_mb2 = [Buf("maskb0"), Buf("maskb1")]
    B_mT2 = [Buf("maskT0"), Buf("maskT1")]
    B_bisA = [Buf("bisA0"), Buf("bisA1")]
    B_bisB = [Buf("bisB0"), Buf("bisB1")]
    B_bisC = [Buf("bisC0"), Buf("bisC1")]
    B_j, B_jA, B_lg, B_O, B_Oo, B_dsT = [Buf(n_) for n_ in ("junkC", "junkA", "lg", "Oacc", "Oo", "dsT")]

    def stage1(j):
        q = j % 2
        Lk = (j + 1) * 128
        qi, bqi, zq, bzq = qiB[q], B_qi[q], zqB[q], B_zq[q]
        score, B_sc = score2[q], B_sc2[q]
        qsl = slice(j * 128, (j + 1) * 128)
        for cch in range(2):
            sc.dma("sp", qi[:, cch, :], L["zqi_d"][cch * 128:(cch + 1) * 128, qsl], writes=[bqi])
        for h in range(8):
            sc.dma("sp", zq[:, h, :], L["zq_d"][h * 128:(h + 1) * 128, qsl], writes=[bzq])
        nkc = (Lk + 511) // 512
        ri = 0
        for kc in range(nkc):
            wd = min(512, Lk - kc * 512)
            ksl = slice(kc * 512, kc * 512 + wd)
            for hi in range(4):
                po = (hi % 2) * 64
                pD, bD = ps[hi], B_ps[hi]
                P(lambda e: e.matmul(pD[:, 0:wd], lhsT=qi[po:po + 64, hi // 2, :], rhs=kiT2[po:po + 64, ksl], start=True, stop=True), [bqi, B_ki], [bD])
                r_, br_ = rl[ri % 2], B_rl[ri % 2]
                ri += 1
                A(lambda e: e.activation(out=r_[:, 0:wd], in_=pD[:, 0:wd], func=AF.Relu), [bD], [br_])
                if hi == 0:
                    V(lambda e: e.tensor_scalar(out=score[:, ksl], in0=r_[:, 0:wd], scalar1=wall[:, j, 0:1], scalar2=None, op0=ALU.mult), [br_, B_wl], [B_sc])
                else:
                    V(lambda e: e.scalar_tensor_tensor(out=score[:, ksl], in0=r_[:, 0:wd], scalar=wall[:, j, hi:hi + 1], in1=score[:, ksl], op0=ALU.mult, op1=ALU.add),
                      [br_, B_wl, B_sc], [B_sc])
        V(lambda e: e.tensor_tensor(out=score[:, qsl], in0=score[:, qsl], in1=negm[:, :], op=ALU.add), [B_sc, B_cc], [B_sc])

    def bisect_iters(j):
        q = j % 2
        Lk = (j + 1) * 128
        score, B_sc, bis = score2[q], B_sc2[q], bis2[q]
        bA, bB, bC = B_bisA[q], B_bisB[q], B_bisC[q]
        if Lk > 256:
            na = (Lk // 2) // 128 * 128
            V(lambda e: e.memset(bis[:, 6:7], 0.0), [bA], [bA])
            step = 16.0
            for it in range(20):
                V(lambda e: e.tensor_scalar(out=bis[:, 7:8], in0=bis[:, 6:7], scalar1=-1.0, scalar2=None, op0=ALU.mult), [bA], [bB])
                A(lambda e: e.activation(out=junkA[:, 0:na], in_=score[:, 0:na], func=AF.Sign, bias=bis[:, 7:8], accum_out=bis[:, 2:3]), [B_sc, bB], [B_jA, bC])
                V(lambda e: e.tensor_scalar(out=junk[:, na:Lk], in0=score[:, na:Lk], scalar1=bis[:, 6:7], scalar2=None, op0=ALU.is_ge, op1=ALU.add, accum_out=bis[:, 3:4]),
                  [B_sc, bA], [B_j, bA])
                V(lambda e: e.scalar_tensor_tensor(out=bis[:, 4:5], in0=bis[:, 2:3], scalar=0.5, in1=bis[:, 3:4], op0=ALU.mult, op1=ALU.add), [bA, bC], [bA])
                V(lambda e: e.tensor_scalar(out=bis[:, 5:6], in0=bis[:, 4:5], scalar1=255.5 - na / 2.0, scalar2=2.0 * step, op0=ALU.is_ge, op1=ALU.mult), [bA], [bA])
                V(lambda e: e.scalar_tensor_tensor(out=bis[:, 6:7], in0=bis[:, 5:6], scalar=-step, in1=bis[:, 6:7], op0=ALU.add, op1=ALU.add), [bA, bB], [bA])
                step *= 0.5
                yield
            V(lambda e: e.tensor_scalar(out=bis[:, 6:7], in0=bis[:, 6:7], scalar1=-4.0 * step, scalar2=None, op0=ALU.add), [bA], [bA])
        else:
            V(lambda e: e.memset(bis[:, 6:7], -1e29), [bA], [bA])

    def stage3(j):
        q = j % 2
        Lk = (j + 1) * 128
        score, B_sc, bis, maskb, B_mb, maskT, B_mT = score2[q], B_sc2[q], bis2[q], maskb2[q], B_mb2[q], maskT2[q], B_mT2[q]
        V(lambda e: e.tensor_scalar(out=maskb[:, 0:Lk], in0=score[:, 0:Lk], scalar1=bis[:, 6:7], scalar2=None, op0=ALU.is_ge), [B_sc, B_bisA[q]], [B_mb])
        for k8 in range((j + 8) // 8):
            nn = min(8, j + 1 - k8 * 8)
            pM = ps[4][:, :].bitcast(BF16)
            for kk_ in range(nn):
                kt = k8 * 8 + kk_
                P(lambda e: e.transpose(pM[:, kk_ * 128:(kk_ + 1) * 128], maskb[:, kt * 128:(kt + 1) * 128], identb[:, :]), [B_mb, B_ident], [B_ps[4]])
            A(lambda e: e.activation(out=maskT[:, k8 * 8:k8 * 8 + nn, :], in_=pM[:, 0:nn * 128].rearrange("p (a b) -> p a b", b=128), func=AF.Copy, scale=30000.0, bias=-30000.0),
              [B_ps[4]], [B_mT])

    def stage4(j, side):
        q = j % 2
        zq, bzq, maskT, B_mT = zqB[q], B_zq[q], maskT2[q], B_mT2[q]
        qsl = slice(j * 128, (j + 1) * 128)
        for kt in range(j + 1):
            near = kt >= j - 1
            bsel = 0 if kt == j else 1
            PTk, bPT = PT2[kt % 2], B_PT2[kt % 2]
            for half in range(2):
                pL, bL = ps[(kt % 2) * 2 + half], B_ps[(kt % 2) * 2 + half]
                P(lambda e: e.matmul(pL[:, :], lhsT=ckvT[:, kt * 128:(kt + 1) * 128], rhs=zq[:, half * 4:(half + 1) * 4, :], start=True, stop=False), [B_kv, bzq], [bL])
                for h4 in range(4):
                    P(lambda e: e.matmul(pL[:, h4 * 128:(h4 + 1) * 128], lhsT=identb[:, :], rhs=maskT[:, kt, :], start=False, stop=(h4 == 3 and not near)), [B_ident, B_mT], [bL])
                if near:
                    P(lambda e: e.matmul(pL[:, :], lhsT=identb[:, :], rhs=biasTb[:, bsel, half * 512:(half + 1) * 512], start=False, stop=True), [B_ident, B_cc], [bL])
                A(lambda e: e.activation(out=PTk[:, half * 4:(half + 1) * 4, :], in_=pL[:, :].rearrange("p (h q) -> p h q", q=128), func=AF.Exp), [bL], [bPT])
            for h in range(8):
                pO, bO = ps[5 + h // 3], B_ps[5 + h // 3]
                P(lambda e: e.matmul(pO[:, (h % 3) * 129:(h % 3 + 1) * 129], lhsT=PTk[:, h, :], rhs=ckv_tok[:, kt, :], start=(kt == 0 and h % 3 == 0), stop=(kt == j),
                                     skip_group_check=True), [bPT, B_kv], [bO])
            if side is not None:
                next(side, None)
        for b3 in range(3):
            nh = 3 if b3 < 2 else 2
            V(lambda e: e.tensor_copy(out=Oacc[:, b3 * 3:b3 * 3 + nh, :], in_=ps[5 + b3][:, 0:nh * 129].rearrange("p (h d) -> p h d", d=129)), [B_ps[5 + b3]], [B_O])
        V(lambda e: e.reciprocal(out=rec[:, :], in_=Oacc[:, :, 128]), [B_O], [B_Oo])
        V(lambda e: e.tensor_tensor(out=Oo[:, :, :], in0=Oacc[:, :, 0:128], in1=rec[:, :].unsqueeze(2).to_broadcast([128, 8, 128]), op=ALU.mult), [B_O, B_Oo], [B_Oo])
        pX = ps[4][:, :].bitcast(BF16)
        for h in range(8):
            P(lambda e: e.transpose(pX[:, h * 128:(h + 1) * 128], Oo[:, h, :], identb[:, :]), [B_Oo, B_ident], [B_ps[4]])
        V(lambda e: e.tensor_copy(out=dsT[:, :, :], in_=pX[:, :].rearrange("p (a b) -> p a b", b=128)), [B_ps[4]], [B_dsT])
        for h in range(8):
            sc.dma("sp", L["dsao_d"][h * 128:(h + 1) * 128, qsl], dsT[:, h, :], reads=[B_dsT])

    stage1(0)
    for _ in bisect_iters(0):
        pass
    stage3(0)
    for j in range(NQB):
        side = None
        if j + 1 < NQB:
            stage1(j + 1)
            side = bisect_iters(j + 1)
        stage4(j, side)
        if side is not None:
            for _ in side:
                pass
            stage3(j + 1)
    ar.release(mC)


def phase_E(L):
    nc, sc, ar, ps, B_ps = L["nc"], L["sc"], L["ar"], L["ps"], L["B_ps"]
    identb, B_ident = L["identb"], L["B_ident"]
    gt_bc, B_gt = L["gt_bc"], L["B_gt"]
    iota128, B_iota = L["iota128"], L["B_iota"]
    dbg = L["dbg"]
    V = lambda fn, r=(), w=(): sc.op("dve", fn, r, w)
    A = lambda fn, r=(), w=(): sc.op("act", fn, r, w)
    P = lambda fn, r=(), w=(): sc.op("pe", fn, r, w)
    G = lambda fn, r=(), w=(): sc.op("pool", fn, r, w)
    ALPHA = 2.0 ** 0.25
    if not L["e0_done_flag"][0]:
        m0 = ar.mark()
        for _ in make_e0(L, 3, 0):
            pass
        ar.release(m0)
    ar.release(L["mark_stg"])
    mE = ar.mark()
    TP = 256
    lnbc = ar.alloc([128, 2, D], F32, "lnbcE")
    B_ln = Buf("lnE")
    sc.dma("sp", lnbc[:, :, :], L["lnbc_d"][:, 2:4, :], writes=[B_ln])
    Gs2 = [ar.alloc([128, 128, TP], BF16, "Gs%d" % i) for i in range(2)]
    h2Tp2 = [ar.alloc([128, 8, TP], BF16, "h2Tp%d" % i) for i in range(2)]
    IT1 = ar.alloc([128, 3, TP], F32, "IT1")
    IT2 = [IT1, IT1]
    ITb2 = [ar.alloc([128, 3, TP], BF16, "ITb%d" % i) for i in range(2)]
    iotab = ar.alloc([128, 128], BF16, "iotab")
    NBT = 8
    eqb = [ar.alloc([128, NBT, 128], BF16, "eqb%d" % i) for i in range(1)]
    Lb = [ar.alloc([128, NBT, 128], BF16, "Lb%d" % i) for i in range(2)]
    Rb = [ar.alloc([128, NBT, 128], BF16, "Rb%d" % i) for i in range(2)]
    B_Gs2 = [Buf("Gs0"), Buf("Gs1")]
    B_h2Tp2 = [Buf("h2Tp0"), Buf("h2Tp1")]
    B_IT2 = [Buf("IT0"), Buf("IT1")]
    B_ITf1 = Buf("ITf")
    B_ITf2 = [B_ITf1, B_ITf1]
    B_eq = [Buf("eqb0")]
    B_eqh = [Buf("eqh0"), Buf("eqh1")]
    V(lambda e: e.tensor_copy(out=iotab[:, :], in_=iota128[:, :]), [B_iota], [B_iota])
    B_Lb = [Buf("Lb0"), Buf("Lb1")]
    B_Rb = [Buf("Rb0"), Buf("Rb1")]
    NS = 4
    uTc = [ar.alloc([128, 1024], BF16, "uTc%d" % i) for i in range(NS)]
    vcb = [ar.alloc([128, 1024], BF16, "vcb%d" % i) for i in range(NS)]
    B_uTc = [Buf("uTc%d" % i) for i in range(NS)]
    B_vcb = [Buf("vcb%d" % i) for i in range(NS)]
    gl = [ar.alloc([128, TP], BF16, "gl%d" % i) for i in range(3)]
    AT = [ar.alloc([128, TP], BF16, "AT%d" % i) for i in range(3)]
    B_gl = [Buf("gl0"), Buf("gl1"), Buf("gl2")]
    B_AT = [Buf("AT0"), Buf("AT1"), Buf("AT2")]
    x1t = ar.alloc([128, D], F32, "x1t")
    oo = ar.alloc([128, D], F32, "ooE")
    jk = oo
    st = ar.alloc([128, 8], F32, "stE")
    B_x1t, B_oo, B_st = Buf("x1t"), Buf("ooE"), Buf("stE")
    B_jk = B_oo
    out_v = L["out_d"].rearrange("(n p) m -> p n m", p=128)
    iota3 = iotab[:, :].unsqueeze(1).to_broadcast([128, NBT, 128])
    npass = L["nblk"] * 2
    NBATCH = TP // NBT

    def emit_loads(p_):
        q = p_ % 2
        tsl = slice(p_ * TP, (p_ + 1) * TP)
        for kc in range(8):
            sc.dma("sp", h2Tp2[q][:, kc, :], L["h2T_d"][kc * 128:(kc + 1) * 128, tsl], writes=[B_h2Tp2[q]])
        for q3 in range(3):
            sc.dma("sp", IT2[q][:, q3, :], L["selT_d"][q3, :, tsl], writes=[B_ITf2[q]])
        A(lambda e: e.activation(out=ITb2[q][:, :, :], in_=IT2[q][:, :, :], func=AF.Copy), [B_ITf2[q]], [B_IT2[q]])

    gcount = [0]

    def gbatch_gen(p_, b):
        q = p_ % 2
        Gs, B_Gs, ITb, B_IT = Gs2[q], B_Gs2[q], ITb2[q], B_IT2[q]
        gi = gcount[0]
        gcount[0] += 1
        Lk, bLk = Lb[gi % 2], B_Lb[gi % 2]
        Rk, bRk = Rb[gi % 2], B_Rb[gi % 2]
        eq_ = eqb[0]
        H = NBT // 2
        for hf in range(2):
            hs_ = slice(hf * H, (hf + 1) * H)
            bs_ = slice(b * NBT + hf * H, b * NBT + (hf + 1) * H)
            io_ = iotab[:, :].unsqueeze(1).to_broadcast([128, H, 128])
            V(lambda e: e.tensor_tensor(out=eq_[:, hs_, :], in0=io_, in1=ITb[:, 0, bs_].unsqueeze(2).to_broadcast([128, H, 128]), op=ALU.is_equal), [B_IT, B_iota], [B_eqh[hf]])
            yield
            V(lambda e: e.tensor_tensor(out=Lk[:, hs_, :], in0=eq_[:, hs_, :], in1=ITb[:, 2, bs_].unsqueeze(2).to_broadcast([128, H, 128]), op=ALU.mult), [B_eqh[hf], B_IT], [bLk])
            yield
            V(lambda e: e.tensor_tensor(out=Rk[:, hs_, :], in0=io_, in1=ITb[:, 1, bs_].unsqueeze(2).to_broadcast([128, H, 128]), op=ALU.is_equal), [B_IT, B_iota], [bRk])
            yield
        yield
        yield
        for t4 in range(NBT // 4):
            pg, bpg = ps[7], B_ps[7]
            for tt in range(4):
                t = t4 * 4 + tt
                P(lambda e: e.matmul(pg[:, tt * 128:(tt + 1) * 128], lhsT=Lk[:, t, :], rhs=Rk[:, t, :], start=True, stop=True), [bLk, bRk], [bpg])
            t0 = b * NBT + t4 * 4
            src = pg[:, :].rearrange("p (t i) -> p i t", i=128)
            A(lambda e: e.activation(out=Gs[:, :, t0:t0 + 4], in_=src, func=AF.Copy), [bpg], [B_Gs])
            yield

    def gall_gen(p_):
        for b in range(NBATCH):
            for _ in gbatch_gen(p_, b):
                yield

    emit_loads(0)
    for _ in gall_gen(0):
        pass
    for p_ in range(npass):
        q = p_ % 2
        Gs, B_Gs, h2Tp, B_h2Tp = Gs2[q], B_Gs2[q], h2Tp2[q], B_h2Tp2[q]
        if p_ + 1 < npass:
            emit_loads(p_ + 1)

        def load_u(c):
            sc.dma("sp", uTc[c % NS][:, :], L["uv_d"][c, :, 0:1024], writes=[B_uTc[c % NS]])

        def emit_hu(c):
            k = c % NS
            if c == 0:
                load_u(0)
                load_u(1)
            if c + 2 < 128:
                load_u(c + 2)
            sc.dma("sp", vcb[k][:, :], L["uv_d"][c, :, 1024:2048], writes=[B_vcb[k]])
            pH, bH = ps[4 + c % 3], B_ps[4 + c % 3]
            for kc in range(8):
                P(lambda e: e.matmul(pH[:, 0:TP], lhsT=uTc[k][:, kc * 128:(kc + 1) * 128], rhs=h2Tp[:, kc, :], start=(kc == 0), stop=(kc == 7)), [B_uTc[k], B_h2Tp], [bH])

        def emit_y2(c):
            k = c % NS
            pH, bH = ps[4 + c % 3], B_ps[4 + c % 3]
            g_, bg_ = gl[c % 3], B_gl[c % 3]
            a_, ba_ = AT[c % 3], B_AT[c % 3]
            A(lambda e: e.activation(out=g_[:, :], in_=pH[:, 0:TP], func=AF.Gelu), [bH], [bg_])
            V(lambda e: e.tensor_tensor(out=a_[:, :], in0=g_[:, :], in1=Gs[:, c, :], op=ALU.mult), [bg_, B_Gs], [ba_])
            for tt in range(2):
                for half in range(2):
                    py, bpy = ps[tt * 2 + half], B_ps[tt * 2 + half]
                    P(lambda e: e.matmul(py[:, :], lhsT=a_[:, tt * 128:(tt + 1) * 128], rhs=vcb[k][:, half * 512:(half + 1) * 512], start=(c == 0), stop=(c == 127)),
                      [ba_, B_vcb[k]], [bpy])
        emit_hu(0)
        emit_hu(1)
        gg = gall_gen(p_ + 1) if p_ + 1 < npass else None
        steps_per_chunk = (NBATCH * 10 + 127) // 128
        for c in range(128):
            if c + 2 < 128:
                emit_hu(c + 2)
            emit_y2(c)
            if gg is not None:
                for _ in range(steps_per_chunk):
                    next(gg, None)
        if gg is not None:
            for _ in gg:
                pass
        for tt in range(2):
            n = p_ * 2 + tt
            sc.dma("sp", x1t[:, :], L["x1s_d"][n * 128:(n + 1) * 128, :], writes=[B_x1t])
            for half in range(2):
                hsl = slice(half * 512, (half + 1) * 512)
                py, bpy = ps[tt * 2 + half], B_ps[tt * 2 + half]
                if "y2dbg" in dbg:
                    V(lambda e: e.tensor_copy(out=oo[:, hsl], in_=py[:, :]), [bpy], [B_oo])
                    sc.dma("sp", L["y2_d"][n * 128:(n + 1) * 128, hsl], oo[:, hsl], reads=[B_oo])
                V(lambda e: e.tensor_tensor(out=oo[:, hsl], in0=py[:, :], in1=gt_bc[:, 3, hsl], op=ALU.mult), [bpy, B_gt], [B_oo])
            V(lambda e: e.scalar_tensor_tensor(out=x1t[:, :], in0=x1t[:, :], scalar=ALPHA, in1=oo[:, :], op0=ALU.mult, op1=ALU.add), [B_x1t, B_oo], [B_x1t])
            A(lambda e: e.activation(out=oo[:, :], in_=x1t[:, :], func=AF.Copy, accum_out=st[:, 0:1]), [B_x1t], [B_st, B_oo])
            A(lambda e: e.activation(out=oo[:, :], in_=x1t[:, :], func=AF.Square, accum_out=st[:, 1:2]), [B_x1t], [B_st, B_oo])
            V(lambda e: e.tensor_scalar(out=st[:, 2:3], in0=st[:, 0:1], scalar1=1.0 / D, scalar2=None, op0=ALU.mult), [B_st], [B_st])
            V(lambda e: e.tensor_tensor(out=st[:, 3:4], in0=st[:, 2:3], in1=st[:, 2:3], op=ALU.mult), [B_st], [B_st])
            V(lambda e: e.scalar_tensor_tensor(out=st[:, 4:5], in0=st[:, 1:2], scalar=1.0 / D, in1=st[:, 3:4], op0=ALU.mult, op1=ALU.subtract), [B_st], [B_st])
            V(lambda e: e.tensor_scalar(out=st[:, 4:5], in0=st[:, 4:5], scalar1=1e-5, scalar2=None, op0=ALU.add), [B_st], [B_st])
            A(lambda e: e.activation(out=st[:, 5:6], in_=st[:, 4:5], func=AF.Sqrt), [B_st], [B_st])
            V(lambda e: e.reciprocal(out=st[:, 5:6], in_=st[:, 5:6]), [B_st], [B_st])
            V(lambda e: e.scalar_tensor_tensor(out=st[:, 6:7], in0=st[:, 2:3], scalar=-1.0, in1=st[:, 5:6], op0=ALU.mult, op1=ALU.mult), [B_st], [B_st])
            A(lambda e: e.activation(out=oo[:, :], in_=x1t[:, :], func=AF.Identity, scale=st[:, 5:6], bias=st[:, 6:7]), [B_x1t, B_st], [B_oo])
            G(lambda e: e.tensor_tensor(out=oo[:, :], in0=oo[:, :], in1=lnbc[:, 0, :], op=ALU.mult), [B_oo, B_ln], [B_oo])
            G(lambda e: e.tensor_tensor(out=oo[:, :], in0=oo[:, :], in1=lnbc[:, 1, :], op=ALU.add), [B_oo, B_ln], [B_oo])
            sc.dma("sp", out_v[:, n, :], oo[:, :], reads=[B_oo])
    ar.release(mE)


def make_e0(L, NB, bank):
    sc, ar, ps, B_ps = L["sc"], L["ar"], L["ps"], L["B_ps"]
    identb, B_ident = L["identb"], L["B_ident"]
    A = lambda fn, r=(), w=(): sc.op("act", fn, r, w)
    P = lambda fn, r=(), w=(): sc.op("pe", fn, r, w)
    G = lambda fn, r=(), w=(): sc.op("pool", fn, r, w)
    NF = 3
    stf = [ar.alloc([128, D], F32, "e0f%d" % i) for i in range(NF)]
    o16 = [ar.alloc([128, D], BF16, "e0h%d" % i) for i in range(NF)]
    uT = [ar.alloc([128, D], BF16, "e0t%d" % i) for i in range(2)]
    B_f = [Buf("e0f%d" % i) for i in range(NF)]
    B_o = [Buf("e0h%d" % i) for i in range(NF)]
    B_t = [Buf("e0t0"), Buf("e0t1")]
    pu_v = L["pu_d"].rearrange("(i1 i2) d -> i2 i1 d", i2=128)
    pv_v = L["pv_d"].rearrange("(i1 i2) d -> i2 i1 d", i2=128)
    NI = 256

    def load(i):
        c, isv = i // 2, i % 2
        sc.dma("sp", stf[i % NF][:, :], (pv_v if isv else pu_v)[c, :, :], writes=[B_f[i % NF]])

    def cast(i):
        G(lambda e: e.tensor_copy(out=o16[i % NF][:, :], in_=stf[i % NF][:, :]), [B_f[i % NF]], [B_o[i % NF]])

    def finish(i):
        c, isv = i // 2, i % 2
        if isv:
            sc.dma("sp", L["uv_d"][c, :, 1024:2048], o16[i % NF][:, :], reads=[B_o[i % NF]])
        else:
            pT = ps[bank][:, :].bitcast(BF16)
            for kc in range(8):
                P(lambda e: e.transpose(pT[:, kc * 128:(kc + 1) * 128], o16[i % NF][:, kc * 128:(kc + 1) * 128], identb[:, :]), [B_o[i % NF], B_ident], [B_ps[bank]])
            A(lambda e: e.activation(out=uT[c % 2][:, :], in_=pT[:, :], func=AF.Copy), [B_ps[bank]], [B_t[c % 2]])
            sc.dma("sp", L["uv_d"][c, :, 0:1024], uT[c % 2][:, :], reads=[B_t[c % 2]])
    load(0)
    load(1)
    cast(0)
    for k in range(NI):
        if k + 2 < NI:
            load(k + 2)
        if k + 1 < NI:
            cast(k + 1)
        finish(k)
        yield
```
